# Optimizing a Trainium2 kernel written in Bass

```python
import math
import jax
import jax.numpy as jnp
from jax import lax
import numpy as np

D_MODEL = 1024
BATCH = 1
SEQ = 16384
DEPTH = 4

GRID_W = 64
CTX_LEN = 256
N_MIXERS = 3
N_HEADS = 16
N_KV_HEADS = 4
HEAD_DIM = 64
Q_PER_KV = N_HEADS // N_KV_HEADS
WINDOW = 128
ATTN_BLOCK = 128
ATTN_SCALE = HEAD_DIM ** -0.5
ROPE_BASE = 10000.0
ROPE_FREQS = HEAD_DIM // 4
CHUNK = 128
GMLP_WIDTH = D_MODEL
GMLP_GROUPS = 8
GMLP_GROUP_DIM = GMLP_WIDTH // GMLP_GROUPS
CONV_WIDTH = 3
N_EXPERTS = 16
N_EXPERT_GROUPS = 4
EXPERTS_PER_GROUP = N_EXPERTS // N_EXPERT_GROUPS
TOP_K = 2
D_EXPERT = 512
MOE_BLOCK = 128
ALPHA = (2 * DEPTH) ** 0.25
BETA = (8 * DEPTH) ** -0.25
LN_EPS = 1e-5

kernel_name = "hybrid_dit_interleaved_moe"


def layer_norm(x, g, b):
    xf = x.astype(jnp.float32)
    mu = jnp.mean(xf, axis=-1, keepdims=True)
    var = jnp.mean(jnp.square(xf - mu), axis=-1, keepdims=True)
    y = (xf - mu) * lax.rsqrt(var + LN_EPS) * g.astype(jnp.float32) + b.astype(jnp.float32)
    return y.astype(x.dtype)


def modulate(h, shift, scale):
    return h * (1 + scale) + shift


def axial_rope_tables(L, dtype):
    rows = L // GRID_W
    row = jnp.repeat(jnp.arange(rows), GRID_W).astype(jnp.float32)
    col = jnp.tile(jnp.arange(GRID_W), rows).astype(jnp.float32)
    freqs = jnp.power(ROPE_BASE, -jnp.arange(ROPE_FREQS, dtype=jnp.float32) / ROPE_FREQS)
    ar = row[:, None] * freqs[None, :]
    ac = col[:, None] * freqs[None, :]
    ang = jnp.concatenate([ar, ar, ac, ac], axis=-1)
    return jnp.cos(ang).astype(dtype), jnp.sin(ang).astype(dtype)


def apply_axial_rope(x, cos, sin):
    L = x.shape[1]
    bshape = (1, L) + (1,) * (x.ndim - 3) + (HEAD_DIM,)
    xr = x.reshape(x.shape[:-1] + (2, 2, ROPE_FREQS))
    rot = jnp.stack([-xr[..., 1, :], xr[..., 0, :]], axis=-2).reshape(x.shape)
    return x * cos.reshape(bshape) + rot * sin.reshape(bshape)


def qkv_project(h, w_qkv, with_q):
    B, L, _ = h.shape
    qd = N_HEADS * HEAD_DIM
    kd = N_KV_HEADS * HEAD_DIM
    if with_q:
        y = h @ w_qkv
        q = y[..., :qd].reshape(B, L, N_KV_HEADS, Q_PER_KV, HEAD_DIM)
        kv = y[..., qd:]
    else:
        q = None
        kv = h @ w_qkv[:, qd:]
    k = kv[..., :kd].reshape(B, L, N_KV_HEADS, HEAD_DIM)
    v = kv[..., kd:].reshape(B, L, N_KV_HEADS, HEAD_DIM)
    return q, k, v


def windowed_attention(q, k, v, kc, vc, sink):
    B, L = q.shape[:2]
    nb = L // ATTN_BLOCK
    n_loc = 3 * ATTN_BLOCK
    n_ctx = kc.shape[1]
    pad = ((0, 0), (ATTN_BLOCK, ATTN_BLOCK), (0, 0), (0, 0))
    kp = jnp.pad(k, pad)
    vp = jnp.pad(v, pad)
    qb = jnp.moveaxis(q.reshape(B, nb, ATTN_BLOCK, N_KV_HEADS, Q_PER_KV, HEAD_DIM), 1, 0)
    sink_col = jnp.broadcast_to(sink[None, :, :, None, None], (B, N_KV_HEADS, Q_PER_KV, ATTN_BLOCK, 1))
    offs_q = jnp.arange(ATTN_BLOCK)
    offs_k = jnp.arange(n_loc)

    def block(args):
        qblk, n = args
        kblk = lax.dynamic_slice_in_dim(kp, n * ATTN_BLOCK, n_loc, axis=1)
        vblk = lax.dynamic_slice_in_dim(vp, n * ATTN_BLOCK, n_loc, axis=1)
        qpos = n * ATTN_BLOCK + offs_q
        kpos = (n - 1) * ATTN_BLOCK + offs_k
        valid = ((jnp.abs(kpos[None, :] - qpos[:, None]) <= WINDOW)
                 & (kpos >= 0)[None, :] & (kpos < L)[None, :])
        s_loc = jnp.einsum("bqkgd,bskd->bkgqs", qblk, kblk, preferred_element_type=jnp.float32) * ATTN_SCALE
        s_loc = jnp.where(valid, s_loc, -jnp.inf)
        s_ctx = jnp.einsum("bqkgd,bckd->bkgqc", qblk, kc, preferred_element_type=jnp.float32) * ATTN_SCALE
        p = jax.nn.softmax(jnp.concatenate([s_loc, s_ctx, sink_col], axis=-1), axis=-1).astype(v.dtype)
        o = (jnp.einsum("bkgqs,bskd->bqkgd", p[..., :n_loc], vblk)
             + jnp.einsum("bkgqc,bckd->bqkgd", p[..., n_loc:n_loc + n_ctx], vc))
        return o

    out = lax.map(block, (qb, jnp.arange(nb)))
    return jnp.moveaxis(out, 0, 1).reshape(B, L, N_HEADS * HEAD_DIM)


def context_attention(qc, kc, vc, sink):
    B, C = qc.shape[:2]
    s = jnp.einsum("bqkgd,bckd->bkgqc", qc, kc, preferred_element_type=jnp.float32) * ATTN_SCALE
    sink_col = jnp.broadcast_to(sink[None, :, :, None, None], (B, N_KV_HEADS, Q_PER_KV, C, 1))
    p = jax.nn.softmax(jnp.concatenate([s, sink_col], axis=-1), axis=-1).astype(vc.dtype)
    o = jnp.einsum("bkgqc,bckd->bqkgd", p[..., :C], vc)
    return o.reshape(B, C, N_HEADS * HEAD_DIM)


def attention_mixer(hc, hl, w_qkv, w_o, sink, cos, sin, want_ctx):
    ql, kl, vl = qkv_project(hl, w_qkv, True)
    ql = apply_axial_rope(ql, cos, sin)
    kl = apply_axial_rope(kl, cos, sin)
    qc, kc, vc = qkv_project(hc, w_qkv, want_ctx)
    sink_f = sink.astype(jnp.float32).reshape(N_KV_HEADS, Q_PER_KV)
    yl = windowed_attention(ql, kl, vl, kc, vc, sink_f) @ w_o
    yc = context_attention(qc, kc, vc, sink_f) @ w_o if want_ctx else None
    return yc, yl


def chunk_gmlp(h, w_in, b_in, ln_g, ln_b, w_s, b_s, w_out):
    B, L, _ = h.shape
    n = L // CHUNK
    z = jax.nn.gelu(h @ w_in + b_in)
    u, v = z[..., :GMLP_WIDTH], z[..., GMLP_WIDTH:]
    v = layer_norm(v, ln_g, ln_b).reshape(B, n, CHUNK, GMLP_GROUPS, GMLP_GROUP_DIM)
    s = jnp.einsum("gpq,bnqgc->bnpgc", w_s, v) + b_s[None, None, :, :, None]
    return (u * s.reshape(B, L, GMLP_WIDTH)) @ w_out


def short_gated_conv(h, w_in, w_conv, w_out):
    D = h.shape[-1]
    proj = h @ w_in
    gb, gc, z = proj[..., :D], proj[..., D:2 * D], proj[..., 2 * D:]
    z = gc * z
    zp = jnp.pad(z, ((0, 0), (1, 1), (0, 0)))
    zc = w_conv[0] * zp[:, :-2] + w_conv[1] * zp[:, 1:-1] + w_conv[2] * zp[:, 2:]
    return (gb * zc) @ w_out


def routed_moe(h, router_w, router_bias, w1, w3, w2):
    T, D = h.shape
    s = jax.nn.sigmoid(jnp.dot(h, router_w, preferred_element_type=jnp.float32))
    s_sel = s + router_bias.astype(jnp.float32)
    grp_score = lax.top_k(s_sel.reshape(T, N_EXPERT_GROUPS, EXPERTS_PER_GROUP), TOP_K)[0].sum(-1)
    g = jnp.argmax(grp_score, axis=-1)
    in_group = (jnp.arange(N_EXPERTS) // EXPERTS_PER_GROUP)[None, :] == g[:, None]
    _, idx = lax.top_k(jnp.where(in_group, s_sel, -jnp.inf), TOP_K)
    sc = jnp.take_along_axis(s, idx, axis=-1)
    gate = sc / jnp.sum(sc, axis=-1, keepdims=True)
    N = T * TOP_K
    e_flat = idx.reshape(N).astype(jnp.int32)
    tok = jnp.repeat(jnp.arange(T, dtype=jnp.int32), TOP_K)
    counts = jnp.bincount(e_flat, length=N_EXPERTS)
    padded = (counts + MOE_BLOCK - 1) // MOE_BLOCK * MOE_BLOCK
    pad_end = jnp.cumsum(padded)
    pad_start = pad_end - padded
    start = jnp.cumsum(counts) - counts
    order = jnp.argsort(e_flat, stable=True)
    e_sorted = e_flat[order]
    dest = pad_start[e_sorted] + jnp.arange(N, dtype=jnp.int32) - start[e_sorted]
    n_blocks = -(-N // MOE_BLOCK) + N_EXPERTS
    P = n_blocks * MOE_BLOCK
    row_tok = jnp.full((P,), T, jnp.int32).at[dest].set(tok[order])
    row_gate = jnp.zeros((P,), jnp.float32).at[dest].set(gate.reshape(N)[order])
    blk_expert = jnp.minimum(
        jnp.searchsorted(pad_end, jnp.arange(n_blocks, dtype=jnp.int32) * MOE_BLOCK, side="right"),
        N_EXPERTS - 1)
    h_pad = jnp.concatenate([h, jnp.zeros((1, D), h.dtype)], axis=0)
    xs = h_pad[row_tok].reshape(n_blocks, MOE_BLOCK, D)

    def expert_block(args):
        xb, e = args
        return (jax.nn.silu(xb @ w1[e]) * (xb @ w3[e])) @ w2[e]

    ys = lax.map(expert_block, (xs, blk_expert)).reshape(P, D)
    out = jax.ops.segment_sum(ys * row_gate[:, None].astype(ys.dtype), row_tok, num_segments=T + 1)
    return out[:T]


def setup_inputs(seed: int = 0) -> dict:
    key = jax.random.key(seed)
    keys = jax.random.split(key, 32)
    kgen = (keys[i] for i in range(32))
    D = D_MODEL

    def nrm(shape, scale):
        return jax.random.normal(next(kgen), shape, jnp.float32) * scale

    n_a = len(range(0, DEPTH, N_MIXERS))
    n_b = len(range(1, DEPTH, N_MIXERS))
    n_c = len(range(2, DEPTH, N_MIXERS))
    qkv_width = (N_HEADS + 2 * N_KV_HEADS) * HEAD_DIM
    return {
        "x": nrm((BATCH, SEQ, D), 1.0),
        "c": nrm((BATCH, D), 1.0),
        "ctx": nrm((BATCH, CTX_LEN, D), 1.0),
        "c_ctx": nrm((D,), 1.0),
        "w_mod": nrm((DEPTH, D, 6 * D), 0.5 * D ** -0.5),
        "b_mod": nrm((DEPTH, 6 * D), 0.02),
        "ln1_g": 1.0 + nrm((DEPTH, D), 0.02),
        "ln1_b": nrm((DEPTH, D), 0.02),
        "ln2_g": 1.0 + nrm((DEPTH, D), 0.02),
        "ln2_b": nrm((DEPTH, D), 0.02),
        "router_w": nrm((D, N_EXPERTS), D ** -0.5),
        "router_bias": nrm((N_EXPERTS,), 0.01),
        "moe_w1": nrm((DEPTH, N_EXPERTS, D, D_EXPERT), D ** -0.5),
        "moe_w3": nrm((DEPTH, N_EXPERTS, D, D_EXPERT), D ** -0.5),
        "moe_w2": nrm((DEPTH, N_EXPERTS, D_EXPERT, D), BETA * D_EXPERT ** -0.5),
        "a_w_qkv": nrm((n_a, D, qkv_width), D ** -0.5),
        "a_w_o": nrm((n_a, N_HEADS * HEAD_DIM, D), BETA * (N_HEADS * HEAD_DIM) ** -0.5),
        "a_sink": nrm((n_a, N_HEADS), 0.5),
        "b_w_in": nrm((n_b, D, 2 * GMLP_WIDTH), D ** -0.5),
        "b_b_in": nrm((n_b, 2 * GMLP_WIDTH), 0.02),
        "b_ln_g": 1.0 + nrm((n_b, GMLP_WIDTH), 0.02),
        "b_ln_b": nrm((n_b, GMLP_WIDTH), 0.02),
        "b_w_s": nrm((n_b, GMLP_GROUPS, CHUNK, CHUNK), 0.5 * CHUNK ** -0.5),
        "b_b_s": 1.0 + nrm((n_b, CHUNK, GMLP_GROUPS), 0.02),
        "b_w_out": nrm((n_b, GMLP_WIDTH, D), BETA * GMLP_WIDTH ** -0.5),
        "c_w_in": nrm((n_c, D, 3 * D), D ** -0.5),
        "c_w_conv": nrm((n_c, CONV_WIDTH, D), CONV_WIDTH ** -0.5),
        "c_w_out": nrm((n_c, D, D), BETA * D ** -0.5),
    }


def reference(x, c, ctx, c_ctx, w_mod, b_mod, ln1_g, ln1_b, ln2_g, ln2_b,
              router_w, router_bias, moe_w1, moe_w3, moe_w2,
              a_w_qkv, a_w_o, a_sink,
              b_w_in, b_b_in, b_ln_g, b_ln_b, b_w_s, b_b_s, b_w_out,
              c_w_in, c_w_conv, c_w_out):
    B, L, D = x.shape
    C = ctx.shape[1]
    cos, sin = axial_rope_tables(L, x.dtype)
    silu_c = jax.nn.silu(c)
    silu_cc = jax.nn.silu(c_ctx)
    for i in range(DEPTH):
        kind, j = i % N_MIXERS, i // N_MIXERS
        want_ctx = i < DEPTH - 1
        mod_l = (silu_c @ w_mod[i] + b_mod[i])[:, None, :]
        mod_c = (silu_cc @ w_mod[i] + b_mod[i])[None, None, :]
        sh_a, sc_a, g_a, sh_f, sc_f, g_f = jnp.split(mod_l, 6, axis=-1)
        csh_a, csc_a, cg_a, csh_f, csc_f, cg_f = jnp.split(mod_c, 6, axis=-1)
        hl = modulate(x, sh_a, sc_a)
        hc = modulate(ctx, csh_a, csc_a)
        if kind == 0:
            yc, yl = attention_mixer(hc, hl, a_w_qkv[j], a_w_o[j], a_sink[j], cos, sin, want_ctx)
        elif kind == 1:
            gm = (b_w_in[j], b_b_in[j], b_ln_g[j], b_ln_b[j], b_w_s[j], b_b_s[j], b_w_out[j])
            yl = chunk_gmlp(hl, *gm)
            yc = chunk_gmlp(hc, *gm) if want_ctx else None
        else:
            sc_w = (c_w_in[j], c_w_conv[j], c_w_out[j])
            yl = short_gated_conv(hl, *sc_w)
            yc = short_gated_conv(hc, *sc_w) if want_ctx else None
        x = layer_norm(ALPHA * x + g_a * yl, ln1_g[i], ln1_b[i])
        if want_ctx:
            ctx = layer_norm(ALPHA * ctx + cg_a * yc, ln1_g[i], ln1_b[i])
        hl = modulate(x, sh_f, sc_f)
        if want_ctx:
            hc = modulate(ctx, csh_f, csc_f)
            tokens = jnp.concatenate([hc.reshape(-1, D), hl.reshape(-1, D)], axis=0)
        else:
            tokens = hl.reshape(-1, D)
        y = routed_moe(tokens, router_w, router_bias, moe_w1[i], moe_w3[i], moe_w2[i])
        x = layer_norm(ALPHA * x + g_f * y[-B * L:].reshape(B, L, D), ln2_g[i], ln2_b[i])
        if want_ctx:
            ctx = layer_norm(ALPHA * ctx + cg_f * y[:B * C].reshape(B, C, D), ln2_g[i], ln2_b[i])
    return x
```

```python
import numpy as np
from contextlib import ExitStack
import concourse.bass as bass
import concourse.mybir as mybir
from concourse.bass_utils import run_bass_kernel_spmd

F32 = mybir.dt.float32
BF16 = mybir.dt.bfloat16
AF = mybir.ActivationFunctionType
ALU = mybir.AluOpType
AX = mybir.AxisListType


class Sched:
    ENG = ("pe", "act", "dve", "pool", "sp")

    def __init__(self, nc, es, pfx=""):
        self.nc = nc
        self.es = es
        self.pfx = pfx
        self.q = {e: [] for e in self.ENG}
        self.semh = {e: nc.alloc_semaphore(name=pfx + "s_" + e) for e in self.ENG}
        self.cnt = {e: 0 for e in self.ENG}
        self.waited = {e: {} for e in self.ENG}
        self.lastw = {}
        self.readers = {}
        self.dcnt = {}
        self.store_sems = set()

    def _deps(self, eng, reads, writes):
        need = {}
        for k in reads:
            if k in self.lastw:
                s, v = self.lastw[k]
                need[s] = max(need.get(s, 0), v)
        for k in writes:
            if k in self.lastw:
                s, v = self.lastw[k]
                need[s] = max(need.get(s, 0), v)
            for (s, v) in self.readers.get(k, ()):
                need[s] = max(need.get(s, 0), v)
        for s, v in need.items():
            if s == eng and eng == "pe":
                continue
            if self.waited[eng].get(s, 0) < v:
                self.q[eng].append(("wait", s, v))
                self.waited[eng][s] = v

    def op(self, eng, fn, reads=(), writes=()):
        psr = [k for k in reads if isinstance(k, str) and k.startswith("ps")]
        if psr:
            reads = [k for k in reads if k not in psr]
            writes = list(writes) + psr
        self._deps(eng, reads, writes)
        self.cnt[eng] += 1
        v = self.cnt[eng]
        self.q[eng].append(("op", fn))
        for k in reads:
            self.readers.setdefault(k, []).append((eng, v))
        for k in writes:
            self.lastw[k] = (eng, v)
            self.readers[k] = []

    def dma(self, queue, out, in_, sem, reads=(), writes=(), store=False, **kw):
        if sem not in self.semh:
            self.semh[sem] = self.nc.alloc_semaphore(name=self.pfx + "d_" + sem)
            self.dcnt[sem] = 0
        self._deps(queue, reads, writes)
        self.dcnt[sem] += 16
        v = self.dcnt[sem]
        self.q[queue].append(("dma", out, in_, sem, kw))
        for k in reads:
            self.readers.setdefault(k, []).append((sem, v))
        for k in writes:
            self.lastw[k] = (sem, v)
            self.readers[k] = []
        if store:
            self.store_sems.add(sem)

    def finish(self):
        for s in sorted(self.store_sems):
            self.q["sp"].append(("wait", s, self.dcnt[s]))

    def close(self):
        self.nc.clear_and_free_semaphores(list(self.semh.values()))
        self.nc.all_engine_barrier()

    def emit(self):
        nc = self.nc
        self.finish()

        def replay(e, engobj):
            for it in self.q[e]:
                if it[0] == "wait":
                    engobj.wait_ge(self.semh[it[1]], it[2])
                elif it[0] == "op":
                    it[1](engobj).then_inc(self.semh[e], 1)
                else:
                    _, out, in_, sem, kw = it
                    engobj.dma_start(out=out, in_=in_, **kw).then_inc(self.semh[sem], 16)

        with nc.Block() as block:
            @block.tensor
            def _(e):
                replay("pe", e)

            @block.scalar
            def _(e):
                replay("act", e)

            @block.vector
            def _(e):
                replay("dve", e)

            @block.gpsimd
            def _(e):
                replay("pool", e)

            @block.sync
            def _(e):
                replay("sp", e)


D = 1024
NE = 16
DEXP = 512
ALPHA = 8.0 ** 0.25
LN_EPS = 1e-5
EPS2 = LN_EPS / (ALPHA * ALPHA)


def common_consts(S, nc, sb, ps):
    ident = sb("ident", [128, 128])
    ones = sb("ones", [128, 128])
    S.op("pool", lambda e: e.memset(ones[:], 1.0), writes=["ones"])
    S.op("pool", lambda e: e.memset(ident[:], 0.0), writes=["ident"])
    S.op("pool", lambda e: e.affine_select(out=ident[:], in_=ident[:], pattern=[[-1, 128]], compare_op=ALU.not_equal,
                                          fill=1.0, base=0, channel_multiplier=1), reads=["ident"], writes=["ident"])
    return ident, ones


def mod_tiles(S, nc, sb, cvec, wmod, bmod, ident, ones, psbig, pbk, stage, stage_keys, SLC, slc_keys, outs):
    ct = sb("ct", [16, 128])
    csil = sb("csil", [128, 16])
    brow = sb("brow", [1, 512])
    S.dma("sp", ct[:], cvec.rearrange("s (k p) -> (s k) p", p=128), "ld_ct", writes=["ct"])
    S.op("pe", lambda e: e.transpose(out=psbig[:, 0:16], in_=ct[0:16, :], identity=ident[0:16, 0:16]),
         reads=["ct", "ident"], writes=[pbk])
    S.op("act", lambda e: e.activation(out=csil[:], in_=psbig[:, 0:16], func=AF.Silu), reads=[pbk], writes=["csil"])
    for s in range(2):
        for k in range(8):
            S.op("act", lambda e, s=s, k=k: e.activation(out=SLC[:, s, k, :], in_=ones[:], func=AF.Copy,
                                                         scale=csil[:, s * 8 + k:s * 8 + k + 1]),
                 reads=["ones", "csil"], writes=slc_keys)
    nv = max(o[0] for o in outs) + 1
    for v in range(nv):
        for h in range(2):
            c0 = v * 1024 + h * 512
            S.dma("sp", stage, wmod[:, c0:c0 + 512].rearrange("(k p) n -> p k n", p=128), "ld_stage", writes=stage_keys)
            S.dma("sp", brow[:], bmod[c0:c0 + 512].rearrange("(a n) -> a n", a=1), "ld_brow", writes=["brow"])
            for s in range(2):
                mine = [o for o in outs if o[0] == v and o[1] == s]
                if not mine:
                    continue
                for k in range(8):
                    S.op("pe", lambda e, s=s, k=k: e.matmul(psbig[:, 0:512], lhsT=SLC[:, s, k, :], rhs=stage[:, k, :],
                                                            start=(k == 0), stop=False),
                         reads=slc_keys + stage_keys, writes=[pbk])
                S.op("pe", lambda e: e.matmul(psbig[:, 0:512], lhsT=ones[0:1, :], rhs=brow[0:1, :], start=False, stop=True),
                     reads=["ones", "brow"], writes=[pbk])
                for (_, _, tile, key, kind) in mine:
                    dst = tile[:, h * 512:(h + 1) * 512]
                    if kind == "plain":
                        S.op("dve", lambda e, dst=dst: e.tensor_copy(out=dst, in_=psbig[:, 0:512]), reads=[pbk], writes=[key])
                    elif kind == "plus1":
                        S.op("dve", lambda e, dst=dst: e.tensor_scalar(out=dst, in0=psbig[:, 0:512], scalar1=1.0, scalar2=None,
                                                                       op0=ALU.add), reads=[pbk], writes=[key])
                    else:
                        S.op("dve", lambda e, dst=dst: e.tensor_scalar(out=dst, in0=psbig[:, 0:512], scalar1=1.0 / ALPHA,
                                                                       scalar2=None, op0=ALU.mult), reads=[pbk], writes=[key])


def layer_norm_tile(S, src, src_key, dst, dst_key, tmp, tmp_key, lng, lng_key, lnb, lnb_key, small, eps_t):
    st, mv, rstd, nmr = small
    for h in range(2):
        S.op("dve", lambda e, h=h: e.bn_stats(out=st[:, h, :], in_=src[:, h * 512:(h + 1) * 512]), reads=[src_key], writes=["ln_st"])
    S.op("dve", lambda e: e.bn_aggr(out=mv[:], in_=st[:].rearrange("p a b -> p (a b)")), reads=["ln_st"], writes=["ln_mv"])
    S.op("act", lambda e: e.activation(out=rstd[:], in_=mv[:, 1:2], func=AF.Sqrt, bias=eps_t[:], scale=1.0),
         reads=["ln_mv", "eps"], writes=["ln_rstd"])
    S.op("dve", lambda e: e.reciprocal(out=rstd[:], in_=rstd[:]), reads=["ln_rstd"], writes=["ln_rstd"])
    S.op("dve", lambda e: e.scalar_tensor_tensor(out=nmr[:], in0=mv[:, 0:1], scalar=-1.0, in1=rstd[:], op0=ALU.mult, op1=ALU.mult),
         reads=["ln_mv", "ln_rstd"], writes=["ln_nmr"])
    S.op("act", lambda e: e.activation(out=tmp, in_=src, func=AF.Identity, bias=nmr[:], scale=rstd[:]),
         reads=[src_key, "ln_nmr", "ln_rstd"], writes=[tmp_key])
    S.op("dve", lambda e: e.tensor_tensor(out=tmp, in0=tmp, in1=lng, op=ALU.mult), reads=[tmp_key, lng_key], writes=[tmp_key])
    S.op("dve", lambda e: e.tensor_tensor(out=dst, in0=tmp, in1=lnb, op=ALU.add), reads=[tmp_key, lnb_key], writes=[dst_key])


def build_moe():
    NT = 18
    nc = bass.Bass("TRN2", target_bir_lowering=False)
    dt = lambda n, s, k: nc.dram_tensor(n, s, F32, kind=k).ap()
    xin = dt("xin", [NT * 128, D], "ExternalInput")
    A = {"cvec": dt("cvec", [2, D], "ExternalInput"), "wmod": dt("wmod", [D, 3 * D], "ExternalInput"),
         "bmod": dt("bmod", [3 * D], "ExternalInput"), "lng": dt("lng", [D], "ExternalInput"), "lnb": dt("lnb", [D], "ExternalInput"),
         "rw": dt("rw", [D, NE], "ExternalInput"), "rb": dt("rb", [NE], "ExternalInput"),
         "w1": dt("w1", [NE, D, DEXP], "ExternalInput"), "w3": dt("w3", [NE, D, DEXP], "ExternalInput"),
         "w2": dt("w2", [NE, DEXP, D], "ExternalInput")}
    xout = dt("xout", [NT * 128, D], "ExternalOutput")
    tiles = [(xin[t * 128:(t + 1) * 128, :], 0 if t < 16 else 1, xout[t * 128:(t + 1) * 128, :]) for t in range(NT)]
    stage_moe(nc, "", tiles, A)
    return nc


def stage_moe(nc, pfx, tiles, A):
    NT = len(tiles)
    cvec, wmod, bmod, lng_d, lnb_d, rw_d, rb_d, w1_d, w3_d, w2_d = (A[k] for k in ("cvec", "wmod", "bmod", "lng", "lnb", "rw", "rb", "w1", "w3", "w2"))
    es = ExitStack()
    with es:
        S = Sched(nc, es, pfx)
        sb = lambda n, s, d=F32: es.enter_context(nc.sbuf_tensor(pfx + n, s, d))
        ps = lambda n, s, d=F32: es.enter_context(nc.psum_tensor(pfx + n, s, d))
        X = sb("X", [128, NT, D])
        HT = sb("HT", [128, 8, NT * 128], BF16)
        W13 = [sb(f"W13_{i}", [128, 2, 8, 256], BF16) for i in range(2)]
        W2 = [sb(f"W2_{i}", [128, 2, D], BF16) for i in range(2)]
        STG = sb("STG", [128, 6 * D])
        MOD = {("sc1", 0): STG[:, 0:D], ("sh", 0): STG[:, D:2 * D], ("sc1", 1): STG[:, 2 * D:3 * D], ("sh", 1): STG[:, 3 * D:4 * D]}
        MKEY = {("sc1", 0): "STG0", ("sh", 0): "STG1", ("sc1", 1): "STG2", ("sh", 1): "STG3"}
        for s_ in range(2):
            MOD[("gf", s_)] = sb(f"Mgf{s_}", [128, D])[:]
            MKEY[("gf", s_)] = f"Mgf{s_}"
        S13 = STG[:, 0:4 * D].rearrange("p (a k n) -> p a k n", a=2, k=8)
        S2 = STG[:, 4 * D:6 * D].rearrange("p (k n) -> p k n", k=2)
        T = [sb(f"T{i}", [128, D]) for i in range(2)]
        SA = sb("SA", [128, 2, 512])
        H1 = [sb(f"H1_{i}", [128, 2, 512], BF16) for i in range(2)]
        rw = sb("rwt", [128, 8, NE])
        rb = sb("rbt", [128, NE])
        GATE = sb("GATE", [128, NT, NE])
        eps_t = sb("eps_t", [128, 1])
        small = (sb("ln_st", [128, 2, 6]), sb("ln_mv", [128, 2]), sb("ln_rstd", [128, 1]), sb("ln_nmr", [128, 1]))
        rt = {n: sb("rt_" + n, [128, 16]) for n in ("s", "ssel", "sm", "sel", "sc")}
        rg = {n: sb("rg_" + n, [128, 4]) for n in ("p01", "q01", "p23", "q23", "t1", "m2", "m3", "gs", "ing", "pen")}
        r1 = {n: sb("r1_" + n, [128, 1]) for n in ("gmax", "den")}
        top8 = sb("top8", [128, 8])
        psY = [ps(f"psY{i}", [128, D]) for i in range(2)]
        psA = [ps(f"psA{i}", [128, 512]) for i in range(2)]
        psB = [ps(f"psB{i}", [128, 512]) for i in range(2)]

        ident, ones = common_consts(S, nc, sb, ps)
        S.op("dve", lambda e: e.memset(eps_t[:], EPS2), writes=["eps"])
        SLC = X[:, 0:2, :].rearrange("p s (k n) -> p s k n", k=8)
        stage = X[:, 2:6, :].rearrange("p a (b n) -> p (a b) n", b=2)
        outs = []
        for s in range(2):
            outs.append((0, s, MOD[("sh", s)], MKEY[("sh", s)], "plain"))
            outs.append((1, s, MOD[("sc1", s)], MKEY[("sc1", s)], "plus1"))
            outs.append((2, s, MOD[("gf", s)], MKEY[("gf", s)], "invalpha"))
        mod_tiles(S, nc, sb, cvec, wmod, bmod, ident, ones, psY[0], "psY0", stage, [("X", t) for t in range(2, 6)],
                  SLC, [("X", 0), ("X", 1)], outs)
        S.dma("sp", rw[:], rw_d.rearrange("(k p) e -> p k e", p=128), "ld_rw", writes=["rw"])
        S.dma("sp", rb[:], rb_d.partition_broadcast(128), "ld_rb", writes=["rb"])

        for t in range(NT):
            s = tiles[t][1]
            xk = ("X", t)
            S.dma("sp", X[:, t, :], tiles[t][0], f"ld_x{t}", writes=[xk])
            S.op("dve", lambda e, t=t, s=s: e.tensor_tensor(out=T[0][:], in0=X[:, t, :], in1=MOD[("sc1", s)], op=ALU.mult),
                 reads=[xk, MKEY[("sc1", s)]], writes=["T0"])
            S.op("dve", lambda e, s=s: e.tensor_tensor(out=T[0][:], in0=T[0][:], in1=MOD[("sh", s)], op=ALU.add),
                 reads=["T0", MKEY[("sh", s)]], writes=["T0"])
            pt = psY[t % 2]
            ptk = f"psY{t % 2}"
            for k in range(8):
                S.op("pe", lambda e, k=k, pt=pt: e.transpose(out=pt[:, k * 128:(k + 1) * 128], in_=T[0][:, k * 128:(k + 1) * 128],
                                                             identity=ident[:]), reads=["T0", "ident"], writes=[ptk])
            S.op("act", lambda e, t=t, pt=pt: e.activation(out=HT[:, :, t * 128:(t + 1) * 128],
                                                           in_=pt[:].rearrange("p (k n) -> p k n", k=8), func=AF.Copy),
                 reads=[ptk], writes=[("HT", t)])
            S.op("dve", lambda e, pt=pt: e.tensor_copy(out=T[1][:], in_=pt[:]), reads=[ptk], writes=["T1"])
            pr = psA[t % 2]
            prk = f"psA{t % 2}"
            for k in range(8):
                S.op("pe", lambda e, k=k, pr=pr: e.matmul(pr[:, 0:NE], lhsT=T[1][:, k * 128:(k + 1) * 128], rhs=rw[:, k, :],
                                                          start=(k == 0), stop=(k == 7)), reads=["T1", "rw"], writes=[prk])
            S.op("act", lambda e, pr=pr: e.activation(out=rt["s"][:], in_=pr[:, 0:NE], func=AF.Sigmoid), reads=[prk], writes=["rt_s"])
            dv = lambda fn, r, w: S.op("dve", fn, reads=r, writes=w)
            dv(lambda e: e.tensor_tensor(out=rt["ssel"][:], in0=rt["s"][:], in1=rb[:], op=ALU.add), ["rt_s", "rb"], ["rt_ssel"])
            sv = rt["ssel"][:].rearrange("p (g j) -> p g j", j=4)
            a, b, c, d = (sv[:, :, j] for j in range(4))
            dv(lambda e: e.tensor_tensor(out=rg["p01"][:], in0=a, in1=b, op=ALU.max), ["rt_ssel"], ["p01"])
            dv(lambda e: e.tensor_tensor(out=rg["q01"][:], in0=a, in1=b, op=ALU.min), ["rt_ssel"], ["q01"])
            dv(lambda e: e.tensor_tensor(out=rg["p23"][:], in0=c, in1=d, op=ALU.max), ["rt_ssel"], ["p23"])
            dv(lambda e: e.tensor_tensor(out=rg["q23"][:], in0=c, in1=d, op=ALU.min), ["rt_ssel"], ["q23"])
            dv(lambda e: e.tensor_tensor(out=rg["t1"][:], in0=rg["p01"][:], in1=rg["p23"][:], op=ALU.max), ["p01", "p23"], ["t1"])
            dv(lambda e: e.tensor_tensor(out=rg["m2"][:], in0=rg["p01"][:], in1=rg["p23"][:], op=ALU.min), ["p01", "p23"], ["m2"])
            dv(lambda e: e.tensor_tensor(out=rg["m3"][:], in0=rg["q01"][:], in1=rg["q23"][:], op=ALU.max), ["q01", "q23"], ["m3"])
            dv(lambda e: e.tensor_tensor(out=rg["m2"][:], in0=rg["m2"][:], in1=rg["m3"][:], op=ALU.max), ["m2", "m3"], ["m2"])
            dv(lambda e: e.tensor_tensor(out=rg["gs"][:], in0=rg["t1"][:], in1=rg["m2"][:], op=ALU.add), ["t1", "m2"], ["gs"])
            dv(lambda e: e.tensor_reduce(out=r1["gmax"][:], in_=rg["gs"][:], axis=AX.X, op=ALU.max), ["gs"], ["gmax"])
            dv(lambda e: e.tensor_scalar(out=rg["ing"][:], in0=rg["gs"][:], scalar1=r1["gmax"][:, 0:1], scalar2=None, op0=ALU.is_ge),
               ["gs", "gmax"], ["ing"])
            dv(lambda e: e.tensor_scalar(out=rg["pen"][:], in0=rg["ing"][:], scalar1=4.0, scalar2=-4.0, op0=ALU.mult, op1=ALU.add),
               ["ing"], ["pen"])
            smv = rt["sm"][:].rearrange("p (g j) -> p g j", j=4)
            for g in range(4):
                dv(lambda e, g=g: e.tensor_scalar(out=smv[:, g, :], in0=sv[:, g, :], scalar1=rg["ing"][:, g:g + 1],
                                                  scalar2=rg["pen"][:, g:g + 1], op0=ALU.mult, op1=ALU.add),
                   ["rt_ssel", "ing", "pen"], ["rt_sm"])
            dv(lambda e: e.max(out=top8[:], in_=rt["sm"][:]), ["rt_sm"], ["top8"])
            dv(lambda e: e.tensor_scalar(out=rt["sel"][:], in0=rt["sm"][:], scalar1=top8[:, 1:2], scalar2=None, op0=ALU.is_ge),
               ["rt_sm", "top8"], ["rt_sel"])
            dv(lambda e: e.tensor_tensor(out=rt["sc"][:], in0=rt["s"][:], in1=rt["sel"][:], op=ALU.mult), ["rt_s", "rt_sel"], ["rt_sc"])
            dv(lambda e: e.tensor_reduce(out=r1["den"][:], in_=rt["sc"][:], axis=AX.X, op=ALU.add), ["rt_sc"], ["den"])
            dv(lambda e: e.reciprocal(out=r1["den"][:], in_=r1["den"][:]), ["den"], ["den"])
            dv(lambda e, t=t: e.tensor_scalar(out=GATE[:, t, :], in0=rt["sc"][:], scalar1=r1["den"][:, 0:1], scalar2=None, op0=ALU.mult),
               ["rt_sc", "den"], [("GATE", t)])

        groups = [(g * 4, min(4, NT - g * 4)) for g in range((NT + 3) // 4)]
        NU = 2 * NE
        stg13_keys = ["STG0", "STG1", "STG2", "STG3"]
        stg2_keys = ["STG4", "STG5"]

        def load_unit(u):
            ex, hf = u // 2, u % 2
            S.dma("sp", S13[:, 0, :, :], w1_d[ex, :, hf * 256:(hf + 1) * 256].rearrange("(k p) n -> p k n", p=128),
                  "ld_s13", writes=stg13_keys)
            S.dma("sp", S13[:, 1, :, :], w3_d[ex, :, hf * 256:(hf + 1) * 256].rearrange("(k p) n -> p k n", p=128),
                  "ld_s13", writes=stg13_keys)
            S.dma("sp", S2, w2_d[ex, hf * 256:(hf + 1) * 256, :].rearrange("(k p) n -> p k n", p=128),
                  "ld_s2", writes=stg2_keys)

        def cast_unit(u):
            sl = u % 2
            for a in range(2):
                S.op("act", lambda e, a=a, sl=sl: e.activation(out=W13[sl][:, a, :, :], in_=S13[:, a, :, :], func=AF.Copy),
                     reads=stg13_keys, writes=[f"W13_{sl}"])
            S.op("act", lambda e, sl=sl: e.activation(out=W2[sl][:], in_=S2, func=AF.Copy), reads=stg2_keys, writes=[f"W2_{sl}"])

        yi = 0
        abi = 0
        load_unit(0)
        cast_unit(0)
        for u in range(NU):
            ex, hf = u // 2, u % 2
            sl = u % 2
            wk = [f"W13_{sl}", f"W2_{sl}"]
            if u + 1 < NU:
                load_unit(u + 1)
            for gi, (t0, ntile) in enumerate(groups):
                if gi == min(2, len(groups) - 1) and u + 1 < NU:
                    cast_unit(u + 1)
                ntok = ntile * 128
                c0 = t0 * 128
                htk = [("HT", t) for t in range(t0, t0 + ntile)]
                hb = H1[abi % 2]
                hk = f"H1_{abi % 2}"
                for dc in range(2):
                    pa, pb = psA[dc], psB[dc]
                    for k in range(8):
                        S.op("pe", lambda e, k=k, dc=dc, pa=pa, ntok=ntok, c0=c0, sl=sl: e.matmul(
                            pa[:, 0:ntok], lhsT=W13[sl][:, 0, k, dc * 128:(dc + 1) * 128], rhs=HT[:, k, c0:c0 + ntok],
                            start=(k == 0), stop=(k == 7)), reads=[wk[0]] + htk, writes=[f"psA{dc}"])
                    for k in range(8):
                        S.op("pe", lambda e, k=k, dc=dc, pb=pb, ntok=ntok, c0=c0, sl=sl: e.matmul(
                            pb[:, 0:ntok], lhsT=W13[sl][:, 1, k, dc * 128:(dc + 1) * 128], rhs=HT[:, k, c0:c0 + ntok],
                            start=(k == 0), stop=(k == 7)), reads=[wk[0]] + htk, writes=[f"psB{dc}"])
                    sa = SA[:, dc, 0:ntok]
                    S.op("act", lambda e, pa=pa, sa=sa, ntok=ntok: e.activation(out=sa, in_=pa[:, 0:ntok], func=AF.Silu),
                         reads=[f"psA{dc}"], writes=[f"SA{dc}"])
                    S.op("dve", lambda e, pb=pb, sa=sa, dc=dc, hb=hb, ntok=ntok: e.tensor_tensor(
                        out=hb[:, dc, 0:ntok], in0=sa, in1=pb[:, 0:ntok], op=ALU.mult),
                        reads=[f"SA{dc}", f"psB{dc}"], writes=[hk])
                abi += 1
                for ti in range(ntile):
                    t = t0 + ti
                    s = tiles[t][1]
                    py = psY[yi % 2]
                    pyk = f"psY{yi % 2}"
                    tm = T[yi % 2]
                    tmk = f"T{yi % 2}"
                    yi += 1
                    for h2 in range(2):
                        for dc in range(2):
                            S.op("pe", lambda e, h2=h2, dc=dc, py=py, ti=ti, hb=hb, sl=sl: e.matmul(
                                py[:, h2 * 512:(h2 + 1) * 512], lhsT=hb[:, dc, ti * 128:(ti + 1) * 128],
                                rhs=W2[sl][:, dc, h2 * 512:(h2 + 1) * 512], start=(dc == 0), stop=(dc == 1)),
                                reads=[hk, wk[1]], writes=[pyk])
                    S.op("dve", lambda e, py=py, tm=tm, t=t, s=s, ex=ex: e.scalar_tensor_tensor(
                        out=tm[:], in0=py[:], scalar=GATE[:, t, ex:ex + 1], in1=MOD[("gf", s)], op0=ALU.mult, op1=ALU.mult),
                        reads=[pyk, ("GATE", t), MKEY[("gf", s)]], writes=[tmk])
                    S.op("pool", lambda e, tm=tm, t=t: e.tensor_tensor(out=X[:, t, :], in0=X[:, t, :], in1=tm[:], op=ALU.add),
                         reads=[tmk, ("X", t)], writes=[("X", t)])

        LNG, LNB = STG[:, 0:D], STG[:, D:2 * D]
        S.dma("sp", LNG, lng_d.partition_broadcast(128), "ld_lng", writes=["STG0"])
        S.dma("sp", LNB, lnb_d.partition_broadcast(128), "ld_lnb", writes=["STG1"])
        for t in range(NT):
            o = STG[:, (2 + t % 2) * D:(3 + t % 2) * D]
            ok = f"STG{2 + t % 2}"
            layer_norm_tile(S, X[:, t, :], ("X", t), o, ok, T[0][:], "T0", LNG, "STG0", LNB, "STG1", small, eps_t)
            S.dma("sp", tiles[t][2], o, f"st{t % 2}", reads=[ok], store=True)
        S.emit()
        S.close()


class Ctx:
    pass


def std_inputs(nc):
    dt = lambda n, s, k="ExternalInput": nc.dram_tensor(n, s, F32, kind=k).ap()
    return dt, {"cvec": dt("cvec", [2, D]), "wmod": dt("wmod", [D, 3 * D]), "bmod": dt("bmod", [3 * D]), "lng": dt("lng", [D]),
                "lnb": dt("lnb", [D])}


def mixer_prologue(nc, es, pfx, A):
    C = Ctx()
    C.nc = nc
    C.es = es
    S = C.S = Sched(nc, es, pfx)
    sb = C.sb = lambda n, s, d=F32: es.enter_context(nc.sbuf_tensor(pfx + n, s, d))
    ps = C.ps = lambda n, s, d=F32: es.enter_context(nc.psum_tensor(pfx + n, s, d))
    C.cvec, C.wmod, C.bmod, C.lng_d, C.lnb_d = A["cvec"], A["wmod"], A["bmod"], A["lng"], A["lnb"]
    C.STG = [sb(f"STG{i}", [128, 4096]) for i in range(2)]
    C.stg_i = 0
    C.MOD = {}
    C.MKEY = {}
    for n in ("sh", "sc1", "ga"):
        for s in range(2):
            C.MOD[(n, s)] = sb(f"M{n}{s}", [128, D])[:]
            C.MKEY[(n, s)] = f"M{n}{s}"
    C.LNG = sb("LNG", [128, D])
    C.LNB = sb("LNB", [128, D])
    C.eps2 = sb("eps2", [128, 1])
    C.eps1 = sb("eps1", [128, 1])
    C.small = (sb("ln_st", [128, 2, 6]), sb("ln_mv", [128, 2]), sb("ln_rstd", [128, 1]), sb("ln_nmr", [128, 1]))
    C.psBig = [ps(f"psBig{i}", [128, D]) for i in range(2)]
    C.ident, C.ones = common_consts(S, nc, sb, ps)
    S.op("dve", lambda e: e.memset(C.eps2[:], EPS2), writes=["eps"])
    S.op("dve", lambda e: e.memset(C.eps1[:], LN_EPS), writes=["eps1"])
    return C


def mixer_mods(C):
    S = C.S
    stage = C.STG[0][:].rearrange("p (k n) -> p k n", k=8)
    SLC = C.STG[1][:, 0:2048].rearrange("p (s k n) -> p s k n", s=2, k=8)
    outs = []
    for s in range(2):
        outs.append((0, s, C.MOD[("sh", s)], C.MKEY[("sh", s)], "plain"))
        outs.append((1, s, C.MOD[("sc1", s)], C.MKEY[("sc1", s)], "plus1"))
        outs.append((2, s, C.MOD[("ga", s)], C.MKEY[("ga", s)], "invalpha"))
    mod_tiles(S, C.nc, C.sb, C.cvec, C.wmod, C.bmod, C.ident, C.ones, C.psBig[0], "psBig0", stage, ["STG0"], SLC, ["STG1"], outs)
    S.dma("sp", C.LNG[:], C.lng_d.partition_broadcast(128), "ld_lng", writes=["LNG"])
    S.dma("sp", C.LNB[:], C.lnb_d.partition_broadcast(128), "ld_lnb", writes=["LNB"])


def load_w_bf16(C, dst, dkey, src, kc, ncols):
    S = C.S
    cw = min(ncols, 512)
    kpp = max(1, min(kc, 4096 // cw))
    n = 0
    for c0 in range(0, ncols, cw):
        for k0 in range(0, kc, kpp):
            kk = min(kpp, kc - k0)
            i = C.stg_i % 2
            C.stg_i += 1
            st = C.STG[i][:, 0:kk * cw].rearrange("p (k n) -> p k n", k=kk)
            S.dma("sp", st, src[k0 * 128:(k0 + kk) * 128, c0:c0 + cw].rearrange("(k p) n -> p k n", p=128), f"ld_stg{i}",
                  writes=[f"STG{i}"])
            eng = "act" if n % 2 == 0 else "dve"
            n += 1
            d = dst[:, k0:k0 + kk, c0:c0 + cw]
            if eng == "act":
                S.op("act", lambda e, d=d, st=st: e.activation(out=d, in_=st, func=AF.Copy), reads=[f"STG{i}"], writes=[dkey])
            else:
                S.op("dve", lambda e, d=d, st=st: e.tensor_copy(out=d, in_=st), reads=[f"STG{i}"], writes=[dkey])


def modulate_T(C, xt, xkey, s, T0, t0key, pst, pskey, HTt, htkey):
    S = C.S
    S.op("dve", lambda e: e.tensor_tensor(out=T0, in0=xt, in1=C.MOD[("sc1", s)], op=ALU.mult), reads=[xkey, C.MKEY[("sc1", s)]], writes=[t0key])
    S.op("dve", lambda e: e.tensor_tensor(out=T0, in0=T0, in1=C.MOD[("sh", s)], op=ALU.add), reads=[t0key, C.MKEY[("sh", s)]], writes=[t0key])
    to_T(C, T0, t0key, pst, pskey, HTt, htkey)


def to_T(C, src, skey, pst, pskey, HTt, htkey):
    S = C.S
    for k in range(8):
        S.op("pe", lambda e, k=k: e.transpose(out=pst[:, k * 128:(k + 1) * 128], in_=src[:, k * 128:(k + 1) * 128], identity=C.ident[:]),
             reads=[skey, "ident"], writes=[pskey])
    S.op("act", lambda e: e.activation(out=HTt, in_=pst[:].rearrange("p (k n) -> p k n", k=8), func=AF.Copy), reads=[pskey], writes=[htkey])


def proj(C, pst_list, HTt, htkey, W, wkey, c0, ncols):
    S = C.S
    off = 0
    for (pa, pk) in pst_list:
        w = min(512, ncols - off)
        for k in range(8):
            S.op("pe", lambda e, k=k, pa=pa, w=w, off=off: e.matmul(pa[:, 0:w], lhsT=HTt[:, k, :], rhs=W[:, k, c0 + off:c0 + off + w],
                                                                    start=(k == 0), stop=(k == 7)), reads=[htkey, wkey], writes=[pk])
        off += w


def residual_ln_store(C, psy, pykey, xt, xkey, s, T1, t1key, T2, t2key, o, okey, out_ap, stsem):
    S = C.S
    S.op("dve", lambda e: e.tensor_tensor(out=T1, in0=psy[:], in1=C.MOD[("ga", s)], op=ALU.mult), reads=[pykey, C.MKEY[("ga", s)]], writes=[t1key])
    S.op("pool", lambda e: e.tensor_tensor(out=T1, in0=T1, in1=xt, op=ALU.add), reads=[t1key, xkey], writes=[t1key])
    layer_norm_tile(S, T1, t1key, o, okey, T2, t2key, C.LNG[:], "LNG", C.LNB[:], "LNB", C.small, C.eps2)
    S.dma("sp", out_ap, o, stsem, reads=[okey], store=True)


def build_gmlp():
    NT = 18
    nc = bass.Bass("TRN2", target_bir_lowering=False)
    dt, A = std_inputs(nc)
    xin = dt("xin", [NT * 128, D])
    A.update({"w_in": dt("w_in", [D, 2 * D]), "b_in": dt("b_in", [2 * D]), "g_ln_g": dt("g_ln_g", [D]), "g_ln_b": dt("g_ln_b", [D]),
              "w_s": dt("w_s", [8, 128, 128]), "b_s": dt("b_s", [128, 8]), "w_out": dt("w_out", [D, D])})
    xout = dt("xout", [NT * 128, D], "ExternalOutput")
    tiles = [(xin[t * 128:(t + 1) * 128, :], 0 if t < 16 else 1, xout[t * 128:(t + 1) * 128, :]) for t in range(NT)]
    stage_gmlp(nc, "", tiles, A)
    return nc


def stage_gmlp(nc, pfx, tiles, A):
    NT = len(tiles)
    es = ExitStack()
    with es:
        C = mixer_prologue(nc, es, pfx, A)
        S, sb, ps = C.S, C.sb, C.ps
        win_d, bin_d, glng_d, glnb_d, ws_d, bs_d, wout_d = (A[k] for k in ("w_in", "b_in", "g_ln_g", "g_ln_b", "w_s", "b_s", "w_out"))
        Win = sb("Win", [128, 8, 2 * D], BF16)
        Wout = sb("Wout", [128, 8, D], BF16)
        wsT = sb("wsT", [128, 8, 128], BF16)
        BIN = sb("BIN", [128, 2 * D])
        GLNG = sb("GLNG", [128, D])
        GLNB = sb("GLNB", [128, D])
        bs = sb("bs", [128, 8])
        Xt = [sb(f"Xt{i}", [128, D]) for i in range(2)]
        OUT = [sb(f"OUT{i}", [128, D]) for i in range(2)]
        T0 = sb("T0", [128, D]); T1 = sb("T1", [128, D]); T2 = sb("T2", [128, D])
        HTt = sb("HTt", [128, 8, 128], BF16)
        HT2 = sb("HT2", [128, 8, 128], BF16)
        Z = sb("Z", [128, 2 * D])
        TZ = sb("TZ", [128, 2 * D])
        TZ2 = sb("TZ2", [128, 2 * D])
        VN = sb("VN", [128, D], BF16)
        US = sb("US", [128, D])
        psZ = [ps(f"psZ{i}", [128, 512]) for i in range(4)]
        mixer_mods(C)
        load_w_bf16(C, Win[:], "Win", win_d, 8, 2 * D)
        load_w_bf16(C, Wout[:], "Wout", wout_d, 8, D)
        S.dma("sp", BIN[:], bin_d.partition_broadcast(128), "ld_bin", writes=["BIN"])
        S.dma("sp", GLNG[:], glng_d.partition_broadcast(128), "ld_glng", writes=["GLNG"])
        S.dma("sp", GLNB[:], glnb_d.partition_broadcast(128), "ld_glnb", writes=["GLNB"])
        S.dma("sp", bs[:], bs_d, "ld_bs", writes=["bs"])
        wst = C.STG[0][:, 0:1024].rearrange("p (g q) -> p g q", g=8)
        S.dma("sp", wst, ws_d.rearrange("g p q -> p g q"), "ld_stg0", writes=["STG0"])
        for g in range(8):
            S.op("pe", lambda e, g=g: e.transpose(out=C.psBig[0][:, g * 128:(g + 1) * 128], in_=wst[:, g, :], identity=C.ident[:]),
                 reads=["STG0", "ident"], writes=["psBig0"])
        S.op("act", lambda e: e.activation(out=wsT[:], in_=C.psBig[0][:].rearrange("p (g n) -> p g n", g=8), func=AF.Copy),
             reads=["psBig0"], writes=["wsT"])
        for t in range(NT):
            s = tiles[t][1]
            xt = Xt[t % 2][:]
            xk = f"Xt{t % 2}"
            S.dma("sp", xt, tiles[t][0], f"ld_x{t % 2}", writes=[xk])
            modulate_T(C, xt, xk, s, T0[:], "T0", C.psBig[0], "psBig0", HTt[:], "HTt")
            proj(C, [(psZ[i], f"psZ{i}") for i in range(4)], HTt, "HTt", Win, "Win", 0, 2 * D)
            for i in range(4):
                S.op("dve", lambda e, i=i: e.tensor_tensor(out=Z[:, i * 512:(i + 1) * 512], in0=psZ[i][:], in1=BIN[:, i * 512:(i + 1) * 512],
                                                           op=ALU.add), reads=[f"psZ{i}", "BIN"], writes=["Z"])
            S.op("act", lambda e: e.activation(out=TZ[:], in_=Z[:], func=AF.Square), reads=["Z"], writes=["TZ"])
            S.op("dve", lambda e: e.tensor_scalar(out=TZ[:], in0=TZ[:], scalar1=0.044715, scalar2=1.0, op0=ALU.mult, op1=ALU.add),
                 reads=["TZ"], writes=["TZ"])
            S.op("pool", lambda e: e.tensor_tensor(out=TZ[:], in0=TZ[:], in1=Z[:], op=ALU.mult), reads=["TZ", "Z"], writes=["TZ"])
            S.op("act", lambda e: e.activation(out=TZ[:], in_=TZ[:], func=AF.Sigmoid, scale=1.5957691216057308), reads=["TZ"], writes=["TZ"])
            S.op("pool", lambda e: e.tensor_tensor(out=TZ2[:], in0=TZ[:], in1=Z[:], op=ALU.mult), reads=["TZ", "Z"], writes=["TZ2"])
            layer_norm_tile(S, TZ2[:, D:2 * D], "TZ2", VN[:], "VN", T2[:], "T2", GLNG[:], "GLNG", GLNB[:], "GLNB", C.small, C.eps1)
            for g in range(8):
                S.op("pe", lambda e, g=g: e.matmul(C.psBig[0][:, g * 128:(g + 1) * 128], lhsT=wsT[:, g, :], rhs=VN[:, g * 128:(g + 1) * 128],
                                                   start=True, stop=True), reads=["wsT", "VN"], writes=["psBig0"])
            for g in range(8):
                S.op("dve", lambda e, g=g: e.scalar_tensor_tensor(out=US[:, g * 128:(g + 1) * 128], in0=C.psBig[0][:, g * 128:(g + 1) * 128],
                                                                  scalar=bs[:, g:g + 1], in1=TZ2[:, g * 128:(g + 1) * 128],
                                                                  op0=ALU.add, op1=ALU.mult), reads=["psBig0", "bs", "TZ2"], writes=["US"])
            to_T(C, US[:], "US", C.psBig[1], "psBig1", HT2[:], "HT2")
            proj(C, [(C.psBig[1][:, 0:512], "psBig1"), (C.psBig[1][:, 512:1024], "psBig1")], HT2, "HT2", Wout, "Wout", 0, D)
            residual_ln_store(C, C.psBig[1], "psBig1", xt, xk, s, T1[:], "T1", T2[:], "T2", OUT[t % 2][:], f"OUT{t % 2}",
                              tiles[t][2], f"st{t % 2}")
        S.emit()
        S.close()


def build_conv():
    nc = bass.Bass("TRN2", target_bir_lowering=False)
    dt, A = std_inputs(nc)
    xin = dt("xin", [20 * 128, D])
    A.update({"valid": dt("valid", [128, 20]), "w_in": dt("w_in", [D, 3 * D]), "w_conv": dt("w_conv", [3, D]), "w_out": dt("w_out", [D, D])})
    xout = dt("xout", [18 * 128, D], "ExternalOutput")
    lat = [(xin[e * 128:(e + 1) * 128, :], 0, e, (xout[(e - 1) * 128:e * 128, :] if 1 <= e <= 16 else None)) for e in range(18)]
    ctx = [(xin[e * 128:(e + 1) * 128, :], 1, e, xout[(e - 2) * 128:(e - 1) * 128, :]) for e in (18, 19)]
    stage_conv(nc, "", [lat, ctx], A, 20)
    return nc


def stage_conv(nc, pfx, seqs, A, nvalid):
    es = ExitStack()
    with es:
        C = mixer_prologue(nc, es, pfx, A)
        S, sb, ps = C.S, C.sb, C.ps
        valid_d, win_d, wconv_d, wout_d = (A[k] for k in ("valid", "w_in", "w_conv", "w_out"))
        Win = sb("Win", [128, 8, 3 * D], BF16)
        Wout = sb("Wout", [128, 8, D], BF16)
        WC = [sb(f"WC{i}", [128, D]) for i in range(3)]
        valid = sb("valid_sb", [128, nvalid])
        Xr = [sb(f"Xr{i}", [128, D]) for i in range(4)]
        Zr = [C.STG[1][:, i * D:(i + 1) * D] for i in range(4)]
        HTr = [sb(f"HTr{i}", [128, 8, 128], BF16) for i in range(4)]
        ZM = sb("ZM", [128, D]); ZP = sb("ZP", [128, D]); TC = sb("TC", [128, D]); TG = sb("TG", [128, D])
        OUT = [sb(f"OUT{i}", [128, D]) for i in range(2)]
        T0 = sb("T0", [128, D]); T1 = sb("T1", [128, D]); T2 = sb("T2", [128, D])
        HT2 = sb("HT2", [128, 8, 128], BF16)
        psP = [ps(f"psP{i}", [128, 512]) for i in range(4)]
        mixer_mods(C)
        load_w_bf16(C, Win[:], "Win", win_d, 8, 3 * D)
        load_w_bf16(C, Wout[:], "Wout", wout_d, 8, D)
        for i in range(3):
            S.dma("sp", WC[i][:], wconv_d[i, :].partition_broadcast(128), f"ld_wc{i}", writes=[f"WC{i}"])
        S.dma("sp", valid[:], valid_d, "ld_valid", writes=["valid"])
        S.op("dve", lambda e: e.memset(ZM[0:1, 0:1], 0.0), writes=["STG1", "Zr0", "Zr1", "Zr2", "Zr3"])

        cnt = {"a": 0, "o": 0}

        def Astep(tile):
            src, s, vcol, _ = tile
            r = cnt["a"] % 4
            cnt["a"] += 1
            xt, xk = Xr[r][:], f"Xr{r}"
            S.dma("sp", xt, src, f"ld_x{r}", writes=[xk])
            modulate_T(C, xt, xk, s, T0[:], "T0", C.psBig[0], "psBig0", HTr[r][:], f"HTr{r}")
            proj(C, [(psP[i], f"psP{i}") for i in range(4)], HTr[r], f"HTr{r}", Win, "Win", D, 2 * D)
            for h in range(2):
                S.op("act", lambda e_, h=h: e_.activation(out=TG[:, h * 512:(h + 1) * 512], in_=psP[h][:], func=AF.Copy, scale=valid[:, vcol:vcol + 1]),
                     reads=[f"psP{h}", "valid"], writes=["TG"])
            for h in range(2):
                S.op("dve", lambda e_, h=h: e_.tensor_tensor(out=Zr[r][:, h * 512:(h + 1) * 512], in0=TG[:, h * 512:(h + 1) * 512], in1=psP[2 + h][:],
                                                            op=ALU.mult), reads=["TG", f"psP{2 + h}"], writes=[f"Zr{r}"])
            return r

        def Bstep(tile, r, prev, nxt):
            _, s, _, out_ap = tile
            ot = cnt["o"]
            cnt["o"] += 1
            xt, xk = Xr[r][:], f"Xr{r}"
            zc, zk = Zr[r], f"Zr{r}"
            if prev is None:
                S.op("dve", lambda e_: e_.memset(ZM[:], 0.0), writes=["ZM"])
            if nxt is None:
                S.op("dve", lambda e_: e_.memset(ZP[:], 0.0), writes=["ZP"])
            S.dma("sp", ZM[1:128, :], zc[0:127, :], "sh_zm", reads=[zk], writes=["ZM"])
            if prev is not None:
                S.dma("sp", ZM[0:1, :], Zr[prev][127:128, :], "sh_zm", reads=[f"Zr{prev}"], writes=["ZM"])
            S.dma("sp", ZP[0:127, :], zc[1:128, :], "sh_zp", reads=[zk], writes=["ZP"])
            if nxt is not None:
                S.dma("sp", ZP[127:128, :], Zr[nxt][0:1, :], "sh_zp", reads=[f"Zr{nxt}"], writes=["ZP"])
            S.op("dve", lambda e_: e_.tensor_tensor(out=ZM[:], in0=ZM[:], in1=WC[0][:], op=ALU.mult), reads=["ZM", "WC0"], writes=["ZM"])
            S.op("pool", lambda e_: e_.tensor_tensor(out=ZP[:], in0=ZP[:], in1=WC[2][:], op=ALU.mult), reads=["ZP", "WC2"], writes=["ZP"])
            S.op("dve", lambda e_: e_.tensor_tensor(out=TC[:], in0=zc, in1=WC[1][:], op=ALU.mult), reads=[zk, "WC1"], writes=["TC"])
            S.op("pool", lambda e_: e_.tensor_tensor(out=TC[:], in0=TC[:], in1=ZM[:], op=ALU.add), reads=["TC", "ZM"], writes=["TC"])
            S.op("dve", lambda e_: e_.tensor_tensor(out=TC[:], in0=TC[:], in1=ZP[:], op=ALU.add), reads=["TC", "ZP"], writes=["TC"])
            proj(C, [(C.psBig[1][:, 0:512], "psBig1"), (C.psBig[1][:, 512:1024], "psBig1")], HTr[r], f"HTr{r}", Win, "Win", 0, D)
            S.op("dve", lambda e_: e_.tensor_tensor(out=TG[:], in0=C.psBig[1][:], in1=TC[:], op=ALU.mult), reads=["psBig1", "TC"], writes=["TG"])
            to_T(C, TG[:], "TG", C.psBig[1], "psBig1", HT2[:], "HT2")
            proj(C, [(C.psBig[1][:, 0:512], "psBig1"), (C.psBig[1][:, 512:1024], "psBig1")], HT2, "HT2", Wout, "Wout", 0, D)
            residual_ln_store(C, C.psBig[1], "psBig1", xt, xk, s, T1[:], "T1", T2[:], "T2", OUT[ot % 2][:], f"OUT{ot % 2}",
                              out_ap, f"st{ot % 2}")

        for seq in seqs:
            slots = {}
            n = len(seq)
            for i in range(n + 1):
                if i < n:
                    slots[i] = Astep(seq[i])
                j = i - 1
                if j >= 0 and seq[j][3] is not None:
                    Bstep(seq[j], slots[j], slots.get(j - 1), slots.get(j + 1) if j + 1 < n else None)
        S.emit()
        S.close()


def build_attn(want_ctx):
    nc = bass.Bass("TRN2", target_bir_lowering=False)
    dt, A = std_inputs(nc)
    NOUT = 18 if want_ctx else 16
    xin = dt("xin", [20 * 128, D])
    A.update({"kbias": dt("kbias", [128, 20]), "cos_t": dt("cos_t", [128, 20 * 64]), "sin_t": dt("sin_t", [128, 20 * 64]),
              "maskp": dt("maskp", [128, 512]), "maskn": dt("maskn", [128, 512]), "w_qkv": dt("w_qkv", [D, 1536]),
              "w_o": dt("w_o", [D, D]), "sink": dt("sink", [16])})
    xout = dt("xout", [NOUT * 128, D], "ExternalOutput")
    kv = [(xin[e * 128:(e + 1) * 128, :], 0 if e < 18 else 1, e, e) for e in range(20)]
    q = [(xin[(o + 1) * 128:(o + 2) * 128, :], 0, o + 1, [(o, "P"), (o + 1, None), (o + 2, "N"), (18, None), (19, None)],
          xout[o * 128:(o + 1) * 128, :]) for o in range(16)]
    if want_ctx:
        q += [(xin[e * 128:(e + 1) * 128, :], 1, e, [(18, None), (19, None)], xout[(e - 2) * 128:(e - 1) * 128, :]) for e in (18, 19)]
    stage_attn(nc, "", kv, q, A, 20)
    return nc


def stage_attn(nc, pfx, kv_tiles, q_tiles, A, ntbl):
    NKT = len(kv_tiles)
    es = ExitStack()
    with es:
        C = mixer_prologue(nc, es, pfx, A)
        S, sb, ps = C.S, C.sb, C.ps
        kbias_d, cos_d, sin_d, maskp_d, maskn_d, wqkv_d, wo_d, sink_d = (A[k] for k in ("kbias", "cos_t", "sin_t", "maskp", "maskn", "w_qkv", "w_o", "sink"))
        Wqkv = sb("Wqkv", [128, 8, 1536], BF16)
        Wo = sb("Wo", [128, 8, D], BF16)
        KT = sb("KT", [128, 2, NKT * 128], BF16)
        V = sb("V", [128, NKT, 256], BF16)
        kbias = sb("kbias_sb", [128, ntbl])
        COS = sb("COS", [128, ntbl, 64])
        SIN = sb("SIN", [128, ntbl, 64])
        MP = sb("MP", [128, 512])
        MN = sb("MN", [128, 512])
        SINKB = sb("SINKB", [128, 2, 512])
        ES = sb("ES", [128, 16])
        identb = sb("identb", [128, 128], BF16)
        onesb = sb("onesb", [128, 64], BF16)
        Xt = [sb(f"Xt{i}", [128, D]) for i in range(2)]
        OUT = [sb(f"OUT{i}", [128, D]) for i in range(2)]
        T0 = sb("T0", [128, D]); T1 = sb("T1", [128, D]); T2 = sb("T2", [128, D])
        HTt = sb("HTt", [128, 8, 128], BF16)
        R1 = sb("R1", [128, D]); R2 = sb("R2", [128, D])
        KR = sb("KR", [128, 256], BF16)
        QRp = sb("QRp", [128, 8, 128], BF16)
        QT = sb("QT", [128, 8, 128], BF16)
        PT = [sb(f"PT{i}", [128, 512], BF16) for i in range(3)]
        DEN = sb("DEN", [128, 512])
        OT = sb("OT", [128, 2, 512], BF16)
        psS = [ps(f"psS{i}", [128, 512]) for i in range(2)]
        psX = ps("psX", [128, 8, 128], BF16)
        psO = C.psBig[1][:, 0:512]
        psD = C.psBig[1][:, 512:1024]
        S.op("pool", lambda e: e.memset(onesb[:], 1.0), writes=["onesb"])
        S.op("pool", lambda e: e.tensor_copy(out=identb[:], in_=C.ident[:]), reads=["ident"], writes=["identb"])
        mixer_mods(C)
        load_w_bf16(C, Wqkv[:], "Wqkv", wqkv_d, 8, 1536)
        for half in range(2):
            i = C.stg_i % 2
            C.stg_i += 1
            st = C.STG[i][:].rearrange("p (c n) -> p c n", c=4)
            for cc in range(4):
                c = half * 4 + cc
                jp, g = c // 4, c % 4
                for r in range(2):
                    row = 512 * jp + 256 * r + 64 * g
                    S.dma("sp", st[r * 64:(r + 1) * 64, cc, :], wo_d[row:row + 64, :], f"ld_stg{i}", writes=[f"STG{i}"])
            S.op("act", lambda e, st=st, half=half: e.activation(out=Wo[:, half * 4:(half + 1) * 4, :], in_=st, func=AF.Copy),
                 reads=[f"STG{i}"], writes=["Wo"])
        S.dma("sp", kbias[:], kbias_d, "ld_kb", writes=["kbias"])
        S.dma("sp", COS[:].rearrange("p t n -> p (t n)"), cos_d, "ld_cos", writes=["COS"])
        S.dma("sp", SIN[:].rearrange("p t n -> p (t n)"), sin_d, "ld_sin", writes=["SIN"])
        S.dma("sp", MP[:], maskp_d, "ld_mp", writes=["MP"])
        S.dma("sp", MN[:], maskn_d, "ld_mn", writes=["MN"])
        S.dma("sp", ES[:], sink_d.partition_broadcast(128), "ld_sink", writes=["ES"])
        S.op("act", lambda e: e.activation(out=ES[:], in_=ES[:], func=AF.Exp), reads=["ES"], writes=["ES"])
        for jp in range(2):
            for g in range(4):
                for r in range(2):
                    h = 8 * jp + 4 * r + g
                    S.op("act", lambda e, jp=jp, g=g, r=r, h=h: e.activation(
                        out=SINKB[r * 64:(r + 1) * 64, jp, g * 128:(g + 1) * 128], in_=C.ones[r * 64:(r + 1) * 64, :], func=AF.Copy,
                        scale=ES[r * 64:(r + 1) * 64, h:h + 1]), reads=["ones", "ES"], writes=["SINKB"])

        def rope(src_ps, pskey, nh, e, out_fn):
            n = nh * 64
            xv = src_ps.rearrange("p (h b f i) -> p h b f i", h=nh, b=2, f=2)
            r1v = R1[:, 0:n].rearrange("p (h n) -> p h n", h=nh)
            r2v = R2[:, 0:n].rearrange("p (h b f i) -> p h b f i", h=nh, b=2, f=2)
            cosb = COS[:, e, :].unsqueeze(1).to_broadcast([128, nh, 64])
            sv = SIN[:, e, :].rearrange("p (b f i) -> p b f i", b=2, f=2)
            S.op("dve", lambda e_: e_.tensor_tensor(out=r1v, in0=src_ps.rearrange("p (h n) -> p h n", h=nh), in1=cosb, op=ALU.mult),
                 reads=[pskey, "COS"], writes=["R1"])
            for f in range(2):
                sb_ = sv[:, :, f, :].unsqueeze(1).to_broadcast([128, nh, 2, 16])
                S.op("dve", lambda e_, f=f, sb_=sb_: e_.tensor_tensor(out=r2v[:, :, :, f, :], in0=xv[:, :, :, 1 - f, :], in1=sb_, op=ALU.mult),
                     reads=[pskey, "SIN"], writes=["R2"])
            out_fn(n)

        for e in range(NKT):
            src_, s, tbl, kbc = kv_tiles[e]
            xt, xk = Xt[e % 2][:], f"Xt{e % 2}"
            S.dma("sp", xt, src_, f"ld_x{e % 2}", writes=[xk])
            modulate_T(C, xt, xk, s, T0[:], "T0", C.psBig[0], "psBig0", HTt[:], "HTt")
            proj(C, [(psS[0], "psS0")], HTt, "HTt", Wqkv, "Wqkv", 1024, 512)

            def kout(n):
                S.op("pool", lambda e_: e_.tensor_tensor(out=KR[:], in0=R1[:, 0:256], in1=R2[:, 0:256], op=ALU.add), reads=["R1", "R2"], writes=["KR"])
            S.op("act", lambda e_, e=e: e_.activation(out=V[:, e, :], in_=psS[0][:, 256:512], func=AF.Copy), reads=["psS0"], writes=["V"])
            rope(psS[0][:, 0:256], "psS0", 4, tbl, kout)
            for jp in range(2):
                S.op("pe", lambda e_, jp=jp: e_.transpose(out=psX[:, jp, :], in_=KR[:, jp * 128:(jp + 1) * 128], identity=identb[:]),
                     reads=["KR", "identb"], writes=["psX"])
            S.op("act", lambda e_, e=e: e_.activation(out=KT[:, :, e * 128:(e + 1) * 128], in_=psX[:, 0:2, :], func=AF.Copy),
                 reads=["psX"], writes=["KT"])

        pti = 0
        for o, (src_, s, tbl, chunks_, out_ap) in enumerate(q_tiles):
            xt, xk = Xt[o % 2][:], f"Xt{o % 2}"
            S.dma("sp", xt, src_, f"ld_x{o % 2}", writes=[xk])
            modulate_T(C, xt, xk, s, T0[:], "T0", C.psBig[0], "psBig0", HTt[:], "HTt")
            proj(C, [(C.psBig[0][:, 0:512], "psBig0"), (C.psBig[0][:, 512:1024], "psBig0")], HTt, "HTt", Wqkv, "Wqkv", 0, D)

            def qout(n):
                for jp in range(2):
                    a = R1[:, jp * 512:(jp + 1) * 512].rearrange("p (r g d) -> p r g d", r=2, g=4)
                    b = R2[:, jp * 512:(jp + 1) * 512].rearrange("p (r g d) -> p r g d", r=2, g=4)
                    o_ = QRp[:, jp * 4:(jp + 1) * 4, :].rearrange("p g (r d) -> p r g d", r=2)
                    S.op("pool", lambda e_, a=a, b=b, o_=o_: e_.tensor_tensor(out=o_, in0=a, in1=b, op=ALU.add), reads=["R1", "R2"], writes=["QRp"])
            rope(C.psBig[0][:], "psBig0", 16, tbl, qout)
            for c in range(8):
                S.op("pe", lambda e_, c=c: e_.transpose(out=psX[:, c, :], in_=QRp[:, c, :], identity=identb[:]), reads=["QRp", "identb"], writes=["psX"])
            S.op("act", lambda e_: e_.activation(out=QT[:], in_=psX[:], func=AF.Copy), reads=["psX"], writes=["QT"])
            chunks = [(kt, {"P": MP, "N": MN, None: None}[m]) for (kt, m) in chunks_]
            for jp in range(2):
                for r in range(2):
                    j = 2 * jp + r
                    lo, hi = r * 64, (r + 1) * 64
                    for ci, (kt, mask) in enumerate(chunks):
                        kbc = kv_tiles[kt][3]
                        pss, psk = psS[pti % 2], f"psS{pti % 2}"
                        pt, ptk = PT[pti % 3], f"PT{pti % 3}"
                        pti += 1
                        S.op("pe", lambda e_, pss=pss, kt=kt, jp=jp, lo=lo, hi=hi: e_.matmul(
                            pss[:], lhsT=KT[lo:hi, jp, kt * 128:(kt + 1) * 128], rhs=QT[lo:hi, jp * 4:(jp + 1) * 4, :], start=True, stop=True),
                            reads=["KT", "QT"], writes=[psk])
                        S.op("act", lambda e_, pss=pss, pt=pt, kbc=kbc: e_.activation(out=pt[:], in_=pss[:], func=AF.Exp, bias=kbias[:, kbc:kbc + 1], scale=0.125),
                             reads=[psk, "kbias"], writes=[ptk])
                        if mask is not None:
                            S.op("dve", lambda e_, pt=pt, mask=mask: e_.tensor_tensor(out=pt[:], in0=pt[:], in1=mask[:], op=ALU.mult),
                                 reads=[ptk, "MP", "MN"], writes=[ptk])
                        first, last = (ci == 0), (ci == len(chunks) - 1)
                        S.op("pe", lambda e_, pt=pt, kt=kt, j=j, lo=lo, hi=hi, first=first, last=last: e_.matmul(
                            psO[lo:hi, :], lhsT=V[:, kt, j * 64:(j + 1) * 64], rhs=pt[:], start=first, stop=last),
                            reads=["V", ptk], writes=["psO"])
                        S.op("pe", lambda e_, pt=pt, lo=lo, hi=hi, first=first, last=last: e_.matmul(
                            psD[lo:hi, :], lhsT=onesb[:, 0:64], rhs=pt[:], start=first, stop=last),
                            reads=["onesb", ptk], writes=["psD"])
                S.op("dve", lambda e_, jp=jp: e_.tensor_tensor(out=DEN[:], in0=psD, in1=SINKB[:, jp, :], op=ALU.add), reads=["psD", "SINKB"], writes=["DEN"])
                S.op("dve", lambda e_: e_.reciprocal(out=DEN[:], in_=DEN[:]), reads=["DEN"], writes=["DEN"])
                S.op("dve", lambda e_, jp=jp: e_.tensor_tensor(out=OT[:, jp, :], in0=psO, in1=DEN[:], op=ALU.mult), reads=["psO", "DEN"], writes=["OT"])
            for half in range(2):
                for c in range(8):
                    jp, g = c // 4, c % 4
                    S.op("pe", lambda e_, half=half, c=c, jp=jp, g=g: e_.matmul(
                        C.psBig[0][:, half * 512:(half + 1) * 512], lhsT=OT[:, jp, g * 128:(g + 1) * 128], rhs=Wo[:, c, half * 512:(half + 1) * 512],
                        start=(c == 0), stop=(c == 7)), reads=["OT", "Wo"], writes=["psBig0"])
            residual_ln_store(C, C.psBig[0], "psBig0", xt, xk, s, T1[:], "T1", T2[:], "T2", OUT[o % 2][:], f"OUT{o % 2}",
                              out_ap, f"st{o % 2}")
        S.emit()
        S.close()


NWIN = 22
NEXT = 20


def build_fused(nstage=8, dbg=False):
    nc = bass.Bass("TRN2", target_bir_lowering=False)
    dt = lambda n, s, k="ExternalInput": nc.dram_tensor(n, s, F32, kind=k).ap()
    xw = dt("xw", [NWIN * 128, D])
    ctx = dt("ctx", [256, D])
    cvec = dt("cvec", [2, D])
    w_mod = dt("w_mod", [4, D, 6 * D]); b_mod = dt("b_mod", [4, 6 * D])
    ln1_g = dt("ln1_g", [4, D]); ln1_b = dt("ln1_b", [4, D]); ln2_g = dt("ln2_g", [4, D]); ln2_b = dt("ln2_b", [4, D])
    rw = dt("router_w", [D, NE]); rb = dt("router_bias", [NE])
    w1 = dt("moe_w1", [4, NE, D, DEXP]); w3 = dt("moe_w3", [4, NE, D, DEXP]); w2 = dt("moe_w2", [4, NE, DEXP, D])
    a_w_qkv = dt("a_w_qkv", [2, D, 1536]); a_w_o = dt("a_w_o", [2, D, D]); a_sink = dt("a_sink", [2, 16])
    b_w_in = dt("b_w_in", [1, D, 2 * D]); b_b_in = dt("b_b_in", [1, 2 * D]); b_ln_g = dt("b_ln_g", [1, D]); b_ln_b = dt("b_ln_b", [1, D])
    b_w_s = dt("b_w_s", [1, 8, 128, 128]); b_b_s = dt("b_b_s", [1, 128, 8]); b_w_out = dt("b_w_out", [1, D, D])
    c_w_in = dt("c_w_in", [1, D, 3 * D]); c_w_conv = dt("c_w_conv", [1, 3, D]); c_w_out = dt("c_w_out", [1, D, D])
    kbias = dt("kbias", [128, 24]); cos_t = dt("cos_t", [128, 24 * 64]); sin_t = dt("sin_t", [128, 24 * 64])
    maskp = dt("maskp", [128, 512]); maskn = dt("maskn", [128, 512]); valid = dt("valid", [128, 22])
    SA = dt("scrA", [22 * 128, D], "ExternalOutput" if dbg else "Internal")
    SB = dt("scrB", [22 * 128, D], "ExternalOutput" if dbg else "Internal")
    xout = dt("xout", [2048, D], "ExternalOutput")
    row = lambda T, i: T[i * 128:(i + 1) * 128, :]

    def AM(L):
        return {"cvec": cvec, "wmod": w_mod[L][:, 3 * D:6 * D], "bmod": b_mod[L][3 * D:6 * D], "lng": ln2_g[L], "lnb": ln2_b[L],
                "rw": rw, "rb": rb, "w1": w1[L], "w3": w3[L], "w2": w2[L]}

    def AX_(L):
        return {"cvec": cvec, "wmod": w_mod[L][:, 0:3 * D], "bmod": b_mod[L][0:3 * D], "lng": ln1_g[L], "lnb": ln1_b[L]}

    def moe(L, pfx, tiles):
        h = (len(tiles) + 1) // 2 if len(tiles) > 18 else len(tiles)
        stage_moe(nc, pfx + "a_", tiles[:h], AM(L))
        if h < len(tiles):
            stage_moe(nc, pfx + "b_", tiles[h:], AM(L))

    attn_tabs = {"kbias": kbias, "cos_t": cos_t, "sin_t": sin_t, "maskp": maskp, "maskn": maskn}
    kv = [(row(xw, v), 0, v, v) for v in range(NWIN)] + [(row(ctx, c), 1, 22 + c, 22 + c) for c in range(2)]
    q = [(row(xw, u + 1), 0, u + 1, [(u, "P"), (u + 1, None), (u + 2, "N"), (22, None), (23, None)], row(SA, u)) for u in range(NEXT)]
    q += [(row(ctx, c), 1, 22 + c, [(22, None), (23, None)], row(SA, 20 + c)) for c in range(2)]
    A = AX_(0); A.update(attn_tabs); A.update({"w_qkv": a_w_qkv[0], "w_o": a_w_o[0], "sink": a_sink[0]})
    stage_attn(nc, "s0_", kv, q, A, 24)
    if nstage <= 1:
        return nc
    allt = lambda Tsrc, Tdst: [(row(Tsrc, u), 0 if u < NEXT else 1, row(Tdst, u)) for u in range(22)]
    moe(0, "m0", allt(SA, SB))
    if nstage <= 2:
        return nc
    A = AX_(1); A.update({"w_in": b_w_in[0], "b_in": b_b_in[0], "g_ln_g": b_ln_g[0], "g_ln_b": b_ln_b[0], "w_s": b_w_s[0], "b_s": b_b_s[0],
                          "w_out": b_w_out[0]})
    stage_gmlp(nc, "s1_", allt(SB, SA), A)
    if nstage <= 3:
        return nc
    moe(1, "m1", allt(SA, SB))
    if nstage <= 4:
        return nc
    lat = [(row(SB, u), 0, u, (row(SA, u) if 1 <= u <= 18 else None)) for u in range(NEXT)]
    cx = [(row(SB, 20 + c), 1, 20 + c, row(SA, 20 + c)) for c in range(2)]
    A = AX_(2); A.update({"valid": valid, "w_in": c_w_in[0], "w_conv": c_w_conv[0], "w_out": c_w_out[0]})
    stage_conv(nc, "s2_", [lat, cx], A, 22)
    if nstage <= 5:
        return nc
    t2 = [(row(SA, u), 0, row(SB, u)) for u in range(1, 19)] + [(row(SA, 20 + c), 1, row(SB, 20 + c)) for c in range(2)]
    moe(2, "m2", t2)
    if nstage <= 6:
        return nc
    kv = [(row(SB, u), 0, u + 1, u + 1) for u in range(1, 19)] + [(row(SB, 20 + c), 1, 22 + c, 22 + c) for c in range(2)]
    q = [(row(SB, u), 0, u + 1, [(u - 2, "P"), (u - 1, None), (u, "N"), (18, None), (19, None)], row(SA, u)) for u in range(2, 18)]
    A = AX_(3); A.update(attn_tabs); A.update({"w_qkv": a_w_qkv[1], "w_o": a_w_o[1], "sink": a_sink[1]})
    stage_attn(nc, "s3_", kv, q, A, 24)
    moe(3, "m3", [(row(SA, u), 0, row(xout, u - 2)) for u in range(2, 18)])
    return nc


_NC = []


def _tables(core):
    L = 16384
    freqs = np.power(np.float32(10000.0), -np.arange(16, dtype=np.float32) / np.float32(16)).astype(np.float32)
    pos = (core * 2048 - 384 + np.arange(NWIN * 128)).astype(np.float32)
    row = np.floor(pos / 64.0).astype(np.float32)
    col = (pos - row * 64).astype(np.float32)
    ar = row[:, None] * freqs[None, :]
    ac = col[:, None] * freqs[None, :]
    ang = np.concatenate([ar, ar, ac, ac], -1)
    cos = np.cos(ang).astype(np.float32)
    sin = np.sin(ang).astype(np.float32)
    sgn = np.tile(np.concatenate([-np.ones(16, np.float32), np.ones(16, np.float32)]), 2)
    sinS = sin * sgn[None, :]
    cos = np.concatenate([cos, np.ones((256, 64), np.float32)], 0)
    sinS = np.concatenate([sinS, np.zeros((256, 64), np.float32)], 0)
    cos_t = np.ascontiguousarray(cos.reshape(24, 128, 64).transpose(1, 0, 2).reshape(128, 24 * 64))
    sin_t = np.ascontiguousarray(sinS.reshape(24, 128, 64).transpose(1, 0, 2).reshape(128, 24 * 64))
    kb = np.zeros((128, 24), np.float32)
    for v in range(NWIN):
        st = core * 2048 - 384 + 128 * v
        if st < 0 or st >= L:
            kb[:, v] = -30000.0
    valid = np.ones((128, 22), np.float32)
    for u in range(NEXT):
        st = core * 2048 - 256 + 128 * u
        if st < 0 or st >= L:
            valid[:, u] = 0.0
    return cos_t, sin_t, kb, valid


def kernel(x, c, ctx, c_ctx, w_mod, b_mod, ln1_g, ln1_b, ln2_g, ln2_b, router_w, router_bias, moe_w1, moe_w3, moe_w2,
           a_w_qkv, a_w_o, a_sink, b_w_in, b_b_in, b_ln_g, b_ln_b, b_w_s, b_b_s, b_w_out, c_w_in, c_w_conv, c_w_out):
    f32 = lambda a: np.ascontiguousarray(np.asarray(a, dtype=np.float32))
    if not _NC:
        _NC.append(build_fused())
    nc = _NC[0]
    xc = f32(x)[0]
    zpad = np.zeros((384, D), np.float32)
    xp = np.concatenate([zpad, xc, zpad], 0)
    kk = np.arange(128)[:, None]
    qq = np.arange(128)[None, :]
    com = {"ctx": f32(ctx)[0], "cvec": np.stack([f32(c)[0], f32(c_ctx)]).astype(np.float32),
           "w_mod": f32(w_mod), "b_mod": f32(b_mod), "ln1_g": f32(ln1_g), "ln1_b": f32(ln1_b), "ln2_g": f32(ln2_g), "ln2_b": f32(ln2_b),
           "router_w": f32(router_w), "router_bias": f32(router_bias), "moe_w1": f32(moe_w1), "moe_w3": f32(moe_w3), "moe_w2": f32(moe_w2),
           "a_w_qkv": f32(a_w_qkv), "a_w_o": f32(a_w_o), "a_sink": f32(a_sink), "b_w_in": f32(b_w_in), "b_b_in": f32(b_b_in),
           "b_ln_g": f32(b_ln_g), "b_ln_b": f32(b_ln_b), "b_w_s": f32(b_w_s), "b_b_s": f32(b_b_s), "b_w_out": f32(b_w_out),
           "c_w_in": f32(c_w_in), "c_w_conv": f32(c_w_conv), "c_w_out": f32(c_w_out),
           "maskp": np.tile((kk >= qq).astype(np.float32), (1, 4)), "maskn": np.tile((kk <= qq).astype(np.float32), (1, 4))}
    in_maps = []
    for core in range(8):
        cos_t, sin_t, kb, valid = _tables(core)
        in_maps.append(dict(com, xw=np.ascontiguousarray(xp[core * 2048:core * 2048 + NWIN * 128]), cos_t=cos_t, sin_t=sin_t, kbias=kb, valid=valid))
    res = run_bass_kernel_spmd(nc, in_maps, core_ids=list(range(8)))
    out = np.concatenate([r["xout"] for r in res.results], 0)
    return out[None].astype(np.float32)
```

```python
import numpy as np
from contextlib import ExitStack
import concourse.bass as bass
import concourse.mybir as mybir
from concourse.bass_utils import run_bass_kernel_spmd

F32 = mybir.dt.float32
BF16 = mybir.dt.bfloat16
AF = mybir.ActivationFunctionType
ALU = mybir.AluOpType
AX = mybir.AxisListType


class Sched:
    ENG = ("pe", "act", "dve", "pool", "sp")

    def __init__(self, nc, es, pfx=""):
        self.nc = nc
        self.es = es
        self.pfx = pfx
        self.q = {e: [] for e in self.ENG}
        self.semh = {e: nc.alloc_semaphore(name=pfx + "s_" + e) for e in self.ENG}
        self.cnt = {e: 0 for e in self.ENG}
        self.waited = {e: {} for e in self.ENG}
        self.lastw = {}
        self.readers = {}
        self.dcnt = {}
        self.store_sems = set()

    def _deps(self, eng, reads, writes):
        need = {}
        for k in reads:
            if k in self.lastw:
                s, v = self.lastw[k]
                need[s] = max(need.get(s, 0), v)
        for k in writes:
            if k in self.lastw:
                s, v = self.lastw[k]
                need[s] = max(need.get(s, 0), v)
            for (s, v) in self.readers.get(k, ()):
                need[s] = max(need.get(s, 0), v)
        for s, v in need.items():
            if s == eng and eng == "pe":
                continue
            if self.waited[eng].get(s, 0) < v:
                self.q[eng].append(("wait", s, v))
                self.waited[eng][s] = v

    def op(self, eng, fn, reads=(), writes=()):
        psr = [k for k in reads if isinstance(k, str) and k.startswith("ps")]
        if psr:
            reads = [k for k in reads if k not in psr]
            writes = list(writes) + psr
        self._deps(eng, reads, writes)
        self.cnt[eng] += 1
        v = self.cnt[eng]
        self.q[eng].append(("op", fn))
        for k in reads:
            self.readers.setdefault(k, []).append((eng, v))
        for k in writes:
            self.lastw[k] = (eng, v)
            self.readers[k] = []

    def dma(self, queue, out, in_, sem, reads=(), writes=(), store=False, **kw):
        if sem not in self.semh:
            self.semh[sem] = self.nc.alloc_semaphore(name=self.pfx + "d_" + sem)
            self.dcnt[sem] = 0
        self._deps(queue, reads, writes)
        self.dcnt[sem] += 16
        v = self.dcnt[sem]
        self.q[queue].append(("dma", out, in_, sem, kw))
        for k in reads:
            self.readers.setdefault(k, []).append((sem, v))
        for k in writes:
            self.lastw[k] = (sem, v)
            self.readers[k] = []
        if store:
            self.store_sems.add(sem)

    def finish(self):
        for s in sorted(self.store_sems):
            self.q["sp"].append(("wait", s, self.dcnt[s]))

    def close(self):
        self.nc.clear_and_free_semaphores(list(self.semh.values()))
        self.nc.all_engine_barrier()

    def emit(self):
        nc = self.nc
        self.finish()

        def replay(e, engobj):
            for it in self.q[e]:
                if it[0] == "wait":
                    engobj.wait_ge(self.semh[it[1]], it[2])
                elif it[0] == "op":
                    it[1](engobj).then_inc(self.semh[e], 1)
                else:
                    _, out, in_, sem, kw = it
                    engobj.dma_start(out=out, in_=in_, **kw).then_inc(self.semh[sem], 16)

        with nc.Block() as block:
            @block.tensor
            def _(e):
                replay("pe", e)

            @block.scalar
            def _(e):
                replay("act", e)

            @block.vector
            def _(e):
                replay("dve", e)

            @block.gpsimd
            def _(e):
                replay("pool", e)

            @block.sync
            def _(e):
                replay("sp", e)


D = 1024
NE = 16
DEXP = 512
ALPHA = 8.0 ** 0.25
LN_EPS = 1e-5
EPS2 = LN_EPS / (ALPHA * ALPHA)


def common_consts(S, nc, sb, ps):
    ident = sb("ident", [128, 128])
    ones = sb("ones", [128, 128])
    S.op("pool", lambda e: e.memset(ones[:], 1.0), writes=["ones"])
    S.op("pool", lambda e: e.memset(ident[:], 0.0), writes=["ident"])
    S.op("pool", lambda e: e.affine_select(out=ident[:], in_=ident[:], pattern=[[-1, 128]], compare_op=ALU.not_equal,
                                          fill=1.0, base=0, channel_multiplier=1), reads=["ident"], writes=["ident"])
    return ident, ones


def mod_tiles(S, nc, sb, cvec, wmod, bmod, ident, ones, psbig, pbk, stage, stage_keys, SLC, slc_keys, outs):
    ct = sb("ct", [16, 128])
    csil = sb("csil", [128, 16])
    brow = sb("brow", [1, 512])
    S.dma("sp", ct[:], cvec.rearrange("s (k p) -> (s k) p", p=128), "ld_ct", writes=["ct"])
    S.op("pe", lambda e: e.transpose(out=psbig[:, 0:16], in_=ct[0:16, :], identity=ident[0:16, 0:16]),
         reads=["ct", "ident"], writes=[pbk])
    S.op("act", lambda e: e.activation(out=csil[:], in_=psbig[:, 0:16], func=AF.Silu), reads=[pbk], writes=["csil"])
    for s in range(2):
        for k in range(8):
            S.op("act", lambda e, s=s, k=k: e.activation(out=SLC[:, s, k, :], in_=ones[:], func=AF.Copy,
                                                         scale=csil[:, s * 8 + k:s * 8 + k + 1]),
                 reads=["ones", "csil"], writes=slc_keys)
    nv = max(o[0] for o in outs) + 1
    stages = stage if isinstance(stage, list) else [stage]
    skeys = stage_keys if isinstance(stage, list) else [stage_keys]
    brows = [brow, sb("brow2", [1, 512])] if isinstance(stage, list) else [brow, brow]
    idx = 0
    for v in range(nv):
        for h in range(2):
            c0 = v * 1024 + h * 512
            stg = stages[idx % len(stages)]
            sk = skeys[idx % len(stages)]
            br = brows[idx % 2]
            brk = f"brow{idx % 2}" if isinstance(stage, list) else "brow0"
            S.dma("sp", stg, wmod[:, c0:c0 + 512].rearrange("(k p) n -> p k n", p=128), f"ld_stage{idx % len(stages)}", writes=sk)
            S.dma("sp", br[:], bmod[c0:c0 + 512].rearrange("(a n) -> a n", a=1), (f"ld_brow{idx % 2}" if isinstance(stage, list) else "ld_brow0"), writes=[brk])
            idx += 1
            for s in range(2):
                mine = [o for o in outs if o[0] == v and o[1] == s]
                if not mine:
                    continue
                for k in range(8):
                    S.op("pe", lambda e, s=s, k=k, stg=stg: e.matmul(psbig[:, 0:512], lhsT=SLC[:, s, k, :], rhs=stg[:, k, :],
                                                                     start=(k == 0), stop=False),
                         reads=slc_keys + sk, writes=[pbk])
                S.op("pe", lambda e, br=br: e.matmul(psbig[:, 0:512], lhsT=ones[0:1, :], rhs=br[0:1, :], start=False, stop=True),
                     reads=["ones", brk], writes=[pbk])
                for (_, _, tile, key, kind) in mine:
                    dst = tile[:, h * 512:(h + 1) * 512]
                    if kind == "plain":
                        S.op("dve", lambda e, dst=dst: e.tensor_copy(out=dst, in_=psbig[:, 0:512]), reads=[pbk], writes=[key])
                    elif kind == "plus1":
                        S.op("dve", lambda e, dst=dst: e.tensor_scalar(out=dst, in0=psbig[:, 0:512], scalar1=1.0, scalar2=None,
                                                                       op0=ALU.add), reads=[pbk], writes=[key])
                    else:
                        S.op("dve", lambda e, dst=dst: e.tensor_scalar(out=dst, in0=psbig[:, 0:512], scalar1=1.0 / ALPHA,
                                                                       scalar2=None, op0=ALU.mult), reads=[pbk], writes=[key])


def layer_norm_tile(S, src, src_key, dst, dst_key, tmp, tmp_key, lng, lng_key, lnb, lnb_key, small, eps_t):
    st, mv, rstd, nmr = small
    for h in range(2):
        S.op("dve", lambda e, h=h: e.bn_stats(out=st[:, h, :], in_=src[:, h * 512:(h + 1) * 512]), reads=[src_key], writes=["ln_st"])
    S.op("dve", lambda e: e.bn_aggr(out=mv[:], in_=st[:].rearrange("p a b -> p (a b)")), reads=["ln_st"], writes=["ln_mv"])
    S.op("act", lambda e: e.activation(out=rstd[:], in_=mv[:, 1:2], func=AF.Sqrt, bias=eps_t[:], scale=1.0),
         reads=["ln_mv", "eps"], writes=["ln_rstd"])
    S.op("dve", lambda e: e.reciprocal(out=rstd[:], in_=rstd[:]), reads=["ln_rstd"], writes=["ln_rstd"])
    S.op("dve", lambda e: e.scalar_tensor_tensor(out=nmr[:], in0=mv[:, 0:1], scalar=-1.0, in1=rstd[:], op0=ALU.mult, op1=ALU.mult),
         reads=["ln_mv", "ln_rstd"], writes=["ln_nmr"])
    S.op("act", lambda e: e.activation(out=tmp, in_=src, func=AF.Identity, bias=nmr[:], scale=rstd[:]),
         reads=[src_key, "ln_nmr", "ln_rstd"], writes=[tmp_key])
    S.op("dve", lambda e: e.tensor_tensor(out=tmp, in0=tmp, in1=lng, op=ALU.mult), reads=[tmp_key, lng_key], writes=[tmp_key])
    S.op("dve", lambda e: e.tensor_tensor(out=dst, in0=tmp, in1=lnb, op=ALU.add), reads=[tmp_key, lnb_key], writes=[dst_key])


def build_moe():
    NT = 18
    nc = bass.Bass("TRN2", target_bir_lowering=False)
    dt = lambda n, s, k: nc.dram_tensor(n, s, F32, kind=k).ap()
    xin = dt("xin", [NT * 128, D], "ExternalInput")
    A = {"cvec": dt("cvec", [2, D], "ExternalInput"), "wmod": dt("wmod", [D, 3 * D], "ExternalInput"),
         "bmod": dt("bmod", [3 * D], "ExternalInput"), "lng": dt("lng", [D], "ExternalInput"), "lnb": dt("lnb", [D], "ExternalInput"),
         "rw": dt("rw", [D, NE], "ExternalInput"), "rb": dt("rb", [NE], "ExternalInput"),
         "w1": dt("w1", [NE, D, DEXP], "ExternalInput"), "w3": dt("w3", [NE, D, DEXP], "ExternalInput"),
         "w2": dt("w2", [NE, DEXP, D], "ExternalInput")}
    xout = dt("xout", [NT * 128, D], "ExternalOutput")
    tiles = [(xin[t * 128:(t + 1) * 128, :], 0 if t < 16 else 1, xout[t * 128:(t + 1) * 128, :]) for t in range(NT)]
    stage_moe(nc, "", tiles, A)
    return nc


def stage_moe(nc, pfx, tiles, A):
    NT = len(tiles)
    cvec, wmod, bmod, lng_d, lnb_d, rw_d, rb_d, w1_d, w3_d, w2_d = (A[k] for k in ("cvec", "wmod", "bmod", "lng", "lnb", "rw", "rb", "w1", "w3", "w2"))
    es = ExitStack()
    with es:
        S = Sched(nc, es, pfx)
        sb = lambda n, s, d=F32: es.enter_context(nc.sbuf_tensor(pfx + n, s, d))
        ps = lambda n, s, d=F32: es.enter_context(nc.psum_tensor(pfx + n, s, d))
        X = sb("X", [128, NT, D])
        HT = sb("HT", [128, 8, NT * 128], BF16)
        W13 = [sb(f"W13_{i}", [128, 2, 8, 256], BF16) for i in range(2)]
        W2 = [sb(f"W2_{i}", [128, 2, D], BF16) for i in range(2)]
        STG = sb("STG", [128, 6 * D])
        MOD = {("sc1", 0): STG[:, 0:D], ("sh", 0): STG[:, D:2 * D], ("sc1", 1): STG[:, 2 * D:3 * D], ("sh", 1): STG[:, 3 * D:4 * D]}
        MKEY = {("sc1", 0): "STG0", ("sh", 0): "STG1", ("sc1", 1): "STG2", ("sh", 1): "STG3"}
        for s_ in range(2):
            MOD[("gf", s_)] = sb(f"Mgf{s_}", [128, D])[:]
            MKEY[("gf", s_)] = f"Mgf{s_}"
        S13 = STG[:, 0:4 * D].rearrange("p (a k n) -> p a k n", a=2, k=8)
        S2 = STG[:, 4 * D:6 * D].rearrange("p (k n) -> p k n", k=2)
        T = [sb(f"T{i}", [128, D]) for i in range(2)]
        SA = sb("SA", [128, 2, 512])
        H1 = [sb(f"H1_{i}", [128, 2, 512], BF16) for i in range(2)]
        rw = sb("rwt", [128, 8, NE])
        rb = sb("rbt", [128, NE])
        GATE = sb("GATE", [128, NT, NE])
        eps_t = sb("eps_t", [128, 1])
        small = (sb("ln_st", [128, 2, 6]), sb("ln_mv", [128, 2]), sb("ln_rstd", [128, 1]), sb("ln_nmr", [128, 1]))
        rt = {n: sb("rt_" + n, [128, 16]) for n in ("s", "ssel", "sm", "sel", "sc")}
        rg = {n: sb("rg_" + n, [128, 4]) for n in ("p01", "q01", "p23", "q23", "t1", "m2", "m3", "gs", "ing", "pen")}
        r1 = {n: sb("r1_" + n, [128, 1]) for n in ("gmax", "den")}
        top8 = sb("top8", [128, 8])
        psY = [ps(f"psY{i}", [128, D]) for i in range(2)]
        psA = [ps(f"psA{i}", [128, 512]) for i in range(2)]
        psB = [ps(f"psB{i}", [128, 512]) for i in range(2)]

        ident, ones = common_consts(S, nc, sb, ps)
        S.op("dve", lambda e: e.memset(eps_t[:], EPS2), writes=["eps"])
        SLC = X[:, 0:2, :].rearrange("p s (k n) -> p s k n", k=8)
        stage = [X[:, 2:6, :].rearrange("p a (b n) -> p (a b) n", b=2), X[:, 6:10, :].rearrange("p a (b n) -> p (a b) n", b=2)]
        outs = []
        for s in range(2):
            outs.append((0, s, MOD[("sh", s)], MKEY[("sh", s)], "plain"))
            outs.append((1, s, MOD[("sc1", s)], MKEY[("sc1", s)], "plus1"))
            outs.append((2, s, MOD[("gf", s)], MKEY[("gf", s)], "invalpha"))
        mod_tiles(S, nc, sb, cvec, wmod, bmod, ident, ones, psY[0], "psY0", stage, [[("X", t) for t in range(2, 6)], [("X", t) for t in range(6, 10)]],
                  SLC, [("X", 0), ("X", 1)], outs)
        S.dma("sp", rw[:], rw_d.rearrange("(k p) e -> p k e", p=128), "ld_rw", writes=["rw"])
        S.dma("sp", rb[:], rb_d.partition_broadcast(128), "ld_rb", writes=["rb"])

        for t in range(NT):
            s = tiles[t][1]
            xk = ("X", t)
            S.dma("sp", X[:, t, :], tiles[t][0], f"ld_x{t}", writes=[xk])
            S.op("dve", lambda e, t=t, s=s: e.tensor_tensor(out=T[0][:], in0=X[:, t, :], in1=MOD[("sc1", s)], op=ALU.mult),
                 reads=[xk, MKEY[("sc1", s)]], writes=["T0"])
            S.op("dve", lambda e, s=s: e.tensor_tensor(out=T[0][:], in0=T[0][:], in1=MOD[("sh", s)], op=ALU.add),
                 reads=["T0", MKEY[("sh", s)]], writes=["T0"])
            pt = psY[t % 2]
            ptk = f"psY{t % 2}"
            for k in range(8):
                S.op("pe", lambda e, k=k, pt=pt: e.transpose(out=pt[:, k * 128:(k + 1) * 128], in_=T[0][:, k * 128:(k + 1) * 128],
                                                             identity=ident[:]), reads=["T0", "ident"], writes=[ptk])
            S.op("act", lambda e, t=t, pt=pt: e.activation(out=HT[:, :, t * 128:(t + 1) * 128],
                                                           in_=pt[:].rearrange("p (k n) -> p k n", k=8), func=AF.Copy),
                 reads=[ptk], writes=[("HT", t)])
            S.op("dve", lambda e, pt=pt: e.tensor_copy(out=T[1][:], in_=pt[:]), reads=[ptk], writes=["T1"])
            pr = psA[t % 2]
            prk = f"psA{t % 2}"
            for k in range(8):
                S.op("pe", lambda e, k=k, pr=pr: e.matmul(pr[:, 0:NE], lhsT=T[1][:, k * 128:(k + 1) * 128], rhs=rw[:, k, :],
                                                          start=(k == 0), stop=(k == 7)), reads=["T1", "rw"], writes=[prk])
            S.op("act", lambda e, pr=pr: e.activation(out=rt["s"][:], in_=pr[:, 0:NE], func=AF.Sigmoid), reads=[prk], writes=["rt_s"])
            dv = lambda fn, r, w: S.op("dve", fn, reads=r, writes=w)
            dv(lambda e: e.tensor_tensor(out=rt["ssel"][:], in0=rt["s"][:], in1=rb[:], op=ALU.add), ["rt_s", "rb"], ["rt_ssel"])
            sv = rt["ssel"][:].rearrange("p (g j) -> p g j", j=4)
            a, b, c, d = (sv[:, :, j] for j in range(4))
            dv(lambda e: e.tensor_tensor(out=rg["p01"][:], in0=a, in1=b, op=ALU.max), ["rt_ssel"], ["p01"])
            dv(lambda e: e.tensor_tensor(out=rg["q01"][:], in0=a, in1=b, op=ALU.min), ["rt_ssel"], ["q01"])
            dv(lambda e: e.tensor_tensor(out=rg["p23"][:], in0=c, in1=d, op=ALU.max), ["rt_ssel"], ["p23"])
            dv(lambda e: e.tensor_tensor(out=rg["q23"][:], in0=c, in1=d, op=ALU.min), ["rt_ssel"], ["q23"])
            dv(lambda e: e.tensor_tensor(out=rg["t1"][:], in0=rg["p01"][:], in1=rg["p23"][:], op=ALU.max), ["p01", "p23"], ["t1"])
            dv(lambda e: e.tensor_tensor(out=rg["m2"][:], in0=rg["p01"][:], in1=rg["p23"][:], op=ALU.min), ["p01", "p23"], ["m2"])
            dv(lambda e: e.tensor_tensor(out=rg["m3"][:], in0=rg["q01"][:], in1=rg["q23"][:], op=ALU.max), ["q01", "q23"], ["m3"])
            dv(lambda e: e.tensor_tensor(out=rg["m2"][:], in0=rg["m2"][:], in1=rg["m3"][:], op=ALU.max), ["m2", "m3"], ["m2"])
            dv(lambda e: e.tensor_tensor(out=rg["gs"][:], in0=rg["t1"][:], in1=rg["m2"][:], op=ALU.add), ["t1", "m2"], ["gs"])
            dv(lambda e: e.tensor_reduce(out=r1["gmax"][:], in_=rg["gs"][:], axis=AX.X, op=ALU.max), ["gs"], ["gmax"])
            dv(lambda e: e.tensor_scalar(out=rg["ing"][:], in0=rg["gs"][:], scalar1=r1["gmax"][:, 0:1], scalar2=None, op0=ALU.is_ge),
               ["gs", "gmax"], ["ing"])
            dv(lambda e: e.tensor_scalar(out=rg["pen"][:], in0=rg["ing"][:], scalar1=4.0, scalar2=-4.0, op0=ALU.mult, op1=ALU.add),
               ["ing"], ["pen"])
            smv = rt["sm"][:].rearrange("p (g j) -> p g j", j=4)
            for g in range(4):
                dv(lambda e, g=g: e.tensor_scalar(out=smv[:, g, :], in0=sv[:, g, :], scalar1=rg["ing"][:, g:g + 1],
                                                  scalar2=rg["pen"][:, g:g + 1], op0=ALU.mult, op1=ALU.add),
                   ["rt_ssel", "ing", "pen"], ["rt_sm"])
            dv(lambda e: e.max(out=top8[:], in_=rt["sm"][:]), ["rt_sm"], ["top8"])
            dv(lambda e: e.tensor_scalar(out=rt["sel"][:], in0=rt["sm"][:], scalar1=top8[:, 1:2], scalar2=None, op0=ALU.is_ge),
               ["rt_sm", "top8"], ["rt_sel"])
            dv(lambda e: e.tensor_tensor(out=rt["sc"][:], in0=rt["s"][:], in1=rt["sel"][:], op=ALU.mult), ["rt_s", "rt_sel"], ["rt_sc"])
            dv(lambda e: e.tensor_reduce(out=r1["den"][:], in_=rt["sc"][:], axis=AX.X, op=ALU.add), ["rt_sc"], ["den"])
            dv(lambda e: e.reciprocal(out=r1["den"][:], in_=r1["den"][:]), ["den"], ["den"])
            dv(lambda e, t=t: e.tensor_scalar(out=GATE[:, t, :], in0=rt["sc"][:], scalar1=r1["den"][:, 0:1], scalar2=None, op0=ALU.mult),
               ["rt_sc", "den"], [("GATE", t)])

        groups = [(g * 4, min(4, NT - g * 4)) for g in range((NT + 3) // 4)]
        NU = 2 * NE
        stg13_keys = ["STG0", "STG1", "STG2", "STG3"]
        stg2_keys = ["STG4", "STG5"]

        def load_unit(u):
            ex, hf = u // 2, u % 2
            S.dma("sp", S13[:, 0, :, :], w1_d[ex, :, hf * 256:(hf + 1) * 256].rearrange("(k p) n -> p k n", p=128),
                  "ld_s13", writes=stg13_keys)
            S.dma("sp", S13[:, 1, :, :], w3_d[ex, :, hf * 256:(hf + 1) * 256].rearrange("(k p) n -> p k n", p=128),
                  "ld_s13", writes=stg13_keys)
            S.dma("sp", S2, w2_d[ex, hf * 256:(hf + 1) * 256, :].rearrange("(k p) n -> p k n", p=128),
                  "ld_s2", writes=stg2_keys)

        def cast_unit(u):
            sl = u % 2
            for a in range(2):
                S.op("act", lambda e, a=a, sl=sl: e.activation(out=W13[sl][:, a, :, :], in_=S13[:, a, :, :], func=AF.Copy),
                     reads=stg13_keys, writes=[f"W13_{sl}"])
            S.op("act", lambda e, sl=sl: e.activation(out=W2[sl][:], in_=S2, func=AF.Copy), reads=stg2_keys, writes=[f"W2_{sl}"])

        yi = 0
        abi = 0
        load_unit(0)
        cast_unit(0)
        for u in range(NU):
            ex, hf = u // 2, u % 2
            sl = u % 2
            wk = [f"W13_{sl}", f"W2_{sl}"]
            if u + 1 < NU:
                load_unit(u + 1)
            for gi, (t0, ntile) in enumerate(groups):
                if gi == min(2, len(groups) - 1) and u + 1 < NU:
                    cast_unit(u + 1)
                ntok = ntile * 128
                c0 = t0 * 128
                htk = [("HT", t) for t in range(t0, t0 + ntile)]
                hb = H1[abi % 2]
                hk = f"H1_{abi % 2}"
                for dc in range(2):
                    pa, pb = psA[dc], psB[dc]
                    for k in range(8):
                        S.op("pe", lambda e, k=k, dc=dc, pa=pa, ntok=ntok, c0=c0, sl=sl: e.matmul(
                            pa[:, 0:ntok], lhsT=W13[sl][:, 0, k, dc * 128:(dc + 1) * 128], rhs=HT[:, k, c0:c0 + ntok],
                            start=(k == 0), stop=(k == 7)), reads=[wk[0]] + htk, writes=[f"psA{dc}"])
                    for k in range(8):
                        S.op("pe", lambda e, k=k, dc=dc, pb=pb, ntok=ntok, c0=c0, sl=sl: e.matmul(
                            pb[:, 0:ntok], lhsT=W13[sl][:, 1, k, dc * 128:(dc + 1) * 128], rhs=HT[:, k, c0:c0 + ntok],
                            start=(k == 0), stop=(k == 7)), reads=[wk[0]] + htk, writes=[f"psB{dc}"])
                    sa = SA[:, dc, 0:ntok]
                    S.op("act", lambda e, pa=pa, sa=sa, ntok=ntok: e.activation(out=sa, in_=pa[:, 0:ntok], func=AF.Silu),
                         reads=[f"psA{dc}"], writes=[f"SA{dc}"])
                    S.op("dve", lambda e, pb=pb, sa=sa, dc=dc, hb=hb, ntok=ntok: e.tensor_tensor(
                        out=hb[:, dc, 0:ntok], in0=sa, in1=pb[:, 0:ntok], op=ALU.mult),
                        reads=[f"SA{dc}", f"psB{dc}"], writes=[hk])
                abi += 1
                for ti in range(ntile):
                    t = t0 + ti
                    s = tiles[t][1]
                    py = psY[yi % 2]
                    pyk = f"psY{yi % 2}"
                    tm = T[yi % 2]
                    tmk = f"T{yi % 2}"
                    yi += 1
                    for h2 in range(2):
                        for dc in range(2):
                            S.op("pe", lambda e, h2=h2, dc=dc, py=py, ti=ti, hb=hb, sl=sl: e.matmul(
                                py[:, h2 * 512:(h2 + 1) * 512], lhsT=hb[:, dc, ti * 128:(ti + 1) * 128],
                                rhs=W2[sl][:, dc, h2 * 512:(h2 + 1) * 512], start=(dc == 0), stop=(dc == 1)),
                                reads=[hk, wk[1]], writes=[pyk])
                    S.op("dve", lambda e, py=py, tm=tm, t=t, s=s, ex=ex: e.scalar_tensor_tensor(
                        out=tm[:], in0=py[:], scalar=GATE[:, t, ex:ex + 1], in1=MOD[("gf", s)], op0=ALU.mult, op1=ALU.mult),
                        reads=[pyk, ("GATE", t), MKEY[("gf", s)]], writes=[tmk])
                    S.op("pool", lambda e, tm=tm, t=t: e.tensor_tensor(out=X[:, t, :], in0=X[:, t, :], in1=tm[:], op=ALU.add),
                         reads=[tmk, ("X", t)], writes=[("X", t)])

        LNG, LNB = STG[:, 0:D], STG[:, D:2 * D]
        S.dma("sp", LNG, lng_d.partition_broadcast(128), "ld_lng", writes=["STG0"])
        S.dma("sp", LNB, lnb_d.partition_broadcast(128), "ld_lnb", writes=["STG1"])
        for t in range(NT):
            o = STG[:, (2 + t % 2) * D:(3 + t % 2) * D]
            ok = f"STG{2 + t % 2}"
            layer_norm_tile(S, X[:, t, :], ("X", t), o, ok, T[0][:], "T0", LNG, "STG0", LNB, "STG1", small, eps_t)
            S.dma("sp", tiles[t][2], o, f"st{t % 2}", reads=[ok], store=True)
        S.emit()
        S.close()


class Ctx:
    pass


def std_inputs(nc):
    dt = lambda n, s, k="ExternalInput": nc.dram_tensor(n, s, F32, kind=k).ap()
    return dt, {"cvec": dt("cvec", [2, D]), "wmod": dt("wmod", [D, 3 * D]), "bmod": dt("bmod", [3 * D]), "lng": dt("lng", [D]),
                "lnb": dt("lnb", [D])}


def mixer_prologue(nc, es, pfx, A):
    C = Ctx()
    C.nc = nc
    C.es = es
    S = C.S = Sched(nc, es, pfx)
    sb = C.sb = lambda n, s, d=F32: es.enter_context(nc.sbuf_tensor(pfx + n, s, d))
    ps = C.ps = lambda n, s, d=F32: es.enter_context(nc.psum_tensor(pfx + n, s, d))
    C.cvec, C.wmod, C.bmod, C.lng_d, C.lnb_d = A["cvec"], A["wmod"], A["bmod"], A["lng"], A["lnb"]
    C.STG = [sb(f"STG{i}", [128, 4096]) for i in range(2)]
    C.stg_i = 0
    C.MOD = {}
    C.MKEY = {}
    for n in ("sh", "sc1", "ga"):
        for s in range(2):
            C.MOD[(n, s)] = sb(f"M{n}{s}", [128, D])[:]
            C.MKEY[(n, s)] = f"M{n}{s}"
    C.LNG = sb("LNG", [128, D])
    C.LNB = sb("LNB", [128, D])
    C.eps2 = sb("eps2", [128, 1])
    C.eps1 = sb("eps1", [128, 1])
    C.small = (sb("ln_st", [128, 2, 6]), sb("ln_mv", [128, 2]), sb("ln_rstd", [128, 1]), sb("ln_nmr", [128, 1]))
    C.psBig = [ps(f"psBig{i}", [128, D]) for i in range(2)]
    C.ident, C.ones = common_consts(S, nc, sb, ps)
    S.op("dve", lambda e: e.memset(C.eps2[:], EPS2), writes=["eps"])
    S.op("dve", lambda e: e.memset(C.eps1[:], LN_EPS), writes=["eps1"])
    return C


def mixer_mods(C):
    S = C.S
    stage = C.STG[0][:].rearrange("p (k n) -> p k n", k=8)
    SLC = C.STG[1][:, 0:2048].rearrange("p (s k n) -> p s k n", s=2, k=8)
    outs = []
    for s in range(2):
        outs.append((0, s, C.MOD[("sh", s)], C.MKEY[("sh", s)], "plain"))
        outs.append((1, s, C.MOD[("sc1", s)], C.MKEY[("sc1", s)], "plus1"))
        outs.append((2, s, C.MOD[("ga", s)], C.MKEY[("ga", s)], "invalpha"))
    mod_tiles(S, C.nc, C.sb, C.cvec, C.wmod, C.bmod, C.ident, C.ones, C.psBig[0], "psBig0", stage, ["STG0"], SLC, ["STG1"], outs)
    S.dma("sp", C.LNG[:], C.lng_d.partition_broadcast(128), "ld_lng", writes=["LNG"])
    S.dma("sp", C.LNB[:], C.lnb_d.partition_broadcast(128), "ld_lnb", writes=["LNB"])


def load_w_bf16(C, dst, dkey, src, kc, ncols):
    S = C.S
    cw = min(ncols, 512)
    kpp = max(1, min(kc, 4096 // cw))
    n = 0
    for c0 in range(0, ncols, cw):
        for k0 in range(0, kc, kpp):
            kk = min(kpp, kc - k0)
            i = C.stg_i % 2
            C.stg_i += 1
            st = C.STG[i][:, 0:kk * cw].rearrange("p (k n) -> p k n", k=kk)
            S.dma("sp", st, src[k0 * 128:(k0 + kk) * 128, c0:c0 + cw].rearrange("(k p) n -> p k n", p=128), f"ld_stg{i}",
                  writes=[f"STG{i}"])
            eng = "act" if n % 2 == 0 else "dve"
            n += 1
            d = dst[:, k0:k0 + kk, c0:c0 + cw]
            if eng == "act":
                S.op("act", lambda e, d=d, st=st: e.activation(out=d, in_=st, func=AF.Copy), reads=[f"STG{i}"], writes=[dkey])
            else:
                S.op("dve", lambda e, d=d, st=st: e.tensor_copy(out=d, in_=st), reads=[f"STG{i}"], writes=[dkey])


def modulate_T(C, xt, xkey, s, T0, t0key, pst, pskey, HTt, htkey):
    S = C.S
    S.op("dve", lambda e: e.tensor_tensor(out=T0, in0=xt, in1=C.MOD[("sc1", s)], op=ALU.mult), reads=[xkey, C.MKEY[("sc1", s)]], writes=[t0key])
    S.op("dve", lambda e: e.tensor_tensor(out=T0, in0=T0, in1=C.MOD[("sh", s)], op=ALU.add), reads=[t0key, C.MKEY[("sh", s)]], writes=[t0key])
    to_T(C, T0, t0key, pst, pskey, HTt, htkey)


def to_T(C, src, skey, pst, pskey, HTt, htkey):
    S = C.S
    for k in range(8):
        S.op("pe", lambda e, k=k: e.transpose(out=pst[:, k * 128:(k + 1) * 128], in_=src[:, k * 128:(k + 1) * 128], identity=C.ident[:]),
             reads=[skey, "ident"], writes=[pskey])
    S.op("act", lambda e: e.activation(out=HTt, in_=pst[:].rearrange("p (k n) -> p k n", k=8), func=AF.Copy), reads=[pskey], writes=[htkey])


def proj(C, pst_list, HTt, htkey, W, wkey, c0, ncols):
    S = C.S
    off = 0
    for (pa, pk) in pst_list:
        w = min(512, ncols - off)
        for k in range(8):
            S.op("pe", lambda e, k=k, pa=pa, w=w, off=off: e.matmul(pa[:, 0:w], lhsT=HTt[:, k, :], rhs=W[:, k, c0 + off:c0 + off + w],
                                                                    start=(k == 0), stop=(k == 7)), reads=[htkey, wkey], writes=[pk])
        off += w


def residual_ln_store(C, psy, pykey, xt, xkey, s, T1, t1key, T2, t2key, o, okey, out_ap, stsem):
    S = C.S
    S.op("dve", lambda e: e.tensor_tensor(out=T1, in0=psy[:], in1=C.MOD[("ga", s)], op=ALU.mult), reads=[pykey, C.MKEY[("ga", s)]], writes=[t1key])
    S.op("pool", lambda e: e.tensor_tensor(out=T1, in0=T1, in1=xt, op=ALU.add), reads=[t1key, xkey], writes=[t1key])
    layer_norm_tile(S, T1, t1key, o, okey, T2, t2key, C.LNG[:], "LNG", C.LNB[:], "LNB", C.small, C.eps2)
    S.dma("sp", out_ap, o, stsem, reads=[okey], store=True)


def build_gmlp():
    NT = 18
    nc = bass.Bass("TRN2", target_bir_lowering=False)
    dt, A = std_inputs(nc)
    xin = dt("xin", [NT * 128, D])
    A.update({"w_in": dt("w_in", [D, 2 * D]), "b_in": dt("b_in", [2 * D]), "g_ln_g": dt("g_ln_g", [D]), "g_ln_b": dt("g_ln_b", [D]),
              "w_s": dt("w_s", [8, 128, 128]), "b_s": dt("b_s", [128, 8]), "w_out": dt("w_out", [D, D])})
    xout = dt("xout", [NT * 128, D], "ExternalOutput")
    tiles = [(xin[t * 128:(t + 1) * 128, :], 0 if t < 16 else 1, xout[t * 128:(t + 1) * 128, :]) for t in range(NT)]
    stage_gmlp(nc, "", tiles, A)
    return nc


def stage_gmlp(nc, pfx, tiles, A):
    NT = len(tiles)
    es = ExitStack()
    with es:
        C = mixer_prologue(nc, es, pfx, A)
        S, sb, ps = C.S, C.sb, C.ps
        win_d, bin_d, glng_d, glnb_d, ws_d, bs_d, wout_d = (A[k] for k in ("w_in", "b_in", "g_ln_g", "g_ln_b", "w_s", "b_s", "w_out"))
        Win = sb("Win", [128, 8, 2 * D], BF16)
        Wout = sb("Wout", [128, 8, D], BF16)
        wsT = sb("wsT", [128, 8, 128], BF16)
        BIN = sb("BIN", [128, 2 * D])
        GLNG = sb("GLNG", [128, D])
        GLNB = sb("GLNB", [128, D])
        bs = sb("bs", [128, 8])
        Xt = [sb(f"Xt{i}", [128, D]) for i in range(2)]
        OUT = [sb(f"OUT{i}", [128, D]) for i in range(2)]
        T0 = sb("T0", [128, D]); T1 = sb("T1", [128, D]); T2 = sb("T2", [128, D])
        HTt = sb("HTt", [128, 8, 128], BF16)
        HT2 = sb("HT2", [128, 8, 128], BF16)
        Z = sb("Z", [128, 2 * D])
        TZ = sb("TZ", [128, 2 * D])
        TZ2 = sb("TZ2", [128, 2 * D])
        VN = sb("VN", [128, D], BF16)
        US = sb("US", [128, D])
        psZ = [ps(f"psZ{i}", [128, 512]) for i in range(4)]
        mixer_mods(C)
        load_w_bf16(C, Win[:], "Win", win_d, 8, 2 * D)
        load_w_bf16(C, Wout[:], "Wout", wout_d, 8, D)
        S.dma("sp", BIN[:], bin_d.partition_broadcast(128), "ld_bin", writes=["BIN"])
        S.dma("sp", GLNG[:], glng_d.partition_broadcast(128), "ld_glng", writes=["GLNG"])
        S.dma("sp", GLNB[:], glnb_d.partition_broadcast(128), "ld_glnb", writes=["GLNB"])
        S.dma("sp", bs[:], bs_d, "ld_bs", writes=["bs"])
        wst = C.STG[0][:, 0:1024].rearrange("p (g q) -> p g q", g=8)
        S.dma("sp", wst, ws_d.rearrange("g p q -> p g q"), "ld_stg0", writes=["STG0"])
        for g in range(8):
            S.op("pe", lambda e, g=g: e.transpose(out=C.psBig[0][:, g * 128:(g + 1) * 128], in_=wst[:, g, :], identity=C.ident[:]),
                 reads=["STG0", "ident"], writes=["psBig0"])
        S.op("act", lambda e: e.activation(out=wsT[:], in_=C.psBig[0][:].rearrange("p (g n) -> p g n", g=8), func=AF.Copy),
             reads=["psBig0"], writes=["wsT"])
        for t in range(NT):
            s = tiles[t][1]
            xt = Xt[t % 2][:]
            xk = f"Xt{t % 2}"
            S.dma("sp", xt, tiles[t][0], f"ld_x{t % 2}", writes=[xk])
            modulate_T(C, xt, xk, s, T0[:], "T0", C.psBig[0], "psBig0", HTt[:], "HTt")
            proj(C, [(psZ[i], f"psZ{i}") for i in range(4)], HTt, "HTt", Win, "Win", 0, 2 * D)
            for i in range(4):
                S.op("dve", lambda e, i=i: e.tensor_tensor(out=Z[:, i * 512:(i + 1) * 512], in0=psZ[i][:], in1=BIN[:, i * 512:(i + 1) * 512],
                                                           op=ALU.add), reads=[f"psZ{i}", "BIN"], writes=["Z"])
            S.op("act", lambda e: e.activation(out=TZ[:], in_=Z[:], func=AF.Square), reads=["Z"], writes=["TZ"])
            S.op("dve", lambda e: e.tensor_scalar(out=TZ[:], in0=TZ[:], scalar1=0.044715, scalar2=1.0, op0=ALU.mult, op1=ALU.add),
                 reads=["TZ"], writes=["TZ"])
            S.op("pool", lambda e: e.tensor_tensor(out=TZ[:], in0=TZ[:], in1=Z[:], op=ALU.mult), reads=["TZ", "Z"], writes=["TZ"])
            S.op("act", lambda e: e.activation(out=TZ[:], in_=TZ[:], func=AF.Sigmoid, scale=1.5957691216057308), reads=["TZ"], writes=["TZ"])
            S.op("pool", lambda e: e.tensor_tensor(out=TZ2[:], in0=TZ[:], in1=Z[:], op=ALU.mult), reads=["TZ", "Z"], writes=["TZ2"])
            layer_norm_tile(S, TZ2[:, D:2 * D], "TZ2", VN[:], "VN", T2[:], "T2", GLNG[:], "GLNG", GLNB[:], "GLNB", C.small, C.eps1)
            for g in range(8):
                S.op("pe", lambda e, g=g: e.matmul(C.psBig[0][:, g * 128:(g + 1) * 128], lhsT=wsT[:, g, :], rhs=VN[:, g * 128:(g + 1) * 128],
                                                   start=True, stop=True), reads=["wsT", "VN"], writes=["psBig0"])
            for g in range(8):
                S.op("dve", lambda e, g=g: e.scalar_tensor_tensor(out=US[:, g * 128:(g + 1) * 128], in0=C.psBig[0][:, g * 128:(g + 1) * 128],
                                                                  scalar=bs[:, g:g + 1], in1=TZ2[:, g * 128:(g + 1) * 128],
                                                                  op0=ALU.add, op1=ALU.mult), reads=["psBig0", "bs", "TZ2"], writes=["US"])
            to_T(C, US[:], "US", C.psBig[1], "psBig1", HT2[:], "HT2")
            proj(C, [(C.psBig[1][:, 0:512], "psBig1"), (C.psBig[1][:, 512:1024], "psBig1")], HT2, "HT2", Wout, "Wout", 0, D)
            residual_ln_store(C, C.psBig[1], "psBig1", xt, xk, s, T1[:], "T1", T2[:], "T2", OUT[t % 2][:], f"OUT{t % 2}",
                              tiles[t][2], f"st{t % 2}")
        S.emit()
        S.close()


def build_conv():
    nc = bass.Bass("TRN2", target_bir_lowering=False)
    dt, A = std_inputs(nc)
    xin = dt("xin", [20 * 128, D])
    A.update({"valid": dt("valid", [128, 20]), "w_in": dt("w_in", [D, 3 * D]), "w_conv": dt("w_conv", [3, D]), "w_out": dt("w_out", [D, D])})
    xout = dt("xout", [18 * 128, D], "ExternalOutput")
    lat = [(xin[e * 128:(e + 1) * 128, :], 0, e, (xout[(e - 1) * 128:e * 128, :] if 1 <= e <= 16 else None)) for e in range(18)]
    ctx = [(xin[e * 128:(e + 1) * 128, :], 1, e, xout[(e - 2) * 128:(e - 1) * 128, :]) for e in (18, 19)]
    stage_conv(nc, "", [lat, ctx], A, 20)
    return nc


def stage_conv(nc, pfx, seqs, A, nvalid):
    es = ExitStack()
    with es:
        C = mixer_prologue(nc, es, pfx, A)
        S, sb, ps = C.S, C.sb, C.ps
        valid_d, win_d, wconv_d, wout_d = (A[k] for k in ("valid", "w_in", "w_conv", "w_out"))
        Win = sb("Win", [128, 8, 3 * D], BF16)
        Wout = sb("Wout", [128, 8, D], BF16)
        WC = [sb(f"WC{i}", [128, D]) for i in range(3)]
        valid = sb("valid_sb", [128, nvalid])
        Xr = [sb(f"Xr{i}", [128, D]) for i in range(4)]
        Zr = [C.STG[1][:, i * D:(i + 1) * D] for i in range(4)]
        HTr = [sb(f"HTr{i}", [128, 8, 128], BF16) for i in range(4)]
        ZM = sb("ZM", [128, D]); ZP = sb("ZP", [128, D]); TC = sb("TC", [128, D]); TG = sb("TG", [128, D])
        OUT = [sb(f"OUT{i}", [128, D]) for i in range(2)]
        T0 = sb("T0", [128, D]); T1 = sb("T1", [128, D]); T2 = sb("T2", [128, D])
        HT2 = sb("HT2", [128, 8, 128], BF16)
        psP = [ps(f"psP{i}", [128, 512]) for i in range(4)]
        mixer_mods(C)
        load_w_bf16(C, Win[:], "Win", win_d, 8, 3 * D)
        load_w_bf16(C, Wout[:], "Wout", wout_d, 8, D)
        for i in range(3):
            S.dma("sp", WC[i][:], wconv_d[i, :].partition_broadcast(128), f"ld_wc{i}", writes=[f"WC{i}"])
        S.dma("sp", valid[:], valid_d, "ld_valid", writes=["valid"])
        S.op("dve", lambda e: e.memset(ZM[0:1, 0:1], 0.0), writes=["STG1", "Zr0", "Zr1", "Zr2", "Zr3"])

        cnt = {"a": 0, "o": 0}

        def Astep(tile):
            src, s, vcol, _ = tile
            r = cnt["a"] % 4
            cnt["a"] += 1
            xt, xk = Xr[r][:], f"Xr{r}"
            S.dma("sp", xt, src, f"ld_x{r}", writes=[xk])
            modulate_T(C, xt, xk, s, T0[:], "T0", C.psBig[0], "psBig0", HTr[r][:], f"HTr{r}")
            proj(C, [(psP[i], f"psP{i}") for i in range(4)], HTr[r], f"HTr{r}", Win, "Win", D, 2 * D)
            for h in range(2):
                S.op("act", lambda e_, h=h: e_.activation(out=TG[:, h * 512:(h + 1) * 512], in_=psP[h][:], func=AF.Copy, scale=valid[:, vcol:vcol + 1]),
                     reads=[f"psP{h}", "valid"], writes=["TG"])
            for h in range(2):
                S.op("dve", lambda e_, h=h: e_.tensor_tensor(out=Zr[r][:, h * 512:(h + 1) * 512], in0=TG[:, h * 512:(h + 1) * 512], in1=psP[2 + h][:],
                                                            op=ALU.mult), reads=["TG", f"psP{2 + h}"], writes=[f"Zr{r}"])
            return r

        def Bstep(tile, r, prev, nxt):
            _, s, _, out_ap = tile
            ot = cnt["o"]
            cnt["o"] += 1
            xt, xk = Xr[r][:], f"Xr{r}"
            zc, zk = Zr[r], f"Zr{r}"
            if prev is None:
                S.op("dve", lambda e_: e_.memset(ZM[:], 0.0), writes=["ZM"])
            if nxt is None:
                S.op("dve", lambda e_: e_.memset(ZP[:], 0.0), writes=["ZP"])
            S.dma("sp", ZM[1:128, :], zc[0:127, :], "sh_zm", reads=[zk], writes=["ZM"])
            if prev is not None:
                S.dma("sp", ZM[0:1, :], Zr[prev][127:128, :], "sh_zm", reads=[f"Zr{prev}"], writes=["ZM"])
            S.dma("sp", ZP[0:127, :], zc[1:128, :], "sh_zp", reads=[zk], writes=["ZP"])
            if nxt is not None:
                S.dma("sp", ZP[127:128, :], Zr[nxt][0:1, :], "sh_zp", reads=[f"Zr{nxt}"], writes=["ZP"])
            S.op("dve", lambda e_: e_.tensor_tensor(out=ZM[:], in0=ZM[:], in1=WC[0][:], op=ALU.mult), reads=["ZM", "WC0"], writes=["ZM"])
            S.op("pool", lambda e_: e_.tensor_tensor(out=ZP[:], in0=ZP[:], in1=WC[2][:], op=ALU.mult), reads=["ZP", "WC2"], writes=["ZP"])
            S.op("dve", lambda e_: e_.tensor_tensor(out=TC[:], in0=zc, in1=WC[1][:], op=ALU.mult), reads=[zk, "WC1"], writes=["TC"])
            S.op("pool", lambda e_: e_.tensor_tensor(out=TC[:], in0=TC[:], in1=ZM[:], op=ALU.add), reads=["TC", "ZM"], writes=["TC"])
            S.op("dve", lambda e_: e_.tensor_tensor(out=TC[:], in0=TC[:], in1=ZP[:], op=ALU.add), reads=["TC", "ZP"], writes=["TC"])
            proj(C, [(C.psBig[1][:, 0:512], "psBig1"), (C.psBig[1][:, 512:1024], "psBig1")], HTr[r], f"HTr{r}", Win, "Win", 0, D)
            S.op("dve", lambda e_: e_.tensor_tensor(out=TG[:], in0=C.psBig[1][:], in1=TC[:], op=ALU.mult), reads=["psBig1", "TC"], writes=["TG"])
            to_T(C, TG[:], "TG", C.psBig[1], "psBig1", HT2[:], "HT2")
            proj(C, [(C.psBig[1][:, 0:512], "psBig1"), (C.psBig[1][:, 512:1024], "psBig1")], HT2, "HT2", Wout, "Wout", 0, D)
            residual_ln_store(C, C.psBig[1], "psBig1", xt, xk, s, T1[:], "T1", T2[:], "T2", OUT[ot % 2][:], f"OUT{ot % 2}",
                              out_ap, f"st{ot % 2}")

        for seq in seqs:
            slots = {}
            n = len(seq)
            for i in range(n + 1):
                if i < n:
                    slots[i] = Astep(seq[i])
                j = i - 1
                if j >= 0 and seq[j][3] is not None:
                    Bstep(seq[j], slots[j], slots.get(j - 1), slots.get(j + 1) if j + 1 < n else None)
        S.emit()
        S.close()


def build_attn(want_ctx):
    nc = bass.Bass("TRN2", target_bir_lowering=False)
    dt, A = std_inputs(nc)
    NOUT = 18 if want_ctx else 16
    xin = dt("xin", [20 * 128, D])
    A.update({"kbias": dt("kbias", [128, 20]), "cos_t": dt("cos_t", [128, 20 * 64]), "sin_t": dt("sin_t", [128, 20 * 64]),
              "maskp": dt("maskp", [128, 512]), "maskn": dt("maskn", [128, 512]), "w_qkv": dt("w_qkv", [D, 1536]),
              "w_o": dt("w_o", [D, D]), "sink": dt("sink", [16])})
    xout = dt("xout", [NOUT * 128, D], "ExternalOutput")
    kv = [(xin[e * 128:(e + 1) * 128, :], 0 if e < 18 else 1, e, e) for e in range(20)]
    q = [(xin[(o + 1) * 128:(o + 2) * 128, :], 0, o + 1, [(o, "P"), (o + 1, None), (o + 2, "N"), (18, None), (19, None)],
          xout[o * 128:(o + 1) * 128, :]) for o in range(16)]
    if want_ctx:
        q += [(xin[e * 128:(e + 1) * 128, :], 1, e, [(18, None), (19, None)], xout[(e - 2) * 128:(e - 1) * 128, :]) for e in (18, 19)]
    stage_attn(nc, "", kv, q, A, 20)
    return nc


def stage_attn(nc, pfx, kv_tiles, q_tiles, A, ntbl):
    NKT = len(kv_tiles)
    es = ExitStack()
    with es:
        C = mixer_prologue(nc, es, pfx, A)
        S, sb, ps = C.S, C.sb, C.ps
        kbias_d, cos_d, sin_d, maskp_d, maskn_d, wqkv_d, wo_d, sink_d = (A[k] for k in ("kbias", "cos_t", "sin_t", "maskp", "maskn", "w_qkv", "w_o", "sink"))
        Wqkv = sb("Wqkv", [128, 8, 1536], BF16)
        Wo = sb("Wo", [128, 8, D], BF16)
        KT = sb("KT", [128, 2, NKT * 128], BF16)
        V = sb("V", [128, NKT, 256], BF16)
        kbias = sb("kbias_sb", [128, ntbl])
        COS = sb("COS", [128, ntbl, 64])
        SIN = sb("SIN", [128, ntbl, 64])
        MP = sb("MP", [128, 512])
        MN = sb("MN", [128, 512])
        SINKB = sb("SINKB", [128, 2, 512])
        ES = sb("ES", [128, 16])
        identb = sb("identb", [128, 128], BF16)
        onesb = sb("onesb", [128, 64], BF16)
        Xt = [sb(f"Xt{i}", [128, D]) for i in range(2)]
        OUT = [sb(f"OUT{i}", [128, D]) for i in range(2)]
        T0 = sb("T0", [128, D]); T1 = sb("T1", [128, D]); T2 = sb("T2", [128, D])
        HTt = sb("HTt", [128, 8, 128], BF16)
        R1 = sb("R1", [128, D]); R2 = sb("R2", [128, D])
        KR = sb("KR", [128, 256], BF16)
        QRp = sb("QRp", [128, 8, 128], BF16)
        QT = sb("QT", [128, 8, 128], BF16)
        PT = [sb(f"PT{i}", [128, 512], BF16) for i in range(3)]
        DEN = sb("DEN", [128, 512])
        OT = sb("OT", [128, 2, 512], BF16)
        psS = [ps(f"psS{i}", [128, 512]) for i in range(2)]
        psX = ps("psX", [128, 8, 128], BF16)
        psO = C.psBig[1][:, 0:512]
        psD = C.psBig[1][:, 512:1024]
        S.op("pool", lambda e: e.memset(onesb[:], 1.0), writes=["onesb"])
        S.op("pool", lambda e: e.tensor_copy(out=identb[:], in_=C.ident[:]), reads=["ident"], writes=["identb"])
        mixer_mods(C)
        load_w_bf16(C, Wqkv[:], "Wqkv", wqkv_d, 8, 1536)
        for half in range(2):
            i = C.stg_i % 2
            C.stg_i += 1
            st = C.STG[i][:].rearrange("p (c n) -> p c n", c=4)
            for cc in range(4):
                c = half * 4 + cc
                jp, g = c // 4, c % 4
                for r in range(2):
                    row = 512 * jp + 256 * r + 64 * g
                    S.dma("sp", st[r * 64:(r + 1) * 64, cc, :], wo_d[row:row + 64, :], f"ld_stg{i}", writes=[f"STG{i}"])
            S.op("act", lambda e, st=st, half=half: e.activation(out=Wo[:, half * 4:(half + 1) * 4, :], in_=st, func=AF.Copy),
                 reads=[f"STG{i}"], writes=["Wo"])
        S.dma("sp", kbias[:], kbias_d, "ld_kb", writes=["kbias"])
        S.dma("sp", COS[:].rearrange("p t n -> p (t n)"), cos_d, "ld_cos", writes=["COS"])
        S.dma("sp", SIN[:].rearrange("p t n -> p (t n)"), sin_d, "ld_sin", writes=["SIN"])
        S.dma("sp", MP[:], maskp_d, "ld_mp", writes=["MP"])
        S.dma("sp", MN[:], maskn_d, "ld_mn", writes=["MN"])
        S.dma("sp", ES[:], sink_d.partition_broadcast(128), "ld_sink", writes=["ES"])
        S.op("act", lambda e: e.activation(out=ES[:], in_=ES[:], func=AF.Exp), reads=["ES"], writes=["ES"])
        for jp in range(2):
            for g in range(4):
                for r in range(2):
                    h = 8 * jp + 4 * r + g
                    S.op("act", lambda e, jp=jp, g=g, r=r, h=h: e.activation(
                        out=SINKB[r * 64:(r + 1) * 64, jp, g * 128:(g + 1) * 128], in_=C.ones[r * 64:(r + 1) * 64, :], func=AF.Copy,
                        scale=ES[r * 64:(r + 1) * 64, h:h + 1]), reads=["ones", "ES"], writes=["SINKB"])

        def rope(src_ps, pskey, nh, e, out_fn):
            n = nh * 64
            xv = src_ps.rearrange("p (h b f i) -> p h b f i", h=nh, b=2, f=2)
            r1v = R1[:, 0:n].rearrange("p (h n) -> p h n", h=nh)
            r2v = R2[:, 0:n].rearrange("p (h b f i) -> p h b f i", h=nh, b=2, f=2)
            cosb = COS[:, e, :].unsqueeze(1).to_broadcast([128, nh, 64])
            sv = SIN[:, e, :].rearrange("p (b f i) -> p b f i", b=2, f=2)
            S.op("dve", lambda e_: e_.tensor_tensor(out=r1v, in0=src_ps.rearrange("p (h n) -> p h n", h=nh), in1=cosb, op=ALU.mult),
                 reads=[pskey, "COS"], writes=["R1"])
            for f in range(2):
                sb_ = sv[:, :, f, :].unsqueeze(1).to_broadcast([128, nh, 2, 16])
                S.op("dve", lambda e_, f=f, sb_=sb_: e_.tensor_tensor(out=r2v[:, :, :, f, :], in0=xv[:, :, :, 1 - f, :], in1=sb_, op=ALU.mult),
                     reads=[pskey, "SIN"], writes=["R2"])
            out_fn(n)

        for e in range(NKT):
            src_, s, tbl, kbc = kv_tiles[e]
            xt, xk = Xt[e % 2][:], f"Xt{e % 2}"
            S.dma("sp", xt, src_, f"ld_x{e % 2}", writes=[xk])
            modulate_T(C, xt, xk, s, T0[:], "T0", C.psBig[0], "psBig0", HTt[:], "HTt")
            proj(C, [(psS[0], "psS0")], HTt, "HTt", Wqkv, "Wqkv", 1024, 512)

            def kout(n):
                S.op("pool", lambda e_: e_.tensor_tensor(out=KR[:], in0=R1[:, 0:256], in1=R2[:, 0:256], op=ALU.add), reads=["R1", "R2"], writes=["KR"])
            S.op("act", lambda e_, e=e: e_.activation(out=V[:, e, :], in_=psS[0][:, 256:512], func=AF.Copy), reads=["psS0"], writes=["V"])
            rope(psS[0][:, 0:256], "psS0", 4, tbl, kout)
            for jp in range(2):
                S.op("pe", lambda e_, jp=jp: e_.transpose(out=psX[:, jp, :], in_=KR[:, jp * 128:(jp + 1) * 128], identity=identb[:]),
                     reads=["KR", "identb"], writes=["psX"])
            S.op("act", lambda e_, e=e: e_.activation(out=KT[:, :, e * 128:(e + 1) * 128], in_=psX[:, 0:2, :], func=AF.Copy),
                 reads=["psX"], writes=["KT"])

        pti = 0
        for o, (src_, s, tbl, chunks_, out_ap) in enumerate(q_tiles):
            xt, xk = Xt[o % 2][:], f"Xt{o % 2}"
            S.dma("sp", xt, src_, f"ld_x{o % 2}", writes=[xk])
            modulate_T(C, xt, xk, s, T0[:], "T0", C.psBig[0], "psBig0", HTt[:], "HTt")
            proj(C, [(C.psBig[0][:, 0:512], "psBig0"), (C.psBig[0][:, 512:1024], "psBig0")], HTt, "HTt", Wqkv, "Wqkv", 0, D)

            def qout(n):
                for jp in range(2):
                    a = R1[:, jp * 512:(jp + 1) * 512].rearrange("p (r g d) -> p r g d", r=2, g=4)
                    b = R2[:, jp * 512:(jp + 1) * 512].rearrange("p (r g d) -> p r g d", r=2, g=4)
                    o_ = QRp[:, jp * 4:(jp + 1) * 4, :].rearrange("p g (r d) -> p r g d", r=2)
                    S.op("pool", lambda e_, a=a, b=b, o_=o_: e_.tensor_tensor(out=o_, in0=a, in1=b, op=ALU.add), reads=["R1", "R2"], writes=["QRp"])
            rope(C.psBig[0][:], "psBig0", 16, tbl, qout)
            for c in range(8):
                S.op("pe", lambda e_, c=c: e_.transpose(out=psX[:, c, :], in_=QRp[:, c, :], identity=identb[:]), reads=["QRp", "identb"], writes=["psX"])
            S.op("act", lambda e_: e_.activation(out=QT[:], in_=psX[:], func=AF.Copy), reads=["psX"], writes=["QT"])
            chunks = [(kt, {"P": MP, "N": MN, None: None}[m]) for (kt, m) in chunks_]
            for jp in range(2):
                for r in range(2):
                    j = 2 * jp + r
                    lo, hi = r * 64, (r + 1) * 64
                    for ci, (kt, mask) in enumerate(chunks):
                        kbc = kv_tiles[kt][3]
                        pss, psk = psS[pti % 2], f"psS{pti % 2}"
                        pt, ptk = PT[pti % 3], f"PT{pti % 3}"
                        pti += 1
                        S.op("pe", lambda e_, pss=pss, kt=kt, jp=jp, lo=lo, hi=hi: e_.matmul(
                            pss[:], lhsT=KT[lo:hi, jp, kt * 128:(kt + 1) * 128], rhs=QT[lo:hi, jp * 4:(jp + 1) * 4, :], start=True, stop=True),
                            reads=["KT", "QT"], writes=[psk])
                        S.op("act", lambda e_, pss=pss, pt=pt, kbc=kbc: e_.activation(out=pt[:], in_=pss[:], func=AF.Exp, bias=kbias[:, kbc:kbc + 1], scale=0.125),
                             reads=[psk, "kbias"], writes=[ptk])
                        if mask is not None:
                            S.op("dve", lambda e_, pt=pt, mask=mask: e_.tensor_tensor(out=pt[:], in0=pt[:], in1=mask[:], op=ALU.mult),
                                 reads=[ptk, "MP", "MN"], writes=[ptk])
                        first, last = (ci == 0), (ci == len(chunks) - 1)
                        S.op("pe", lambda e_, pt=pt, kt=kt, j=j, lo=lo, hi=hi, first=first, last=last: e_.matmul(
                            psO[lo:hi, :], lhsT=V[:, kt, j * 64:(j + 1) * 64], rhs=pt[:], start=first, stop=last),
                            reads=["V", ptk], writes=["psO"])
                        S.op("pe", lambda e_, pt=pt, lo=lo, hi=hi, first=first, last=last: e_.matmul(
                            psD[lo:hi, :], lhsT=onesb[:, 0:64], rhs=pt[:], start=first, stop=last),
                            reads=["onesb", ptk], writes=["psD"])
                S.op("dve", lambda e_, jp=jp: e_.tensor_tensor(out=DEN[:], in0=psD, in1=SINKB[:, jp, :], op=ALU.add), reads=["psD", "SINKB"], writes=["DEN"])
                S.op("dve", lambda e_: e_.reciprocal(out=DEN[:], in_=DEN[:]), reads=["DEN"], writes=["DEN"])
                S.op("dve", lambda e_, jp=jp: e_.tensor_tensor(out=OT[:, jp, :], in0=psO, in1=DEN[:], op=ALU.mult), reads=["psO", "DEN"], writes=["OT"])
            for half in range(2):
                for c in range(8):
                    jp, g = c // 4, c % 4
                    S.op("pe", lambda e_, half=half, c=c, jp=jp, g=g: e_.matmul(
                        C.psBig[0][:, half * 512:(half + 1) * 512], lhsT=OT[:, jp, g * 128:(g + 1) * 128], rhs=Wo[:, c, half * 512:(half + 1) * 512],
                        start=(c == 0), stop=(c == 7)), reads=["OT", "Wo"], writes=["psBig0"])
            residual_ln_store(C, C.psBig[0], "psBig0", xt, xk, s, T1[:], "T1", T2[:], "T2", OUT[o % 2][:], f"OUT{o % 2}",
                              out_ap, f"st{o % 2}")
        S.emit()
        S.close()


NWIN = 22
NEXT = 20


def build_fused(nstage=8, dbg=False):
    nc = bass.Bass("TRN2", target_bir_lowering=False)
    dt = lambda n, s, k="ExternalInput": nc.dram_tensor(n, s, F32, kind=k).ap()
    xw = dt("xw", [NWIN * 128, D])
    ctx = dt("ctx", [256, D])
    cvec = dt("cvec", [2, D])
    w_mod = dt("w_mod", [4, D, 6 * D]); b_mod = dt("b_mod", [4, 6 * D])
    ln1_g = dt("ln1_g", [4, D]); ln1_b = dt("ln1_b", [4, D]); ln2_g = dt("ln2_g", [4, D]); ln2_b = dt("ln2_b", [4, D])
    rw = dt("router_w", [D, NE]); rb = dt("router_bias", [NE])
    w1 = dt("moe_w1", [4, NE, D, DEXP]); w3 = dt("moe_w3", [4, NE, D, DEXP]); w2 = dt("moe_w2", [4, NE, DEXP, D])
    a_w_qkv = dt("a_w_qkv", [2, D, 1536]); a_w_o = dt("a_w_o", [2, D, D]); a_sink = dt("a_sink", [2, 16])
    b_w_in = dt("b_w_in", [1, D, 2 * D]); b_b_in = dt("b_b_in", [1, 2 * D]); b_ln_g = dt("b_ln_g", [1, D]); b_ln_b = dt("b_ln_b", [1, D])
    b_w_s = dt("b_w_s", [1, 8, 128, 128]); b_b_s = dt("b_b_s", [1, 128, 8]); b_w_out = dt("b_w_out", [1, D, D])
    c_w_in = dt("c_w_in", [1, D, 3 * D]); c_w_conv = dt("c_w_conv", [1, 3, D]); c_w_out = dt("c_w_out", [1, D, D])
    kbias = dt("kbias", [128, 24]); cos_t = dt("cos_t", [128, 24 * 64]); sin_t = dt("sin_t", [128, 24 * 64])
    maskp = dt("maskp", [128, 512]); maskn = dt("maskn", [128, 512]); valid = dt("valid", [128, 22])
    SA = dt("scrA", [22 * 128, D], "ExternalOutput" if dbg else "Internal")
    SB = dt("scrB", [22 * 128, D], "ExternalOutput" if dbg else "Internal")
    xout = dt("xout", [2048, D], "ExternalOutput")
    row = lambda T, i: T[i * 128:(i + 1) * 128, :]

    def AM(L):
        return {"cvec": cvec, "wmod": w_mod[L][:, 3 * D:6 * D], "bmod": b_mod[L][3 * D:6 * D], "lng": ln2_g[L], "lnb": ln2_b[L],
                "rw": rw, "rb": rb, "w1": w1[L], "w3": w3[L], "w2": w2[L]}

    def AX_(L):
        return {"cvec": cvec, "wmod": w_mod[L][:, 0:3 * D], "bmod": b_mod[L][0:3 * D], "lng": ln1_g[L], "lnb": ln1_b[L]}

    def moe(L, pfx, tiles):
        h = (len(tiles) + 1) // 2 if len(tiles) > 20 else len(tiles)
        stage_moe(nc, pfx + "a_", tiles[:h], AM(L))
        if h < len(tiles):
            stage_moe(nc, pfx + "b_", tiles[h:], AM(L))

    attn_tabs = {"kbias": kbias, "cos_t": cos_t, "sin_t": sin_t, "maskp": maskp, "maskn": maskn}
    kv = [(row(xw, v), 0, v, v) for v in range(NWIN)] + [(row(ctx, c), 1, 22 + c, 22 + c) for c in range(2)]
    q = [(row(xw, u + 1), 0, u + 1, [(u, "P"), (u + 1, None), (u + 2, "N"), (22, None), (23, None)], row(SA, u)) for u in range(NEXT)]
    q += [(row(ctx, c), 1, 22 + c, [(22, None), (23, None)], row(SA, 20 + c)) for c in range(2)]
    A = AX_(0); A.update(attn_tabs); A.update({"w_qkv": a_w_qkv[0], "w_o": a_w_o[0], "sink": a_sink[0]})
    stage_attn(nc, "s0_", kv, q, A, 24)
    if nstage <= 1:
        return nc
    allt = lambda Tsrc, Tdst: [(row(Tsrc, u), 0 if u < NEXT else 1, row(Tdst, u)) for u in range(22)]
    moe(0, "m0", allt(SA, SB))
    if nstage <= 2:
        return nc
    A = AX_(1); A.update({"w_in": b_w_in[0], "b_in": b_b_in[0], "g_ln_g": b_ln_g[0], "g_ln_b": b_ln_b[0], "w_s": b_w_s[0], "b_s": b_b_s[0],
                          "w_out": b_w_out[0]})
    stage_gmlp(nc, "s1_", allt(SB, SA), A)
    if nstage <= 3:
        return nc
    moe(1, "m1", allt(SA, SB))
    if nstage <= 4:
        return nc
    lat = [(row(SB, u), 0, u, (row(SA, u) if 1 <= u <= 18 else None)) for u in range(NEXT)]
    cx = [(row(SB, 20 + c), 1, 20 + c, row(SA, 20 + c)) for c in range(2)]
    A = AX_(2); A.update({"valid": valid, "w_in": c_w_in[0], "w_conv": c_w_conv[0], "w_out": c_w_out[0]})
    stage_conv(nc, "s2_", [lat, cx], A, 22)
    if nstage <= 5:
        return nc
    t2 = [(row(SA, u), 0, row(SB, u)) for u in range(1, 19)] + [(row(SA, 20 + c), 1, row(SB, 20 + c)) for c in range(2)]
    moe(2, "m2", t2)
    if nstage <= 6:
        return nc
    kv = [(row(SB, u), 0, u + 1, u + 1) for u in range(1, 19)] + [(row(SB, 20 + c), 1, 22 + c, 22 + c) for c in range(2)]
    q = [(row(SB, u), 0, u + 1, [(u - 2, "P"), (u - 1, None), (u, "N"), (18, None), (19, None)], row(SA, u)) for u in range(2, 18)]
    A = AX_(3); A.update(attn_tabs); A.update({"w_qkv": a_w_qkv[1], "w_o": a_w_o[1], "sink": a_sink[1]})
    stage_attn(nc, "s3_", kv, q, A, 24)
    moe(3, "m3", [(row(SA, u), 0, row(xout, u - 2)) for u in range(2, 18)])
    return nc


_NC = []


def _tables(core):
    L = 16384
    freqs = np.power(np.float32(10000.0), -np.arange(16, dtype=np.float32) / np.float32(16)).astype(np.float32)
    pos = (core * 2048 - 384 + np.arange(NWIN * 128)).astype(np.float32)
    row = np.floor(pos / 64.0).astype(np.float32)
    col = (pos - row * 64).astype(np.float32)
    ar = row[:, None] * freqs[None, :]
    ac = col[:, None] * freqs[None, :]
    ang = np.concatenate([ar, ar, ac, ac], -1)
    cos = np.cos(ang).astype(np.float32)
    sin = np.sin(ang).astype(np.float32)
    sgn = np.tile(np.concatenate([-np.ones(16, np.float32), np.ones(16, np.float32)]), 2)
    sinS = sin * sgn[None, :]
    cos = np.concatenate([cos, np.ones((256, 64), np.float32)], 0)
    sinS = np.concatenate([sinS, np.zeros((256, 64), np.float32)], 0)
    cos_t = np.ascontiguousarray(cos.reshape(24, 128, 64).transpose(1, 0, 2).reshape(128, 24 * 64))
    sin_t = np.ascontiguousarray(sinS.reshape(24, 128, 64).transpose(1, 0, 2).reshape(128, 24 * 64))
    kb = np.zeros((128, 24), np.float32)
    for v in range(NWIN):
        st = core * 2048 - 384 + 128 * v
        if st < 0 or st >= L:
            kb[:, v] = -30000.0
    valid = np.ones((128, 22), np.float32)
    for u in range(NEXT):
        st = core * 2048 - 256 + 128 * u
        if st < 0 or st >= L:
            valid[:, u] = 0.0
    return cos_t, sin_t, kb, valid


def kernel(x, c, ctx, c_ctx, w_mod, b_mod, ln1_g, ln1_b, ln2_g, ln2_b, router_w, router_bias, moe_w1, moe_w3, moe_w2,
           a_w_qkv, a_w_o, a_sink, b_w_in, b_b_in, b_ln_g, b_ln_b, b_w_s, b_b_s, b_w_out, c_w_in, c_w_conv, c_w_out):
    f32 = lambda a: np.ascontiguousarray(np.asarray(a, dtype=np.float32))
    if not _NC:
        _NC.append(build_fused())
    nc = _NC[0]
    xc = f32(x)[0]
    zpad = np.zeros((384, D), np.float32)
    xp = np.concatenate([zpad, xc, zpad], 0)
    kk = np.arange(128)[:, None]
    qq = np.arange(128)[None, :]
    com = {"ctx": f32(ctx)[0], "cvec": np.stack([f32(c)[0], f32(c_ctx)]).astype(np.float32),
           "w_mod": f32(w_mod), "b_mod": f32(b_mod), "ln1_g": f32(ln1_g), "ln1_b": f32(ln1_b), "ln2_g": f32(ln2_g), "ln2_b": f32(ln2_b),
           "router_w": f32(router_w), "router_bias": f32(router_bias), "moe_w1": f32(moe_w1), "moe_w3": f32(moe_w3), "moe_w2": f32(moe_w2),
           "a_w_qkv": f32(a_w_qkv), "a_w_o": f32(a_w_o), "a_sink": f32(a_sink), "b_w_in": f32(b_w_in), "b_b_in": f32(b_b_in),
           "b_ln_g": f32(b_ln_g), "b_ln_b": f32(b_ln_b), "b_w_s": f32(b_w_s), "b_b_s": f32(b_b_s), "b_w_out": f32(b_w_out),
           "c_w_in": f32(c_w_in), "c_w_conv": f32(c_w_conv), "c_w_out": f32(c_w_out),
           "maskp": np.tile((kk >= qq).astype(np.float32), (1, 4)), "maskn": np.tile((kk <= qq).astype(np.float32), (1, 4))}
    in_maps = []
    for core in range(8):
        cos_t, sin_t, kb, valid = _tables(core)
        in_maps.append(dict(com, xw=np.ascontiguousarray(xp[core * 2048:core * 2048 + NWIN * 128]), cos_t=cos_t, sin_t=sin_t, kbias=kb, valid=valid))
    res = run_bass_kernel_spmd(nc, in_maps, core_ids=list(range(8)))
    out = np.concatenate([r["xout"] for r in res.results], 0)
    return out[None].astype(np.float32)
```

```python
import numpy as np
from contextlib import ExitStack
import concourse.bass as bass
import concourse.mybir as mybir
from concourse.bass_utils import run_bass_kernel_spmd

F32 = mybir.dt.float32
BF16 = mybir.dt.bfloat16
AF = mybir.ActivationFunctionType
ALU = mybir.AluOpType
AX = mybir.AxisListType


class Sched:
    ENG = ("pe", "act", "dve", "pool", "sp")

    def __init__(self, nc, es, pfx=""):
        self.nc = nc
        self.es = es
        self.pfx = pfx
        self.q = {e: [] for e in self.ENG}
        self.semh = {e: nc.alloc_semaphore(name=pfx + "s_" + e) for e in self.ENG}
        self.cnt = {e: 0 for e in self.ENG}
        self.waited = {e: {} for e in self.ENG}
        self.lastw = {}
        self.readers = {}
        self.dcnt = {}
        self.store_sems = set()

    def _deps(self, eng, reads, writes):
        need = {}
        for k in reads:
            if k in self.lastw:
                s, v = self.lastw[k]
                need[s] = max(need.get(s, 0), v)
        for k in writes:
            if k in self.lastw:
                s, v = self.lastw[k]
                need[s] = max(need.get(s, 0), v)
            for (s, v) in self.readers.get(k, ()):
                need[s] = max(need.get(s, 0), v)
        for s, v in need.items():
            if s == eng and eng == "pe":
                continue
            if self.waited[eng].get(s, 0) < v:
                self.q[eng].append(("wait", s, v))
                self.waited[eng][s] = v

    def op(self, eng, fn, reads=(), writes=()):
        psr = [k for k in reads if isinstance(k, str) and k.startswith("ps")]
        if psr:
            reads = [k for k in reads if k not in psr]
            writes = list(writes) + psr
        self._deps(eng, reads, writes)
        self.cnt[eng] += 1
        v = self.cnt[eng]
        self.q[eng].append(("op", fn))
        for k in reads:
            self.readers.setdefault(k, []).append((eng, v))
        for k in writes:
            self.lastw[k] = (eng, v)
            self.readers[k] = []

    def dma(self, queue, out, in_, sem, reads=(), writes=(), store=False, **kw):
        if sem not in self.semh:
            self.semh[sem] = self.nc.alloc_semaphore(name=self.pfx + "d_" + sem)
            self.dcnt[sem] = 0
        self._deps(queue, reads, writes)
        self.dcnt[sem] += 16
        v = self.dcnt[sem]
        self.q[queue].append(("dma", out, in_, sem, kw))
        for k in reads:
            self.readers.setdefault(k, []).append((sem, v))
        for k in writes:
            self.lastw[k] = (sem, v)
            self.readers[k] = []
        if store:
            self.store_sems.add(sem)

    def finish(self):
        for s in sorted(self.store_sems):
            self.q["sp"].append(("wait", s, self.dcnt[s]))

    def close(self):
        self.nc.clear_and_free_semaphores(list(self.semh.values()))
        self.nc.all_engine_barrier()

    def emit(self):
        nc = self.nc
        self.finish()

        def replay(e, engobj):
            for it in self.q[e]:
                if it[0] == "wait":
                    engobj.wait_ge(self.semh[it[1]], it[2])
                elif it[0] == "op":
                    it[1](engobj).then_inc(self.semh[e], 1)
                else:
                    _, out, in_, sem, kw = it
                    engobj.dma_start(out=out, in_=in_, **kw).then_inc(self.semh[sem], 16)

        with nc.Block() as block:
            @block.tensor
            def _(e):
                replay("pe", e)

            @block.scalar
            def _(e):
                replay("act", e)

            @block.vector
            def _(e):
                replay("dve", e)

            @block.gpsimd
            def _(e):
                replay("pool", e)

            @block.sync
            def _(e):
                replay("sp", e)


D = 1024
NE = 16
DEXP = 512
ALPHA = 8.0 ** 0.25
LN_EPS = 1e-5
EPS2 = LN_EPS / (ALPHA * ALPHA)


def common_consts(S, nc, sb, ps):
    ident = sb("ident", [128, 128])
    ones = sb("ones", [128, 128])
    S.op("pool", lambda e: e.memset(ones[:], 1.0), writes=["ones"])
    S.op("pool", lambda e: e.memset(ident[:], 0.0), writes=["ident"])
    S.op("pool", lambda e: e.affine_select(out=ident[:], in_=ident[:], pattern=[[-1, 128]], compare_op=ALU.not_equal,
                                          fill=1.0, base=0, channel_multiplier=1), reads=["ident"], writes=["ident"])
    return ident, ones


def mod_tiles(S, nc, sb, cvec, wmod, bmod, ident, ones, psbig, pbk, stage, stage_keys, SLC, slc_keys, outs):
    ct = sb("ct", [16, 128])
    csil = sb("csil", [128, 16])
    brow = sb("brow", [1, 512])
    S.dma("sp", ct[:], cvec.rearrange("s (k p) -> (s k) p", p=128), "ld_ct", writes=["ct"])
    S.op("pe", lambda e: e.transpose(out=psbig[:, 0:16], in_=ct[0:16, :], identity=ident[0:16, 0:16]),
         reads=["ct", "ident"], writes=[pbk])
    S.op("act", lambda e: e.activation(out=csil[:], in_=psbig[:, 0:16], func=AF.Silu), reads=[pbk], writes=["csil"])
    for s in range(2):
        for k in range(8):
            S.op("act", lambda e, s=s, k=k: e.activation(out=SLC[:, s, k, :], in_=ones[:], func=AF.Copy,
                                                         scale=csil[:, s * 8 + k:s * 8 + k + 1]),
                 reads=["ones", "csil"], writes=slc_keys)
    nv = max(o[0] for o in outs) + 1
    stages = stage if isinstance(stage, list) else [stage]
    skeys = stage_keys if isinstance(stage, list) else [stage_keys]
    brows = [brow, sb("brow2", [1, 512])] if isinstance(stage, list) else [brow, brow]
    idx = 0
    for v in range(nv):
        for h in range(2):
            c0 = v * 1024 + h * 512
            stg = stages[idx % len(stages)]
            sk = skeys[idx % len(stages)]
            br = brows[idx % 2]
            brk = f"brow{idx % 2}" if isinstance(stage, list) else "brow0"
            S.dma("sp", stg, wmod[:, c0:c0 + 512].rearrange("(k p) n -> p k n", p=128), f"ld_stage{idx % len(stages)}", writes=sk)
            S.dma("sp", br[:], bmod[c0:c0 + 512].rearrange("(a n) -> a n", a=1), (f"ld_brow{idx % 2}" if isinstance(stage, list) else "ld_brow0"), writes=[brk])
            idx += 1
            for s in range(2):
                mine = [o for o in outs if o[0] == v and o[1] == s]
                if not mine:
                    continue
                for k in range(8):
                    S.op("pe", lambda e, s=s, k=k, stg=stg: e.matmul(psbig[:, 0:512], lhsT=SLC[:, s, k, :], rhs=stg[:, k, :],
                                                                     start=(k == 0), stop=False),
                         reads=slc_keys + sk, writes=[pbk])
                S.op("pe", lambda e, br=br: e.matmul(psbig[:, 0:512], lhsT=ones[0:1, :], rhs=br[0:1, :], start=False, stop=True),
                     reads=["ones", brk], writes=[pbk])
                for (_, _, tile, key, kind) in mine:
                    dst = tile[:, h * 512:(h + 1) * 512]
                    if kind == "plain":
                        S.op("dve", lambda e, dst=dst: e.tensor_copy(out=dst, in_=psbig[:, 0:512]), reads=[pbk], writes=[key])
                    elif kind == "plus1":
                        S.op("dve", lambda e, dst=dst: e.tensor_scalar(out=dst, in0=psbig[:, 0:512], scalar1=1.0, scalar2=None,
                                                                       op0=ALU.add), reads=[pbk], writes=[key])
                    else:
                        S.op("dve", lambda e, dst=dst: e.tensor_scalar(out=dst, in0=psbig[:, 0:512], scalar1=1.0 / ALPHA,
                                                                       scalar2=None, op0=ALU.mult), reads=[pbk], writes=[key])


def layer_norm_tile(S, src, src_key, dst, dst_key, tmp, tmp_key, lng, lng_key, lnb, lnb_key, small, eps_t):
    st, mv, rstd, nmr = small
    for h in range(2):
        S.op("dve", lambda e, h=h: e.bn_stats(out=st[:, h, :], in_=src[:, h * 512:(h + 1) * 512]), reads=[src_key], writes=["ln_st"])
    S.op("dve", lambda e: e.bn_aggr(out=mv[:], in_=st[:].rearrange("p a b -> p (a b)")), reads=["ln_st"], writes=["ln_mv"])
    S.op("act", lambda e: e.activation(out=rstd[:], in_=mv[:, 1:2], func=AF.Sqrt, bias=eps_t[:], scale=1.0),
         reads=["ln_mv", "eps"], writes=["ln_rstd"])
    S.op("dve", lambda e: e.reciprocal(out=rstd[:], in_=rstd[:]), reads=["ln_rstd"], writes=["ln_rstd"])
    S.op("dve", lambda e: e.scalar_tensor_tensor(out=nmr[:], in0=mv[:, 0:1], scalar=-1.0, in1=rstd[:], op0=ALU.mult, op1=ALU.mult),
         reads=["ln_mv", "ln_rstd"], writes=["ln_nmr"])
    S.op("act", lambda e: e.activation(out=tmp, in_=src, func=AF.Identity, bias=nmr[:], scale=rstd[:]),
         reads=[src_key, "ln_nmr", "ln_rstd"], writes=[tmp_key])
    S.op("dve", lambda e: e.tensor_tensor(out=tmp, in0=tmp, in1=lng, op=ALU.mult), reads=[tmp_key, lng_key], writes=[tmp_key])
    S.op("dve", lambda e: e.tensor_tensor(out=dst, in0=tmp, in1=lnb, op=ALU.add), reads=[tmp_key, lnb_key], writes=[dst_key])


def build_moe():
    NT = 18
    nc = bass.Bass("TRN2", target_bir_lowering=False)
    dt = lambda n, s, k: nc.dram_tensor(n, s, F32, kind=k).ap()
    xin = dt("xin", [NT * 128, D], "ExternalInput")
    A = {"cvec": dt("cvec", [2, D], "ExternalInput"), "wmod": dt("wmod", [D, 3 * D], "ExternalInput"),
         "bmod": dt("bmod", [3 * D], "ExternalInput"), "lng": dt("lng", [D], "ExternalInput"), "lnb": dt("lnb", [D], "ExternalInput"),
         "rw": dt("rw", [D, NE], "ExternalInput"), "rb": dt("rb", [NE], "ExternalInput"),
         "w1": dt("w1", [NE, D, DEXP], "ExternalInput"), "w3": dt("w3", [NE, D, DEXP], "ExternalInput"),
         "w2": dt("w2", [NE, DEXP, D], "ExternalInput")}
    xout = dt("xout", [NT * 128, D], "ExternalOutput")
    tiles = [(xin[t * 128:(t + 1) * 128, :], 0 if t < 16 else 1, xout[t * 128:(t + 1) * 128, :]) for t in range(NT)]
    stage_moe(nc, "", tiles, A)
    return nc


def stage_moe(nc, pfx, tiles, A):
    NT = len(tiles)
    cvec, wmod, bmod, lng_d, lnb_d, rw_d, rb_d, w1_d, w3_d, w2_d = (A[k] for k in ("cvec", "wmod", "bmod", "lng", "lnb", "rw", "rb", "w1", "w3", "w2"))
    es = ExitStack()
    with es:
        S = Sched(nc, es, pfx)
        sb = lambda n, s, d=F32: es.enter_context(nc.sbuf_tensor(pfx + n, s, d))
        ps = lambda n, s, d=F32: es.enter_context(nc.psum_tensor(pfx + n, s, d))
        X = sb("X", [128, NT, D])
        HT = sb("HT", [128, 8, NT * 128], BF16)
        W13 = [sb(f"W13_{i}", [128, 2, 8, 256], BF16) for i in range(2)]
        W2 = [sb(f"W2_{i}", [128, 2, D], BF16) for i in range(2)]
        has_ctx = any(tl[1] == 1 for tl in tiles)
        W2c = [sb(f"W2c_{i}", [128, 2, D], BF16) for i in range(2)] if has_ctx else None
        STG = sb("STG", [128, 6 * D])
        MOD = {("sc1", 0): STG[:, 0:D], ("sh", 0): STG[:, D:2 * D], ("sc1", 1): STG[:, 2 * D:3 * D], ("sh", 1): STG[:, 3 * D:4 * D]}
        MKEY = {("sc1", 0): "STG0", ("sh", 0): "STG1", ("sc1", 1): "STG2", ("sh", 1): "STG3"}
        for s_ in range(2):
            MOD[("gf", s_)] = sb(f"Mgf{s_}", [128, D])[:]
            MKEY[("gf", s_)] = f"Mgf{s_}"
        S13 = STG[:, 0:4 * D].rearrange("p (a k n) -> p a k n", a=2, k=8)
        S2 = STG[:, 4 * D:6 * D].rearrange("p (k n) -> p k n", k=2)
        T = [sb(f"T{i}", [128, D]) for i in range(2)]
        SA = sb("SA", [128, 2, 512])
        H1 = [sb(f"H1_{i}", [128, 2, 512], BF16) for i in range(2)]
        rw = sb("rwt", [128, 8, NE])
        rb = sb("rbt", [128, NE])
        GATE = sb("GATE", [128, NT, NE])
        eps_t = sb("eps_t", [128, 1])
        small = (sb("ln_st", [128, 2, 6]), sb("ln_mv", [128, 2]), sb("ln_rstd", [128, 1]), sb("ln_nmr", [128, 1]))
        rt = {n: sb("rt_" + n, [128, 16]) for n in ("s", "ssel", "sm", "sel", "sc")}
        rg = {n: sb("rg_" + n, [128, 4]) for n in ("p01", "q01", "p23", "q23", "t1", "m2", "m3", "gs", "ing", "pen")}
        r1 = {n: sb("r1_" + n, [128, 1]) for n in ("gmax", "den")}
        top8 = sb("top8", [128, 8])
        psY = [ps(f"psY{i}", [128, D]) for i in range(2)]
        psA = [ps(f"psA{i}", [128, 512]) for i in range(2)]
        psB = [ps(f"psB{i}", [128, 512]) for i in range(2)]

        ident, ones = common_consts(S, nc, sb, ps)
        S.op("dve", lambda e: e.memset(eps_t[:], EPS2), writes=["eps"])
        SLC = X[:, 0:2, :].rearrange("p s (k n) -> p s k n", k=8)
        stage = [X[:, 2:6, :].rearrange("p a (b n) -> p (a b) n", b=2), X[:, 6:10, :].rearrange("p a (b n) -> p (a b) n", b=2)]
        outs = []
        for s in range(2):
            outs.append((0, s, MOD[("sh", s)], MKEY[("sh", s)], "plain"))
            outs.append((1, s, MOD[("sc1", s)], MKEY[("sc1", s)], "plus1"))
            outs.append((2, s, MOD[("gf", s)], MKEY[("gf", s)], "invalpha"))
        mod_tiles(S, nc, sb, cvec, wmod, bmod, ident, ones, psY[0], "psY0", stage, [[("X", t) for t in range(2, 6)], [("X", t) for t in range(6, 10)]],
                  SLC, [("X", 0), ("X", 1)], outs)
        S.dma("sp", rw[:], rw_d.rearrange("(k p) e -> p k e", p=128), "ld_rw", writes=["rw"])
        S.dma("sp", rb[:], rb_d.partition_broadcast(128), "ld_rb", writes=["rb"])

        for t in range(NT):
            s = tiles[t][1]
            xk = ("X", t)
            S.dma("sp", X[:, t, :], tiles[t][0], f"ld_x{t}", writes=[xk])
            S.op("dve", lambda e, t=t, s=s: e.tensor_tensor(out=T[0][:], in0=X[:, t, :], in1=MOD[("sc1", s)], op=ALU.mult),
                 reads=[xk, MKEY[("sc1", s)]], writes=["T0"])
            S.op("dve", lambda e, s=s: e.tensor_tensor(out=T[0][:], in0=T[0][:], in1=MOD[("sh", s)], op=ALU.add),
                 reads=["T0", MKEY[("sh", s)]], writes=["T0"])
            pt = psY[t % 2]
            ptk = f"psY{t % 2}"
            for k in range(8):
                S.op("pe", lambda e, k=k, pt=pt: e.transpose(out=pt[:, k * 128:(k + 1) * 128], in_=T[0][:, k * 128:(k + 1) * 128],
                                                             identity=ident[:]), reads=["T0", "ident"], writes=[ptk])
            S.op("act", lambda e, t=t, pt=pt: e.activation(out=HT[:, :, t * 128:(t + 1) * 128],
                                                           in_=pt[:].rearrange("p (k n) -> p k n", k=8), func=AF.Copy),
                 reads=[ptk], writes=[("HT", t)])
            S.op("dve", lambda e, pt=pt: e.tensor_copy(out=T[1][:], in_=pt[:]), reads=[ptk], writes=["T1"])
            pr = psA[t % 2]
            prk = f"psA{t % 2}"
            for k in range(8):
                S.op("pe", lambda e, k=k, pr=pr: e.matmul(pr[:, 0:NE], lhsT=T[1][:, k * 128:(k + 1) * 128], rhs=rw[:, k, :],
                                                          start=(k == 0), stop=(k == 7)), reads=["T1", "rw"], writes=[prk])
            S.op("act", lambda e, pr=pr: e.activation(out=rt["s"][:], in_=pr[:, 0:NE], func=AF.Sigmoid), reads=[prk], writes=["rt_s"])
            dv = lambda fn, r, w: S.op("dve", fn, reads=r, writes=w)
            dv(lambda e: e.tensor_tensor(out=rt["ssel"][:], in0=rt["s"][:], in1=rb[:], op=ALU.add), ["rt_s", "rb"], ["rt_ssel"])
            sv = rt["ssel"][:].rearrange("p (g j) -> p g j", j=4)
            a, b, c, d = (sv[:, :, j] for j in range(4))
            dv(lambda e: e.tensor_tensor(out=rg["p01"][:], in0=a, in1=b, op=ALU.max), ["rt_ssel"], ["p01"])
            dv(lambda e: e.tensor_tensor(out=rg["q01"][:], in0=a, in1=b, op=ALU.min), ["rt_ssel"], ["q01"])
            dv(lambda e: e.tensor_tensor(out=rg["p23"][:], in0=c, in1=d, op=ALU.max), ["rt_ssel"], ["p23"])
            dv(lambda e: e.tensor_tensor(out=rg["q23"][:], in0=c, in1=d, op=ALU.min), ["rt_ssel"], ["q23"])
            dv(lambda e: e.tensor_tensor(out=rg["t1"][:], in0=rg["p01"][:], in1=rg["p23"][:], op=ALU.max), ["p01", "p23"], ["t1"])
            dv(lambda e: e.tensor_tensor(out=rg["m2"][:], in0=rg["p01"][:], in1=rg["p23"][:], op=ALU.min), ["p01", "p23"], ["m2"])
            dv(lambda e: e.tensor_tensor(out=rg["m3"][:], in0=rg["q01"][:], in1=rg["q23"][:], op=ALU.max), ["q01", "q23"], ["m3"])
            dv(lambda e: e.tensor_tensor(out=rg["m2"][:], in0=rg["m2"][:], in1=rg["m3"][:], op=ALU.max), ["m2", "m3"], ["m2"])
            dv(lambda e: e.tensor_tensor(out=rg["gs"][:], in0=rg["t1"][:], in1=rg["m2"][:], op=ALU.add), ["t1", "m2"], ["gs"])
            dv(lambda e: e.tensor_reduce(out=r1["gmax"][:], in_=rg["gs"][:], axis=AX.X, op=ALU.max), ["gs"], ["gmax"])
            dv(lambda e: e.tensor_scalar(out=rg["ing"][:], in0=rg["gs"][:], scalar1=r1["gmax"][:, 0:1], scalar2=None, op0=ALU.is_ge),
               ["gs", "gmax"], ["ing"])
            dv(lambda e: e.tensor_scalar(out=rg["pen"][:], in0=rg["ing"][:], scalar1=4.0, scalar2=-4.0, op0=ALU.mult, op1=ALU.add),
               ["ing"], ["pen"])
            smv = rt["sm"][:].rearrange("p (g j) -> p g j", j=4)
            for g in range(4):
                dv(lambda e, g=g: e.tensor_scalar(out=smv[:, g, :], in0=sv[:, g, :], scalar1=rg["ing"][:, g:g + 1],
                                                  scalar2=rg["pen"][:, g:g + 1], op0=ALU.mult, op1=ALU.add),
                   ["rt_ssel", "ing", "pen"], ["rt_sm"])
            dv(lambda e: e.max(out=top8[:], in_=rt["sm"][:]), ["rt_sm"], ["top8"])
            dv(lambda e: e.tensor_scalar(out=rt["sel"][:], in0=rt["sm"][:], scalar1=top8[:, 1:2], scalar2=None, op0=ALU.is_ge),
               ["rt_sm", "top8"], ["rt_sel"])
            dv(lambda e: e.tensor_tensor(out=rt["sc"][:], in0=rt["s"][:], in1=rt["sel"][:], op=ALU.mult), ["rt_s", "rt_sel"], ["rt_sc"])
            dv(lambda e: e.tensor_reduce(out=r1["den"][:], in_=rt["sc"][:], axis=AX.X, op=ALU.add), ["rt_sc"], ["den"])
            dv(lambda e: e.reciprocal(out=r1["den"][:], in_=r1["den"][:]), ["den"], ["den"])
            dv(lambda e, t=t: e.tensor_scalar(out=GATE[:, t, :], in0=rt["sc"][:], scalar1=r1["den"][:, 0:1], scalar2=None, op0=ALU.mult),
               ["rt_sc", "den"], [("GATE", t)])

        groups = [(g * 4, min(4, NT - g * 4)) for g in range((NT + 3) // 4)]
        NU = 2 * NE
        stg13_keys = ["STG0", "STG1", "STG2", "STG3"]
        stg2_keys = ["STG4", "STG5"]

        def load_unit(u):
            ex, hf = u // 2, u % 2
            S.dma("sp", S13[:, 0, :, :], w1_d[ex, :, hf * 256:(hf + 1) * 256].rearrange("(k p) n -> p k n", p=128),
                  "ld_s13", writes=stg13_keys)
            S.dma("sp", S13[:, 1, :, :], w3_d[ex, :, hf * 256:(hf + 1) * 256].rearrange("(k p) n -> p k n", p=128),
                  "ld_s13", writes=stg13_keys)
            S.dma("sp", S2, w2_d[ex, hf * 256:(hf + 1) * 256, :].rearrange("(k p) n -> p k n", p=128),
                  "ld_s2", writes=stg2_keys)

        def cast_unit(u):
            sl = u % 2
            for a in range(2):
                S.op("act", lambda e, a=a, sl=sl: e.activation(out=W13[sl][:, a, :, :], in_=S13[:, a, :, :], func=AF.Copy),
                     reads=stg13_keys, writes=[f"W13_{sl}"])
            gl = MOD[("gf", 0)].unsqueeze(1).to_broadcast([128, 2, D])
            S.op("dve", lambda e, sl=sl, gl=gl: e.tensor_tensor(out=W2[sl][:], in0=S2, in1=gl, op=ALU.mult),
                 reads=stg2_keys + [MKEY[("gf", 0)]], writes=[f"W2_{sl}"])
            if has_ctx:
                gc_ = MOD[("gf", 1)].unsqueeze(1).to_broadcast([128, 2, D])
                S.op("pool", lambda e, sl=sl, gc_=gc_: e.tensor_tensor(out=W2c[sl][:], in0=S2, in1=gc_, op=ALU.mult),
                     reads=stg2_keys + [MKEY[("gf", 1)]], writes=[f"W2c_{sl}"])

        yi = 0
        abi = 0
        load_unit(0)
        cast_unit(0)
        work = [(u, gi, t0, ntile) for u in range(NU) for gi, (t0, ntile) in enumerate(groups)]
        cast_gi = min(2, len(groups) - 1)
        yi_box = [0]

        def AB(idx):
            u, gi, t0, ntile = work[idx]
            sl = u % 2
            wk0 = f"W13_{sl}"
            ntok = ntile * 128
            c0 = t0 * 128
            htk = [("HT", t) for t in range(t0, t0 + ntile)]
            hb = H1[idx % 2]
            hk = f"H1_{idx % 2}"
            for dc in range(2):
                pa, pb = psA[dc], psB[dc]
                for k in range(8):
                    S.op("pe", lambda e, k=k, dc=dc, pa=pa: e.matmul(
                        pa[:, 0:ntok], lhsT=W13[sl][:, 0, k, dc * 128:(dc + 1) * 128], rhs=HT[:, k, c0:c0 + ntok],
                        start=(k == 0), stop=(k == 7)), reads=[wk0] + htk, writes=[f"psA{dc}"])
                for k in range(8):
                    S.op("pe", lambda e, k=k, dc=dc, pb=pb: e.matmul(
                        pb[:, 0:ntok], lhsT=W13[sl][:, 1, k, dc * 128:(dc + 1) * 128], rhs=HT[:, k, c0:c0 + ntok],
                        start=(k == 0), stop=(k == 7)), reads=[wk0] + htk, writes=[f"psB{dc}"])
                sa = SA[:, dc, 0:ntok]
                S.op("act", lambda e, pa=pa, sa=sa: e.activation(out=sa, in_=pa[:, 0:ntok], func=AF.Silu),
                     reads=[f"psA{dc}"], writes=[f"SA{dc}"])
                S.op("dve", lambda e, pb=pb, sa=sa, dc=dc: e.tensor_tensor(out=hb[:, dc, 0:ntok], in0=sa, in1=pb[:, 0:ntok], op=ALU.mult),
                     reads=[f"SA{dc}", f"psB{dc}"], writes=[hk])

        def Y(idx):
            u, gi, t0, ntile = work[idx]
            sl = u % 2
            ex = u // 2
            wk1 = f"W2_{sl}"
            hb = H1[idx % 2]
            hk = f"H1_{idx % 2}"
            for ti in range(ntile):
                t = t0 + ti
                s = tiles[t][1]
                yi = yi_box[0]
                yi_box[0] += 1
                py = psY[yi % 2]
                pyk = f"psY{yi % 2}"
                w2t, w2k = (W2[sl], f"W2_{sl}") if s == 0 else (W2c[sl], f"W2c_{sl}")
                for h2 in range(2):
                    for dc in range(2):
                        S.op("pe", lambda e, h2=h2, dc=dc, py=py, ti=ti, w2t=w2t: e.matmul(
                            py[:, h2 * 512:(h2 + 1) * 512], lhsT=hb[:, dc, ti * 128:(ti + 1) * 128],
                            rhs=w2t[:, dc, h2 * 512:(h2 + 1) * 512], start=(dc == 0), stop=(dc == 1)),
                            reads=[hk, w2k], writes=[pyk])
                S.op("dve", lambda e, py=py, t=t: e.scalar_tensor_tensor(
                    out=X[:, t, :], in0=py[:], scalar=GATE[:, t, ex:ex + 1], in1=X[:, t, :], op0=ALU.mult, op1=ALU.add),
                    reads=[pyk, ("GATE", t), ("X", t)], writes=[("X", t)])

        for idx in range(len(work)):
            u, gi, _, _ = work[idx]
            if gi == 0 and u + 1 < NU:
                load_unit(u + 1)
            if gi == cast_gi and u + 1 < NU:
                cast_unit(u + 1)
            AB(idx)
            if idx >= 1:
                Y(idx - 1)
        Y(len(work) - 1)

        LNG, LNB = STG[:, 0:D], STG[:, D:2 * D]
        S.dma("sp", LNG, lng_d.partition_broadcast(128), "ld_lng", writes=["STG0"])
        S.dma("sp", LNB, lnb_d.partition_broadcast(128), "ld_lnb", writes=["STG1"])
        for t in range(NT):
            o = STG[:, (2 + t % 2) * D:(3 + t % 2) * D]
            ok = f"STG{2 + t % 2}"
            layer_norm_tile(S, X[:, t, :], ("X", t), o, ok, T[0][:], "T0", LNG, "STG0", LNB, "STG1", small, eps_t)
            S.dma("sp", tiles[t][2], o, f"st{t % 2}", reads=[ok], store=True)
        S.emit()
        S.close()


class Ctx:
    pass


def std_inputs(nc):
    dt = lambda n, s, k="ExternalInput": nc.dram_tensor(n, s, F32, kind=k).ap()
    return dt, {"cvec": dt("cvec", [2, D]), "wmod": dt("wmod", [D, 3 * D]), "bmod": dt("bmod", [3 * D]), "lng": dt("lng", [D]),
                "lnb": dt("lnb", [D])}


def mixer_prologue(nc, es, pfx, A):
    C = Ctx()
    C.nc = nc
    C.es = es
    S = C.S = Sched(nc, es, pfx)
    sb = C.sb = lambda n, s, d=F32: es.enter_context(nc.sbuf_tensor(pfx + n, s, d))
    ps = C.ps = lambda n, s, d=F32: es.enter_context(nc.psum_tensor(pfx + n, s, d))
    C.cvec, C.wmod, C.bmod, C.lng_d, C.lnb_d = A["cvec"], A["wmod"], A["bmod"], A["lng"], A["lnb"]
    C.STG = [sb(f"STG{i}", [128, 4096]) for i in range(2)]
    C.stg_i = 0
    C.MOD = {}
    C.MKEY = {}
    for n in ("sh", "sc1", "ga"):
        for s in range(2):
            C.MOD[(n, s)] = sb(f"M{n}{s}", [128, D])[:]
            C.MKEY[(n, s)] = f"M{n}{s}"
    C.LNG = sb("LNG", [128, D])
    C.LNB = sb("LNB", [128, D])
    C.eps2 = sb("eps2", [128, 1])
    C.eps1 = sb("eps1", [128, 1])
    C.small = (sb("ln_st", [128, 2, 6]), sb("ln_mv", [128, 2]), sb("ln_rstd", [128, 1]), sb("ln_nmr", [128, 1]))
    C.psBig = [ps(f"psBig{i}", [128, D]) for i in range(2)]
    C.ident, C.ones = common_consts(S, nc, sb, ps)
    S.op("dve", lambda e: e.memset(C.eps2[:], EPS2), writes=["eps"])
    S.op("dve", lambda e: e.memset(C.eps1[:], LN_EPS), writes=["eps1"])
    return C


def mixer_mods(C):
    S = C.S
    stage = C.STG[0][:].rearrange("p (k n) -> p k n", k=8)
    SLC = C.STG[1][:, 0:2048].rearrange("p (s k n) -> p s k n", s=2, k=8)
    outs = []
    for s in range(2):
        outs.append((0, s, C.MOD[("sh", s)], C.MKEY[("sh", s)], "plain"))
        outs.append((1, s, C.MOD[("sc1", s)], C.MKEY[("sc1", s)], "plus1"))
        outs.append((2, s, C.MOD[("ga", s)], C.MKEY[("ga", s)], "invalpha"))
    mod_tiles(S, C.nc, C.sb, C.cvec, C.wmod, C.bmod, C.ident, C.ones, C.psBig[0], "psBig0", stage, ["STG0"], SLC, ["STG1"], outs)
    S.dma("sp", C.LNG[:], C.lng_d.partition_broadcast(128), "ld_lng", writes=["LNG"])
    S.dma("sp", C.LNB[:], C.lnb_d.partition_broadcast(128), "ld_lnb", writes=["LNB"])


def load_w_bf16(C, dst, dkey, src, kc, ncols):
    S = C.S
    cw = min(ncols, 512)
    kpp = max(1, min(kc, 4096 // cw))
    n = 0
    for c0 in range(0, ncols, cw):
        for k0 in range(0, kc, kpp):
            kk = min(kpp, kc - k0)
            i = C.stg_i % 2
            C.stg_i += 1
            st = C.STG[i][:, 0:kk * cw].rearrange("p (k n) -> p k n", k=kk)
            S.dma("sp", st, src[k0 * 128:(k0 + kk) * 128, c0:c0 + cw].rearrange("(k p) n -> p k n", p=128), f"ld_stg{i}",
                  writes=[f"STG{i}"])
            eng = "act" if n % 2 == 0 else "dve"
            n += 1
            d = dst[:, k0:k0 + kk, c0:c0 + cw]
            if eng == "act":
                S.op("act", lambda e, d=d, st=st: e.activation(out=d, in_=st, func=AF.Copy), reads=[f"STG{i}"], writes=[dkey])
            else:
                S.op("dve", lambda e, d=d, st=st: e.tensor_copy(out=d, in_=st), reads=[f"STG{i}"], writes=[dkey])


def modulate_T(C, xt, xkey, s, T0, t0key, pst, pskey, HTt, htkey):
    S = C.S
    S.op("dve", lambda e: e.tensor_tensor(out=T0, in0=xt, in1=C.MOD[("sc1", s)], op=ALU.mult), reads=[xkey, C.MKEY[("sc1", s)]], writes=[t0key])
    S.op("dve", lambda e: e.tensor_tensor(out=T0, in0=T0, in1=C.MOD[("sh", s)], op=ALU.add), reads=[t0key, C.MKEY[("sh", s)]], writes=[t0key])
    to_T(C, T0, t0key, pst, pskey, HTt, htkey)


def to_T(C, src, skey, pst, pskey, HTt, htkey):
    S = C.S
    for k in range(8):
        S.op("pe", lambda e, k=k: e.transpose(out=pst[:, k * 128:(k + 1) * 128], in_=src[:, k * 128:(k + 1) * 128], identity=C.ident[:]),
             reads=[skey, "ident"], writes=[pskey])
    S.op("act", lambda e: e.activation(out=HTt, in_=pst[:].rearrange("p (k n) -> p k n", k=8), func=AF.Copy), reads=[pskey], writes=[htkey])


def proj(C, pst_list, HTt, htkey, W, wkey, c0, ncols):
    S = C.S
    off = 0
    for (pa, pk) in pst_list:
        w = min(512, ncols - off)
        for k in range(8):
            S.op("pe", lambda e, k=k, pa=pa, w=w, off=off: e.matmul(pa[:, 0:w], lhsT=HTt[:, k, :], rhs=W[:, k, c0 + off:c0 + off + w],
                                                                    start=(k == 0), stop=(k == 7)), reads=[htkey, wkey], writes=[pk])
        off += w


def residual_ln_store(C, psy, pykey, xt, xkey, s, T1, t1key, T2, t2key, o, okey, out_ap, stsem):
    S = C.S
    S.op("dve", lambda e: e.tensor_tensor(out=T1, in0=psy[:], in1=C.MOD[("ga", s)], op=ALU.mult), reads=[pykey, C.MKEY[("ga", s)]], writes=[t1key])
    S.op("pool", lambda e: e.tensor_tensor(out=T1, in0=T1, in1=xt, op=ALU.add), reads=[t1key, xkey], writes=[t1key])
    layer_norm_tile(S, T1, t1key, o, okey, T2, t2key, C.LNG[:], "LNG", C.LNB[:], "LNB", C.small, C.eps2)
    S.dma("sp", out_ap, o, stsem, reads=[okey], store=True)


def build_gmlp():
    NT = 18
    nc = bass.Bass("TRN2", target_bir_lowering=False)
    dt, A = std_inputs(nc)
    xin = dt("xin", [NT * 128, D])
    A.update({"w_in": dt("w_in", [D, 2 * D]), "b_in": dt("b_in", [2 * D]), "g_ln_g": dt("g_ln_g", [D]), "g_ln_b": dt("g_ln_b", [D]),
              "w_s": dt("w_s", [8, 128, 128]), "b_s": dt("b_s", [128, 8]), "w_out": dt("w_out", [D, D])})
    xout = dt("xout", [NT * 128, D], "ExternalOutput")
    tiles = [(xin[t * 128:(t + 1) * 128, :], 0 if t < 16 else 1, xout[t * 128:(t + 1) * 128, :]) for t in range(NT)]
    stage_gmlp(nc, "", tiles, A)
    return nc


def stage_gmlp(nc, pfx, tiles, A):
    NT = len(tiles)
    es = ExitStack()
    with es:
        C = mixer_prologue(nc, es, pfx, A)
        S, sb, ps = C.S, C.sb, C.ps
        win_d, bin_d, glng_d, glnb_d, ws_d, bs_d, wout_d = (A[k] for k in ("w_in", "b_in", "g_ln_g", "g_ln_b", "w_s", "b_s", "w_out"))
        Win = sb("Win", [128, 8, 2 * D], BF16)
        Wout = sb("Wout", [128, 8, D], BF16)
        wsT = sb("wsT", [128, 8, 128], BF16)
        BIN = sb("BIN", [128, 2 * D])
        GLNG = sb("GLNG", [128, D])
        GLNB = sb("GLNB", [128, D])
        bs = sb("bs", [128, 8])
        Xt = [sb(f"Xt{i}", [128, D]) for i in range(2)]
        OUT = [sb(f"OUT{i}", [128, D]) for i in range(2)]
        T0 = sb("T0", [128, D]); T1 = sb("T1", [128, D]); T2 = sb("T2", [128, D])
        HTt = sb("HTt", [128, 8, 128], BF16)
        HT2 = sb("HT2", [128, 8, 128], BF16)
        Z = sb("Z", [128, 2 * D])
        TZ = sb("TZ", [128, 2 * D])
        TZ2 = sb("TZ2", [128, 2 * D])
        VN = sb("VN", [128, D], BF16)
        US = sb("US", [128, D])
        psZ = [ps(f"psZ{i}", [128, 512]) for i in range(4)]
        mixer_mods(C)
        load_w_bf16(C, Win[:], "Win", win_d, 8, 2 * D)
        load_w_bf16(C, Wout[:], "Wout", wout_d, 8, D)
        S.dma("sp", BIN[:], bin_d.partition_broadcast(128), "ld_bin", writes=["BIN"])
        S.dma("sp", GLNG[:], glng_d.partition_broadcast(128), "ld_glng", writes=["GLNG"])
        S.dma("sp", GLNB[:], glnb_d.partition_broadcast(128), "ld_glnb", writes=["GLNB"])
        S.dma("sp", bs[:], bs_d, "ld_bs", writes=["bs"])
        wst = C.STG[0][:, 0:1024].rearrange("p (g q) -> p g q", g=8)
        S.dma("sp", wst, ws_d.rearrange("g p q -> p g q"), "ld_stg0", writes=["STG0"])
        for g in range(8):
            S.op("pe", lambda e, g=g: e.transpose(out=C.psBig[0][:, g * 128:(g + 1) * 128], in_=wst[:, g, :], identity=C.ident[:]),
                 reads=["STG0", "ident"], writes=["psBig0"])
        S.op("act", lambda e: e.activation(out=wsT[:], in_=C.psBig[0][:].rearrange("p (g n) -> p g n", g=8), func=AF.Copy),
             reads=["psBig0"], writes=["wsT"])
        for t in range(NT):
            s = tiles[t][1]
            xt = Xt[t % 2][:]
            xk = f"Xt{t % 2}"
            S.dma("sp", xt, tiles[t][0], f"ld_x{t % 2}", writes=[xk])
            modulate_T(C, xt, xk, s, T0[:], "T0", C.psBig[0], "psBig0", HTt[:], "HTt")
            proj(C, [(psZ[i], f"psZ{i}") for i in range(4)], HTt, "HTt", Win, "Win", 0, 2 * D)
            for i in range(4):
                S.op("dve", lambda e, i=i: e.tensor_tensor(out=Z[:, i * 512:(i + 1) * 512], in0=psZ[i][:], in1=BIN[:, i * 512:(i + 1) * 512],
                                                           op=ALU.add), reads=[f"psZ{i}", "BIN"], writes=["Z"])
            S.op("act", lambda e: e.activation(out=TZ[:], in_=Z[:], func=AF.Square), reads=["Z"], writes=["TZ"])
            S.op("dve", lambda e: e.tensor_scalar(out=TZ[:], in0=TZ[:], scalar1=0.044715, scalar2=1.0, op0=ALU.mult, op1=ALU.add),
                 reads=["TZ"], writes=["TZ"])
            S.op("pool", lambda e: e.tensor_tensor(out=TZ[:], in0=TZ[:], in1=Z[:], op=ALU.mult), reads=["TZ", "Z"], writes=["TZ"])
            S.op("act", lambda e: e.activation(out=TZ[:], in_=TZ[:], func=AF.Sigmoid, scale=1.5957691216057308), reads=["TZ"], writes=["TZ"])
            S.op("pool", lambda e: e.tensor_tensor(out=TZ2[:], in0=TZ[:], in1=Z[:], op=ALU.mult), reads=["TZ", "Z"], writes=["TZ2"])
            layer_norm_tile(S, TZ2[:, D:2 * D], "TZ2", VN[:], "VN", T2[:], "T2", GLNG[:], "GLNG", GLNB[:], "GLNB", C.small, C.eps1)
            for g in range(8):
                S.op("pe", lambda e, g=g: e.matmul(C.psBig[0][:, g * 128:(g + 1) * 128], lhsT=wsT[:, g, :], rhs=VN[:, g * 128:(g + 1) * 128],
                                                   start=True, stop=True), reads=["wsT", "VN"], writes=["psBig0"])
            for g in range(8):
                S.op("dve", lambda e, g=g: e.scalar_tensor_tensor(out=US[:, g * 128:(g + 1) * 128], in0=C.psBig[0][:, g * 128:(g + 1) * 128],
                                                                  scalar=bs[:, g:g + 1], in1=TZ2[:, g * 128:(g + 1) * 128],
                                                                  op0=ALU.add, op1=ALU.mult), reads=["psBig0", "bs", "TZ2"], writes=["US"])
            to_T(C, US[:], "US", C.psBig[1], "psBig1", HT2[:], "HT2")
            proj(C, [(C.psBig[1][:, 0:512], "psBig1"), (C.psBig[1][:, 512:1024], "psBig1")], HT2, "HT2", Wout, "Wout", 0, D)
            residual_ln_store(C, C.psBig[1], "psBig1", xt, xk, s, T1[:], "T1", T2[:], "T2", OUT[t % 2][:], f"OUT{t % 2}",
                              tiles[t][2], f"st{t % 2}")
        S.emit()
        S.close()


def build_conv():
    nc = bass.Bass("TRN2", target_bir_lowering=False)
    dt, A = std_inputs(nc)
    xin = dt("xin", [20 * 128, D])
    A.update({"valid": dt("valid", [128, 20]), "w_in": dt("w_in", [D, 3 * D]), "w_conv": dt("w_conv", [3, D]), "w_out": dt("w_out", [D, D])})
    xout = dt("xout", [18 * 128, D], "ExternalOutput")
    lat = [(xin[e * 128:(e + 1) * 128, :], 0, e, (xout[(e - 1) * 128:e * 128, :] if 1 <= e <= 16 else None)) for e in range(18)]
    ctx = [(xin[e * 128:(e + 1) * 128, :], 1, e, xout[(e - 2) * 128:(e - 1) * 128, :]) for e in (18, 19)]
    stage_conv(nc, "", [lat, ctx], A, 20)
    return nc


def stage_conv(nc, pfx, seqs, A, nvalid):
    es = ExitStack()
    with es:
        C = mixer_prologue(nc, es, pfx, A)
        S, sb, ps = C.S, C.sb, C.ps
        valid_d, win_d, wconv_d, wout_d = (A[k] for k in ("valid", "w_in", "w_conv", "w_out"))
        Win = sb("Win", [128, 8, 3 * D], BF16)
        Wout = sb("Wout", [128, 8, D], BF16)
        WC = [sb(f"WC{i}", [128, D]) for i in range(3)]
        valid = sb("valid_sb", [128, nvalid])
        Xr = [sb(f"Xr{i}", [128, D]) for i in range(4)]
        Zr = [C.STG[1][:, i * D:(i + 1) * D] for i in range(4)]
        HTr = [sb(f"HTr{i}", [128, 8, 128], BF16) for i in range(4)]
        ZM = sb("ZM", [128, D]); ZP = sb("ZP", [128, D]); TC = sb("TC", [128, D]); TG = sb("TG", [128, D])
        OUT = [sb(f"OUT{i}", [128, D]) for i in range(2)]
        T0 = sb("T0", [128, D]); T1 = sb("T1", [128, D]); T2 = sb("T2", [128, D])
        HT2 = sb("HT2", [128, 8, 128], BF16)
        psP = [ps(f"psP{i}", [128, 512]) for i in range(4)]
        mixer_mods(C)
        load_w_bf16(C, Win[:], "Win", win_d, 8, 3 * D)
        load_w_bf16(C, Wout[:], "Wout", wout_d, 8, D)
        for i in range(3):
            S.dma("sp", WC[i][:], wconv_d[i, :].partition_broadcast(128), f"ld_wc{i}", writes=[f"WC{i}"])
        S.dma("sp", valid[:], valid_d, "ld_valid", writes=["valid"])
        S.op("dve", lambda e: e.memset(ZM[0:1, 0:1], 0.0), writes=["STG1", "Zr0", "Zr1", "Zr2", "Zr3"])

        cnt = {"a": 0, "o": 0}

        def Astep(tile):
            src, s, vcol, _ = tile
            r = cnt["a"] % 4
            cnt["a"] += 1
            xt, xk = Xr[r][:], f"Xr{r}"
            S.dma("sp", xt, src, f"ld_x{r}", writes=[xk])
            modulate_T(C, xt, xk, s, T0[:], "T0", C.psBig[0], "psBig0", HTr[r][:], f"HTr{r}")
            proj(C, [(psP[i], f"psP{i}") for i in range(4)], HTr[r], f"HTr{r}", Win, "Win", D, 2 * D)
            for h in range(2):
                S.op("act", lambda e_, h=h: e_.activation(out=TG[:, h * 512:(h + 1) * 512], in_=psP[h][:], func=AF.Copy, scale=valid[:, vcol:vcol + 1]),
                     reads=[f"psP{h}", "valid"], writes=["TG"])
            for h in range(2):
                S.op("dve", lambda e_, h=h: e_.tensor_tensor(out=Zr[r][:, h * 512:(h + 1) * 512], in0=TG[:, h * 512:(h + 1) * 512], in1=psP[2 + h][:],
                                                            op=ALU.mult), reads=["TG", f"psP{2 + h}"], writes=[f"Zr{r}"])
            return r

        def Bstep(tile, r, prev, nxt):
            _, s, _, out_ap = tile
            ot = cnt["o"]
            cnt["o"] += 1
            xt, xk = Xr[r][:], f"Xr{r}"
            zc, zk = Zr[r], f"Zr{r}"
            if prev is None:
                S.op("dve", lambda e_: e_.memset(ZM[:], 0.0), writes=["ZM"])
            if nxt is None:
                S.op("dve", lambda e_: e_.memset(ZP[:], 0.0), writes=["ZP"])
            S.dma("sp", ZM[1:128, :], zc[0:127, :], "sh_zm", reads=[zk], writes=["ZM"])
            if prev is not None:
                S.dma("sp", ZM[0:1, :], Zr[prev][127:128, :], "sh_zm", reads=[f"Zr{prev}"], writes=["ZM"])
            S.dma("sp", ZP[0:127, :], zc[1:128, :], "sh_zp", reads=[zk], writes=["ZP"])
            if nxt is not None:
                S.dma("sp", ZP[127:128, :], Zr[nxt][0:1, :], "sh_zp", reads=[f"Zr{nxt}"], writes=["ZP"])
            S.op("dve", lambda e_: e_.tensor_tensor(out=ZM[:], in0=ZM[:], in1=WC[0][:], op=ALU.mult), reads=["ZM", "WC0"], writes=["ZM"])
            S.op("pool", lambda e_: e_.tensor_tensor(out=ZP[:], in0=ZP[:], in1=WC[2][:], op=ALU.mult), reads=["ZP", "WC2"], writes=["ZP"])
            S.op("dve", lambda e_: e_.tensor_tensor(out=TC[:], in0=zc, in1=WC[1][:], op=ALU.mult), reads=[zk, "WC1"], writes=["TC"])
            S.op("pool", lambda e_: e_.tensor_tensor(out=TC[:], in0=TC[:], in1=ZM[:], op=ALU.add), reads=["TC", "ZM"], writes=["TC"])
            S.op("dve", lambda e_: e_.tensor_tensor(out=TC[:], in0=TC[:], in1=ZP[:], op=ALU.add), reads=["TC", "ZP"], writes=["TC"])
            proj(C, [(C.psBig[1][:, 0:512], "psBig1"), (C.psBig[1][:, 512:1024], "psBig1")], HTr[r], f"HTr{r}", Win, "Win", 0, D)
            S.op("dve", lambda e_: e_.tensor_tensor(out=TG[:], in0=C.psBig[1][:], in1=TC[:], op=ALU.mult), reads=["psBig1", "TC"], writes=["TG"])
            to_T(C, TG[:], "TG", C.psBig[1], "psBig1", HT2[:], "HT2")
            proj(C, [(C.psBig[1][:, 0:512], "psBig1"), (C.psBig[1][:, 512:1024], "psBig1")], HT2, "HT2", Wout, "Wout", 0, D)
            residual_ln_store(C, C.psBig[1], "psBig1", xt, xk, s, T1[:], "T1", T2[:], "T2", OUT[ot % 2][:], f"OUT{ot % 2}",
                              out_ap, f"st{ot % 2}")

        for seq in seqs:
            slots = {}
            n = len(seq)
            for i in range(n + 1):
                if i < n:
                    slots[i] = Astep(seq[i])
                j = i - 1
                if j >= 0 and seq[j][3] is not None:
                    Bstep(seq[j], slots[j], slots.get(j - 1), slots.get(j + 1) if j + 1 < n else None)
        S.emit()
        S.close()


def build_attn(want_ctx):
    nc = bass.Bass("TRN2", target_bir_lowering=False)
    dt, A = std_inputs(nc)
    NOUT = 18 if want_ctx else 16
    xin = dt("xin", [20 * 128, D])
    A.update({"kbias": dt("kbias", [128, 20]), "cos_t": dt("cos_t", [128, 20 * 64]), "sin_t": dt("sin_t", [128, 20 * 64]),
              "maskp": dt("maskp", [128, 512]), "maskn": dt("maskn", [128, 512]), "w_qkv": dt("w_qkv", [D, 1536]),
              "w_o": dt("w_o", [D, D]), "sink": dt("sink", [16])})
    xout = dt("xout", [NOUT * 128, D], "ExternalOutput")
    kv = [(xin[e * 128:(e + 1) * 128, :], 0 if e < 18 else 1, e, e) for e in range(20)]
    q = [(xin[(o + 1) * 128:(o + 2) * 128, :], 0, o + 1, [(o, "P"), (o + 1, None), (o + 2, "N"), (18, None), (19, None)],
          xout[o * 128:(o + 1) * 128, :]) for o in range(16)]
    if want_ctx:
        q += [(xin[e * 128:(e + 1) * 128, :], 1, e, [(18, None), (19, None)], xout[(e - 2) * 128:(e - 1) * 128, :]) for e in (18, 19)]
    stage_attn(nc, "", kv, q, A, 20)
    return nc


def stage_attn(nc, pfx, kv_tiles, q_tiles, A, ntbl):
    NKT = len(kv_tiles)
    es = ExitStack()
    with es:
        C = mixer_prologue(nc, es, pfx, A)
        S, sb, ps = C.S, C.sb, C.ps
        kbias_d, cos_d, sin_d, maskp_d, maskn_d, wqkv_d, wo_d, sink_d = (A[k] for k in ("kbias", "cos_t", "sin_t", "maskp", "maskn", "w_qkv", "w_o", "sink"))
        Wqkv = sb("Wqkv", [128, 8, 1536], BF16)
        Wo = sb("Wo", [128, 8, D], BF16)
        KT = sb("KT", [128, 2, NKT * 128], BF16)
        V = sb("V", [128, NKT, 256], BF16)
        kbias = sb("kbias_sb", [128, ntbl])
        COS = sb("COS", [128, ntbl, 64])
        SIN = sb("SIN", [128, ntbl, 64])
        MP = sb("MP", [128, 512])
        MN = sb("MN", [128, 512])
        SINKB = sb("SINKB", [128, 2, 512])
        ES = sb("ES", [128, 16])
        identb = sb("identb", [128, 128], BF16)
        onesb = sb("onesb", [128, 64], BF16)
        Xt = [sb(f"Xt{i}", [128, D]) for i in range(2)]
        OUT = [sb(f"OUT{i}", [128, D]) for i in range(2)]
        T0 = sb("T0", [128, D]); T1 = sb("T1", [128, D]); T2 = sb("T2", [128, D])
        HTt = sb("HTt", [128, 8, 128], BF16)
        R1 = sb("R1", [128, D]); R2 = sb("R2", [128, D])
        KR = sb("KR", [128, 256], BF16)
        QRp = sb("QRp", [128, 8, 128], BF16)
        QT = sb("QT", [128, 8, 128], BF16)
        PT = [sb(f"PT{i}", [128, 512], BF16) for i in range(3)]
        DEN = sb("DEN", [128, 512])
        OT = sb("OT", [128, 2, 512], BF16)
        psS = [ps(f"psS{i}", [128, 512]) for i in range(2)]
        psX = ps("psX", [128, 8, 128], BF16)
        psO = C.psBig[1][:, 0:512]
        psD = C.psBig[1][:, 512:1024]
        S.op("pool", lambda e: e.memset(onesb[:], 1.0), writes=["onesb"])
        S.op("pool", lambda e: e.tensor_copy(out=identb[:], in_=C.ident[:]), reads=["ident"], writes=["identb"])
        mixer_mods(C)
        load_w_bf16(C, Wqkv[:], "Wqkv", wqkv_d, 8, 1536)
        for half in range(2):
            i = C.stg_i % 2
            C.stg_i += 1
            st = C.STG[i][:].rearrange("p (c n) -> p c n", c=4)
            for cc in range(4):
                c = half * 4 + cc
                jp, g = c // 4, c % 4
                for r in range(2):
                    row = 512 * jp + 256 * r + 64 * g
                    S.dma("sp", st[r * 64:(r + 1) * 64, cc, :], wo_d[row:row + 64, :], f"ld_stg{i}", writes=[f"STG{i}"])
            S.op("act", lambda e, st=st, half=half: e.activation(out=Wo[:, half * 4:(half + 1) * 4, :], in_=st, func=AF.Copy),
                 reads=[f"STG{i}"], writes=["Wo"])
        S.dma("sp", kbias[:], kbias_d, "ld_kb", writes=["kbias"])
        S.dma("sp", COS[:].rearrange("p t n -> p (t n)"), cos_d, "ld_cos", writes=["COS"])
        S.dma("sp", SIN[:].rearrange("p t n -> p (t n)"), sin_d, "ld_sin", writes=["SIN"])
        S.dma("sp", MP[:], maskp_d, "ld_mp", writes=["MP"])
        S.dma("sp", MN[:], maskn_d, "ld_mn", writes=["MN"])
        S.dma("sp", ES[:], sink_d.partition_broadcast(128), "ld_sink", writes=["ES"])
        S.op("act", lambda e: e.activation(out=ES[:], in_=ES[:], func=AF.Exp), reads=["ES"], writes=["ES"])
        for jp in range(2):
            for g in range(4):
                for r in range(2):
                    h = 8 * jp + 4 * r + g
                    S.op("act", lambda e, jp=jp, g=g, r=r, h=h: e.activation(
                        out=SINKB[r * 64:(r + 1) * 64, jp, g * 128:(g + 1) * 128], in_=C.ones[r * 64:(r + 1) * 64, :], func=AF.Copy,
                        scale=ES[r * 64:(r + 1) * 64, h:h + 1]), reads=["ones", "ES"], writes=["SINKB"])

        def rope(src_ps, pskey, nh, e, out_fn):
            n = nh * 64
            xv = src_ps.rearrange("p (h b f i) -> p h b f i", h=nh, b=2, f=2)
            r1v = R1[:, 0:n].rearrange("p (h n) -> p h n", h=nh)
            r2v = R2[:, 0:n].rearrange("p (h b f i) -> p h b f i", h=nh, b=2, f=2)
            cosb = COS[:, e, :].unsqueeze(1).to_broadcast([128, nh, 64])
            sv = SIN[:, e, :].rearrange("p (b f i) -> p b f i", b=2, f=2)
            S.op("dve", lambda e_: e_.tensor_tensor(out=r1v, in0=src_ps.rearrange("p (h n) -> p h n", h=nh), in1=cosb, op=ALU.mult),
                 reads=[pskey, "COS"], writes=["R1"])
            for f in range(2):
                sb_ = sv[:, :, f, :].unsqueeze(1).to_broadcast([128, nh, 2, 16])
                S.op("dve", lambda e_, f=f, sb_=sb_: e_.tensor_tensor(out=r2v[:, :, :, f, :], in0=xv[:, :, :, 1 - f, :], in1=sb_, op=ALU.mult),
                     reads=[pskey, "SIN"], writes=["R2"])
            out_fn(n)

        for e in range(NKT):
            src_, s, tbl, kbc = kv_tiles[e]
            xt, xk = Xt[e % 2][:], f"Xt{e % 2}"
            S.dma("sp", xt, src_, f"ld_x{e % 2}", writes=[xk])
            modulate_T(C, xt, xk, s, T0[:], "T0", C.psBig[0], "psBig0", HTt[:], "HTt")
            proj(C, [(psS[0], "psS0")], HTt, "HTt", Wqkv, "Wqkv", 1024, 512)

            def kout(n):
                S.op("pool", lambda e_: e_.tensor_tensor(out=KR[:], in0=R1[:, 0:256], in1=R2[:, 0:256], op=ALU.add), reads=["R1", "R2"], writes=["KR"])
            S.op("act", lambda e_, e=e: e_.activation(out=V[:, e, :], in_=psS[0][:, 256:512], func=AF.Copy), reads=["psS0"], writes=["V"])
            rope(psS[0][:, 0:256], "psS0", 4, tbl, kout)
            for jp in range(2):
                S.op("pe", lambda e_, jp=jp: e_.transpose(out=psX[:, jp, :], in_=KR[:, jp * 128:(jp + 1) * 128], identity=identb[:]),
                     reads=["KR", "identb"], writes=["psX"])
            S.op("act", lambda e_, e=e: e_.activation(out=KT[:, :, e * 128:(e + 1) * 128], in_=psX[:, 0:2, :], func=AF.Copy),
                 reads=["psX"], writes=["KT"])

        pti = 0
        for o, (src_, s, tbl, chunks_, out_ap) in enumerate(q_tiles):
            xt, xk = Xt[o % 2][:], f"Xt{o % 2}"
            S.dma("sp", xt, src_, f"ld_x{o % 2}", writes=[xk])
            modulate_T(C, xt, xk, s, T0[:], "T0", C.psBig[0], "psBig0", HTt[:], "HTt")
            proj(C, [(C.psBig[0][:, 0:512], "psBig0"), (C.psBig[0][:, 512:1024], "psBig0")], HTt, "HTt", Wqkv, "Wqkv", 0, D)

            def qout(n):
                for jp in range(2):
                    a = R1[:, jp * 512:(jp + 1) * 512].rearrange("p (r g d) -> p r g d", r=2, g=4)
                    b = R2[:, jp * 512:(jp + 1) * 512].rearrange("p (r g d) -> p r g d", r=2, g=4)
                    o_ = QRp[:, jp * 4:(jp + 1) * 4, :].rearrange("p g (r d) -> p r g d", r=2)
                    S.op("pool", lambda e_, a=a, b=b, o_=o_: e_.tensor_tensor(out=o_, in0=a, in1=b, op=ALU.add), reads=["R1", "R2"], writes=["QRp"])
            rope(C.psBig[0][:], "psBig0", 16, tbl, qout)
            for c in range(8):
                S.op("pe", lambda e_, c=c: e_.transpose(out=psX[:, c, :], in_=QRp[:, c, :], identity=identb[:]), reads=["QRp", "identb"], writes=["psX"])
            S.op("act", lambda e_: e_.activation(out=QT[:], in_=psX[:], func=AF.Copy), reads=["psX"], writes=["QT"])
            chunks = [(kt, {"P": MP, "N": MN, None: None}[m]) for (kt, m) in chunks_]
            for jp in range(2):
                for r in range(2):
                    j = 2 * jp + r
                    lo, hi = r * 64, (r + 1) * 64
                    for ci, (kt, mask) in enumerate(chunks):
                        kbc = kv_tiles[kt][3]
                        pss, psk = psS[pti % 2], f"psS{pti % 2}"
                        pt, ptk = PT[pti % 3], f"PT{pti % 3}"
                        pti += 1
                        S.op("pe", lambda e_, pss=pss, kt=kt, jp=jp, lo=lo, hi=hi: e_.matmul(
                            pss[:], lhsT=KT[lo:hi, jp, kt * 128:(kt + 1) * 128], rhs=QT[lo:hi, jp * 4:(jp + 1) * 4, :], start=True, stop=True),
                            reads=["KT", "QT"], writes=[psk])
                        S.op("act", lambda e_, pss=pss, pt=pt, kbc=kbc: e_.activation(out=pt[:], in_=pss[:], func=AF.Exp, bias=kbias[:, kbc:kbc + 1], scale=0.125),
                             reads=[psk, "kbias"], writes=[ptk])
                        if mask is not None:
                            S.op("dve", lambda e_, pt=pt, mask=mask: e_.tensor_tensor(out=pt[:], in0=pt[:], in1=mask[:], op=ALU.mult),
                                 reads=[ptk, "MP", "MN"], writes=[ptk])
                        first, last = (ci == 0), (ci == len(chunks) - 1)
                        S.op("pe", lambda e_, pt=pt, kt=kt, j=j, lo=lo, hi=hi, first=first, last=last: e_.matmul(
                            psO[lo:hi, :], lhsT=V[:, kt, j * 64:(j + 1) * 64], rhs=pt[:], start=first, stop=last),
                            reads=["V", ptk], writes=["psO"])
                        S.op("pe", lambda e_, pt=pt, lo=lo, hi=hi, first=first, last=last: e_.matmul(
                            psD[lo:hi, :], lhsT=onesb[:, 0:64], rhs=pt[:], start=first, stop=last),
                            reads=["onesb", ptk], writes=["psD"])
                S.op("dve", lambda e_, jp=jp: e_.tensor_tensor(out=DEN[:], in0=psD, in1=SINKB[:, jp, :], op=ALU.add), reads=["psD", "SINKB"], writes=["DEN"])
                S.op("dve", lambda e_: e_.reciprocal(out=DEN[:], in_=DEN[:]), reads=["DEN"], writes=["DEN"])
                S.op("dve", lambda e_, jp=jp: e_.tensor_tensor(out=OT[:, jp, :], in0=psO, in1=DEN[:], op=ALU.mult), reads=["psO", "DEN"], writes=["OT"])
            for half in range(2):
                for c in range(8):
                    jp, g = c // 4, c % 4
                    S.op("pe", lambda e_, half=half, c=c, jp=jp, g=g: e_.matmul(
                        C.psBig[0][:, half * 512:(half + 1) * 512], lhsT=OT[:, jp, g * 128:(g + 1) * 128], rhs=Wo[:, c, half * 512:(half + 1) * 512],
                        start=(c == 0), stop=(c == 7)), reads=["OT", "Wo"], writes=["psBig0"])
            residual_ln_store(C, C.psBig[0], "psBig0", xt, xk, s, T1[:], "T1", T2[:], "T2", OUT[o % 2][:], f"OUT{o % 2}",
                              out_ap, f"st{o % 2}")
        S.emit()
        S.close()


NWIN = 22
NEXT = 20


def build_fused(nstage=8, dbg=False):
    nc = bass.Bass("TRN2", target_bir_lowering=False)
    dt = lambda n, s, k="ExternalInput": nc.dram_tensor(n, s, F32, kind=k).ap()
    xw = dt("xw", [NWIN * 128, D])
    ctx = dt("ctx", [256, D])
    cvec = dt("cvec", [2, D])
    w_mod = dt("w_mod", [4, D, 6 * D]); b_mod = dt("b_mod", [4, 6 * D])
    ln1_g = dt("ln1_g", [4, D]); ln1_b = dt("ln1_b", [4, D]); ln2_g = dt("ln2_g", [4, D]); ln2_b = dt("ln2_b", [4, D])
    rw = dt("router_w", [D, NE]); rb = dt("router_bias", [NE])
    w1 = dt("moe_w1", [4, NE, D, DEXP]); w3 = dt("moe_w3", [4, NE, D, DEXP]); w2 = dt("moe_w2", [4, NE, DEXP, D])
    a_w_qkv = dt("a_w_qkv", [2, D, 1536]); a_w_o = dt("a_w_o", [2, D, D]); a_sink = dt("a_sink", [2, 16])
    b_w_in = dt("b_w_in", [1, D, 2 * D]); b_b_in = dt("b_b_in", [1, 2 * D]); b_ln_g = dt("b_ln_g", [1, D]); b_ln_b = dt("b_ln_b", [1, D])
    b_w_s = dt("b_w_s", [1, 8, 128, 128]); b_b_s = dt("b_b_s", [1, 128, 8]); b_w_out = dt("b_w_out", [1, D, D])
    c_w_in = dt("c_w_in", [1, D, 3 * D]); c_w_conv = dt("c_w_conv", [1, 3, D]); c_w_out = dt("c_w_out", [1, D, D])
    kbias = dt("kbias", [128, 24]); cos_t = dt("cos_t", [128, 24 * 64]); sin_t = dt("sin_t", [128, 24 * 64])
    maskp = dt("maskp", [128, 512]); maskn = dt("maskn", [128, 512]); valid = dt("valid", [128, 22])
    SA = dt("scrA", [22 * 128, D], "ExternalOutput" if dbg else "Internal")
    SB = dt("scrB", [22 * 128, D], "ExternalOutput" if dbg else "Internal")
    xout = dt("xout", [2048, D], "ExternalOutput")
    row = lambda T, i: T[i * 128:(i + 1) * 128, :]

    def AM(L):
        return {"cvec": cvec, "wmod": w_mod[L][:, 3 * D:6 * D], "bmod": b_mod[L][3 * D:6 * D], "lng": ln2_g[L], "lnb": ln2_b[L],
                "rw": rw, "rb": rb, "w1": w1[L], "w3": w3[L], "w2": w2[L]}

    def AX_(L):
        return {"cvec": cvec, "wmod": w_mod[L][:, 0:3 * D], "bmod": b_mod[L][0:3 * D], "lng": ln1_g[L], "lnb": ln1_b[L]}

    def moe(L, pfx, tiles):
        h = (len(tiles) + 1) // 2 if len(tiles) > 18 else len(tiles)
        stage_moe(nc, pfx + "a_", tiles[:h], AM(L))
        if h < len(tiles):
            stage_moe(nc, pfx + "b_", tiles[h:], AM(L))

    attn_tabs = {"kbias": kbias, "cos_t": cos_t, "sin_t": sin_t, "maskp": maskp, "maskn": maskn}
    kv = [(row(xw, v), 0, v, v) for v in range(NWIN)] + [(row(ctx, c), 1, 22 + c, 22 + c) for c in range(2)]
    q = [(row(xw, u + 1), 0, u + 1, [(u, "P"), (u + 1, None), (u + 2, "N"), (22, None), (23, None)], row(SA, u)) for u in range(NEXT)]
    q += [(row(ctx, c), 1, 22 + c, [(22, None), (23, None)], row(SA, 20 + c)) for c in range(2)]
    A = AX_(0); A.update(attn_tabs); A.update({"w_qkv": a_w_qkv[0], "w_o": a_w_o[0], "sink": a_sink[0]})
    stage_attn(nc, "s0_", kv, q, A, 24)
    if nstage <= 1:
        return nc
    allt = lambda Tsrc, Tdst: [(row(Tsrc, u), 0 if u < NEXT else 1, row(Tdst, u)) for u in range(22)]
    moe(0, "m0", allt(SA, SB))
    if nstage <= 2:
        return nc
    A = AX_(1); A.update({"w_in": b_w_in[0], "b_in": b_b_in[0], "g_ln_g": b_ln_g[0], "g_ln_b": b_ln_b[0], "w_s": b_w_s[0], "b_s": b_b_s[0],
                          "w_out": b_w_out[0]})
    stage_gmlp(nc, "s1_", allt(SB, SA), A)
    if nstage <= 3:
        return nc
    moe(1, "m1", allt(SA, SB))
    if nstage <= 4:
        return nc
    lat = [(row(SB, u), 0, u, (row(SA, u) if 1 <= u <= 18 else None)) for u in range(NEXT)]
    cx = [(row(SB, 20 + c), 1, 20 + c, row(SA, 20 + c)) for c in range(2)]
    A = AX_(2); A.update({"valid": valid, "w_in": c_w_in[0], "w_conv": c_w_conv[0], "w_out": c_w_out[0]})
    stage_conv(nc, "s2_", [lat, cx], A, 22)
    if nstage <= 5:
        return nc
    t2 = [(row(SA, u), 0, row(SB, u)) for u in range(1, 19)] + [(row(SA, 20 + c), 1, row(SB, 20 + c)) for c in range(2)]
    moe(2, "m2", t2)
    if nstage <= 6:
        return nc
    kv = [(row(SB, u), 0, u + 1, u + 1) for u in range(1, 19)] + [(row(SB, 20 + c), 1, 22 + c, 22 + c) for c in range(2)]
    q = [(row(SB, u), 0, u + 1, [(u - 2, "P"), (u - 1, None), (u, "N"), (18, None), (19, None)], row(SA, u)) for u in range(2, 18)]
    A = AX_(3); A.update(attn_tabs); A.update({"w_qkv": a_w_qkv[1], "w_o": a_w_o[1], "sink": a_sink[1]})
    stage_attn(nc, "s3_", kv, q, A, 24)
    moe(3, "m3", [(row(SA, u), 0, row(xout, u - 2)) for u in range(2, 18)])
    return nc


_NC = []


def _tables(core):
    L = 16384
    freqs = np.power(np.float32(10000.0), -np.arange(16, dtype=np.float32) / np.float32(16)).astype(np.float32)
    pos = (core * 2048 - 384 + np.arange(NWIN * 128)).astype(np.float32)
    row = np.floor(pos / 64.0).astype(np.float32)
    col = (pos - row * 64).astype(np.float32)
    ar = row[:, None] * freqs[None, :]
    ac = col[:, None] * freqs[None, :]
    ang = np.concatenate([ar, ar, ac, ac], -1)
    cos = np.cos(ang).astype(np.float32)
    sin = np.sin(ang).astype(np.float32)
    sgn = np.tile(np.concatenate([-np.ones(16, np.float32), np.ones(16, np.float32)]), 2)
    sinS = sin * sgn[None, :]
    cos = np.concatenate([cos, np.ones((256, 64), np.float32)], 0)
    sinS = np.concatenate([sinS, np.zeros((256, 64), np.float32)], 0)
    cos_t = np.ascontiguousarray(cos.reshape(24, 128, 64).transpose(1, 0, 2).reshape(128, 24 * 64))
    sin_t = np.ascontiguousarray(sinS.reshape(24, 128, 64).transpose(1, 0, 2).reshape(128, 24 * 64))
    kb = np.zeros((128, 24), np.float32)
    for v in range(NWIN):
        st = core * 2048 - 384 + 128 * v
        if st < 0 or st >= L:
            kb[:, v] = -30000.0
    valid = np.ones((128, 22), np.float32)
    for u in range(NEXT):
        st = core * 2048 - 256 + 128 * u
        if st < 0 or st >= L:
            valid[:, u] = 0.0
    return cos_t, sin_t, kb, valid


def kernel(x, c, ctx, c_ctx, w_mod, b_mod, ln1_g, ln1_b, ln2_g, ln2_b, router_w, router_bias, moe_w1, moe_w3, moe_w2,
           a_w_qkv, a_w_o, a_sink, b_w_in, b_b_in, b_ln_g, b_ln_b, b_w_s, b_b_s, b_w_out, c_w_in, c_w_conv, c_w_out):
    f32 = lambda a: np.ascontiguousarray(np.asarray(a, dtype=np.float32))
    if not _NC:
        _NC.append(build_fused())
    nc = _NC[0]
    xc = f32(x)[0]
    zpad = np.zeros((384, D), np.float32)
    xp = np.concatenate([zpad, xc, zpad], 0)
    kk = np.arange(128)[:, None]
    qq = np.arange(128)[None, :]
    com = {"ctx": f32(ctx)[0], "cvec": np.stack([f32(c)[0], f32(c_ctx)]).astype(np.float32),
           "w_mod": f32(w_mod), "b_mod": f32(b_mod), "ln1_g": f32(ln1_g), "ln1_b": f32(ln1_b), "ln2_g": f32(ln2_g), "ln2_b": f32(ln2_b),
           "router_w": f32(router_w), "router_bias": f32(router_bias), "moe_w1": f32(moe_w1), "moe_w3": f32(moe_w3), "moe_w2": f32(moe_w2),
           "a_w_qkv": f32(a_w_qkv), "a_w_o": f32(a_w_o), "a_sink": f32(a_sink), "b_w_in": f32(b_w_in), "b_b_in": f32(b_b_in),
           "b_ln_g": f32(b_ln_g), "b_ln_b": f32(b_ln_b), "b_w_s": f32(b_w_s), "b_b_s": f32(b_b_s), "b_w_out": f32(b_w_out),
           "c_w_in": f32(c_w_in), "c_w_conv": f32(c_w_conv), "c_w_out": f32(c_w_out),
           "maskp": np.tile((kk >= qq).astype(np.float32), (1, 4)), "maskn": np.tile((kk <= qq).astype(np.float32), (1, 4))}
    in_maps = []
    for core in range(8):
        cos_t, sin_t, kb, valid = _tables(core)
        in_maps.append(dict(com, xw=np.ascontiguousarray(xp[core * 2048:core * 2048 + NWIN * 128]), cos_t=cos_t, sin_t=sin_t, kbias=kb, valid=valid))
    res = run_bass_kernel_spmd(nc, in_maps, core_ids=list(range(8)))
    out = np.concatenate([r["xout"] for r in res.results], 0)
    return out[None].astype(np.float32)
```

```python
import numpy as np
from contextlib import ExitStack
import concourse.bass as bass
import concourse.mybir as mybir
from concourse.bass_utils import run_bass_kernel_spmd

F32 = mybir.dt.float32
BF16 = mybir.dt.bfloat16
AF = mybir.ActivationFunctionType
ALU = mybir.AluOpType
AX = mybir.AxisListType


SAME_ENGINE_INORDER = False


class Sched:
    ENG = ("pe", "act", "dve", "pool", "sp")

    def __init__(self, nc, es, pfx=""):
        self.nc = nc
        self.es = es
        self.pfx = pfx
        self.q = {e: [] for e in self.ENG}
        self.semh = {e: nc.alloc_semaphore(name=pfx + "s_" + e) for e in self.ENG}
        self.cnt = {e: 0 for e in self.ENG}
        self.waited = {e: {} for e in self.ENG}
        self.lastw = {}
        self.readers = {}
        self.dcnt = {}
        self.store_sems = set()

    def _deps(self, eng, reads, writes):
        need = {}
        for k in reads:
            if k in self.lastw:
                s, v = self.lastw[k]
                need[s] = max(need.get(s, 0), v)
        for k in writes:
            if k in self.lastw:
                s, v = self.lastw[k]
                need[s] = max(need.get(s, 0), v)
            for (s, v) in self.readers.get(k, ()):
                need[s] = max(need.get(s, 0), v)
        for s, v in need.items():
            if s == eng and (eng == "pe" or SAME_ENGINE_INORDER):
                continue
            if self.waited[eng].get(s, 0) < v:
                self.q[eng].append(("wait", s, v))
                self.waited[eng][s] = v

    def op(self, eng, fn, reads=(), writes=()):
        psr = [k for k in reads if isinstance(k, str) and k.startswith("ps")]
        if psr:
            reads = [k for k in reads if k not in psr]
            writes = list(writes) + psr
        self._deps(eng, reads, writes)
        self.cnt[eng] += 1
        v = self.cnt[eng]
        self.q[eng].append(("op", fn))
        for k in reads:
            self.readers.setdefault(k, []).append((eng, v))
        for k in writes:
            self.lastw[k] = (eng, v)
            self.readers[k] = []

    def dma(self, queue, out, in_, sem, reads=(), writes=(), store=False, **kw):
        if sem not in self.semh:
            self.semh[sem] = self.nc.alloc_semaphore(name=self.pfx + "d_" + sem)
            self.dcnt[sem] = 0
        self._deps(queue, reads, writes)
        self.dcnt[sem] += 16
        v = self.dcnt[sem]
        self.q[queue].append(("dma", out, in_, sem, kw))
        for k in reads:
            self.readers.setdefault(k, []).append((sem, v))
        for k in writes:
            self.lastw[k] = (sem, v)
            self.readers[k] = []
        if store:
            self.store_sems.add(sem)

    def finish(self):
        for s in sorted(self.store_sems):
            self.q["sp"].append(("wait", s, self.dcnt[s]))

    def close(self):
        self.nc.clear_and_free_semaphores(list(self.semh.values()))
        self.nc.all_engine_barrier()

    def emit(self):
        nc = self.nc
        self.finish()

        def replay(e, engobj):
            for it in self.q[e]:
                if it[0] == "wait":
                    engobj.wait_ge(self.semh[it[1]], it[2])
                elif it[0] == "op":
                    it[1](engobj).then_inc(self.semh[e], 1)
                else:
                    _, out, in_, sem, kw = it
                    engobj.dma_start(out=out, in_=in_, **kw).then_inc(self.semh[sem], 16)

        with nc.Block() as block:
            @block.tensor
            def _(e):
                replay("pe", e)

            @block.scalar
            def _(e):
                replay("act", e)

            @block.vector
            def _(e):
                replay("dve", e)

            @block.gpsimd
            def _(e):
                replay("pool", e)

            @block.sync
            def _(e):
                replay("sp", e)


D = 1024
NE = 16
DEXP = 512
ALPHA = 8.0 ** 0.25
LN_EPS = 1e-5
EPS2 = LN_EPS / (ALPHA * ALPHA)


def common_consts(S, nc, sb, ps):
    ident = sb("ident", [128, 128])
    ones = sb("ones", [128, 128])
    S.op("pool", lambda e: e.memset(ones[:], 1.0), writes=["ones"])
    S.op("pool", lambda e: e.memset(ident[:], 0.0), writes=["ident"])
    S.op("pool", lambda e: e.affine_select(out=ident[:], in_=ident[:], pattern=[[-1, 128]], compare_op=ALU.not_equal,
                                          fill=1.0, base=0, channel_multiplier=1), reads=["ident"], writes=["ident"])
    return ident, ones


def mod_tiles(S, nc, sb, cvec, wmod, bmod, ident, ones, psbig, pbk, stage, stage_keys, SLC, slc_keys, outs):
    ct = sb("ct", [16, 128])
    csil = sb("csil", [128, 16])
    brow = sb("brow", [1, 512])
    S.dma("sp", ct[:], cvec.rearrange("s (k p) -> (s k) p", p=128), "ld_ct", writes=["ct"])
    S.op("pe", lambda e: e.transpose(out=psbig[:, 0:16], in_=ct[0:16, :], identity=ident[0:16, 0:16]),
         reads=["ct", "ident"], writes=[pbk])
    S.op("act", lambda e: e.activation(out=csil[:], in_=psbig[:, 0:16], func=AF.Silu), reads=[pbk], writes=["csil"])
    for s in range(2):
        for k in range(8):
            S.op("act", lambda e, s=s, k=k: e.activation(out=SLC[:, s, k, :], in_=ones[:], func=AF.Copy,
                                                         scale=csil[:, s * 8 + k:s * 8 + k + 1]),
                 reads=["ones", "csil"], writes=slc_keys)
    nv = max(o[0] for o in outs) + 1
    stages = stage if isinstance(stage, list) else [stage]
    skeys = stage_keys if isinstance(stage, list) else [stage_keys]
    brows = [brow, sb("brow2", [1, 512])] if isinstance(stage, list) else [brow, brow]
    idx = 0
    for v in range(nv):
        for h in range(2):
            c0 = v * 1024 + h * 512
            stg = stages[idx % len(stages)]
            sk = skeys[idx % len(stages)]
            br = brows[idx % 2]
            brk = f"brow{idx % 2}" if isinstance(stage, list) else "brow0"
            S.dma("sp", stg, wmod[:, c0:c0 + 512].rearrange("(k p) n -> p k n", p=128), f"ld_stage{idx % len(stages)}", writes=sk)
            S.dma("sp", br[:], bmod[c0:c0 + 512].rearrange("(a n) -> a n", a=1), (f"ld_brow{idx % 2}" if isinstance(stage, list) else "ld_brow0"), writes=[brk])
            idx += 1
            for s in range(2):
                mine = [o for o in outs if o[0] == v and o[1] == s]
                if not mine:
                    continue
                for k in range(8):
                    S.op("pe", lambda e, s=s, k=k, stg=stg: e.matmul(psbig[:, 0:512], lhsT=SLC[:, s, k, :], rhs=stg[:, k, :],
                                                                     start=(k == 0), stop=False),
                         reads=slc_keys + sk, writes=[pbk])
                S.op("pe", lambda e, br=br: e.matmul(psbig[:, 0:512], lhsT=ones[0:1, :], rhs=br[0:1, :], start=False, stop=True),
                     reads=["ones", brk], writes=[pbk])
                for (_, _, tile, key, kind) in mine:
                    dst = tile[:, h * 512:(h + 1) * 512]
                    if kind == "plain":
                        S.op("dve", lambda e, dst=dst: e.tensor_copy(out=dst, in_=psbig[:, 0:512]), reads=[pbk], writes=[key])
                    elif kind == "plus1":
                        S.op("dve", lambda e, dst=dst: e.tensor_scalar(out=dst, in0=psbig[:, 0:512], scalar1=1.0, scalar2=None,
                                                                       op0=ALU.add), reads=[pbk], writes=[key])
                    else:
                        S.op("dve", lambda e, dst=dst: e.tensor_scalar(out=dst, in0=psbig[:, 0:512], scalar1=1.0 / ALPHA,
                                                                       scalar2=None, op0=ALU.mult), reads=[pbk], writes=[key])


def layer_norm_tile(S, src, src_key, dst, dst_key, tmp, tmp_key, lng, lng_key, lnb, lnb_key, small, eps_t):
    st, mv, rstd, nmr = small
    for h in range(2):
        S.op("dve", lambda e, h=h: e.bn_stats(out=st[:, h, :], in_=src[:, h * 512:(h + 1) * 512]), reads=[src_key], writes=["ln_st"])
    S.op("dve", lambda e: e.bn_aggr(out=mv[:], in_=st[:].rearrange("p a b -> p (a b)")), reads=["ln_st"], writes=["ln_mv"])
    S.op("act", lambda e: e.activation(out=rstd[:], in_=mv[:, 1:2], func=AF.Sqrt, bias=eps_t[:], scale=1.0),
         reads=["ln_mv", "eps"], writes=["ln_rstd"])
    S.op("dve", lambda e: e.reciprocal(out=rstd[:], in_=rstd[:]), reads=["ln_rstd"], writes=["ln_rstd"])
    S.op("dve", lambda e: e.scalar_tensor_tensor(out=nmr[:], in0=mv[:, 0:1], scalar=-1.0, in1=rstd[:], op0=ALU.mult, op1=ALU.mult),
         reads=["ln_mv", "ln_rstd"], writes=["ln_nmr"])
    S.op("act", lambda e: e.activation(out=tmp, in_=src, func=AF.Identity, bias=nmr[:], scale=rstd[:]),
         reads=[src_key, "ln_nmr", "ln_rstd"], writes=[tmp_key])
    S.op("dve", lambda e: e.tensor_tensor(out=tmp, in0=tmp, in1=lng, op=ALU.mult), reads=[tmp_key, lng_key], writes=[tmp_key])
    S.op("dve", lambda e: e.tensor_tensor(out=dst, in0=tmp, in1=lnb, op=ALU.add), reads=[tmp_key, lnb_key], writes=[dst_key])


def build_moe():
    NT = 18
    nc = bass.Bass("TRN2", target_bir_lowering=False)
    dt = lambda n, s, k: nc.dram_tensor(n, s, F32, kind=k).ap()
    xin = dt("xin", [NT * 128, D], "ExternalInput")
    A = {"cvec": dt("cvec", [2, D], "ExternalInput"), "wmod": dt("wmod", [D, 3 * D], "ExternalInput"),
         "bmod": dt("bmod", [3 * D], "ExternalInput"), "lng": dt("lng", [D], "ExternalInput"), "lnb": dt("lnb", [D], "ExternalInput"),
         "rw": dt("rw", [D, NE], "ExternalInput"), "rb": dt("rb", [NE], "ExternalInput"),
         "w1": dt("w1", [NE, D, DEXP], "ExternalInput"), "w3": dt("w3", [NE, D, DEXP], "ExternalInput"),
         "w2": dt("w2", [NE, DEXP, D], "ExternalInput")}
    xout = dt("xout", [NT * 128, D], "ExternalOutput")
    tiles = [(xin[t * 128:(t + 1) * 128, :], 0 if t < 16 else 1, xout[t * 128:(t + 1) * 128, :]) for t in range(NT)]
    stage_moe(nc, "", tiles, A)
    return nc


def stage_moe(nc, pfx, tiles, A):
    NT = len(tiles)
    cvec, wmod, bmod, lng_d, lnb_d, rw_d, rb_d, w1_d, w3_d, w2_d = (A[k] for k in ("cvec", "wmod", "bmod", "lng", "lnb", "rw", "rb", "w1", "w3", "w2"))
    es = ExitStack()
    with es:
        S = Sched(nc, es, pfx)
        sb = lambda n, s, d=F32: es.enter_context(nc.sbuf_tensor(pfx + n, s, d))
        ps = lambda n, s, d=F32: es.enter_context(nc.psum_tensor(pfx + n, s, d))
        X = sb("X", [128, NT, D])
        HT = sb("HT", [128, 8, NT * 128], BF16)
        W13 = [sb(f"W13_{i}", [128, 2, 8, 256], BF16) for i in range(2)]
        W2 = [sb(f"W2_{i}", [128, 2, D], BF16) for i in range(2)]
        has_ctx = any(tl[1] == 1 for tl in tiles)
        W2c = [sb(f"W2c_{i}", [128, 2, D], BF16) for i in range(2)] if has_ctx else None
        STG = sb("STG", [128, 6 * D])
        MOD = {("sc1", 0): STG[:, 0:D], ("sh", 0): STG[:, D:2 * D], ("sc1", 1): STG[:, 2 * D:3 * D], ("sh", 1): STG[:, 3 * D:4 * D]}
        MKEY = {("sc1", 0): "STG0", ("sh", 0): "STG1", ("sc1", 1): "STG2", ("sh", 1): "STG3"}
        for s_ in range(2):
            MOD[("gf", s_)] = sb(f"Mgf{s_}", [128, D])[:]
            MKEY[("gf", s_)] = f"Mgf{s_}"
        S13 = STG[:, 0:4 * D].rearrange("p (a k n) -> p a k n", a=2, k=8)
        S2 = STG[:, 4 * D:6 * D].rearrange("p (k n) -> p k n", k=2)
        T = [sb(f"T{i}", [128, D]) for i in range(2)]
        SA = sb("SA", [128, 2, 512])
        H1 = [sb(f"H1_{i}", [128, 2, 512], BF16) for i in range(2)]
        rw = sb("rwt", [128, 8, NE])
        rb = sb("rbt", [128, NE])
        GATE = sb("GATE", [128, NT, NE])
        eps_t = sb("eps_t", [128, 1])
        small = (sb("ln_st", [128, 2, 6]), sb("ln_mv", [128, 2]), sb("ln_rstd", [128, 1]), sb("ln_nmr", [128, 1]))
        rt = {n: sb("rt_" + n, [128, 16]) for n in ("s", "ssel", "sm", "sel", "sc")}
        rg = {n: sb("rg_" + n, [128, 4]) for n in ("p01", "q01", "p23", "q23", "t1", "m2", "m3", "gs", "ing", "pen")}
        r1 = {n: sb("r1_" + n, [128, 1]) for n in ("gmax", "den")}
        top8 = sb("top8", [128, 8])
        psY = [ps(f"psY{i}", [128, D]) for i in range(2)]
        psA = [ps(f"psA{i}", [128, 512]) for i in range(2)]
        psB = [ps(f"psB{i}", [128, 512]) for i in range(2)]

        ident, ones = common_consts(S, nc, sb, ps)
        S.op("dve", lambda e: e.memset(eps_t[:], EPS2), writes=["eps"])
        SLC = X[:, 0:2, :].rearrange("p s (k n) -> p s k n", k=8)
        stage = [X[:, 2:6, :].rearrange("p a (b n) -> p (a b) n", b=2), X[:, 6:10, :].rearrange("p a (b n) -> p (a b) n", b=2)]
        outs = []
        for s in range(2):
            outs.append((0, s, MOD[("sh", s)], MKEY[("sh", s)], "plain"))
            outs.append((1, s, MOD[("sc1", s)], MKEY[("sc1", s)], "plus1"))
            outs.append((2, s, MOD[("gf", s)], MKEY[("gf", s)], "invalpha"))
        mod_tiles(S, nc, sb, cvec, wmod, bmod, ident, ones, psY[0], "psY0", stage, [[("X", t) for t in range(2, 6)], [("X", t) for t in range(6, 10)]],
                  SLC, [("X", 0), ("X", 1)], outs)
        S.dma("sp", rw[:], rw_d.rearrange("(k p) e -> p k e", p=128), "ld_rw", writes=["rw"])
        S.dma("sp", rb[:], rb_d.partition_broadcast(128), "ld_rb", writes=["rb"])

        for t in range(NT):
            s = tiles[t][1]
            xk = ("X", t)
            S.dma("sp", X[:, t, :], tiles[t][0], f"ld_x{t}", writes=[xk])
            S.op("dve", lambda e, t=t, s=s: e.tensor_tensor(out=T[0][:], in0=X[:, t, :], in1=MOD[("sc1", s)], op=ALU.mult),
                 reads=[xk, MKEY[("sc1", s)]], writes=["T0"])
            S.op("dve", lambda e, s=s: e.tensor_tensor(out=T[0][:], in0=T[0][:], in1=MOD[("sh", s)], op=ALU.add),
                 reads=["T0", MKEY[("sh", s)]], writes=["T0"])
            pt = psY[t % 2]
            ptk = f"psY{t % 2}"
            for k in range(8):
                S.op("pe", lambda e, k=k, pt=pt: e.transpose(out=pt[:, k * 128:(k + 1) * 128], in_=T[0][:, k * 128:(k + 1) * 128],
                                                             identity=ident[:]), reads=["T0", "ident"], writes=[ptk])
            S.op("act", lambda e, t=t, pt=pt: e.activation(out=HT[:, :, t * 128:(t + 1) * 128],
                                                           in_=pt[:].rearrange("p (k n) -> p k n", k=8), func=AF.Copy),
                 reads=[ptk], writes=[("HT", t)])
            S.op("dve", lambda e, pt=pt: e.tensor_copy(out=T[1][:], in_=pt[:]), reads=[ptk], writes=["T1"])
            pr = psA[t % 2]
            prk = f"psA{t % 2}"
            for k in range(8):
                S.op("pe", lambda e, k=k, pr=pr: e.matmul(pr[:, 0:NE], lhsT=T[1][:, k * 128:(k + 1) * 128], rhs=rw[:, k, :],
                                                          start=(k == 0), stop=(k == 7)), reads=["T1", "rw"], writes=[prk])
            S.op("act", lambda e, pr=pr: e.activation(out=rt["s"][:], in_=pr[:, 0:NE], func=AF.Sigmoid), reads=[prk], writes=["rt_s"])
            dv = lambda fn, r, w: S.op("dve", fn, reads=r, writes=w)
            dv(lambda e: e.tensor_tensor(out=rt["ssel"][:], in0=rt["s"][:], in1=rb[:], op=ALU.add), ["rt_s", "rb"], ["rt_ssel"])
            sv = rt["ssel"][:].rearrange("p (g j) -> p g j", j=4)
            a, b, c, d = (sv[:, :, j] for j in range(4))
            dv(lambda e: e.tensor_tensor(out=rg["p01"][:], in0=a, in1=b, op=ALU.max), ["rt_ssel"], ["p01"])
            dv(lambda e: e.tensor_tensor(out=rg["q01"][:], in0=a, in1=b, op=ALU.min), ["rt_ssel"], ["q01"])
            dv(lambda e: e.tensor_tensor(out=rg["p23"][:], in0=c, in1=d, op=ALU.max), ["rt_ssel"], ["p23"])
            dv(lambda e: e.tensor_tensor(out=rg["q23"][:], in0=c, in1=d, op=ALU.min), ["rt_ssel"], ["q23"])
            dv(lambda e: e.tensor_tensor(out=rg["t1"][:], in0=rg["p01"][:], in1=rg["p23"][:], op=ALU.max), ["p01", "p23"], ["t1"])
            dv(lambda e: e.tensor_tensor(out=rg["m2"][:], in0=rg["p01"][:], in1=rg["p23"][:], op=ALU.min), ["p01", "p23"], ["m2"])
            dv(lambda e: e.tensor_tensor(out=rg["m3"][:], in0=rg["q01"][:], in1=rg["q23"][:], op=ALU.max), ["q01", "q23"], ["m3"])
            dv(lambda e: e.tensor_tensor(out=rg["m2"][:], in0=rg["m2"][:], in1=rg["m3"][:], op=ALU.max), ["m2", "m3"], ["m2"])
            dv(lambda e: e.tensor_tensor(out=rg["gs"][:], in0=rg["t1"][:], in1=rg["m2"][:], op=ALU.add), ["t1", "m2"], ["gs"])
            dv(lambda e: e.tensor_reduce(out=r1["gmax"][:], in_=rg["gs"][:], axis=AX.X, op=ALU.max), ["gs"], ["gmax"])
            dv(lambda e: e.tensor_scalar(out=rg["ing"][:], in0=rg["gs"][:], scalar1=r1["gmax"][:, 0:1], scalar2=None, op0=ALU.is_ge),
               ["gs", "gmax"], ["ing"])
            dv(lambda e: e.tensor_scalar(out=rg["pen"][:], in0=rg["ing"][:], scalar1=4.0, scalar2=-4.0, op0=ALU.mult, op1=ALU.add),
               ["ing"], ["pen"])
            smv = rt["sm"][:].rearrange("p (g j) -> p g j", j=4)
            for g in range(4):
                dv(lambda e, g=g: e.tensor_scalar(out=smv[:, g, :], in0=sv[:, g, :], scalar1=rg["ing"][:, g:g + 1],
                                                  scalar2=rg["pen"][:, g:g + 1], op0=ALU.mult, op1=ALU.add),
                   ["rt_ssel", "ing", "pen"], ["rt_sm"])
            dv(lambda e: e.max(out=top8[:], in_=rt["sm"][:]), ["rt_sm"], ["top8"])
            dv(lambda e: e.tensor_scalar(out=rt["sel"][:], in0=rt["sm"][:], scalar1=top8[:, 1:2], scalar2=None, op0=ALU.is_ge),
               ["rt_sm", "top8"], ["rt_sel"])
            dv(lambda e: e.tensor_tensor(out=rt["sc"][:], in0=rt["s"][:], in1=rt["sel"][:], op=ALU.mult), ["rt_s", "rt_sel"], ["rt_sc"])
            dv(lambda e: e.tensor_reduce(out=r1["den"][:], in_=rt["sc"][:], axis=AX.X, op=ALU.add), ["rt_sc"], ["den"])
            dv(lambda e: e.reciprocal(out=r1["den"][:], in_=r1["den"][:]), ["den"], ["den"])
            dv(lambda e, t=t: e.tensor_scalar(out=GATE[:, t, :], in0=rt["sc"][:], scalar1=r1["den"][:, 0:1], scalar2=None, op0=ALU.mult),
               ["rt_sc", "den"], [("GATE", t)])

        groups = [(g * 4, min(4, NT - g * 4)) for g in range((NT + 3) // 4)]
        NU = 2 * NE
        stg13_keys = ["STG0", "STG1", "STG2", "STG3"]
        stg2_keys = ["STG4", "STG5"]

        def load_unit(u):
            ex, hf = u // 2, u % 2
            S.dma("sp", S13[:, 0, :, :], w1_d[ex, :, hf * 256:(hf + 1) * 256].rearrange("(k p) n -> p k n", p=128),
                  "ld_s13", writes=stg13_keys)
            S.dma("sp", S13[:, 1, :, :], w3_d[ex, :, hf * 256:(hf + 1) * 256].rearrange("(k p) n -> p k n", p=128),
                  "ld_s13", writes=stg13_keys)
            S.dma("sp", S2, w2_d[ex, hf * 256:(hf + 1) * 256, :].rearrange("(k p) n -> p k n", p=128),
                  "ld_s2", writes=stg2_keys)

        def cast_unit(u):
            sl = u % 2
            for a in range(2):
                S.op("act", lambda e, a=a, sl=sl: e.activation(out=W13[sl][:, a, :, :], in_=S13[:, a, :, :], func=AF.Copy),
                     reads=stg13_keys, writes=[f"W13_{sl}"])
            gl = MOD[("gf", 0)].unsqueeze(1).to_broadcast([128, 2, D])
            S.op("dve", lambda e, sl=sl, gl=gl: e.tensor_tensor(out=W2[sl][:], in0=S2, in1=gl, op=ALU.mult),
                 reads=stg2_keys + [MKEY[("gf", 0)]], writes=[f"W2_{sl}"])
            if has_ctx:
                gc_ = MOD[("gf", 1)].unsqueeze(1).to_broadcast([128, 2, D])
                S.op("pool", lambda e, sl=sl, gc_=gc_: e.tensor_tensor(out=W2c[sl][:], in0=S2, in1=gc_, op=ALU.mult),
                     reads=stg2_keys + [MKEY[("gf", 1)]], writes=[f"W2c_{sl}"])

        yi = 0
        abi = 0
        load_unit(0)
        cast_unit(0)
        work = [(u, gi, t0, ntile) for u in range(NU) for gi, (t0, ntile) in enumerate(groups)]
        cast_gi = min(2, len(groups) - 1)
        yi_box = [0]

        def AB(idx):
            u, gi, t0, ntile = work[idx]
            sl = u % 2
            wk0 = f"W13_{sl}"
            ntok = ntile * 128
            c0 = t0 * 128
            htk = [("HT", t) for t in range(t0, t0 + ntile)]
            hb = H1[idx % 2]
            hk = f"H1_{idx % 2}"
            for dc in range(2):
                pa, pb = psA[dc], psB[dc]
                for k in range(8):
                    S.op("pe", lambda e, k=k, dc=dc, pa=pa: e.matmul(
                        pa[:, 0:ntok], lhsT=W13[sl][:, 0, k, dc * 128:(dc + 1) * 128], rhs=HT[:, k, c0:c0 + ntok],
                        start=(k == 0), stop=(k == 7)), reads=[wk0] + htk, writes=[f"psA{dc}"])
                for k in range(8):
                    S.op("pe", lambda e, k=k, dc=dc, pb=pb: e.matmul(
                        pb[:, 0:ntok], lhsT=W13[sl][:, 1, k, dc * 128:(dc + 1) * 128], rhs=HT[:, k, c0:c0 + ntok],
                        start=(k == 0), stop=(k == 7)), reads=[wk0] + htk, writes=[f"psB{dc}"])
                sa = SA[:, dc, 0:ntok]
                S.op("act", lambda e, pa=pa, sa=sa: e.activation(out=sa, in_=pa[:, 0:ntok], func=AF.Silu),
                     reads=[f"psA{dc}"], writes=[f"SA{dc}"])
                S.op("dve", lambda e, pb=pb, sa=sa, dc=dc: e.tensor_tensor(out=hb[:, dc, 0:ntok], in0=sa, in1=pb[:, 0:ntok], op=ALU.mult),
                     reads=[f"SA{dc}", f"psB{dc}"], writes=[hk])

        def Y(idx):
            u, gi, t0, ntile = work[idx]
            sl = u % 2
            ex = u // 2
            wk1 = f"W2_{sl}"
            hb = H1[idx % 2]
            hk = f"H1_{idx % 2}"
            for ti in range(ntile):
                t = t0 + ti
                s = tiles[t][1]
                yi = yi_box[0]
                yi_box[0] += 1
                py = psY[yi % 2]
                pyk = f"psY{yi % 2}"
                w2t, w2k = (W2[sl], f"W2_{sl}") if s == 0 else (W2c[sl], f"W2c_{sl}")
                for h2 in range(2):
                    for dc in range(2):
                        S.op("pe", lambda e, h2=h2, dc=dc, py=py, ti=ti, w2t=w2t: e.matmul(
                            py[:, h2 * 512:(h2 + 1) * 512], lhsT=hb[:, dc, ti * 128:(ti + 1) * 128],
                            rhs=w2t[:, dc, h2 * 512:(h2 + 1) * 512], start=(dc == 0), stop=(dc == 1)),
                            reads=[hk, w2k], writes=[pyk])
                S.op("dve", lambda e, py=py, t=t: e.scalar_tensor_tensor(
                    out=X[:, t, :], in0=py[:], scalar=GATE[:, t, ex:ex + 1], in1=X[:, t, :], op0=ALU.mult, op1=ALU.add),
                    reads=[pyk, ("GATE", t), ("X", t)], writes=[("X", t)])

        for idx in range(len(work)):
            u, gi, _, _ = work[idx]
            if gi == 0 and u + 1 < NU:
                load_unit(u + 1)
            if gi == cast_gi and u + 1 < NU:
                cast_unit(u + 1)
            AB(idx)
            if idx >= 1:
                Y(idx - 1)
        Y(len(work) - 1)

        LNG, LNB = STG[:, 0:D], STG[:, D:2 * D]
        S.dma("sp", LNG, lng_d.partition_broadcast(128), "ld_lng", writes=["STG0"])
        S.dma("sp", LNB, lnb_d.partition_broadcast(128), "ld_lnb", writes=["STG1"])
        for t in range(NT):
            o = STG[:, (2 + t % 2) * D:(3 + t % 2) * D]
            ok = f"STG{2 + t % 2}"
            layer_norm_tile(S, X[:, t, :], ("X", t), o, ok, T[0][:], "T0", LNG, "STG0", LNB, "STG1", small, eps_t)
            S.dma("sp", tiles[t][2], o, f"st{t % 2}", reads=[ok], store=True)
        S.emit()
        S.close()


class Ctx:
    pass


def std_inputs(nc):
    dt = lambda n, s, k="ExternalInput": nc.dram_tensor(n, s, F32, kind=k).ap()
    return dt, {"cvec": dt("cvec", [2, D]), "wmod": dt("wmod", [D, 3 * D]), "bmod": dt("bmod", [3 * D]), "lng": dt("lng", [D]),
                "lnb": dt("lnb", [D])}


def mixer_prologue(nc, es, pfx, A):
    C = Ctx()
    C.nc = nc
    C.es = es
    S = C.S = Sched(nc, es, pfx)
    sb = C.sb = lambda n, s, d=F32: es.enter_context(nc.sbuf_tensor(pfx + n, s, d))
    ps = C.ps = lambda n, s, d=F32: es.enter_context(nc.psum_tensor(pfx + n, s, d))
    C.cvec, C.wmod, C.bmod, C.lng_d, C.lnb_d = A["cvec"], A["wmod"], A["bmod"], A["lng"], A["lnb"]
    C.STG = [sb(f"STG{i}", [128, 4096]) for i in range(2)]
    C.stg_i = 0
    C.MOD = {}
    C.MKEY = {}
    for n in ("sh", "sc1", "ga"):
        for s in range(2):
            C.MOD[(n, s)] = sb(f"M{n}{s}", [128, D])[:]
            C.MKEY[(n, s)] = f"M{n}{s}"
    C.LNG = sb("LNG", [128, D])
    C.LNB = sb("LNB", [128, D])
    C.eps2 = sb("eps2", [128, 1])
    C.eps1 = sb("eps1", [128, 1])
    C.small = (sb("ln_st", [128, 2, 6]), sb("ln_mv", [128, 2]), sb("ln_rstd", [128, 1]), sb("ln_nmr", [128, 1]))
    C.psBig = [ps(f"psBig{i}", [128, D]) for i in range(2)]
    C.ident, C.ones = common_consts(S, nc, sb, ps)
    S.op("dve", lambda e: e.memset(C.eps2[:], EPS2), writes=["eps"])
    S.op("dve", lambda e: e.memset(C.eps1[:], LN_EPS), writes=["eps1"])
    return C


def mixer_mods(C):
    S = C.S
    stage = C.STG[0][:].rearrange("p (k n) -> p k n", k=8)
    SLC = C.STG[1][:, 0:2048].rearrange("p (s k n) -> p s k n", s=2, k=8)
    outs = []
    for s in range(2):
        outs.append((0, s, C.MOD[("sh", s)], C.MKEY[("sh", s)], "plain"))
        outs.append((1, s, C.MOD[("sc1", s)], C.MKEY[("sc1", s)], "plus1"))
        outs.append((2, s, C.MOD[("ga", s)], C.MKEY[("ga", s)], "invalpha"))
    mod_tiles(S, C.nc, C.sb, C.cvec, C.wmod, C.bmod, C.ident, C.ones, C.psBig[0], "psBig0", stage, ["STG0"], SLC, ["STG1"], outs)
    S.dma("sp", C.LNG[:], C.lng_d.partition_broadcast(128), "ld_lng", writes=["LNG"])
    S.dma("sp", C.LNB[:], C.lnb_d.partition_broadcast(128), "ld_lnb", writes=["LNB"])


def load_w_bf16(C, dst, dkey, src, kc, ncols):
    S = C.S
    cw = min(ncols, 512)
    kpp = max(1, min(kc, 4096 // cw))
    n = 0
    for c0 in range(0, ncols, cw):
        for k0 in range(0, kc, kpp):
            kk = min(kpp, kc - k0)
            i = C.stg_i % 2
            C.stg_i += 1
            st = C.STG[i][:, 0:kk * cw].rearrange("p (k n) -> p k n", k=kk)
            S.dma("sp", st, src[k0 * 128:(k0 + kk) * 128, c0:c0 + cw].rearrange("(k p) n -> p k n", p=128), f"ld_stg{i}",
                  writes=[f"STG{i}"])
            eng = "act" if n % 2 == 0 else "dve"
            n += 1
            d = dst[:, k0:k0 + kk, c0:c0 + cw]
            if eng == "act":
                S.op("act", lambda e, d=d, st=st: e.activation(out=d, in_=st, func=AF.Copy), reads=[f"STG{i}"], writes=[dkey])
            else:
                S.op("dve", lambda e, d=d, st=st: e.tensor_copy(out=d, in_=st), reads=[f"STG{i}"], writes=[dkey])


def modulate_T(C, xt, xkey, s, T0, t0key, pst, pskey, HTt, htkey):
    S = C.S
    S.op("dve", lambda e: e.tensor_tensor(out=T0, in0=xt, in1=C.MOD[("sc1", s)], op=ALU.mult), reads=[xkey, C.MKEY[("sc1", s)]], writes=[t0key])
    S.op("dve", lambda e: e.tensor_tensor(out=T0, in0=T0, in1=C.MOD[("sh", s)], op=ALU.add), reads=[t0key, C.MKEY[("sh", s)]], writes=[t0key])
    to_T(C, T0, t0key, pst, pskey, HTt, htkey)


def to_T(C, src, skey, pst, pskey, HTt, htkey):
    S = C.S
    for k in range(8):
        S.op("pe", lambda e, k=k: e.transpose(out=pst[:, k * 128:(k + 1) * 128], in_=src[:, k * 128:(k + 1) * 128], identity=C.ident[:]),
             reads=[skey, "ident"], writes=[pskey])
    S.op("act", lambda e: e.activation(out=HTt, in_=pst[:].rearrange("p (k n) -> p k n", k=8), func=AF.Copy), reads=[pskey], writes=[htkey])


def proj(C, pst_list, HTt, htkey, W, wkey, c0, ncols):
    S = C.S
    off = 0
    for (pa, pk) in pst_list:
        w = min(512, ncols - off)
        for k in range(8):
            S.op("pe", lambda e, k=k, pa=pa, w=w, off=off: e.matmul(pa[:, 0:w], lhsT=HTt[:, k, :], rhs=W[:, k, c0 + off:c0 + off + w],
                                                                    start=(k == 0), stop=(k == 7)), reads=[htkey, wkey], writes=[pk])
        off += w


def residual_ln_store(C, psy, pykey, xt, xkey, s, T1, t1key, T2, t2key, o, okey, out_ap, stsem):
    S = C.S
    S.op("dve", lambda e: e.tensor_tensor(out=T1, in0=psy[:], in1=C.MOD[("ga", s)], op=ALU.mult), reads=[pykey, C.MKEY[("ga", s)]], writes=[t1key])
    S.op("pool", lambda e: e.tensor_tensor(out=T1, in0=T1, in1=xt, op=ALU.add), reads=[t1key, xkey], writes=[t1key])
    layer_norm_tile(S, T1, t1key, o, okey, T2, t2key, C.LNG[:], "LNG", C.LNB[:], "LNB", C.small, C.eps2)
    S.dma("sp", out_ap, o, stsem, reads=[okey], store=True)


def build_gmlp():
    NT = 18
    nc = bass.Bass("TRN2", target_bir_lowering=False)
    dt, A = std_inputs(nc)
    xin = dt("xin", [NT * 128, D])
    A.update({"w_in": dt("w_in", [D, 2 * D]), "b_in": dt("b_in", [2 * D]), "g_ln_g": dt("g_ln_g", [D]), "g_ln_b": dt("g_ln_b", [D]),
              "w_s": dt("w_s", [8, 128, 128]), "b_s": dt("b_s", [128, 8]), "w_out": dt("w_out", [D, D])})
    xout = dt("xout", [NT * 128, D], "ExternalOutput")
    tiles = [(xin[t * 128:(t + 1) * 128, :], 0 if t < 16 else 1, xout[t * 128:(t + 1) * 128, :]) for t in range(NT)]
    stage_gmlp(nc, "", tiles, A)
    return nc


def stage_gmlp(nc, pfx, tiles, A):
    NT = len(tiles)
    es = ExitStack()
    with es:
        C = mixer_prologue(nc, es, pfx, A)
        S, sb, ps = C.S, C.sb, C.ps
        win_d, bin_d, glng_d, glnb_d, ws_d, bs_d, wout_d = (A[k] for k in ("w_in", "b_in", "g_ln_g", "g_ln_b", "w_s", "b_s", "w_out"))
        Win = sb("Win", [128, 8, 2 * D], BF16)
        Wout = sb("Wout", [128, 8, D], BF16)
        wsT = sb("wsT", [128, 8, 128], BF16)
        BIN = sb("BIN", [128, 2 * D])
        GLNG = sb("GLNG", [128, D])
        GLNB = sb("GLNB", [128, D])
        bs = sb("bs", [128, 8])
        Xt = [sb(f"Xt{i}", [128, D]) for i in range(2)]
        OUT = [sb(f"OUT{i}", [128, D]) for i in range(2)]
        T0 = sb("T0", [128, D]); T1 = sb("T1", [128, D]); T2 = sb("T2", [128, D])
        HTt = sb("HTt", [128, 8, 128], BF16)
        HT2 = sb("HT2", [128, 8, 128], BF16)
        Z = sb("Z", [128, 2 * D])
        TZ = sb("TZ", [128, 2 * D])
        TZ2 = sb("TZ2", [128, 2 * D])
        VN = sb("VN", [128, D], BF16)
        US = sb("US", [128, D])
        psZ = [ps(f"psZ{i}", [128, 512]) for i in range(4)]
        mixer_mods(C)
        load_w_bf16(C, Win[:], "Win", win_d, 8, 2 * D)
        load_w_bf16(C, Wout[:], "Wout", wout_d, 8, D)
        S.dma("sp", BIN[:], bin_d.partition_broadcast(128), "ld_bin", writes=["BIN"])
        S.dma("sp", GLNG[:], glng_d.partition_broadcast(128), "ld_glng", writes=["GLNG"])
        S.dma("sp", GLNB[:], glnb_d.partition_broadcast(128), "ld_glnb", writes=["GLNB"])
        S.dma("sp", bs[:], bs_d, "ld_bs", writes=["bs"])
        wst = C.STG[0][:, 0:1024].rearrange("p (g q) -> p g q", g=8)
        S.dma("sp", wst, ws_d.rearrange("g p q -> p g q"), "ld_stg0", writes=["STG0"])
        for g in range(8):
            S.op("pe", lambda e, g=g: e.transpose(out=C.psBig[0][:, g * 128:(g + 1) * 128], in_=wst[:, g, :], identity=C.ident[:]),
                 reads=["STG0", "ident"], writes=["psBig0"])
        S.op("act", lambda e: e.activation(out=wsT[:], in_=C.psBig[0][:].rearrange("p (g n) -> p g n", g=8), func=AF.Copy),
             reads=["psBig0"], writes=["wsT"])
        for t in range(NT):
            s = tiles[t][1]
            xt = Xt[t % 2][:]
            xk = f"Xt{t % 2}"
            S.dma("sp", xt, tiles[t][0], f"ld_x{t % 2}", writes=[xk])
            modulate_T(C, xt, xk, s, T0[:], "T0", C.psBig[0], "psBig0", HTt[:], "HTt")
            proj(C, [(psZ[i], f"psZ{i}") for i in range(4)], HTt, "HTt", Win, "Win", 0, 2 * D)
            for i in range(4):
                S.op("dve", lambda e, i=i: e.tensor_tensor(out=Z[:, i * 512:(i + 1) * 512], in0=psZ[i][:], in1=BIN[:, i * 512:(i + 1) * 512],
                                                           op=ALU.add), reads=[f"psZ{i}", "BIN"], writes=["Z"])
            S.op("act", lambda e: e.activation(out=TZ[:], in_=Z[:], func=AF.Square), reads=["Z"], writes=["TZ"])
            S.op("dve", lambda e: e.tensor_scalar(out=TZ[:], in0=TZ[:], scalar1=0.044715, scalar2=1.0, op0=ALU.mult, op1=ALU.add),
                 reads=["TZ"], writes=["TZ"])
            S.op("pool", lambda e: e.tensor_tensor(out=TZ[:], in0=TZ[:], in1=Z[:], op=ALU.mult), reads=["TZ", "Z"], writes=["TZ"])
            S.op("act", lambda e: e.activation(out=TZ[:], in_=TZ[:], func=AF.Sigmoid, scale=1.5957691216057308), reads=["TZ"], writes=["TZ"])
            S.op("pool", lambda e: e.tensor_tensor(out=TZ2[:], in0=TZ[:], in1=Z[:], op=ALU.mult), reads=["TZ", "Z"], writes=["TZ2"])
            layer_norm_tile(S, TZ2[:, D:2 * D], "TZ2", VN[:], "VN", T2[:], "T2", GLNG[:], "GLNG", GLNB[:], "GLNB", C.small, C.eps1)
            for g in range(8):
                S.op("pe", lambda e, g=g: e.matmul(C.psBig[0][:, g * 128:(g + 1) * 128], lhsT=wsT[:, g, :], rhs=VN[:, g * 128:(g + 1) * 128],
                                                   start=True, stop=True), reads=["wsT", "VN"], writes=["psBig0"])
            for g in range(8):
                S.op("dve", lambda e, g=g: e.scalar_tensor_tensor(out=US[:, g * 128:(g + 1) * 128], in0=C.psBig[0][:, g * 128:(g + 1) * 128],
                                                                  scalar=bs[:, g:g + 1], in1=TZ2[:, g * 128:(g + 1) * 128],
                                                                  op0=ALU.add, op1=ALU.mult), reads=["psBig0", "bs", "TZ2"], writes=["US"])
            to_T(C, US[:], "US", C.psBig[1], "psBig1", HT2[:], "HT2")
            proj(C, [(C.psBig[1][:, 0:512], "psBig1"), (C.psBig[1][:, 512:1024], "psBig1")], HT2, "HT2", Wout, "Wout", 0, D)
            residual_ln_store(C, C.psBig[1], "psBig1", xt, xk, s, T1[:], "T1", T2[:], "T2", OUT[t % 2][:], f"OUT{t % 2}",
                              tiles[t][2], f"st{t % 2}")
        S.emit()
        S.close()


def build_conv():
    nc = bass.Bass("TRN2", target_bir_lowering=False)
    dt, A = std_inputs(nc)
    xin = dt("xin", [20 * 128, D])
    A.update({"valid": dt("valid", [128, 20]), "w_in": dt("w_in", [D, 3 * D]), "w_conv": dt("w_conv", [3, D]), "w_out": dt("w_out", [D, D])})
    xout = dt("xout", [18 * 128, D], "ExternalOutput")
    lat = [(xin[e * 128:(e + 1) * 128, :], 0, e, (xout[(e - 1) * 128:e * 128, :] if 1 <= e <= 16 else None)) for e in range(18)]
    ctx = [(xin[e * 128:(e + 1) * 128, :], 1, e, xout[(e - 2) * 128:(e - 1) * 128, :]) for e in (18, 19)]
    stage_conv(nc, "", [lat, ctx], A, 20)
    return nc


def stage_conv(nc, pfx, seqs, A, nvalid):
    es = ExitStack()
    with es:
        C = mixer_prologue(nc, es, pfx, A)
        S, sb, ps = C.S, C.sb, C.ps
        valid_d, win_d, wconv_d, wout_d = (A[k] for k in ("valid", "w_in", "w_conv", "w_out"))
        Win = sb("Win", [128, 8, 3 * D], BF16)
        Wout = sb("Wout", [128, 8, D], BF16)
        WC = [sb(f"WC{i}", [128, D]) for i in range(3)]
        valid = sb("valid_sb", [128, nvalid])
        Xr = [sb(f"Xr{i}", [128, D]) for i in range(4)]
        Zr = [C.STG[1][:, i * D:(i + 1) * D] for i in range(4)]
        HTr = [sb(f"HTr{i}", [128, 8, 128], BF16) for i in range(4)]
        ZM = sb("ZM", [128, D]); ZP = sb("ZP", [128, D]); TC = sb("TC", [128, D]); TG = sb("TG", [128, D])
        OUT = [sb(f"OUT{i}", [128, D]) for i in range(2)]
        T0 = sb("T0", [128, D]); T1 = sb("T1", [128, D]); T2 = sb("T2", [128, D])
        HT2 = sb("HT2", [128, 8, 128], BF16)
        psP = [ps(f"psP{i}", [128, 512]) for i in range(4)]
        mixer_mods(C)
        load_w_bf16(C, Win[:], "Win", win_d, 8, 3 * D)
        load_w_bf16(C, Wout[:], "Wout", wout_d, 8, D)
        for i in range(3):
            S.dma("sp", WC[i][:], wconv_d[i, :].partition_broadcast(128), f"ld_wc{i}", writes=[f"WC{i}"])
        S.dma("sp", valid[:], valid_d, "ld_valid", writes=["valid"])
        S.op("dve", lambda e: e.memset(ZM[0:1, 0:1], 0.0), writes=["STG1", "Zr0", "Zr1", "Zr2", "Zr3"])

        cnt = {"a": 0, "o": 0}

        def Astep(tile):
            src, s, vcol, _ = tile
            r = cnt["a"] % 4
            cnt["a"] += 1
            xt, xk = Xr[r][:], f"Xr{r}"
            S.dma("sp", xt, src, f"ld_x{r}", writes=[xk])
            modulate_T(C, xt, xk, s, T0[:], "T0", C.psBig[0], "psBig0", HTr[r][:], f"HTr{r}")
            proj(C, [(psP[i], f"psP{i}") for i in range(4)], HTr[r], f"HTr{r}", Win, "Win", D, 2 * D)
            for h in range(2):
                S.op("act", lambda e_, h=h: e_.activation(out=TG[:, h * 512:(h + 1) * 512], in_=psP[h][:], func=AF.Copy, scale=valid[:, vcol:vcol + 1]),
                     reads=[f"psP{h}", "valid"], writes=["TG"])
            for h in range(2):
                S.op("dve", lambda e_, h=h: e_.tensor_tensor(out=Zr[r][:, h * 512:(h + 1) * 512], in0=TG[:, h * 512:(h + 1) * 512], in1=psP[2 + h][:],
                                                            op=ALU.mult), reads=["TG", f"psP{2 + h}"], writes=[f"Zr{r}"])
            return r

        def Bstep(tile, r, prev, nxt):
            _, s, _, out_ap = tile
            ot = cnt["o"]
            cnt["o"] += 1
            xt, xk = Xr[r][:], f"Xr{r}"
            zc, zk = Zr[r], f"Zr{r}"
            if prev is None:
                S.op("dve", lambda e_: e_.memset(ZM[:], 0.0), writes=["ZM"])
            if nxt is None:
                S.op("dve", lambda e_: e_.memset(ZP[:], 0.0), writes=["ZP"])
            S.dma("sp", ZM[1:128, :], zc[0:127, :], "sh_zm", reads=[zk], writes=["ZM"])
            if prev is not None:
                S.dma("sp", ZM[0:1, :], Zr[prev][127:128, :], "sh_zm", reads=[f"Zr{prev}"], writes=["ZM"])
            S.dma("sp", ZP[0:127, :], zc[1:128, :], "sh_zp", reads=[zk], writes=["ZP"])
            if nxt is not None:
                S.dma("sp", ZP[127:128, :], Zr[nxt][0:1, :], "sh_zp", reads=[f"Zr{nxt}"], writes=["ZP"])
            S.op("dve", lambda e_: e_.tensor_tensor(out=ZM[:], in0=ZM[:], in1=WC[0][:], op=ALU.mult), reads=["ZM", "WC0"], writes=["ZM"])
            S.op("pool", lambda e_: e_.tensor_tensor(out=ZP[:], in0=ZP[:], in1=WC[2][:], op=ALU.mult), reads=["ZP", "WC2"], writes=["ZP"])
            S.op("dve", lambda e_: e_.tensor_tensor(out=TC[:], in0=zc, in1=WC[1][:], op=ALU.mult), reads=[zk, "WC1"], writes=["TC"])
            S.op("pool", lambda e_: e_.tensor_tensor(out=TC[:], in0=TC[:], in1=ZM[:], op=ALU.add), reads=["TC", "ZM"], writes=["TC"])
            S.op("dve", lambda e_: e_.tensor_tensor(out=TC[:], in0=TC[:], in1=ZP[:], op=ALU.add), reads=["TC", "ZP"], writes=["TC"])
            proj(C, [(C.psBig[1][:, 0:512], "psBig1"), (C.psBig[1][:, 512:1024], "psBig1")], HTr[r], f"HTr{r}", Win, "Win", 0, D)
            S.op("dve", lambda e_: e_.tensor_tensor(out=TG[:], in0=C.psBig[1][:], in1=TC[:], op=ALU.mult), reads=["psBig1", "TC"], writes=["TG"])
            to_T(C, TG[:], "TG", C.psBig[1], "psBig1", HT2[:], "HT2")
            proj(C, [(C.psBig[1][:, 0:512], "psBig1"), (C.psBig[1][:, 512:1024], "psBig1")], HT2, "HT2", Wout, "Wout", 0, D)
            residual_ln_store(C, C.psBig[1], "psBig1", xt, xk, s, T1[:], "T1", T2[:], "T2", OUT[ot % 2][:], f"OUT{ot % 2}",
                              out_ap, f"st{ot % 2}")

        for seq in seqs:
            slots = {}
            n = len(seq)
            for i in range(n + 1):
                if i < n:
                    slots[i] = Astep(seq[i])
                j = i - 1
                if j >= 0 and seq[j][3] is not None:
                    Bstep(seq[j], slots[j], slots.get(j - 1), slots.get(j + 1) if j + 1 < n else None)
        S.emit()
        S.close()


def build_attn(want_ctx):
    nc = bass.Bass("TRN2", target_bir_lowering=False)
    dt, A = std_inputs(nc)
    NOUT = 18 if want_ctx else 16
    xin = dt("xin", [20 * 128, D])
    A.update({"kbias": dt("kbias", [128, 20]), "cos_t": dt("cos_t", [128, 20 * 64]), "sin_t": dt("sin_t", [128, 20 * 64]),
              "maskp": dt("maskp", [128, 512]), "maskn": dt("maskn", [128, 512]), "w_qkv": dt("w_qkv", [D, 1536]),
              "w_o": dt("w_o", [D, D]), "sink": dt("sink", [16])})
    xout = dt("xout", [NOUT * 128, D], "ExternalOutput")
    kv = [(xin[e * 128:(e + 1) * 128, :], 0 if e < 18 else 1, e, e) for e in range(20)]
    q = [(xin[(o + 1) * 128:(o + 2) * 128, :], 0, o + 1, [(o, "P"), (o + 1, None), (o + 2, "N"), (18, None), (19, None)],
          xout[o * 128:(o + 1) * 128, :]) for o in range(16)]
    if want_ctx:
        q += [(xin[e * 128:(e + 1) * 128, :], 1, e, [(18, None), (19, None)], xout[(e - 2) * 128:(e - 1) * 128, :]) for e in (18, 19)]
    stage_attn(nc, "", kv, q, A, 20)
    return nc


def stage_attn(nc, pfx, kv_tiles, q_tiles, A, ntbl):
    NKT = len(kv_tiles)
    es = ExitStack()
    with es:
        C = mixer_prologue(nc, es, pfx, A)
        S, sb, ps = C.S, C.sb, C.ps
        kbias_d, cos_d, sin_d, maskp_d, maskn_d, wqkv_d, wo_d, sink_d = (A[k] for k in ("kbias", "cos_t", "sin_t", "maskp", "maskn", "w_qkv", "w_o", "sink"))
        Wqkv = sb("Wqkv", [128, 8, 1536], BF16)
        Wo = sb("Wo", [128, 8, D], BF16)
        KT = sb("KT", [128, 2, NKT * 128], BF16)
        V = sb("V", [128, NKT, 256], BF16)
        kbias = sb("kbias_sb", [128, ntbl])
        COS = sb("COS", [128, ntbl, 64])
        SIN = sb("SIN", [128, ntbl, 64])
        MP = sb("MP", [128, 512])
        MN = sb("MN", [128, 512])
        SINKB = sb("SINKB", [128, 2, 512])
        ES = sb("ES", [128, 16])
        identb = sb("identb", [128, 128], BF16)
        onesb = sb("onesb", [128, 64], BF16)
        Xt = [sb(f"Xt{i}", [128, D]) for i in range(2)]
        OUT = [sb(f"OUT{i}", [128, D]) for i in range(2)]
        T0 = sb("T0", [128, D]); T1 = sb("T1", [128, D]); T2 = sb("T2", [128, D])
        HTt = sb("HTt", [128, 8, 128], BF16)
        R1 = sb("R1", [128, D]); R2 = sb("R2", [128, D])
        KR = sb("KR", [128, 256], BF16)
        QRp = sb("QRp", [128, 8, 128], BF16)
        QT = sb("QT", [128, 8, 128], BF16)
        PT = [sb(f"PT{i}", [128, 512], BF16) for i in range(3)]
        DEN = sb("DEN", [128, 512])
        OT = sb("OT", [128, 2, 512], BF16)
        psS = [ps(f"psS{i}", [128, 512]) for i in range(2)]
        psX = ps("psX", [128, 8, 128], BF16)
        psO = C.psBig[1][:, 0:512]
        psD = C.psBig[1][:, 512:1024]
        S.op("pool", lambda e: e.memset(onesb[:], 1.0), writes=["onesb"])
        S.op("pool", lambda e: e.tensor_copy(out=identb[:], in_=C.ident[:]), reads=["ident"], writes=["identb"])
        mixer_mods(C)
        load_w_bf16(C, Wqkv[:], "Wqkv", wqkv_d, 8, 1536)
        for half in range(2):
            i = C.stg_i % 2
            C.stg_i += 1
            st = C.STG[i][:].rearrange("p (c n) -> p c n", c=4)
            for cc in range(4):
                c = half * 4 + cc
                jp, g = c // 4, c % 4
                for r in range(2):
                    row = 512 * jp + 256 * r + 64 * g
                    S.dma("sp", st[r * 64:(r + 1) * 64, cc, :], wo_d[row:row + 64, :], f"ld_stg{i}", writes=[f"STG{i}"])
            S.op("act", lambda e, st=st, half=half: e.activation(out=Wo[:, half * 4:(half + 1) * 4, :], in_=st, func=AF.Copy),
                 reads=[f"STG{i}"], writes=["Wo"])
        S.dma("sp", kbias[:], kbias_d, "ld_kb", writes=["kbias"])
        S.dma("sp", COS[:].rearrange("p t n -> p (t n)"), cos_d, "ld_cos", writes=["COS"])
        S.dma("sp", SIN[:].rearrange("p t n -> p (t n)"), sin_d, "ld_sin", writes=["SIN"])
        S.dma("sp", MP[:], maskp_d, "ld_mp", writes=["MP"])
        S.dma("sp", MN[:], maskn_d, "ld_mn", writes=["MN"])
        S.dma("sp", ES[:], sink_d.partition_broadcast(128), "ld_sink", writes=["ES"])
        S.op("act", lambda e: e.activation(out=ES[:], in_=ES[:], func=AF.Exp), reads=["ES"], writes=["ES"])
        for jp in range(2):
            for g in range(4):
                for r in range(2):
                    h = 8 * jp + 4 * r + g
                    S.op("act", lambda e, jp=jp, g=g, r=r, h=h: e.activation(
                        out=SINKB[r * 64:(r + 1) * 64, jp, g * 128:(g + 1) * 128], in_=C.ones[r * 64:(r + 1) * 64, :], func=AF.Copy,
                        scale=ES[r * 64:(r + 1) * 64, h:h + 1]), reads=["ones", "ES"], writes=["SINKB"])

        def rope(src_ps, pskey, nh, e, out_fn):
            n = nh * 64
            xv = src_ps.rearrange("p (h b f i) -> p h b f i", h=nh, b=2, f=2)
            r1v = R1[:, 0:n].rearrange("p (h n) -> p h n", h=nh)
            r2v = R2[:, 0:n].rearrange("p (h b f i) -> p h b f i", h=nh, b=2, f=2)
            cosb = COS[:, e, :].unsqueeze(1).to_broadcast([128, nh, 64])
            sv = SIN[:, e, :].rearrange("p (b f i) -> p b f i", b=2, f=2)
            S.op("dve", lambda e_: e_.tensor_tensor(out=r1v, in0=src_ps.rearrange("p (h n) -> p h n", h=nh), in1=cosb, op=ALU.mult),
                 reads=[pskey, "COS"], writes=["R1"])
            for f in range(2):
                sb_ = sv[:, :, f, :].unsqueeze(1).to_broadcast([128, nh, 2, 16])
                S.op("dve", lambda e_, f=f, sb_=sb_: e_.tensor_tensor(out=r2v[:, :, :, f, :], in0=xv[:, :, :, 1 - f, :], in1=sb_, op=ALU.mult),
                     reads=[pskey, "SIN"], writes=["R2"])
            out_fn(n)

        for e in range(NKT):
            src_, s, tbl, kbc = kv_tiles[e]
            xt, xk = Xt[e % 2][:], f"Xt{e % 2}"
            S.dma("sp", xt, src_, f"ld_x{e % 2}", writes=[xk])
            modulate_T(C, xt, xk, s, T0[:], "T0", C.psBig[0], "psBig0", HTt[:], "HTt")
            proj(C, [(psS[0], "psS0")], HTt, "HTt", Wqkv, "Wqkv", 1024, 512)

            def kout(n):
                S.op("pool", lambda e_: e_.tensor_tensor(out=KR[:], in0=R1[:, 0:256], in1=R2[:, 0:256], op=ALU.add), reads=["R1", "R2"], writes=["KR"])
            S.op("act", lambda e_, e=e: e_.activation(out=V[:, e, :], in_=psS[0][:, 256:512], func=AF.Copy), reads=["psS0"], writes=["V"])
            rope(psS[0][:, 0:256], "psS0", 4, tbl, kout)
            for jp in range(2):
                S.op("pe", lambda e_, jp=jp: e_.transpose(out=psX[:, jp, :], in_=KR[:, jp * 128:(jp + 1) * 128], identity=identb[:]),
                     reads=["KR", "identb"], writes=["psX"])
            S.op("act", lambda e_, e=e: e_.activation(out=KT[:, :, e * 128:(e + 1) * 128], in_=psX[:, 0:2, :], func=AF.Copy),
                 reads=["psX"], writes=["KT"])

        pti = 0
        for o, (src_, s, tbl, chunks_, out_ap) in enumerate(q_tiles):
            xt, xk = Xt[o % 2][:], f"Xt{o % 2}"
            S.dma("sp", xt, src_, f"ld_x{o % 2}", writes=[xk])
            modulate_T(C, xt, xk, s, T0[:], "T0", C.psBig[0], "psBig0", HTt[:], "HTt")
            proj(C, [(C.psBig[0][:, 0:512], "psBig0"), (C.psBig[0][:, 512:1024], "psBig0")], HTt, "HTt", Wqkv, "Wqkv", 0, D)

            def qout(n):
                for jp in range(2):
                    a = R1[:, jp * 512:(jp + 1) * 512].rearrange("p (r g d) -> p r g d", r=2, g=4)
                    b = R2[:, jp * 512:(jp + 1) * 512].rearrange("p (r g d) -> p r g d", r=2, g=4)
                    o_ = QRp[:, jp * 4:(jp + 1) * 4, :].rearrange("p g (r d) -> p r g d", r=2)
                    S.op("pool", lambda e_, a=a, b=b, o_=o_: e_.tensor_tensor(out=o_, in0=a, in1=b, op=ALU.add), reads=["R1", "R2"], writes=["QRp"])
            rope(C.psBig[0][:], "psBig0", 16, tbl, qout)
            for c in range(8):
                S.op("pe", lambda e_, c=c: e_.transpose(out=psX[:, c, :], in_=QRp[:, c, :], identity=identb[:]), reads=["QRp", "identb"], writes=["psX"])
            S.op("act", lambda e_: e_.activation(out=QT[:], in_=psX[:], func=AF.Copy), reads=["psX"], writes=["QT"])
            chunks = [(kt, {"P": MP, "N": MN, None: None}[m]) for (kt, m) in chunks_]
            for jp in range(2):
                for r in range(2):
                    j = 2 * jp + r
                    lo, hi = r * 64, (r + 1) * 64
                    for ci, (kt, mask) in enumerate(chunks):
                        kbc = kv_tiles[kt][3]
                        pss, psk = psS[pti % 2], f"psS{pti % 2}"
                        pt, ptk = PT[pti % 3], f"PT{pti % 3}"
                        pti += 1
                        S.op("pe", lambda e_, pss=pss, kt=kt, jp=jp, lo=lo, hi=hi: e_.matmul(
                            pss[:], lhsT=KT[lo:hi, jp, kt * 128:(kt + 1) * 128], rhs=QT[lo:hi, jp * 4:(jp + 1) * 4, :], start=True, stop=True),
                            reads=["KT", "QT"], writes=[psk])
                        S.op("act", lambda e_, pss=pss, pt=pt, kbc=kbc: e_.activation(out=pt[:], in_=pss[:], func=AF.Exp, bias=kbias[:, kbc:kbc + 1], scale=0.125),
                             reads=[psk, "kbias"], writes=[ptk])
                        if mask is not None:
                            S.op("dve", lambda e_, pt=pt, mask=mask: e_.tensor_tensor(out=pt[:], in0=pt[:], in1=mask[:], op=ALU.mult),
                                 reads=[ptk, "MP", "MN"], writes=[ptk])
                        first, last = (ci == 0), (ci == len(chunks) - 1)
                        S.op("pe", lambda e_, pt=pt, kt=kt, j=j, lo=lo, hi=hi, first=first, last=last: e_.matmul(
                            psO[lo:hi, :], lhsT=V[:, kt, j * 64:(j + 1) * 64], rhs=pt[:], start=first, stop=last),
                            reads=["V", ptk], writes=["psO"])
                        S.op("pe", lambda e_, pt=pt, lo=lo, hi=hi, first=first, last=last: e_.matmul(
                            psD[lo:hi, :], lhsT=onesb[:, 0:64], rhs=pt[:], start=first, stop=last),
                            reads=["onesb", ptk], writes=["psD"])
                S.op("dve", lambda e_, jp=jp: e_.tensor_tensor(out=DEN[:], in0=psD, in1=SINKB[:, jp, :], op=ALU.add), reads=["psD", "SINKB"], writes=["DEN"])
                S.op("dve", lambda e_: e_.reciprocal(out=DEN[:], in_=DEN[:]), reads=["DEN"], writes=["DEN"])
                S.op("dve", lambda e_, jp=jp: e_.tensor_tensor(out=OT[:, jp, :], in0=psO, in1=DEN[:], op=ALU.mult), reads=["psO", "DEN"], writes=["OT"])
            for half in range(2):
                for c in range(8):
                    jp, g = c // 4, c % 4
                    S.op("pe", lambda e_, half=half, c=c, jp=jp, g=g: e_.matmul(
                        C.psBig[0][:, half * 512:(half + 1) * 512], lhsT=OT[:, jp, g * 128:(g + 1) * 128], rhs=Wo[:, c, half * 512:(half + 1) * 512],
                        start=(c == 0), stop=(c == 7)), reads=["OT", "Wo"], writes=["psBig0"])
            residual_ln_store(C, C.psBig[0], "psBig0", xt, xk, s, T1[:], "T1", T2[:], "T2", OUT[o % 2][:], f"OUT{o % 2}",
                              out_ap, f"st{o % 2}")
        S.emit()
        S.close()


NWIN = 22
NEXT = 20


def build_fused(nstage=8, dbg=False):
    nc = bass.Bass("TRN2", target_bir_lowering=False)
    dt = lambda n, s, k="ExternalInput": nc.dram_tensor(n, s, F32, kind=k).ap()
    xw = dt("xw", [NWIN * 128, D])
    ctx = dt("ctx", [256, D])
    cvec = dt("cvec", [2, D])
    w_mod = dt("w_mod", [4, D, 6 * D]); b_mod = dt("b_mod", [4, 6 * D])
    ln1_g = dt("ln1_g", [4, D]); ln1_b = dt("ln1_b", [4, D]); ln2_g = dt("ln2_g", [4, D]); ln2_b = dt("ln2_b", [4, D])
    rw = dt("router_w", [D, NE]); rb = dt("router_bias", [NE])
    w1 = dt("moe_w1", [4, NE, D, DEXP]); w3 = dt("moe_w3", [4, NE, D, DEXP]); w2 = dt("moe_w2", [4, NE, DEXP, D])
    a_w_qkv = dt("a_w_qkv", [2, D, 1536]); a_w_o = dt("a_w_o", [2, D, D]); a_sink = dt("a_sink", [2, 16])
    b_w_in = dt("b_w_in", [1, D, 2 * D]); b_b_in = dt("b_b_in", [1, 2 * D]); b_ln_g = dt("b_ln_g", [1, D]); b_ln_b = dt("b_ln_b", [1, D])
    b_w_s = dt("b_w_s", [1, 8, 128, 128]); b_b_s = dt("b_b_s", [1, 128, 8]); b_w_out = dt("b_w_out", [1, D, D])
    c_w_in = dt("c_w_in", [1, D, 3 * D]); c_w_conv = dt("c_w_conv", [1, 3, D]); c_w_out = dt("c_w_out", [1, D, D])
    kbias = dt("kbias", [128, 24]); cos_t = dt("cos_t", [128, 24 * 64]); sin_t = dt("sin_t", [128, 24 * 64])
    maskp = dt("maskp", [128, 512]); maskn = dt("maskn", [128, 512]); valid = dt("valid", [128, 22])
    SA = dt("scrA", [22 * 128, D], "ExternalOutput" if dbg else "Internal")
    SB = dt("scrB", [22 * 128, D], "ExternalOutput" if dbg else "Internal")
    xout = dt("xout", [2048, D], "ExternalOutput")
    row = lambda T, i: T[i * 128:(i + 1) * 128, :]

    def AM(L):
        return {"cvec": cvec, "wmod": w_mod[L][:, 3 * D:6 * D], "bmod": b_mod[L][3 * D:6 * D], "lng": ln2_g[L], "lnb": ln2_b[L],
                "rw": rw, "rb": rb, "w1": w1[L], "w3": w3[L], "w2": w2[L]}

    def AX_(L):
        return {"cvec": cvec, "wmod": w_mod[L][:, 0:3 * D], "bmod": b_mod[L][0:3 * D], "lng": ln1_g[L], "lnb": ln1_b[L]}

    def moe(L, pfx, tiles):
        h = (len(tiles) + 1) // 2 if len(tiles) > 18 else len(tiles)
        stage_moe(nc, pfx + "a_", tiles[:h], AM(L))
        if h < len(tiles):
            stage_moe(nc, pfx + "b_", tiles[h:], AM(L))

    attn_tabs = {"kbias": kbias, "cos_t": cos_t, "sin_t": sin_t, "maskp": maskp, "maskn": maskn}
    kv = [(row(xw, v), 0, v, v) for v in range(NWIN)] + [(row(ctx, c), 1, 22 + c, 22 + c) for c in range(2)]
    q = [(row(xw, u + 1), 0, u + 1, [(u, "P"), (u + 1, None), (u + 2, "N"), (22, None), (23, None)], row(SA, u)) for u in range(NEXT)]
    q += [(row(ctx, c), 1, 22 + c, [(22, None), (23, None)], row(SA, 20 + c)) for c in range(2)]
    A = AX_(0); A.update(attn_tabs); A.update({"w_qkv": a_w_qkv[0], "w_o": a_w_o[0], "sink": a_sink[0]})
    stage_attn(nc, "s0_", kv, q, A, 24)
    if nstage <= 1:
        return nc
    allt = lambda Tsrc, Tdst: [(row(Tsrc, u), 0 if u < NEXT else 1, row(Tdst, u)) for u in range(22)]
    moe(0, "m0", allt(SA, SB))
    if nstage <= 2:
        return nc
    A = AX_(1); A.update({"w_in": b_w_in[0], "b_in": b_b_in[0], "g_ln_g": b_ln_g[0], "g_ln_b": b_ln_b[0], "w_s": b_w_s[0], "b_s": b_b_s[0],
                          "w_out": b_w_out[0]})
    stage_gmlp(nc, "s1_", allt(SB, SA), A)
    if nstage <= 3:
        return nc
    moe(1, "m1", allt(SA, SB))
    if nstage <= 4:
        return nc
    lat = [(row(SB, u), 0, u, (row(SA, u) if 1 <= u <= 18 else None)) for u in range(NEXT)]
    cx = [(row(SB, 20 + c), 1, 20 + c, row(SA, 20 + c)) for c in range(2)]
    A = AX_(2); A.update({"valid": valid, "w_in": c_w_in[0], "w_conv": c_w_conv[0], "w_out": c_w_out[0]})
    stage_conv(nc, "s2_", [lat, cx], A, 22)
    if nstage <= 5:
        return nc
    t2 = [(row(SA, u), 0, row(SB, u)) for u in range(1, 19)] + [(row(SA, 20 + c), 1, row(SB, 20 + c)) for c in range(2)]
    moe(2, "m2", t2)
    if nstage <= 6:
        return nc
    kv = [(row(SB, u), 0, u + 1, u + 1) for u in range(1, 19)] + [(row(SB, 20 + c), 1, 22 + c, 22 + c) for c in range(2)]
    q = [(row(SB, u), 0, u + 1, [(u - 2, "P"), (u - 1, None), (u, "N"), (18, None), (19, None)], row(SA, u)) for u in range(2, 18)]
    A = AX_(3); A.update(attn_tabs); A.update({"w_qkv": a_w_qkv[1], "w_o": a_w_o[1], "sink": a_sink[1]})
    stage_attn(nc, "s3_", kv, q, A, 24)
    moe(3, "m3", [(row(SA, u), 0, row(xout, u - 2)) for u in range(2, 18)])
    return nc


_NC = []


def _tables(core):
    L = 16384
    freqs = np.power(np.float32(10000.0), -np.arange(16, dtype=np.float32) / np.float32(16)).astype(np.float32)
    pos = (core * 2048 - 384 + np.arange(NWIN * 128)).astype(np.float32)
    row = np.floor(pos / 64.0).astype(np.float32)
    col = (pos - row * 64).astype(np.float32)
    ar = row[:, None] * freqs[None, :]
    ac = col[:, None] * freqs[None, :]
    ang = np.concatenate([ar, ar, ac, ac], -1)
    cos = np.cos(ang).astype(np.float32)
    sin = np.sin(ang).astype(np.float32)
    sgn = np.tile(np.concatenate([-np.ones(16, np.float32), np.ones(16, np.float32)]), 2)
    sinS = sin * sgn[None, :]
    cos = np.concatenate([cos, np.ones((256, 64), np.float32)], 0)
    sinS = np.concatenate([sinS, np.zeros((256, 64), np.float32)], 0)
    cos_t = np.ascontiguousarray(cos.reshape(24, 128, 64).transpose(1, 0, 2).reshape(128, 24 * 64))
    sin_t = np.ascontiguousarray(sinS.reshape(24, 128, 64).transpose(1, 0, 2).reshape(128, 24 * 64))
    kb = np.zeros((128, 24), np.float32)
    for v in range(NWIN):
        st = core * 2048 - 384 + 128 * v
        if st < 0 or st >= L:
            kb[:, v] = -30000.0
    valid = np.ones((128, 22), np.float32)
    for u in range(NEXT):
        st = core * 2048 - 256 + 128 * u
        if st < 0 or st >= L:
            valid[:, u] = 0.0
    return cos_t, sin_t, kb, valid


def kernel(x, c, ctx, c_ctx, w_mod, b_mod, ln1_g, ln1_b, ln2_g, ln2_b, router_w, router_bias, moe_w1, moe_w3, moe_w2,
           a_w_qkv, a_w_o, a_sink, b_w_in, b_b_in, b_ln_g, b_ln_b, b_w_s, b_b_s, b_w_out, c_w_in, c_w_conv, c_w_out):
    f32 = lambda a: np.ascontiguousarray(np.asarray(a, dtype=np.float32))
    if not _NC:
        _NC.append(build_fused())
    nc = _NC[0]
    xc = f32(x)[0]
    zpad = np.zeros((384, D), np.float32)
    xp = np.concatenate([zpad, xc, zpad], 0)
    kk = np.arange(128)[:, None]
    qq = np.arange(128)[None, :]
    com = {"ctx": f32(ctx)[0], "cvec": np.stack([f32(c)[0], f32(c_ctx)]).astype(np.float32),
           "w_mod": f32(w_mod), "b_mod": f32(b_mod), "ln1_g": f32(ln1_g), "ln1_b": f32(ln1_b), "ln2_g": f32(ln2_g), "ln2_b": f32(ln2_b),
           "router_w": f32(router_w), "router_bias": f32(router_bias), "moe_w1": f32(moe_w1), "moe_w3": f32(moe_w3), "moe_w2": f32(moe_w2),
           "a_w_qkv": f32(a_w_qkv), "a_w_o": f32(a_w_o), "a_sink": f32(a_sink), "b_w_in": f32(b_w_in), "b_b_in": f32(b_b_in),
           "b_ln_g": f32(b_ln_g), "b_ln_b": f32(b_ln_b), "b_w_s": f32(b_w_s), "b_b_s": f32(b_b_s), "b_w_out": f32(b_w_out),
           "c_w_in": f32(c_w_in), "c_w_conv": f32(c_w_conv), "c_w_out": f32(c_w_out),
           "maskp": np.tile((kk >= qq).astype(np.float32), (1, 4)), "maskn": np.tile((kk <= qq).astype(np.float32), (1, 4))}
    in_maps = []
    for core in range(8):
        cos_t, sin_t, kb, valid = _tables(core)
        in_maps.append(dict(com, xw=np.ascontiguousarray(xp[core * 2048:core * 2048 + NWIN * 128]), cos_t=cos_t, sin_t=sin_t, kbias=kb, valid=valid))
    res = run_bass_kernel_spmd(nc, in_maps, core_ids=list(range(8)))
    out = np.concatenate([r["xout"] for r in res.results], 0)
    return out[None].astype(np.float32)
```

```python
import numpy as np
from contextlib import ExitStack
import concourse.bass as bass
import concourse.mybir as mybir
from concourse.bass_utils import run_bass_kernel_spmd

F32 = mybir.dt.float32
BF16 = mybir.dt.bfloat16
AF = mybir.ActivationFunctionType
ALU = mybir.AluOpType
AX = mybir.AxisListType


SAME_ENGINE_INORDER = False


class Sched:
    ENG = ("pe", "act", "dve", "pool", "sp")

    def __init__(self, nc, es, pfx=""):
        self.nc = nc
        self.es = es
        self.pfx = pfx
        self.q = {e: [] for e in self.ENG}
        self.semh = {e: nc.alloc_semaphore(name=pfx + "s_" + e) for e in self.ENG}
        self.cnt = {e: 0 for e in self.ENG}
        self.waited = {e: {} for e in self.ENG}
        self.lastw = {}
        self.readers = {}
        self.dcnt = {}
        self.store_sems = set()

    def _deps(self, eng, reads, writes):
        need = {}
        for k in reads:
            if k in self.lastw:
                s, v = self.lastw[k]
                need[s] = max(need.get(s, 0), v)
        for k in writes:
            if k in self.lastw:
                s, v = self.lastw[k]
                need[s] = max(need.get(s, 0), v)
            for (s, v) in self.readers.get(k, ()):
                need[s] = max(need.get(s, 0), v)
        for s, v in need.items():
            if s == eng and (eng == "pe" or SAME_ENGINE_INORDER):
                continue
            if self.waited[eng].get(s, 0) < v:
                self.q[eng].append(("wait", s, v))
                self.waited[eng][s] = v

    def op(self, eng, fn, reads=(), writes=()):
        psr = [k for k in reads if isinstance(k, str) and k.startswith("ps")]
        if psr:
            reads = [k for k in reads if k not in psr]
            writes = list(writes) + psr
        self._deps(eng, reads, writes)
        self.cnt[eng] += 1
        v = self.cnt[eng]
        self.q[eng].append(("op", fn))
        for k in reads:
            self.readers.setdefault(k, []).append((eng, v))
        for k in writes:
            self.lastw[k] = (eng, v)
            self.readers[k] = []

    def dma(self, queue, out, in_, sem, reads=(), writes=(), store=False, **kw):
        if sem not in self.semh:
            self.semh[sem] = self.nc.alloc_semaphore(name=self.pfx + "d_" + sem)
            self.dcnt[sem] = 0
        self._deps(queue, reads, writes)
        self.dcnt[sem] += 16
        v = self.dcnt[sem]
        self.q[queue].append(("dma", out, in_, sem, kw))
        for k in reads:
            self.readers.setdefault(k, []).append((sem, v))
        for k in writes:
            self.lastw[k] = (sem, v)
            self.readers[k] = []
        if store:
            self.store_sems.add(sem)

    def finish(self):
        for s in sorted(self.store_sems):
            self.q["sp"].append(("wait", s, self.dcnt[s]))

    def close(self):
        self.nc.clear_and_free_semaphores(list(self.semh.values()))
        self.nc.all_engine_barrier()

    def emit(self):
        nc = self.nc
        self.finish()

        def replay(e, engobj):
            for it in self.q[e]:
                if it[0] == "wait":
                    engobj.wait_ge(self.semh[it[1]], it[2])
                elif it[0] == "op":
                    it[1](engobj).then_inc(self.semh[e], 1)
                else:
                    _, out, in_, sem, kw = it
                    engobj.dma_start(out=out, in_=in_, **kw).then_inc(self.semh[sem], 16)

        with nc.Block() as block:
            @block.tensor
            def _(e):
                replay("pe", e)

            @block.scalar
            def _(e):
                replay("act", e)

            @block.vector
            def _(e):
                replay("dve", e)

            @block.gpsimd
            def _(e):
                replay("pool", e)

            @block.sync
            def _(e):
                replay("sp", e)


D = 1024
NE = 16
DEXP = 512
ALPHA = 8.0 ** 0.25
LN_EPS = 1e-5
EPS2 = LN_EPS / (ALPHA * ALPHA)


def common_consts(S, nc, sb, ps):
    ident = sb("ident", [128, 128])
    ones = sb("ones", [128, 128])
    S.op("pool", lambda e: e.memset(ones[:], 1.0), writes=["ones"])
    S.op("pool", lambda e: e.memset(ident[:], 0.0), writes=["ident"])
    S.op("pool", lambda e: e.affine_select(out=ident[:], in_=ident[:], pattern=[[-1, 128]], compare_op=ALU.not_equal,
                                          fill=1.0, base=0, channel_multiplier=1), reads=["ident"], writes=["ident"])
    return ident, ones


def mod_tiles(S, nc, sb, cvec, wmod, bmod, ident, ones, psbig, pbk, stage, stage_keys, SLC, slc_keys, outs):
    ct = sb("ct", [16, 128])
    csil = sb("csil", [128, 16])
    brow = sb("brow", [1, 512])
    S.dma("sp", ct[:], cvec.rearrange("s (k p) -> (s k) p", p=128), "ld_ct", writes=["ct"])
    S.op("pe", lambda e: e.transpose(out=psbig[:, 0:16], in_=ct[0:16, :], identity=ident[0:16, 0:16]),
         reads=["ct", "ident"], writes=[pbk])
    S.op("act", lambda e: e.activation(out=csil[:], in_=psbig[:, 0:16], func=AF.Silu), reads=[pbk], writes=["csil"])
    for s in range(2):
        for k in range(8):
            S.op("act", lambda e, s=s, k=k: e.activation(out=SLC[:, s, k, :], in_=ones[:], func=AF.Copy,
                                                         scale=csil[:, s * 8 + k:s * 8 + k + 1]),
                 reads=["ones", "csil"], writes=slc_keys)
    nv = max(o[0] for o in outs) + 1
    stages = stage if isinstance(stage, list) else [stage]
    skeys = stage_keys if isinstance(stage, list) else [stage_keys]
    brows = [brow, sb("brow2", [1, 512])] if isinstance(stage, list) else [brow, brow]
    idx = 0
    for v in range(nv):
        for h in range(2):
            c0 = v * 1024 + h * 512
            stg = stages[idx % len(stages)]
            sk = skeys[idx % len(stages)]
            br = brows[idx % 2]
            brk = f"brow{idx % 2}" if isinstance(stage, list) else "brow0"
            S.dma("sp", stg, wmod[:, c0:c0 + 512].rearrange("(k p) n -> p k n", p=128), f"ld_stage{idx % len(stages)}", writes=sk)
            S.dma("sp", br[:], bmod[c0:c0 + 512].rearrange("(a n) -> a n", a=1), (f"ld_brow{idx % 2}" if isinstance(stage, list) else "ld_brow0"), writes=[brk])
            idx += 1
            for s in range(2):
                mine = [o for o in outs if o[0] == v and o[1] == s]
                if not mine:
                    continue
                for k in range(8):
                    S.op("pe", lambda e, s=s, k=k, stg=stg: e.matmul(psbig[:, 0:512], lhsT=SLC[:, s, k, :], rhs=stg[:, k, :],
                                                                     start=(k == 0), stop=False),
                         reads=slc_keys + sk, writes=[pbk])
                S.op("pe", lambda e, br=br: e.matmul(psbig[:, 0:512], lhsT=ones[0:1, :], rhs=br[0:1, :], start=False, stop=True),
                     reads=["ones", brk], writes=[pbk])
                for (_, _, tile, key, kind) in mine:
                    dst = tile[:, h * 512:(h + 1) * 512]
                    if kind == "plain":
                        S.op("dve", lambda e, dst=dst: e.tensor_copy(out=dst, in_=psbig[:, 0:512]), reads=[pbk], writes=[key])
                    elif kind == "plus1":
                        S.op("dve", lambda e, dst=dst: e.tensor_scalar(out=dst, in0=psbig[:, 0:512], scalar1=1.0, scalar2=None,
                                                                       op0=ALU.add), reads=[pbk], writes=[key])
                    else:
                        S.op("dve", lambda e, dst=dst: e.tensor_scalar(out=dst, in0=psbig[:, 0:512], scalar1=1.0 / ALPHA,
                                                                       scalar2=None, op0=ALU.mult), reads=[pbk], writes=[key])


def layer_norm_tile(S, src, src_key, dst, dst_key, tmp, tmp_key, lng, lng_key, lnb, lnb_key, small, eps_t):
    st, mv, rstd, nmr = small
    for h in range(2):
        S.op("dve", lambda e, h=h: e.bn_stats(out=st[:, h, :], in_=src[:, h * 512:(h + 1) * 512]), reads=[src_key], writes=["ln_st"])
    S.op("dve", lambda e: e.bn_aggr(out=mv[:], in_=st[:].rearrange("p a b -> p (a b)")), reads=["ln_st"], writes=["ln_mv"])
    S.op("act", lambda e: e.activation(out=rstd[:], in_=mv[:, 1:2], func=AF.Sqrt, bias=eps_t[:], scale=1.0),
         reads=["ln_mv", "eps"], writes=["ln_rstd"])
    S.op("dve", lambda e: e.reciprocal(out=rstd[:], in_=rstd[:]), reads=["ln_rstd"], writes=["ln_rstd"])
    S.op("dve", lambda e: e.scalar_tensor_tensor(out=nmr[:], in0=mv[:, 0:1], scalar=-1.0, in1=rstd[:], op0=ALU.mult, op1=ALU.mult),
         reads=["ln_mv", "ln_rstd"], writes=["ln_nmr"])
    S.op("act", lambda e: e.activation(out=tmp, in_=src, func=AF.Identity, bias=nmr[:], scale=rstd[:]),
         reads=[src_key, "ln_nmr", "ln_rstd"], writes=[tmp_key])
    S.op("dve", lambda e: e.tensor_tensor(out=tmp, in0=tmp, in1=lng, op=ALU.mult), reads=[tmp_key, lng_key], writes=[tmp_key])
    S.op("dve", lambda e: e.tensor_tensor(out=dst, in0=tmp, in1=lnb, op=ALU.add), reads=[tmp_key, lnb_key], writes=[dst_key])


def build_moe():
    NT = 18
    nc = bass.Bass("TRN2", target_bir_lowering=False)
    dt = lambda n, s, k: nc.dram_tensor(n, s, F32, kind=k).ap()
    xin = dt("xin", [NT * 128, D], "ExternalInput")
    A = {"cvec": dt("cvec", [2, D], "ExternalInput"), "wmod": dt("wmod", [D, 3 * D], "ExternalInput"),
         "bmod": dt("bmod", [3 * D], "ExternalInput"), "lng": dt("lng", [D], "ExternalInput"), "lnb": dt("lnb", [D], "ExternalInput"),
         "rw": dt("rw", [D, NE], "ExternalInput"), "rb": dt("rb", [NE], "ExternalInput"),
         "w1": dt("w1", [NE, D, DEXP], "ExternalInput"), "w3": dt("w3", [NE, D, DEXP], "ExternalInput"),
         "w2": dt("w2", [NE, DEXP, D], "ExternalInput")}
    xout = dt("xout", [NT * 128, D], "ExternalOutput")
    tiles = [(xin[t * 128:(t + 1) * 128, :], 0 if t < 16 else 1, xout[t * 128:(t + 1) * 128, :]) for t in range(NT)]
    stage_moe(nc, "", tiles, A)
    return nc


def stage_moe(nc, pfx, tiles, A):
    NT = len(tiles)
    cvec, wmod, bmod, lng_d, lnb_d, rw_d, rb_d, w1_d, w3_d, w2_d = (A[k] for k in ("cvec", "wmod", "bmod", "lng", "lnb", "rw", "rb", "w1", "w3", "w2"))
    es = ExitStack()
    with es:
        S = Sched(nc, es, pfx)
        sb = lambda n, s, d=F32: es.enter_context(nc.sbuf_tensor(pfx + n, s, d))
        ps = lambda n, s, d=F32: es.enter_context(nc.psum_tensor(pfx + n, s, d))
        X = sb("X", [128, NT, D])
        HT = sb("HT", [128, 8, NT * 128], BF16)
        W13 = [sb(f"W13_{i}", [128, 2, 8, 256], BF16) for i in range(2)]
        W2 = [sb(f"W2_{i}", [128, 2, D], BF16) for i in range(2)]
        has_ctx = any(tl[1] == 1 for tl in tiles)
        W2c = [sb(f"W2c_{i}", [128, 2, D], BF16) for i in range(2)] if has_ctx else None
        STG = sb("STG", [128, 6 * D])
        MOD = {("sc1", 0): STG[:, 0:D], ("sh", 0): STG[:, D:2 * D], ("sc1", 1): STG[:, 2 * D:3 * D], ("sh", 1): STG[:, 3 * D:4 * D]}
        MKEY = {("sc1", 0): "STG0", ("sh", 0): "STG1", ("sc1", 1): "STG2", ("sh", 1): "STG3"}
        for s_ in range(2):
            MOD[("gf", s_)] = sb(f"Mgf{s_}", [128, D])[:]
            MKEY[("gf", s_)] = f"Mgf{s_}"
        S13 = STG[:, 0:4 * D].rearrange("p (a k n) -> p a k n", a=2, k=8)
        S2 = STG[:, 4 * D:6 * D].rearrange("p (k n) -> p k n", k=2)
        T = [sb(f"T{i}", [128, D]) for i in range(2)]
        SA = sb("SA", [128, 2, 512])
        H1 = [sb(f"H1_{i}", [128, 2, 512], BF16) for i in range(2)]
        rw = sb("rwt", [128, 8, NE])
        rb = sb("rbt", [128, NE])
        GATE = sb("GATE", [128, NT, NE])
        eps_t = sb("eps_t", [128, 1])
        small = (sb("ln_st", [128, 2, 6]), sb("ln_mv", [128, 2]), sb("ln_rstd", [128, 1]), sb("ln_nmr", [128, 1]))
        rt = {n: sb("rt_" + n, [128, 16]) for n in ("s", "ssel", "sm", "sel", "sc")}
        rg = {n: sb("rg_" + n, [128, 4]) for n in ("p01", "q01", "p23", "q23", "t1", "m2", "m3", "gs", "ing", "pen")}
        r1 = {n: sb("r1_" + n, [128, 1]) for n in ("gmax", "den")}
        top8 = sb("top8", [128, 8])
        psY = [ps(f"psY{i}", [128, D]) for i in range(2)]
        psA = [ps(f"psA{i}", [128, 512]) for i in range(2)]
        psB = [ps(f"psB{i}", [128, 512]) for i in range(2)]

        ident, ones = common_consts(S, nc, sb, ps)
        S.op("dve", lambda e: e.memset(eps_t[:], EPS2), writes=["eps"])
        SLC = X[:, 0:2, :].rearrange("p s (k n) -> p s k n", k=8)
        stage = [X[:, 2:6, :].rearrange("p a (b n) -> p (a b) n", b=2), X[:, 6:10, :].rearrange("p a (b n) -> p (a b) n", b=2)]
        outs = []
        for s in range(2):
            outs.append((0, s, MOD[("sh", s)], MKEY[("sh", s)], "plain"))
            outs.append((1, s, MOD[("sc1", s)], MKEY[("sc1", s)], "plus1"))
            outs.append((2, s, MOD[("gf", s)], MKEY[("gf", s)], "invalpha"))
        mod_tiles(S, nc, sb, cvec, wmod, bmod, ident, ones, psY[0], "psY0", stage, [[("X", t) for t in range(2, 6)], [("X", t) for t in range(6, 10)]],
                  SLC, [("X", 0), ("X", 1)], outs)
        S.dma("sp", rw[:], rw_d.rearrange("(k p) e -> p k e", p=128), "ld_rw", writes=["rw"])
        S.dma("sp", rb[:], rb_d.partition_broadcast(128), "ld_rb", writes=["rb"])

        for t in range(NT):
            s = tiles[t][1]
            xk = ("X", t)
            S.dma("sp", X[:, t, :], tiles[t][0], f"ld_x{t}", writes=[xk])
            S.op("dve", lambda e, t=t, s=s: e.tensor_tensor(out=T[0][:], in0=X[:, t, :], in1=MOD[("sc1", s)], op=ALU.mult),
                 reads=[xk, MKEY[("sc1", s)]], writes=["T0"])
            S.op("dve", lambda e, s=s: e.tensor_tensor(out=T[0][:], in0=T[0][:], in1=MOD[("sh", s)], op=ALU.add),
                 reads=["T0", MKEY[("sh", s)]], writes=["T0"])
            pt = psY[t % 2]
            ptk = f"psY{t % 2}"
            for k in range(8):
                S.op("pe", lambda e, k=k, pt=pt: e.transpose(out=pt[:, k * 128:(k + 1) * 128], in_=T[0][:, k * 128:(k + 1) * 128],
                                                             identity=ident[:]), reads=["T0", "ident"], writes=[ptk])
            S.op("act", lambda e, t=t, pt=pt: e.activation(out=HT[:, :, t * 128:(t + 1) * 128],
                                                           in_=pt[:].rearrange("p (k n) -> p k n", k=8), func=AF.Copy),
                 reads=[ptk], writes=[("HT", t)])
            S.op("dve", lambda e, pt=pt: e.tensor_copy(out=T[1][:], in_=pt[:]), reads=[ptk], writes=["T1"])
            pr = psA[t % 2]
            prk = f"psA{t % 2}"
            for k in range(8):
                S.op("pe", lambda e, k=k, pr=pr: e.matmul(pr[:, 0:NE], lhsT=T[1][:, k * 128:(k + 1) * 128], rhs=rw[:, k, :],
                                                          start=(k == 0), stop=(k == 7)), reads=["T1", "rw"], writes=[prk])
            S.op("act", lambda e, pr=pr: e.activation(out=rt["s"][:], in_=pr[:, 0:NE], func=AF.Sigmoid), reads=[prk], writes=["rt_s"])
            dv = lambda fn, r, w: S.op("dve", fn, reads=r, writes=w)
            dv(lambda e: e.tensor_tensor(out=rt["ssel"][:], in0=rt["s"][:], in1=rb[:], op=ALU.add), ["rt_s", "rb"], ["rt_ssel"])
            sv = rt["ssel"][:].rearrange("p (g j) -> p g j", j=4)
            a, b, c, d = (sv[:, :, j] for j in range(4))
            dv(lambda e: e.tensor_tensor(out=rg["p01"][:], in0=a, in1=b, op=ALU.max), ["rt_ssel"], ["p01"])
            dv(lambda e: e.tensor_tensor(out=rg["q01"][:], in0=a, in1=b, op=ALU.min), ["rt_ssel"], ["q01"])
            dv(lambda e: e.tensor_tensor(out=rg["p23"][:], in0=c, in1=d, op=ALU.max), ["rt_ssel"], ["p23"])
            dv(lambda e: e.tensor_tensor(out=rg["q23"][:], in0=c, in1=d, op=ALU.min), ["rt_ssel"], ["q23"])
            dv(lambda e: e.tensor_tensor(out=rg["t1"][:], in0=rg["p01"][:], in1=rg["p23"][:], op=ALU.max), ["p01", "p23"], ["t1"])
            dv(lambda e: e.tensor_tensor(out=rg["m2"][:], in0=rg["p01"][:], in1=rg["p23"][:], op=ALU.min), ["p01", "p23"], ["m2"])
            dv(lambda e: e.tensor_tensor(out=rg["m3"][:], in0=rg["q01"][:], in1=rg["q23"][:], op=ALU.max), ["q01", "q23"], ["m3"])
            dv(lambda e: e.tensor_tensor(out=rg["m2"][:], in0=rg["m2"][:], in1=rg["m3"][:], op=ALU.max), ["m2", "m3"], ["m2"])
            dv(lambda e: e.tensor_tensor(out=rg["gs"][:], in0=rg["t1"][:], in1=rg["m2"][:], op=ALU.add), ["t1", "m2"], ["gs"])
            dv(lambda e: e.tensor_reduce(out=r1["gmax"][:], in_=rg["gs"][:], axis=AX.X, op=ALU.max), ["gs"], ["gmax"])
            dv(lambda e: e.tensor_scalar(out=rg["ing"][:], in0=rg["gs"][:], scalar1=r1["gmax"][:, 0:1], scalar2=None, op0=ALU.is_ge),
               ["gs", "gmax"], ["ing"])
            dv(lambda e: e.tensor_scalar(out=rg["pen"][:], in0=rg["ing"][:], scalar1=4.0, scalar2=-4.0, op0=ALU.mult, op1=ALU.add),
               ["ing"], ["pen"])
            smv = rt["sm"][:].rearrange("p (g j) -> p g j", j=4)
            for g in range(4):
                dv(lambda e, g=g: e.tensor_scalar(out=smv[:, g, :], in0=sv[:, g, :], scalar1=rg["ing"][:, g:g + 1],
                                                  scalar2=rg["pen"][:, g:g + 1], op0=ALU.mult, op1=ALU.add),
                   ["rt_ssel", "ing", "pen"], ["rt_sm"])
            dv(lambda e: e.max(out=top8[:], in_=rt["sm"][:]), ["rt_sm"], ["top8"])
            dv(lambda e: e.tensor_scalar(out=rt["sel"][:], in0=rt["sm"][:], scalar1=top8[:, 1:2], scalar2=None, op0=ALU.is_ge),
               ["rt_sm", "top8"], ["rt_sel"])
            dv(lambda e: e.tensor_tensor(out=rt["sc"][:], in0=rt["s"][:], in1=rt["sel"][:], op=ALU.mult), ["rt_s", "rt_sel"], ["rt_sc"])
            dv(lambda e: e.tensor_reduce(out=r1["den"][:], in_=rt["sc"][:], axis=AX.X, op=ALU.add), ["rt_sc"], ["den"])
            dv(lambda e: e.reciprocal(out=r1["den"][:], in_=r1["den"][:]), ["den"], ["den"])
            dv(lambda e, t=t: e.tensor_scalar(out=GATE[:, t, :], in0=rt["sc"][:], scalar1=r1["den"][:, 0:1], scalar2=None, op0=ALU.mult),
               ["rt_sc", "den"], [("GATE", t)])

        groups = [(g * 4, min(4, NT - g * 4)) for g in range((NT + 3) // 4)]
        NU = 2 * NE
        stg13_keys = ["STG0", "STG1", "STG2", "STG3"]
        stg2_keys = ["STG4", "STG5"]

        def load_unit(u):
            ex, hf = u // 2, u % 2
            S.dma("sp", S13[:, 0, :, :], w1_d[ex, :, hf * 256:(hf + 1) * 256].rearrange("(k p) n -> p k n", p=128),
                  "ld_s13", writes=stg13_keys)
            S.dma("sp", S13[:, 1, :, :], w3_d[ex, :, hf * 256:(hf + 1) * 256].rearrange("(k p) n -> p k n", p=128),
                  "ld_s13", writes=stg13_keys)
            S.dma("sp", S2, w2_d[ex, hf * 256:(hf + 1) * 256, :].rearrange("(k p) n -> p k n", p=128),
                  "ld_s2", writes=stg2_keys)

        def cast_unit(u):
            sl = u % 2
            for a in range(2):
                S.op("act", lambda e, a=a, sl=sl: e.activation(out=W13[sl][:, a, :, :], in_=S13[:, a, :, :], func=AF.Copy),
                     reads=stg13_keys, writes=[f"W13_{sl}"])
            gl = MOD[("gf", 0)].unsqueeze(1).to_broadcast([128, 2, D])
            S.op("dve", lambda e, sl=sl, gl=gl: e.tensor_tensor(out=W2[sl][:], in0=S2, in1=gl, op=ALU.mult),
                 reads=stg2_keys + [MKEY[("gf", 0)]], writes=[f"W2_{sl}"])
            if has_ctx:
                gc_ = MOD[("gf", 1)].unsqueeze(1).to_broadcast([128, 2, D])
                S.op("pool", lambda e, sl=sl, gc_=gc_: e.tensor_tensor(out=W2c[sl][:], in0=S2, in1=gc_, op=ALU.mult),
                     reads=stg2_keys + [MKEY[("gf", 1)]], writes=[f"W2c_{sl}"])

        yi = 0
        abi = 0
        load_unit(0)
        cast_unit(0)
        work = [(u, gi, t0, ntile) for u in range(NU) for gi, (t0, ntile) in enumerate(groups)]
        cast_gi = min(2, len(groups) - 1)
        yi_box = [0]

        def AB(idx):
            u, gi, t0, ntile = work[idx]
            sl = u % 2
            wk0 = f"W13_{sl}"
            ntok = ntile * 128
            c0 = t0 * 128
            htk = [("HT", t) for t in range(t0, t0 + ntile)]
            hb = H1[idx % 2]
            hk = f"H1_{idx % 2}"
            for dc in range(2):
                pa, pb = psA[dc], psB[dc]
                for k in range(8):
                    S.op("pe", lambda e, k=k, dc=dc, pa=pa: e.matmul(
                        pa[:, 0:ntok], lhsT=W13[sl][:, 0, k, dc * 128:(dc + 1) * 128], rhs=HT[:, k, c0:c0 + ntok],
                        start=(k == 0), stop=(k == 7)), reads=[wk0] + htk, writes=[f"psA{dc}"])
                for k in range(8):
                    S.op("pe", lambda e, k=k, dc=dc, pb=pb: e.matmul(
                        pb[:, 0:ntok], lhsT=W13[sl][:, 1, k, dc * 128:(dc + 1) * 128], rhs=HT[:, k, c0:c0 + ntok],
                        start=(k == 0), stop=(k == 7)), reads=[wk0] + htk, writes=[f"psB{dc}"])
                sa = SA[:, dc, 0:ntok]
                S.op("act", lambda e, pa=pa, sa=sa: e.activation(out=sa, in_=pa[:, 0:ntok], func=AF.Silu),
                     reads=[f"psA{dc}"], writes=[f"SA{dc}"])
                S.op("dve", lambda e, pb=pb, sa=sa, dc=dc: e.tensor_tensor(out=hb[:, dc, 0:ntok], in0=sa, in1=pb[:, 0:ntok], op=ALU.mult),
                     reads=[f"SA{dc}", f"psB{dc}"], writes=[hk])

        def Y(idx):
            u, gi, t0, ntile = work[idx]
            sl = u % 2
            ex = u // 2
            wk1 = f"W2_{sl}"
            hb = H1[idx % 2]
            hk = f"H1_{idx % 2}"
            for ti in range(ntile):
                t = t0 + ti
                s = tiles[t][1]
                yi = yi_box[0]
                yi_box[0] += 1
                py = psY[yi % 2]
                pyk = f"psY{yi % 2}"
                w2t, w2k = (W2[sl], f"W2_{sl}") if s == 0 else (W2c[sl], f"W2c_{sl}")
                for h2 in range(2):
                    for dc in range(2):
                        S.op("pe", lambda e, h2=h2, dc=dc, py=py, ti=ti, w2t=w2t: e.matmul(
                            py[:, h2 * 512:(h2 + 1) * 512], lhsT=hb[:, dc, ti * 128:(ti + 1) * 128],
                            rhs=w2t[:, dc, h2 * 512:(h2 + 1) * 512], start=(dc == 0), stop=(dc == 1)),
                            reads=[hk, w2k], writes=[pyk])
                S.op("dve", lambda e, py=py, t=t: e.scalar_tensor_tensor(
                    out=X[:, t, :], in0=py[:], scalar=GATE[:, t, ex:ex + 1], in1=X[:, t, :], op0=ALU.mult, op1=ALU.add),
                    reads=[pyk, ("GATE", t), ("X", t)], writes=[("X", t)])

        for idx in range(len(work)):
            u, gi, _, _ = work[idx]
            if gi == 0 and u + 1 < NU:
                load_unit(u + 1)
            if gi == cast_gi and u + 1 < NU:
                cast_unit(u + 1)
            AB(idx)
            if idx >= 1:
                Y(idx - 1)
        Y(len(work) - 1)

        LNG, LNB = STG[:, 0:D], STG[:, D:2 * D]
        S.dma("sp", LNG, lng_d.partition_broadcast(128), "ld_lng", writes=["STG0"])
        S.dma("sp", LNB, lnb_d.partition_broadcast(128), "ld_lnb", writes=["STG1"])
        for t in range(NT):
            o = STG[:, (2 + t % 2) * D:(3 + t % 2) * D]
            ok = f"STG{2 + t % 2}"
            layer_norm_tile(S, X[:, t, :], ("X", t), o, ok, T[0][:], "T0", LNG, "STG0", LNB, "STG1", small, eps_t)
            S.dma("sp", tiles[t][2], o, f"st{t % 2}", reads=[ok], store=True)
        S.emit()
        S.close()


class Ctx:
    pass


def std_inputs(nc):
    dt = lambda n, s, k="ExternalInput": nc.dram_tensor(n, s, F32, kind=k).ap()
    return dt, {"cvec": dt("cvec", [2, D]), "wmod": dt("wmod", [D, 3 * D]), "bmod": dt("bmod", [3 * D]), "lng": dt("lng", [D]),
                "lnb": dt("lnb", [D])}


def mixer_prologue(nc, es, pfx, A):
    C = Ctx()
    C.nc = nc
    C.es = es
    S = C.S = Sched(nc, es, pfx)
    sb = C.sb = lambda n, s, d=F32: es.enter_context(nc.sbuf_tensor(pfx + n, s, d))
    ps = C.ps = lambda n, s, d=F32: es.enter_context(nc.psum_tensor(pfx + n, s, d))
    C.cvec, C.wmod, C.bmod, C.lng_d, C.lnb_d = A["cvec"], A["wmod"], A["bmod"], A["lng"], A["lnb"]
    C.STG = [sb(f"STG{i}", [128, 4096]) for i in range(2)]
    C.stg_i = 0
    C.MOD = {}
    C.MKEY = {}
    for n in ("sh", "sc1", "ga"):
        for s in range(2):
            C.MOD[(n, s)] = sb(f"M{n}{s}", [128, D])[:]
            C.MKEY[(n, s)] = f"M{n}{s}"
    C.LNG = sb("LNG", [128, D])
    C.LNB = sb("LNB", [128, D])
    C.eps2 = sb("eps2", [128, 1])
    C.eps1 = sb("eps1", [128, 1])
    C.small = (sb("ln_st", [128, 2, 6]), sb("ln_mv", [128, 2]), sb("ln_rstd", [128, 1]), sb("ln_nmr", [128, 1]))
    C.psBig = [ps(f"psBig{i}", [128, D]) for i in range(2)]
    C.ident, C.ones = common_consts(S, nc, sb, ps)
    S.op("dve", lambda e: e.memset(C.eps2[:], EPS2), writes=["eps"])
    S.op("dve", lambda e: e.memset(C.eps1[:], LN_EPS), writes=["eps1"])
    return C


def mixer_mods(C):
    S = C.S
    stage = C.STG[0][:].rearrange("p (k n) -> p k n", k=8)
    SLC = C.STG[1][:, 0:2048].rearrange("p (s k n) -> p s k n", s=2, k=8)
    outs = []
    for s in range(2):
        outs.append((0, s, C.MOD[("sh", s)], C.MKEY[("sh", s)], "plain"))
        outs.append((1, s, C.MOD[("sc1", s)], C.MKEY[("sc1", s)], "plus1"))
        outs.append((2, s, C.MOD[("ga", s)], C.MKEY[("ga", s)], "invalpha"))
    mod_tiles(S, C.nc, C.sb, C.cvec, C.wmod, C.bmod, C.ident, C.ones, C.psBig[0], "psBig0", stage, ["STG0"], SLC, ["STG1"], outs)
    S.dma("sp", C.LNG[:], C.lng_d.partition_broadcast(128), "ld_lng", writes=["LNG"])
    S.dma("sp", C.LNB[:], C.lnb_d.partition_broadcast(128), "ld_lnb", writes=["LNB"])


def load_w_bf16(C, dst, dkey, src, kc, ncols):
    S = C.S
    cw = min(ncols, 512)
    kpp = max(1, min(kc, 4096 // cw))
    n = 0
    for c0 in range(0, ncols, cw):
        for k0 in range(0, kc, kpp):
            kk = min(kpp, kc - k0)
            i = C.stg_i % 2
            C.stg_i += 1
            st = C.STG[i][:, 0:kk * cw].rearrange("p (k n) -> p k n", k=kk)
            S.dma("sp", st, src[k0 * 128:(k0 + kk) * 128, c0:c0 + cw].rearrange("(k p) n -> p k n", p=128), f"ld_stg{i}",
                  writes=[f"STG{i}"])
            eng = "act" if n % 2 == 0 else "dve"
            n += 1
            d = dst[:, k0:k0 + kk, c0:c0 + cw]
            if eng == "act":
                S.op("act", lambda e, d=d, st=st: e.activation(out=d, in_=st, func=AF.Copy), reads=[f"STG{i}"], writes=[dkey])
            else:
                S.op("dve", lambda e, d=d, st=st: e.tensor_copy(out=d, in_=st), reads=[f"STG{i}"], writes=[dkey])


def modulate_T(C, xt, xkey, s, T0, t0key, pst, pskey, HTt, htkey):
    S = C.S
    S.op("dve", lambda e: e.tensor_tensor(out=T0, in0=xt, in1=C.MOD[("sc1", s)], op=ALU.mult), reads=[xkey, C.MKEY[("sc1", s)]], writes=[t0key])
    S.op("dve", lambda e: e.tensor_tensor(out=T0, in0=T0, in1=C.MOD[("sh", s)], op=ALU.add), reads=[t0key, C.MKEY[("sh", s)]], writes=[t0key])
    to_T(C, T0, t0key, pst, pskey, HTt, htkey)


def to_T(C, src, skey, pst, pskey, HTt, htkey):
    S = C.S
    for k in range(8):
        S.op("pe", lambda e, k=k: e.transpose(out=pst[:, k * 128:(k + 1) * 128], in_=src[:, k * 128:(k + 1) * 128], identity=C.ident[:]),
             reads=[skey, "ident"], writes=[pskey])
    S.op("act", lambda e: e.activation(out=HTt, in_=pst[:].rearrange("p (k n) -> p k n", k=8), func=AF.Copy), reads=[pskey], writes=[htkey])


def proj(C, pst_list, HTt, htkey, W, wkey, c0, ncols):
    S = C.S
    off = 0
    for (pa, pk) in pst_list:
        w = min(512, ncols - off)
        for k in range(8):
            S.op("pe", lambda e, k=k, pa=pa, w=w, off=off: e.matmul(pa[:, 0:w], lhsT=HTt[:, k, :], rhs=W[:, k, c0 + off:c0 + off + w],
                                                                    start=(k == 0), stop=(k == 7)), reads=[htkey, wkey], writes=[pk])
        off += w


def residual_ln_store(C, psy, pykey, xt, xkey, s, T1, t1key, T2, t2key, o, okey, out_ap, stsem):
    S = C.S
    S.op("dve", lambda e: e.tensor_tensor(out=T1, in0=psy[:], in1=C.MOD[("ga", s)], op=ALU.mult), reads=[pykey, C.MKEY[("ga", s)]], writes=[t1key])
    S.op("pool", lambda e: e.tensor_tensor(out=T1, in0=T1, in1=xt, op=ALU.add), reads=[t1key, xkey], writes=[t1key])
    layer_norm_tile(S, T1, t1key, o, okey, T2, t2key, C.LNG[:], "LNG", C.LNB[:], "LNB", C.small, C.eps2)
    S.dma("sp", out_ap, o, stsem, reads=[okey], store=True)


def build_gmlp():
    NT = 18
    nc = bass.Bass("TRN2", target_bir_lowering=False)
    dt, A = std_inputs(nc)
    xin = dt("xin", [NT * 128, D])
    A.update({"w_in": dt("w_in", [D, 2 * D]), "b_in": dt("b_in", [2 * D]), "g_ln_g": dt("g_ln_g", [D]), "g_ln_b": dt("g_ln_b", [D]),
              "w_s": dt("w_s", [8, 128, 128]), "b_s": dt("b_s", [128, 8]), "w_out": dt("w_out", [D, D])})
    xout = dt("xout", [NT * 128, D], "ExternalOutput")
    tiles = [(xin[t * 128:(t + 1) * 128, :], 0 if t < 16 else 1, xout[t * 128:(t + 1) * 128, :]) for t in range(NT)]
    stage_gmlp(nc, "", tiles, A)
    return nc


def stage_gmlp(nc, pfx, tiles, A):
    NT = len(tiles)
    es = ExitStack()
    with es:
        C = mixer_prologue(nc, es, pfx, A)
        S, sb, ps = C.S, C.sb, C.ps
        win_d, bin_d, glng_d, glnb_d, ws_d, bs_d, wout_d = (A[k] for k in ("w_in", "b_in", "g_ln_g", "g_ln_b", "w_s", "b_s", "w_out"))
        Win = sb("Win", [128, 8, 2 * D], BF16)
        Wout = sb("Wout", [128, 8, D], BF16)
        wsT = sb("wsT", [128, 8, 128], BF16)
        BIN = sb("BIN", [128, 2 * D])
        GLNG = sb("GLNG", [128, D])
        GLNB = sb("GLNB", [128, D])
        bs = sb("bs", [128, 8])
        Xt = [sb(f"Xt{i}", [128, D]) for i in range(2)]
        OUT = [sb(f"OUT{i}", [128, D]) for i in range(2)]
        T0 = sb("T0", [128, D]); T1 = sb("T1", [128, D]); T2 = sb("T2", [128, D])
        HTt = sb("HTt", [128, 8, 128], BF16)
        HT2 = sb("HT2", [128, 8, 128], BF16)
        Z = sb("Z", [128, 2 * D])
        TZ = sb("TZ", [128, 2 * D])
        TZ2 = sb("TZ2", [128, 2 * D])
        VN = sb("VN", [128, D], BF16)
        US = sb("US", [128, D])
        psZ = [ps(f"psZ{i}", [128, 512]) for i in range(4)]
        mixer_mods(C)
        load_w_bf16(C, Win[:], "Win", win_d, 8, 2 * D)
        load_w_bf16(C, Wout[:], "Wout", wout_d, 8, D)
        S.dma("sp", BIN[:], bin_d.partition_broadcast(128), "ld_bin", writes=["BIN"])
        S.dma("sp", GLNG[:], glng_d.partition_broadcast(128), "ld_glng", writes=["GLNG"])
        S.dma("sp", GLNB[:], glnb_d.partition_broadcast(128), "ld_glnb", writes=["GLNB"])
        S.dma("sp", bs[:], bs_d, "ld_bs", writes=["bs"])
        wst = C.STG[0][:, 0:1024].rearrange("p (g q) -> p g q", g=8)
        S.dma("sp", wst, ws_d.rearrange("g p q -> p g q"), "ld_stg0", writes=["STG0"])
        for g in range(8):
            S.op("pe", lambda e, g=g: e.transpose(out=C.psBig[0][:, g * 128:(g + 1) * 128], in_=wst[:, g, :], identity=C.ident[:]),
                 reads=["STG0", "ident"], writes=["psBig0"])
        S.op("act", lambda e: e.activation(out=wsT[:], in_=C.psBig[0][:].rearrange("p (g n) -> p g n", g=8), func=AF.Copy),
             reads=["psBig0"], writes=["wsT"])
        for t in range(NT):
            s = tiles[t][1]
            xt = Xt[t % 2][:]
            xk = f"Xt{t % 2}"
            S.dma("sp", xt, tiles[t][0], f"ld_x{t % 2}", writes=[xk])
            modulate_T(C, xt, xk, s, T0[:], "T0", C.psBig[0], "psBig0", HTt[:], "HTt")
            proj(C, [(psZ[i], f"psZ{i}") for i in range(4)], HTt, "HTt", Win, "Win", 0, 2 * D)
            for i in range(4):
                S.op("dve", lambda e, i=i: e.tensor_tensor(out=Z[:, i * 512:(i + 1) * 512], in0=psZ[i][:], in1=BIN[:, i * 512:(i + 1) * 512],
                                                           op=ALU.add), reads=[f"psZ{i}", "BIN"], writes=["Z"])
            S.op("act", lambda e: e.activation(out=TZ[:], in_=Z[:], func=AF.Square), reads=["Z"], writes=["TZ"])
            S.op("dve", lambda e: e.tensor_scalar(out=TZ[:], in0=TZ[:], scalar1=0.044715, scalar2=1.0, op0=ALU.mult, op1=ALU.add),
                 reads=["TZ"], writes=["TZ"])
            S.op("pool", lambda e: e.tensor_tensor(out=TZ[:], in0=TZ[:], in1=Z[:], op=ALU.mult), reads=["TZ", "Z"], writes=["TZ"])
            S.op("act", lambda e: e.activation(out=TZ[:], in_=TZ[:], func=AF.Sigmoid, scale=1.5957691216057308), reads=["TZ"], writes=["TZ"])
            S.op("pool", lambda e: e.tensor_tensor(out=TZ2[:], in0=TZ[:], in1=Z[:], op=ALU.mult), reads=["TZ", "Z"], writes=["TZ2"])
            layer_norm_tile(S, TZ2[:, D:2 * D], "TZ2", VN[:], "VN", T2[:], "T2", GLNG[:], "GLNG", GLNB[:], "GLNB", C.small, C.eps1)
            for g in range(8):
                S.op("pe", lambda e, g=g: e.matmul(C.psBig[0][:, g * 128:(g + 1) * 128], lhsT=wsT[:, g, :], rhs=VN[:, g * 128:(g + 1) * 128],
                                                   start=True, stop=True), reads=["wsT", "VN"], writes=["psBig0"])
            for g in range(8):
                S.op("dve", lambda e, g=g: e.scalar_tensor_tensor(out=US[:, g * 128:(g + 1) * 128], in0=C.psBig[0][:, g * 128:(g + 1) * 128],
                                                                  scalar=bs[:, g:g + 1], in1=TZ2[:, g * 128:(g + 1) * 128],
                                                                  op0=ALU.add, op1=ALU.mult), reads=["psBig0", "bs", "TZ2"], writes=["US"])
            to_T(C, US[:], "US", C.psBig[1], "psBig1", HT2[:], "HT2")
            proj(C, [(C.psBig[1][:, 0:512], "psBig1"), (C.psBig[1][:, 512:1024], "psBig1")], HT2, "HT2", Wout, "Wout", 0, D)
            residual_ln_store(C, C.psBig[1], "psBig1", xt, xk, s, T1[:], "T1", T2[:], "T2", OUT[t % 2][:], f"OUT{t % 2}",
                              tiles[t][2], f"st{t % 2}")
        S.emit()
        S.close()


def build_conv():
    nc = bass.Bass("TRN2", target_bir_lowering=False)
    dt, A = std_inputs(nc)
    xin = dt("xin", [20 * 128, D])
    A.update({"valid": dt("valid", [128, 20]), "w_in": dt("w_in", [D, 3 * D]), "w_conv": dt("w_conv", [3, D]), "w_out": dt("w_out", [D, D])})
    xout = dt("xout", [18 * 128, D], "ExternalOutput")
    lat = [(xin[e * 128:(e + 1) * 128, :], 0, e, (xout[(e - 1) * 128:e * 128, :] if 1 <= e <= 16 else None)) for e in range(18)]
    ctx = [(xin[e * 128:(e + 1) * 128, :], 1, e, xout[(e - 2) * 128:(e - 1) * 128, :]) for e in (18, 19)]
    stage_conv(nc, "", [lat, ctx], A, 20)
    return nc


def stage_conv(nc, pfx, seqs, A, nvalid):
    es = ExitStack()
    with es:
        C = mixer_prologue(nc, es, pfx, A)
        S, sb, ps = C.S, C.sb, C.ps
        valid_d, win_d, wconv_d, wout_d = (A[k] for k in ("valid", "w_in", "w_conv", "w_out"))
        Win = sb("Win", [128, 8, 3 * D], BF16)
        Wout = sb("Wout", [128, 8, D], BF16)
        WC = [sb(f"WC{i}", [128, D]) for i in range(3)]
        valid = sb("valid_sb", [128, nvalid])
        Xr = [sb(f"Xr{i}", [128, D]) for i in range(4)]
        Zr = [C.STG[1][:, i * D:(i + 1) * D] for i in range(4)]
        HTr = [sb(f"HTr{i}", [128, 8, 128], BF16) for i in range(4)]
        ZM = sb("ZM", [128, D]); ZP = sb("ZP", [128, D]); TC = sb("TC", [128, D]); TG = sb("TG", [128, D])
        OUT = [sb(f"OUT{i}", [128, D]) for i in range(2)]
        T0 = sb("T0", [128, D]); T1 = sb("T1", [128, D]); T2 = sb("T2", [128, D])
        HT2 = sb("HT2", [128, 8, 128], BF16)
        psP = [ps(f"psP{i}", [128, 512]) for i in range(4)]
        mixer_mods(C)
        load_w_bf16(C, Win[:], "Win", win_d, 8, 3 * D)
        load_w_bf16(C, Wout[:], "Wout", wout_d, 8, D)
        for i in range(3):
            S.dma("sp", WC[i][:], wconv_d[i, :].partition_broadcast(128), f"ld_wc{i}", writes=[f"WC{i}"])
        S.dma("sp", valid[:], valid_d, "ld_valid", writes=["valid"])
        S.op("dve", lambda e: e.memset(ZM[0:1, 0:1], 0.0), writes=["STG1", "Zr0", "Zr1", "Zr2", "Zr3"])

        cnt = {"a": 0, "o": 0}

        def Astep(tile):
            src, s, vcol, _ = tile
            r = cnt["a"] % 4
            cnt["a"] += 1
            xt, xk = Xr[r][:], f"Xr{r}"
            S.dma("sp", xt, src, f"ld_x{r}", writes=[xk])
            modulate_T(C, xt, xk, s, T0[:], "T0", C.psBig[0], "psBig0", HTr[r][:], f"HTr{r}")
            proj(C, [(psP[i], f"psP{i}") for i in range(4)], HTr[r], f"HTr{r}", Win, "Win", D, 2 * D)
            for h in range(2):
                S.op("act", lambda e_, h=h: e_.activation(out=TG[:, h * 512:(h + 1) * 512], in_=psP[h][:], func=AF.Copy, scale=valid[:, vcol:vcol + 1]),
                     reads=[f"psP{h}", "valid"], writes=["TG"])
            for h in range(2):
                S.op("dve", lambda e_, h=h: e_.tensor_tensor(out=Zr[r][:, h * 512:(h + 1) * 512], in0=TG[:, h * 512:(h + 1) * 512], in1=psP[2 + h][:],
                                                            op=ALU.mult), reads=["TG", f"psP{2 + h}"], writes=[f"Zr{r}"])
            return r

        def Bstep(tile, r, prev, nxt):
            _, s, _, out_ap = tile
            ot = cnt["o"]
            cnt["o"] += 1
            xt, xk = Xr[r][:], f"Xr{r}"
            zc, zk = Zr[r], f"Zr{r}"
            if prev is None:
                S.op("dve", lambda e_: e_.memset(ZM[:], 0.0), writes=["ZM"])
            if nxt is None:
                S.op("dve", lambda e_: e_.memset(ZP[:], 0.0), writes=["ZP"])
            S.dma("sp", ZM[1:128, :], zc[0:127, :], "sh_zm", reads=[zk], writes=["ZM"])
            if prev is not None:
                S.dma("sp", ZM[0:1, :], Zr[prev][127:128, :], "sh_zm", reads=[f"Zr{prev}"], writes=["ZM"])
            S.dma("sp", ZP[0:127, :], zc[1:128, :], "sh_zp", reads=[zk], writes=["ZP"])
            if nxt is not None:
                S.dma("sp", ZP[127:128, :], Zr[nxt][0:1, :], "sh_zp", reads=[f"Zr{nxt}"], writes=["ZP"])
            S.op("dve", lambda e_: e_.tensor_tensor(out=ZM[:], in0=ZM[:], in1=WC[0][:], op=ALU.mult), reads=["ZM", "WC0"], writes=["ZM"])
            S.op("pool", lambda e_: e_.tensor_tensor(out=ZP[:], in0=ZP[:], in1=WC[2][:], op=ALU.mult), reads=["ZP", "WC2"], writes=["ZP"])
            S.op("dve", lambda e_: e_.tensor_tensor(out=TC[:], in0=zc, in1=WC[1][:], op=ALU.mult), reads=[zk, "WC1"], writes=["TC"])
            S.op("pool", lambda e_: e_.tensor_tensor(out=TC[:], in0=TC[:], in1=ZM[:], op=ALU.add), reads=["TC", "ZM"], writes=["TC"])
            S.op("dve", lambda e_: e_.tensor_tensor(out=TC[:], in0=TC[:], in1=ZP[:], op=ALU.add), reads=["TC", "ZP"], writes=["TC"])
            proj(C, [(C.psBig[1][:, 0:512], "psBig1"), (C.psBig[1][:, 512:1024], "psBig1")], HTr[r], f"HTr{r}", Win, "Win", 0, D)
            S.op("dve", lambda e_: e_.tensor_tensor(out=TG[:], in0=C.psBig[1][:], in1=TC[:], op=ALU.mult), reads=["psBig1", "TC"], writes=["TG"])
            to_T(C, TG[:], "TG", C.psBig[1], "psBig1", HT2[:], "HT2")
            proj(C, [(C.psBig[1][:, 0:512], "psBig1"), (C.psBig[1][:, 512:1024], "psBig1")], HT2, "HT2", Wout, "Wout", 0, D)
            residual_ln_store(C, C.psBig[1], "psBig1", xt, xk, s, T1[:], "T1", T2[:], "T2", OUT[ot % 2][:], f"OUT{ot % 2}",
                              out_ap, f"st{ot % 2}")

        for seq in seqs:
            slots = {}
            n = len(seq)
            for i in range(n + 1):
                if i < n:
                    slots[i] = Astep(seq[i])
                j = i - 1
                if j >= 0 and seq[j][3] is not None:
                    Bstep(seq[j], slots[j], slots.get(j - 1), slots.get(j + 1) if j + 1 < n else None)
        S.emit()
        S.close()


def build_attn(want_ctx):
    nc = bass.Bass("TRN2", target_bir_lowering=False)
    dt, A = std_inputs(nc)
    NOUT = 18 if want_ctx else 16
    xin = dt("xin", [20 * 128, D])
    A.update({"kbias": dt("kbias", [128, 20]), "cos_t": dt("cos_t", [128, 20 * 64]), "sin_t": dt("sin_t", [128, 20 * 64]),
              "maskp": dt("maskp", [128, 512]), "maskn": dt("maskn", [128, 512]), "w_qkv": dt("w_qkv", [D, 1536]),
              "w_o": dt("w_o", [D, D]), "sink": dt("sink", [16])})
    xout = dt("xout", [NOUT * 128, D], "ExternalOutput")
    kv = [(xin[e * 128:(e + 1) * 128, :], 0 if e < 18 else 1, e, e) for e in range(20)]
    q = [(xin[(o + 1) * 128:(o + 2) * 128, :], 0, o + 1, [(o, "P"), (o + 1, None), (o + 2, "N"), (18, None), (19, None)],
          xout[o * 128:(o + 1) * 128, :]) for o in range(16)]
    if want_ctx:
        q += [(xin[e * 128:(e + 1) * 128, :], 1, e, [(18, None), (19, None)], xout[(e - 2) * 128:(e - 1) * 128, :]) for e in (18, 19)]
    stage_attn(nc, "", kv, q, A, 20)
    return nc


def stage_attn(nc, pfx, kv_tiles, q_tiles, A, ntbl):
    NKT = len(kv_tiles)
    es = ExitStack()
    with es:
        C = mixer_prologue(nc, es, pfx, A)
        S, sb, ps = C.S, C.sb, C.ps
        kbias_d, cos_d, sin_d, maskp_d, maskn_d, wqkv_d, wo_d, sink_d = (A[k] for k in ("kbias", "cos_t", "sin_t", "maskp", "maskn", "w_qkv", "w_o", "sink"))
        Wqkv = sb("Wqkv", [128, 8, 1536], BF16)
        Wo = sb("Wo", [128, 8, D], BF16)
        KT = sb("KT", [128, 2, NKT * 128], BF16)
        V = sb("V", [128, NKT, 256], BF16)
        kbias = sb("kbias_sb", [128, ntbl])
        COS = sb("COS", [128, ntbl, 64])
        SIN = sb("SIN", [128, ntbl, 64])
        MP = sb("MP", [128, 512])
        MN = sb("MN", [128, 512])
        SINKB = sb("SINKB", [128, 2, 512])
        ES = sb("ES", [128, 16])
        identb = sb("identb", [128, 128], BF16)
        onesb = sb("onesb", [128, 64], BF16)
        Xt = [sb(f"Xt{i}", [128, D]) for i in range(2)]
        OUT = [sb(f"OUT{i}", [128, D]) for i in range(2)]
        T0 = sb("T0", [128, D]); T1 = sb("T1", [128, D]); T2 = sb("T2", [128, D])
        HTt = sb("HTt", [128, 8, 128], BF16)
        R1 = sb("R1", [128, D]); R2 = sb("R2", [128, D])
        KR = sb("KR", [128, 256], BF16)
        QRp = sb("QRp", [128, 8, 128], BF16)
        QT = sb("QT", [128, 8, 128], BF16)
        PT = [sb(f"PT{i}", [128, 512], BF16) for i in range(3)]
        DEN = sb("DEN", [128, 512])
        OT = sb("OT", [128, 2, 512], BF16)
        psS = [ps(f"psS{i}", [128, 512]) for i in range(2)]
        psX = ps("psX", [128, 8, 128], BF16)
        psO = C.psBig[1][:, 0:512]
        psD = C.psBig[1][:, 512:1024]
        S.op("pool", lambda e: e.memset(onesb[:], 1.0), writes=["onesb"])
        S.op("pool", lambda e: e.tensor_copy(out=identb[:], in_=C.ident[:]), reads=["ident"], writes=["identb"])
        mixer_mods(C)
        load_w_bf16(C, Wqkv[:], "Wqkv", wqkv_d, 8, 1536)
        for half in range(2):
            i = C.stg_i % 2
            C.stg_i += 1
            st = C.STG[i][:].rearrange("p (c n) -> p c n", c=4)
            for cc in range(4):
                c = half * 4 + cc
                jp, g = c // 4, c % 4
                for r in range(2):
                    row = 512 * jp + 256 * r + 64 * g
                    S.dma("sp", st[r * 64:(r + 1) * 64, cc, :], wo_d[row:row + 64, :], f"ld_stg{i}", writes=[f"STG{i}"])
            S.op("act", lambda e, st=st, half=half: e.activation(out=Wo[:, half * 4:(half + 1) * 4, :], in_=st, func=AF.Copy),
                 reads=[f"STG{i}"], writes=["Wo"])
        S.dma("sp", kbias[:], kbias_d, "ld_kb", writes=["kbias"])
        S.dma("sp", COS[:].rearrange("p t n -> p (t n)"), cos_d, "ld_cos", writes=["COS"])
        S.dma("sp", SIN[:].rearrange("p t n -> p (t n)"), sin_d, "ld_sin", writes=["SIN"])
        S.dma("sp", MP[:], maskp_d, "ld_mp", writes=["MP"])
        S.dma("sp", MN[:], maskn_d, "ld_mn", writes=["MN"])
        S.dma("sp", ES[:], sink_d.partition_broadcast(128), "ld_sink", writes=["ES"])
        S.op("act", lambda e: e.activation(out=ES[:], in_=ES[:], func=AF.Exp), reads=["ES"], writes=["ES"])
        for jp in range(2):
            for g in range(4):
                for r in range(2):
                    h = 8 * jp + 4 * r + g
                    S.op("act", lambda e, jp=jp, g=g, r=r, h=h: e.activation(
                        out=SINKB[r * 64:(r + 1) * 64, jp, g * 128:(g + 1) * 128], in_=C.ones[r * 64:(r + 1) * 64, :], func=AF.Copy,
                        scale=ES[r * 64:(r + 1) * 64, h:h + 1]), reads=["ones", "ES"], writes=["SINKB"])

        T0s = [T0[:], C.STG[0][:, 0:D]]
        R1s = [R1[:], C.STG[0][:, D:2 * D]]
        R2s = [R2[:], C.STG[0][:, 2 * D:3 * D]]
        HTs = [HTt, sb("HTtb", [128, 8, 128], BF16)]
        QRs = [QRp, sb("QRpb", [128, 8, 128], BF16)]
        QTs = [QT, sb("QTb", [128, 8, 128], BF16)]
        S.op("dve", lambda e_: e_.memset(DEN[0:1, 0:1], 0.0), writes=["STG0", "T0_1", "R1_1", "R2_1"])

        def rope(src_ps, pskey, nh, tbl, b):
            n = nh * 64
            xv = src_ps.rearrange("p (h b f i) -> p h b f i", h=nh, b=2, f=2)
            r1v = R1s[b][:, 0:n].rearrange("p (h n) -> p h n", h=nh)
            r2v = R2s[b][:, 0:n].rearrange("p (h b f i) -> p h b f i", h=nh, b=2, f=2)
            cosb = COS[:, tbl, :].unsqueeze(1).to_broadcast([128, nh, 64])
            sv = SIN[:, tbl, :].rearrange("p (b f i) -> p b f i", b=2, f=2)
            S.op("dve", lambda e_: e_.tensor_tensor(out=r1v, in0=src_ps.rearrange("p (h n) -> p h n", h=nh), in1=cosb, op=ALU.mult),
                 reads=[pskey, "COS"], writes=[f"R1_{b}"])
            for f in range(2):
                sb_ = sv[:, :, f, :].unsqueeze(1).to_broadcast([128, nh, 2, 16])
                S.op("dve", lambda e_, f=f, sb_=sb_: e_.tensor_tensor(out=r2v[:, :, :, f, :], in0=xv[:, :, :, 1 - f, :], in1=sb_, op=ALU.mult),
                     reads=[pskey, "SIN"], writes=[f"R2_{b}"])

        def a_front(e):
            src_, s, tbl, kbc = kv_tiles[e]
            b = e % 2
            xt, xk = Xt[b][:], f"Xt{b}"
            S.dma("sp", xt, src_, f"ld_x{b}", writes=[xk])
            modulate_T(C, xt, xk, s, T0s[b], f"T0_{b}", C.psBig[0], "psBig0", HTs[b][:], f"HTt{b}")
            proj(C, [(psS[b], f"psS{b}")], HTs[b], f"HTt{b}", Wqkv, "Wqkv", 1024, 512)

        def a_back(e):
            src_, s, tbl, kbc = kv_tiles[e]
            b = e % 2
            S.op("act", lambda e_: e_.activation(out=V[:, e, :], in_=psS[b][:, 256:512], func=AF.Copy), reads=[f"psS{b}"], writes=["V"])
            rope(psS[b][:, 0:256], f"psS{b}", 4, tbl, b)
            S.op("pool", lambda e_: e_.tensor_tensor(out=KR[:], in0=R1s[b][:, 0:256], in1=R2s[b][:, 0:256], op=ALU.add),
                 reads=[f"R1_{b}", f"R2_{b}"], writes=["KR"])
            for jp in range(2):
                S.op("pe", lambda e_, jp=jp: e_.transpose(out=psX[:, jp, :], in_=KR[:, jp * 128:(jp + 1) * 128], identity=identb[:]),
                     reads=["KR", "identb"], writes=["psX"])
            S.op("act", lambda e_: e_.activation(out=KT[:, :, e * 128:(e + 1) * 128], in_=psX[:, 0:2, :], func=AF.Copy),
                 reads=["psX"], writes=["KT"])

        for e in range(NKT):
            a_front(e)
            if e >= 1:
                a_back(e - 1)
        a_back(NKT - 1)

        pti_box = [0]

        def b_front(i):
            src_, s, tbl, chunks_, out_ap = q_tiles[i]
            b = i % 2
            xt, xk = Xt[b][:], f"Xt{b}"
            S.dma("sp", xt, src_, f"ld_x{b}", writes=[xk])
            modulate_T(C, xt, xk, s, T0s[b], f"T0_{b}", C.psBig[0], "psBig0", HTs[b][:], f"HTt{b}")
            proj(C, [(C.psBig[0][:, 0:512], "psBig0"), (C.psBig[0][:, 512:1024], "psBig0")], HTs[b], f"HTt{b}", Wqkv, "Wqkv", 0, D)
            rope(C.psBig[0][:], "psBig0", 16, tbl, b)
            for jp in range(2):
                a_ = R1s[b][:, jp * 512:(jp + 1) * 512].rearrange("p (r g d) -> p r g d", r=2, g=4)
                b_ = R2s[b][:, jp * 512:(jp + 1) * 512].rearrange("p (r g d) -> p r g d", r=2, g=4)
                o_ = QRs[b][:, jp * 4:(jp + 1) * 4, :].rearrange("p g (r d) -> p r g d", r=2)
                S.op("pool", lambda e_, a_=a_, b_=b_, o_=o_: e_.tensor_tensor(out=o_, in0=a_, in1=b_, op=ALU.add),
                     reads=[f"R1_{b}", f"R2_{b}"], writes=[f"QRp{b}"])
            for c in range(8):
                S.op("pe", lambda e_, c=c: e_.transpose(out=psX[:, c, :], in_=QRs[b][:, c, :], identity=identb[:]),
                     reads=[f"QRp{b}", "identb"], writes=["psX"])
            S.op("act", lambda e_: e_.activation(out=QTs[b][:], in_=psX[:], func=AF.Copy), reads=["psX"], writes=[f"QT{b}"])

        def b_rest(i):
            src_, s, tbl, chunks_, out_ap = q_tiles[i]
            b = i % 2
            o = i
            xt, xk = Xt[b][:], f"Xt{b}"
            QTc, qtk = QTs[b], f"QT{b}"
            chunks = [(kt, {"P": MP, "N": MN, None: None}[m]) for (kt, m) in chunks_]
            steps = []
            for jp in range(2):
                for r in range(2):
                    for ci, (kt, mask) in enumerate(chunks):
                        steps.append((jp, r, ci, kt, mask))
            bufs = {}

            def score(n):
                jp, r, ci, kt, mask = steps[n]
                lo, hi = r * 64, (r + 1) * 64
                kbc = kv_tiles[kt][3]
                pti = pti_box[0]
                pti_box[0] += 1
                pss, psk = psS[pti % 2], f"psS{pti % 2}"
                pt, ptk = PT[pti % 3], f"PT{pti % 3}"
                bufs[n] = (pt, ptk)
                S.op("pe", lambda e_: e_.matmul(pss[:], lhsT=KT[lo:hi, jp, kt * 128:(kt + 1) * 128], rhs=QTc[lo:hi, jp * 4:(jp + 1) * 4, :],
                                                start=True, stop=True), reads=["KT", qtk], writes=[psk])
                S.op("act", lambda e_: e_.activation(out=pt[:], in_=pss[:], func=AF.Exp, bias=kbias[:, kbc:kbc + 1], scale=0.125),
                     reads=[psk, "kbias"], writes=[ptk])
                if mask is not None:
                    S.op("dve", lambda e_: e_.tensor_tensor(out=pt[:], in0=pt[:], in1=mask[:], op=ALU.mult), reads=[ptk, "MP", "MN"], writes=[ptk])

            def pv(n):
                jp, r, ci, kt, mask = steps[n]
                lo, hi = r * 64, (r + 1) * 64
                j = 2 * jp + r
                pt, ptk = bufs.pop(n)
                first, last = (ci == 0), (ci == len(chunks) - 1)
                S.op("pe", lambda e_: e_.matmul(psO[lo:hi, :], lhsT=V[:, kt, j * 64:(j + 1) * 64], rhs=pt[:], start=first, stop=last),
                     reads=["V", ptk], writes=["psBig1"])
                S.op("pe", lambda e_: e_.matmul(psD[lo:hi, :], lhsT=onesb[:, 0:64], rhs=pt[:], start=first, stop=last),
                     reads=["onesb", ptk], writes=["psBig1"])
                if r == 1 and last:
                    S.op("dve", lambda e_: e_.tensor_tensor(out=DEN[:], in0=psD, in1=SINKB[:, jp, :], op=ALU.add), reads=["psBig1", "SINKB"], writes=["DEN"])
                    S.op("dve", lambda e_: e_.reciprocal(out=DEN[:], in_=DEN[:]), reads=["DEN"], writes=["DEN"])
                    S.op("dve", lambda e_: e_.tensor_tensor(out=OT[:, jp, :], in0=psO, in1=DEN[:], op=ALU.mult), reads=["psBig1", "DEN"], writes=["OT"])

            score(0)
            for n in range(len(steps)):
                if n + 1 < len(steps):
                    score(n + 1)
                pv(n)
            for half in range(2):
                for c in range(8):
                    jp, g = c // 4, c % 4
                    S.op("pe", lambda e_, half=half, c=c, jp=jp, g=g: e_.matmul(
                        C.psBig[0][:, half * 512:(half + 1) * 512], lhsT=OT[:, jp, g * 128:(g + 1) * 128], rhs=Wo[:, c, half * 512:(half + 1) * 512],
                        start=(c == 0), stop=(c == 7)), reads=["OT", "Wo"], writes=["psBig0"])
            residual_ln_store(C, C.psBig[0], "psBig0", xt, xk, s, T1[:], "T1", T2[:], "T2", OUT[o % 2][:], f"OUT{o % 2}",
                              out_ap, f"st{o % 2}")

        nq = len(q_tiles)
        b_front(0)
        for i in range(nq):
            if i + 1 < nq:
                b_front(i + 1)
            b_rest(i)
        S.emit()
        S.close()


NWIN = 22
NEXT = 20


def build_fused(nstage=8, dbg=False):
    nc = bass.Bass("TRN2", target_bir_lowering=False)
    dt = lambda n, s, k="ExternalInput": nc.dram_tensor(n, s, F32, kind=k).ap()
    xw = dt("xw", [NWIN * 128, D])
    ctx = dt("ctx", [256, D])
    cvec = dt("cvec", [2, D])
    w_mod = dt("w_mod", [4, D, 6 * D]); b_mod = dt("b_mod", [4, 6 * D])
    ln1_g = dt("ln1_g", [4, D]); ln1_b = dt("ln1_b", [4, D]); ln2_g = dt("ln2_g", [4, D]); ln2_b = dt("ln2_b", [4, D])
    rw = dt("router_w", [D, NE]); rb = dt("router_bias", [NE])
    w1 = dt("moe_w1", [4, NE, D, DEXP]); w3 = dt("moe_w3", [4, NE, D, DEXP]); w2 = dt("moe_w2", [4, NE, DEXP, D])
    a_w_qkv = dt("a_w_qkv", [2, D, 1536]); a_w_o = dt("a_w_o", [2, D, D]); a_sink = dt("a_sink", [2, 16])
    b_w_in = dt("b_w_in", [1, D, 2 * D]); b_b_in = dt("b_b_in", [1, 2 * D]); b_ln_g = dt("b_ln_g", [1, D]); b_ln_b = dt("b_ln_b", [1, D])
    b_w_s = dt("b_w_s", [1, 8, 128, 128]); b_b_s = dt("b_b_s", [1, 128, 8]); b_w_out = dt("b_w_out", [1, D, D])
    c_w_in = dt("c_w_in", [1, D, 3 * D]); c_w_conv = dt("c_w_conv", [1, 3, D]); c_w_out = dt("c_w_out", [1, D, D])
    kbias = dt("kbias", [128, 24]); cos_t = dt("cos_t", [128, 24 * 64]); sin_t = dt("sin_t", [128, 24 * 64])
    maskp = dt("maskp", [128, 512]); maskn = dt("maskn", [128, 512]); valid = dt("valid", [128, 22])
    SA = dt("scrA", [22 * 128, D], "ExternalOutput" if dbg else "Internal")
    SB = dt("scrB", [22 * 128, D], "ExternalOutput" if dbg else "Internal")
    xout = dt("xout", [2048, D], "ExternalOutput")
    row = lambda T, i: T[i * 128:(i + 1) * 128, :]

    def AM(L):
        return {"cvec": cvec, "wmod": w_mod[L][:, 3 * D:6 * D], "bmod": b_mod[L][3 * D:6 * D], "lng": ln2_g[L], "lnb": ln2_b[L],
                "rw": rw, "rb": rb, "w1": w1[L], "w3": w3[L], "w2": w2[L]}

    def AX_(L):
        return {"cvec": cvec, "wmod": w_mod[L][:, 0:3 * D], "bmod": b_mod[L][0:3 * D], "lng": ln1_g[L], "lnb": ln1_b[L]}

    def moe(L, pfx, tiles):
        h = (len(tiles) + 1) // 2 if len(tiles) > 18 else len(tiles)
        stage_moe(nc, pfx + "a_", tiles[:h], AM(L))
        if h < len(tiles):
            stage_moe(nc, pfx + "b_", tiles[h:], AM(L))

    attn_tabs = {"kbias": kbias, "cos_t": cos_t, "sin_t": sin_t, "maskp": maskp, "maskn": maskn}
    kv = [(row(xw, v), 0, v, v) for v in range(NWIN)] + [(row(ctx, c), 1, 22 + c, 22 + c) for c in range(2)]
    q = [(row(xw, u + 1), 0, u + 1, [(u, "P"), (u + 1, None), (u + 2, "N"), (22, None), (23, None)], row(SA, u)) for u in range(NEXT)]
    q += [(row(ctx, c), 1, 22 + c, [(22, None), (23, None)], row(SA, 20 + c)) for c in range(2)]
    A = AX_(0); A.update(attn_tabs); A.update({"w_qkv": a_w_qkv[0], "w_o": a_w_o[0], "sink": a_sink[0]})
    stage_attn(nc, "s0_", kv, q, A, 24)
    if nstage <= 1:
        return nc
    allt = lambda Tsrc, Tdst: [(row(Tsrc, u), 0 if u < NEXT else 1, row(Tdst, u)) for u in range(22)]
    moe(0, "m0", allt(SA, SB))
    if nstage <= 2:
        return nc
    A = AX_(1); A.update({"w_in": b_w_in[0], "b_in": b_b_in[0], "g_ln_g": b_ln_g[0], "g_ln_b": b_ln_b[0], "w_s": b_w_s[0], "b_s": b_b_s[0],
                          "w_out": b_w_out[0]})
    stage_gmlp(nc, "s1_", allt(SB, SA), A)
    if nstage <= 3:
        return nc
    moe(1, "m1", allt(SA, SB))
    if nstage <= 4:
        return nc
    lat = [(row(SB, u), 0, u, (row(SA, u) if 1 <= u <= 18 else None)) for u in range(NEXT)]
    cx = [(row(SB, 20 + c), 1, 20 + c, row(SA, 20 + c)) for c in range(2)]
    A = AX_(2); A.update({"valid": valid, "w_in": c_w_in[0], "w_conv": c_w_conv[0], "w_out": c_w_out[0]})
    stage_conv(nc, "s2_", [lat, cx], A, 22)
    if nstage <= 5:
        return nc
    t2 = [(row(SA, u), 0, row(SB, u)) for u in range(1, 19)] + [(row(SA, 20 + c), 1, row(SB, 20 + c)) for c in range(2)]
    moe(2, "m2", t2)
    if nstage <= 6:
        return nc
    kv = [(row(SB, u), 0, u + 1, u + 1) for u in range(1, 19)] + [(row(SB, 20 + c), 1, 22 + c, 22 + c) for c in range(2)]
    q = [(row(SB, u), 0, u + 1, [(u - 2, "P"), (u - 1, None), (u, "N"), (18, None), (19, None)], row(SA, u)) for u in range(2, 18)]
    A = AX_(3); A.update(attn_tabs); A.update({"w_qkv": a_w_qkv[1], "w_o": a_w_o[1], "sink": a_sink[1]})
    stage_attn(nc, "s3_", kv, q, A, 24)
    moe(3, "m3", [(row(SA, u), 0, row(xout, u - 2)) for u in range(2, 18)])
    return nc


_NC = []


def _tables(core):
    L = 16384
    freqs = np.power(np.float32(10000.0), -np.arange(16, dtype=np.float32) / np.float32(16)).astype(np.float32)
    pos = (core * 2048 - 384 + np.arange(NWIN * 128)).astype(np.float32)
    row = np.floor(pos / 64.0).astype(np.float32)
    col = (pos - row * 64).astype(np.float32)
    ar = row[:, None] * freqs[None, :]
    ac = col[:, None] * freqs[None, :]
    ang = np.concatenate([ar, ar, ac, ac], -1)
    cos = np.cos(ang).astype(np.float32)
    sin = np.sin(ang).astype(np.float32)
    sgn = np.tile(np.concatenate([-np.ones(16, np.float32), np.ones(16, np.float32)]), 2)
    sinS = sin * sgn[None, :]
    cos = np.concatenate([cos, np.ones((256, 64), np.float32)], 0)
    sinS = np.concatenate([sinS, np.zeros((256, 64), np.float32)], 0)
    cos_t = np.ascontiguousarray(cos.reshape(24, 128, 64).transpose(1, 0, 2).reshape(128, 24 * 64))
    sin_t = np.ascontiguousarray(sinS.reshape(24, 128, 64).transpose(1, 0, 2).reshape(128, 24 * 64))
    kb = np.zeros((128, 24), np.float32)
    for v in range(NWIN):
        st = core * 2048 - 384 + 128 * v
        if st < 0 or st >= L:
            kb[:, v] = -30000.0
    valid = np.ones((128, 22), np.float32)
    for u in range(NEXT):
        st = core * 2048 - 256 + 128 * u
        if st < 0 or st >= L:
            valid[:, u] = 0.0
    return cos_t, sin_t, kb, valid


def kernel(x, c, ctx, c_ctx, w_mod, b_mod, ln1_g, ln1_b, ln2_g, ln2_b, router_w, router_bias, moe_w1, moe_w3, moe_w2,
           a_w_qkv, a_w_o, a_sink, b_w_in, b_b_in, b_ln_g, b_ln_b, b_w_s, b_b_s, b_w_out, c_w_in, c_w_conv, c_w_out):
    f32 = lambda a: np.ascontiguousarray(np.asarray(a, dtype=np.float32))
    if not _NC:
        _NC.append(build_fused())
    nc = _NC[0]
    xc = f32(x)[0]
    zpad = np.zeros((384, D), np.float32)
    xp = np.concatenate([zpad, xc, zpad], 0)
    kk = np.arange(128)[:, None]
    qq = np.arange(128)[None, :]
    com = {"ctx": f32(ctx)[0], "cvec": np.stack([f32(c)[0], f32(c_ctx)]).astype(np.float32),
           "w_mod": f32(w_mod), "b_mod": f32(b_mod), "ln1_g": f32(ln1_g), "ln1_b": f32(ln1_b), "ln2_g": f32(ln2_g), "ln2_b": f32(ln2_b),
           "router_w": f32(router_w), "router_bias": f32(router_bias), "moe_w1": f32(moe_w1), "moe_w3": f32(moe_w3), "moe_w2": f32(moe_w2),
           "a_w_qkv": f32(a_w_qkv), "a_w_o": f32(a_w_o), "a_sink": f32(a_sink), "b_w_in": f32(b_w_in), "b_b_in": f32(b_b_in),
           "b_ln_g": f32(b_ln_g), "b_ln_b": f32(b_ln_b), "b_w_s": f32(b_w_s), "b_b_s": f32(b_b_s), "b_w_out": f32(b_w_out),
           "c_w_in": f32(c_w_in), "c_w_conv": f32(c_w_conv), "c_w_out": f32(c_w_out),
           "maskp": np.tile((kk >= qq).astype(np.float32), (1, 4)), "maskn": np.tile((kk <= qq).astype(np.float32), (1, 4))}
    in_maps = []
    for core in range(8):
        cos_t, sin_t, kb, valid = _tables(core)
        in_maps.append(dict(com, xw=np.ascontiguousarray(xp[core * 2048:core * 2048 + NWIN * 128]), cos_t=cos_t, sin_t=sin_t, kbias=kb, valid=valid))
    res = run_bass_kernel_spmd(nc, in_maps, core_ids=list(range(8)))
    out = np.concatenate([r["xout"] for r in res.results], 0)
    return out[None].astype(np.float32)
```

```python
import numpy as np
from contextlib import ExitStack
import concourse.bass as bass
import concourse.mybir as mybir
from concourse.bass_utils import run_bass_kernel_spmd

F32 = mybir.dt.float32
BF16 = mybir.dt.bfloat16
AF = mybir.ActivationFunctionType
ALU = mybir.AluOpType
AX = mybir.AxisListType


SAME_ENGINE_INORDER = False


class Sched:
    ENG = ("pe", "act", "dve", "pool", "sp")

    def __init__(self, nc, es, pfx=""):
        self.nc = nc
        self.es = es
        self.pfx = pfx
        self.q = {e: [] for e in self.ENG}
        self.semh = {e: nc.alloc_semaphore(name=pfx + "s_" + e) for e in self.ENG}
        self.cnt = {e: 0 for e in self.ENG}
        self.waited = {e: {} for e in self.ENG}
        self.lastw = {}
        self.readers = {}
        self.dcnt = {}
        self.store_sems = set()

    def _deps(self, eng, reads, writes):
        need = {}
        for k in reads:
            if k in self.lastw:
                s, v = self.lastw[k]
                need[s] = max(need.get(s, 0), v)
        for k in writes:
            if k in self.lastw:
                s, v = self.lastw[k]
                need[s] = max(need.get(s, 0), v)
            for (s, v) in self.readers.get(k, ()):
                need[s] = max(need.get(s, 0), v)
        for s, v in need.items():
            if s == eng and (eng == "pe" or SAME_ENGINE_INORDER):
                continue
            if self.waited[eng].get(s, 0) < v:
                self.q[eng].append(("wait", s, v))
                self.waited[eng][s] = v

    def op(self, eng, fn, reads=(), writes=()):
        psr = [k for k in reads if isinstance(k, str) and k.startswith("ps")]
        if psr:
            reads = [k for k in reads if k not in psr]
            writes = list(writes) + psr
        self._deps(eng, reads, writes)
        self.cnt[eng] += 1
        v = self.cnt[eng]
        self.q[eng].append(("op", fn))
        for k in reads:
            self.readers.setdefault(k, []).append((eng, v))
        for k in writes:
            self.lastw[k] = (eng, v)
            self.readers[k] = []

    def dma(self, queue, out, in_, sem, reads=(), writes=(), store=False, **kw):
        if sem not in self.semh:
            self.semh[sem] = self.nc.alloc_semaphore(name=self.pfx + "d_" + sem)
            self.dcnt[sem] = 0
        self._deps(queue, reads, writes)
        self.dcnt[sem] += 16
        v = self.dcnt[sem]
        self.q[queue].append(("dma", out, in_, sem, kw))
        for k in reads:
            self.readers.setdefault(k, []).append((sem, v))
        for k in writes:
            self.lastw[k] = (sem, v)
            self.readers[k] = []
        if store:
            self.store_sems.add(sem)

    def finish(self):
        for s in sorted(self.store_sems):
            self.q["sp"].append(("wait", s, self.dcnt[s]))

    def close(self):
        self.nc.clear_and_free_semaphores(list(self.semh.values()))
        self.nc.all_engine_barrier()

    def emit(self):
        nc = self.nc
        self.finish()

        def replay(e, engobj):
            for it in self.q[e]:
                if it[0] == "wait":
                    engobj.wait_ge(self.semh[it[1]], it[2])
                elif it[0] == "op":
                    it[1](engobj).then_inc(self.semh[e], 1)
                else:
                    _, out, in_, sem, kw = it
                    engobj.dma_start(out=out, in_=in_, **kw).then_inc(self.semh[sem], 16)

        with nc.Block() as block:
            @block.tensor
            def _(e):
                replay("pe", e)

            @block.scalar
            def _(e):
                replay("act", e)

            @block.vector
            def _(e):
                replay("dve", e)

            @block.gpsimd
            def _(e):
                replay("pool", e)

            @block.sync
            def _(e):
                replay("sp", e)


D = 1024
NE = 16
DEXP = 512
ALPHA = 8.0 ** 0.25
LN_EPS = 1e-5
EPS2 = LN_EPS / (ALPHA * ALPHA)


def common_consts(S, nc, sb, ps):
    ident = sb("ident", [128, 128])
    ones = sb("ones", [128, 128])
    S.op("pool", lambda e: e.memset(ones[:], 1.0), writes=["ones"])
    S.op("pool", lambda e: e.memset(ident[:], 0.0), writes=["ident"])
    S.op("pool", lambda e: e.affine_select(out=ident[:], in_=ident[:], pattern=[[-1, 128]], compare_op=ALU.not_equal,
                                          fill=1.0, base=0, channel_multiplier=1), reads=["ident"], writes=["ident"])
    return ident, ones


def mod_tiles(S, nc, sb, cvec, wmod, bmod, ident, ones, psbig, pbk, stage, stage_keys, SLC, slc_keys, outs):
    ct = sb("ct", [16, 128])
    csil = sb("csil", [128, 16])
    brow = sb("brow", [1, 512])
    S.dma("sp", ct[:], cvec.rearrange("s (k p) -> (s k) p", p=128), "ld_ct", writes=["ct"])
    S.op("pe", lambda e: e.transpose(out=psbig[:, 0:16], in_=ct[0:16, :], identity=ident[0:16, 0:16]),
         reads=["ct", "ident"], writes=[pbk])
    S.op("act", lambda e: e.activation(out=csil[:], in_=psbig[:, 0:16], func=AF.Silu), reads=[pbk], writes=["csil"])
    for s in range(2):
        for k in range(8):
            S.op("act", lambda e, s=s, k=k: e.activation(out=SLC[:, s, k, :], in_=ones[:], func=AF.Copy,
                                                         scale=csil[:, s * 8 + k:s * 8 + k + 1]),
                 reads=["ones", "csil"], writes=slc_keys)
    nv = max(o[0] for o in outs) + 1
    stages = stage if isinstance(stage, list) else [stage]
    skeys = stage_keys if isinstance(stage, list) else [stage_keys]
    brows = [brow, sb("brow2", [1, 512])] if isinstance(stage, list) else [brow, brow]
    idx = 0
    for v in range(nv):
        for h in range(2):
            c0 = v * 1024 + h * 512
            stg = stages[idx % len(stages)]
            sk = skeys[idx % len(stages)]
            br = brows[idx % 2]
            brk = f"brow{idx % 2}" if isinstance(stage, list) else "brow0"
            S.dma("sp", stg, wmod[:, c0:c0 + 512].rearrange("(k p) n -> p k n", p=128), f"ld_stage{idx % len(stages)}", writes=sk)
            S.dma("sp", br[:], bmod[c0:c0 + 512].rearrange("(a n) -> a n", a=1), (f"ld_brow{idx % 2}" if isinstance(stage, list) else "ld_brow0"), writes=[brk])
            idx += 1
            for s in range(2):
                mine = [o for o in outs if o[0] == v and o[1] == s]
                if not mine:
                    continue
                for k in range(8):
                    S.op("pe", lambda e, s=s, k=k, stg=stg: e.matmul(psbig[:, 0:512], lhsT=SLC[:, s, k, :], rhs=stg[:, k, :],
                                                                     start=(k == 0), stop=False),
                         reads=slc_keys + sk, writes=[pbk])
                S.op("pe", lambda e, br=br: e.matmul(psbig[:, 0:512], lhsT=ones[0:1, :], rhs=br[0:1, :], start=False, stop=True),
                     reads=["ones", brk], writes=[pbk])
                for (_, _, tile, key, kind) in mine:
                    dst = tile[:, h * 512:(h + 1) * 512]
                    if kind == "plain":
                        S.op("dve", lambda e, dst=dst: e.tensor_copy(out=dst, in_=psbig[:, 0:512]), reads=[pbk], writes=[key])
                    elif kind == "plus1":
                        S.op("dve", lambda e, dst=dst: e.tensor_scalar(out=dst, in0=psbig[:, 0:512], scalar1=1.0, scalar2=None,
                                                                       op0=ALU.add), reads=[pbk], writes=[key])
                    else:
                        S.op("dve", lambda e, dst=dst: e.tensor_scalar(out=dst, in0=psbig[:, 0:512], scalar1=1.0 / ALPHA,
                                                                       scalar2=None, op0=ALU.mult), reads=[pbk], writes=[key])


def layer_norm_tile(S, src, src_key, dst, dst_key, tmp, tmp_key, lng, lng_key, lnb, lnb_key, small, eps_t):
    st, mv, rstd, nmr = small
    for h in range(2):
        S.op("dve", lambda e, h=h: e.bn_stats(out=st[:, h, :], in_=src[:, h * 512:(h + 1) * 512]), reads=[src_key], writes=["ln_st"])
    S.op("dve", lambda e: e.bn_aggr(out=mv[:], in_=st[:].rearrange("p a b -> p (a b)")), reads=["ln_st"], writes=["ln_mv"])
    S.op("act", lambda e: e.activation(out=rstd[:], in_=mv[:, 1:2], func=AF.Sqrt, bias=eps_t[:], scale=1.0),
         reads=["ln_mv", "eps"], writes=["ln_rstd"])
    S.op("dve", lambda e: e.reciprocal(out=rstd[:], in_=rstd[:]), reads=["ln_rstd"], writes=["ln_rstd"])
    S.op("dve", lambda e: e.scalar_tensor_tensor(out=nmr[:], in0=mv[:, 0:1], scalar=-1.0, in1=rstd[:], op0=ALU.mult, op1=ALU.mult),
         reads=["ln_mv", "ln_rstd"], writes=["ln_nmr"])
    S.op("act", lambda e: e.activation(out=tmp, in_=src, func=AF.Identity, bias=nmr[:], scale=rstd[:]),
         reads=[src_key, "ln_nmr", "ln_rstd"], writes=[tmp_key])
    S.op("dve", lambda e: e.tensor_tensor(out=tmp, in0=tmp, in1=lng, op=ALU.mult), reads=[tmp_key, lng_key], writes=[tmp_key])
    S.op("dve", lambda e: e.tensor_tensor(out=dst, in0=tmp, in1=lnb, op=ALU.add), reads=[tmp_key, lnb_key], writes=[dst_key])


def build_moe():
    NT = 18
    nc = bass.Bass("TRN2", target_bir_lowering=False)
    dt = lambda n, s, k: nc.dram_tensor(n, s, F32, kind=k).ap()
    xin = dt("xin", [NT * 128, D], "ExternalInput")
    A = {"cvec": dt("cvec", [2, D], "ExternalInput"), "wmod": dt("wmod", [D, 3 * D], "ExternalInput"),
         "bmod": dt("bmod", [3 * D], "ExternalInput"), "lng": dt("lng", [D], "ExternalInput"), "lnb": dt("lnb", [D], "ExternalInput"),
         "rw": dt("rw", [D, NE], "ExternalInput"), "rb": dt("rb", [NE], "ExternalInput"),
         "w1": dt("w1", [NE, D, DEXP], "ExternalInput"), "w3": dt("w3", [NE, D, DEXP], "ExternalInput"),
         "w2": dt("w2", [NE, DEXP, D], "ExternalInput")}
    xout = dt("xout", [NT * 128, D], "ExternalOutput")
    tiles = [(xin[t * 128:(t + 1) * 128, :], 0 if t < 16 else 1, xout[t * 128:(t + 1) * 128, :]) for t in range(NT)]
    stage_moe(nc, "", tiles, A)
    return nc


def stage_moe(nc, pfx, tiles, A):
    NT = len(tiles)
    cvec, wmod, bmod, lng_d, lnb_d, rw_d, rb_d, w1_d, w3_d, w2_d = (A[k] for k in ("cvec", "wmod", "bmod", "lng", "lnb", "rw", "rb", "w1", "w3", "w2"))
    es = ExitStack()
    with es:
        S = Sched(nc, es, pfx)
        sb = lambda n, s, d=F32: es.enter_context(nc.sbuf_tensor(pfx + n, s, d))
        ps = lambda n, s, d=F32: es.enter_context(nc.psum_tensor(pfx + n, s, d))
        X = sb("X", [128, NT, D])
        HT = sb("HT", [128, 8, NT * 128], BF16)
        W13 = [sb(f"W13_{i}", [128, 2, 8, 256], BF16) for i in range(2)]
        W2 = [sb(f"W2_{i}", [128, 2, D], BF16) for i in range(2)]
        has_ctx = any(tl[1] == 1 for tl in tiles)
        W2c = [sb(f"W2c_{i}", [128, 2, D], BF16) for i in range(2)] if has_ctx else None
        STG = sb("STG", [128, 6 * D])
        MOD = {("sc1", 0): STG[:, 0:D], ("sh", 0): STG[:, D:2 * D], ("sc1", 1): STG[:, 2 * D:3 * D], ("sh", 1): STG[:, 3 * D:4 * D]}
        MKEY = {("sc1", 0): "STG0", ("sh", 0): "STG1", ("sc1", 1): "STG2", ("sh", 1): "STG3"}
        for s_ in range(2):
            MOD[("gf", s_)] = sb(f"Mgf{s_}", [128, D])[:]
            MKEY[("gf", s_)] = f"Mgf{s_}"
        S13 = STG[:, 0:4 * D].rearrange("p (a k n) -> p a k n", a=2, k=8)
        S2 = STG[:, 4 * D:6 * D].rearrange("p (k n) -> p k n", k=2)
        T = [sb(f"T{i}", [128, D]) for i in range(2)]
        SA = sb("SA", [128, 2, 512])
        H1 = [sb(f"H1_{i}", [128, 2, 512], BF16) for i in range(2)]
        rw = sb("rwt", [128, 8, NE])
        rb = sb("rbt", [128, NE])
        GATE = sb("GATE", [128, NT, NE])
        eps_t = sb("eps_t", [128, 1])
        small = (sb("ln_st", [128, 2, 6]), sb("ln_mv", [128, 2]), sb("ln_rstd", [128, 1]), sb("ln_nmr", [128, 1]))
        rt = {n: sb("rt_" + n, [128, NT * 16]) for n in ("s", "ssel", "sm", "sel", "sc")}
        rg = {n: sb("rg_" + n, [128, NT * 4]) for n in ("p01", "q01", "p23", "q23", "t1", "m2", "m3", "gs", "ing", "pen")}
        r1 = {n: sb("r1_" + n, [128, NT]) for n in ("gmax", "den", "m1", "m2")}
        psY = [ps(f"psY{i}", [128, D]) for i in range(2)]
        psA = [ps(f"psA{i}", [128, 512]) for i in range(2)]
        psB = [ps(f"psB{i}", [128, 512]) for i in range(2)]

        ident, ones = common_consts(S, nc, sb, ps)
        S.op("dve", lambda e: e.memset(eps_t[:], EPS2), writes=["eps"])
        SLC = X[:, 0:2, :].rearrange("p s (k n) -> p s k n", k=8)
        stage = [X[:, 2:6, :].rearrange("p a (b n) -> p (a b) n", b=2), X[:, 6:10, :].rearrange("p a (b n) -> p (a b) n", b=2)]
        outs = []
        for s in sorted(set(tl[1] for tl in tiles)):
            outs.append((0, s, MOD[("sh", s)], MKEY[("sh", s)], "plain"))
            outs.append((1, s, MOD[("sc1", s)], MKEY[("sc1", s)], "plus1"))
            outs.append((2, s, MOD[("gf", s)], MKEY[("gf", s)], "invalpha"))
        mod_tiles(S, nc, sb, cvec, wmod, bmod, ident, ones, psY[0], "psY0", stage, [[("X", t) for t in range(2, 6)], [("X", t) for t in range(6, 10)]],
                  SLC, [("X", 0), ("X", 1)], outs)
        S.dma("sp", rw[:], rw_d.rearrange("(k p) e -> p k e", p=128), "ld_rw", writes=["rw"])
        S.dma("sp", rb[:], rb_d.partition_broadcast(128), "ld_rb", writes=["rb"])

        for t in range(NT):
            s = tiles[t][1]
            xk = ("X", t)
            S.dma("sp", X[:, t, :], tiles[t][0], f"ld_x{t}", writes=[xk])
            S.op("dve", lambda e, t=t, s=s: e.tensor_tensor(out=T[0][:], in0=X[:, t, :], in1=MOD[("sc1", s)], op=ALU.mult),
                 reads=[xk, MKEY[("sc1", s)]], writes=["T0"])
            S.op("dve", lambda e, s=s: e.tensor_tensor(out=T[0][:], in0=T[0][:], in1=MOD[("sh", s)], op=ALU.add),
                 reads=["T0", MKEY[("sh", s)]], writes=["T0"])
            pt = psY[t % 2]
            ptk = f"psY{t % 2}"
            for k in range(8):
                S.op("pe", lambda e, k=k, pt=pt: e.transpose(out=pt[:, k * 128:(k + 1) * 128], in_=T[0][:, k * 128:(k + 1) * 128],
                                                             identity=ident[:]), reads=["T0", "ident"], writes=[ptk])
            S.op("act", lambda e, t=t, pt=pt: e.activation(out=HT[:, :, t * 128:(t + 1) * 128],
                                                           in_=pt[:].rearrange("p (k n) -> p k n", k=8), func=AF.Copy),
                 reads=[ptk], writes=[("HT", t)])
            S.op("dve", lambda e, pt=pt: e.tensor_copy(out=T[1][:], in_=pt[:]), reads=[ptk], writes=["T1"])
            pr = psA[t % 2]
            prk = f"psA{t % 2}"
            for k in range(8):
                S.op("pe", lambda e, k=k, pr=pr: e.matmul(pr[:, 0:NE], lhsT=T[1][:, k * 128:(k + 1) * 128], rhs=rw[:, k, :],
                                                          start=(k == 0), stop=(k == 7)), reads=["T1", "rw"], writes=[prk])
            S.op("act", lambda e, pr=pr, t=t: e.activation(out=rt["s"][:, t * 16:(t + 1) * 16], in_=pr[:, 0:NE], func=AF.Sigmoid),
                 reads=[prk], writes=["rt_s"])

        dv = lambda fn, r, w: S.op("dve", fn, reads=r, writes=w)
        G = NT * 4
        t3 = lambda ap: ap.rearrange("p (t e) -> p t e", t=NT)
        dv(lambda e: e.tensor_tensor(out=t3(rt["ssel"][:]), in0=t3(rt["s"][:]), in1=rb[:].unsqueeze(1).to_broadcast([128, NT, 16]), op=ALU.add),
           ["rt_s", "rb"], ["rt_ssel"])
        sv = rt["ssel"][:].rearrange("p (g j) -> p g j", j=4)
        a, b, c, d = (sv[:, :, j] for j in range(4))
        dv(lambda e: e.tensor_tensor(out=rg["p01"][:], in0=a, in1=b, op=ALU.max), ["rt_ssel"], ["p01"])
        dv(lambda e: e.tensor_tensor(out=rg["q01"][:], in0=a, in1=b, op=ALU.min), ["rt_ssel"], ["q01"])
        dv(lambda e: e.tensor_tensor(out=rg["p23"][:], in0=c, in1=d, op=ALU.max), ["rt_ssel"], ["p23"])
        dv(lambda e: e.tensor_tensor(out=rg["q23"][:], in0=c, in1=d, op=ALU.min), ["rt_ssel"], ["q23"])
        dv(lambda e: e.tensor_tensor(out=rg["t1"][:], in0=rg["p01"][:], in1=rg["p23"][:], op=ALU.max), ["p01", "p23"], ["t1"])
        dv(lambda e: e.tensor_tensor(out=rg["m2"][:], in0=rg["p01"][:], in1=rg["p23"][:], op=ALU.min), ["p01", "p23"], ["m2"])
        dv(lambda e: e.tensor_tensor(out=rg["m3"][:], in0=rg["q01"][:], in1=rg["q23"][:], op=ALU.max), ["q01", "q23"], ["m3"])
        dv(lambda e: e.tensor_tensor(out=rg["m2"][:], in0=rg["m2"][:], in1=rg["m3"][:], op=ALU.max), ["m2", "m3"], ["m2"])
        dv(lambda e: e.tensor_tensor(out=rg["gs"][:], in0=rg["t1"][:], in1=rg["m2"][:], op=ALU.add), ["t1", "m2"], ["gs"])
        g3 = lambda ap: ap.rearrange("p (t g) -> p t g", t=NT)
        dv(lambda e: e.tensor_reduce(out=r1["gmax"][:], in_=g3(rg["gs"][:]), axis=AX.X, op=ALU.max), ["gs"], ["gmax"])
        dv(lambda e: e.tensor_tensor(out=g3(rg["ing"][:]), in0=g3(rg["gs"][:]), in1=r1["gmax"][:].unsqueeze(2).to_broadcast([128, NT, 4]),
                                     op=ALU.is_ge), ["gs", "gmax"], ["ing"])
        dv(lambda e: e.tensor_scalar(out=rg["pen"][:], in0=rg["ing"][:], scalar1=4.0, scalar2=-4.0, op0=ALU.mult, op1=ALU.add),
           ["ing"], ["pen"])
        smv = rt["sm"][:].rearrange("p (g j) -> p g j", j=4)
        dv(lambda e: e.tensor_tensor(out=smv, in0=sv, in1=rg["ing"][:].unsqueeze(2).to_broadcast([128, G, 4]), op=ALU.mult),
           ["rt_ssel", "ing"], ["rt_sm"])
        dv(lambda e: e.tensor_tensor(out=smv, in0=smv, in1=rg["pen"][:].unsqueeze(2).to_broadcast([128, G, 4]), op=ALU.add),
           ["rt_sm", "pen"], ["rt_sm"])
        bc16 = lambda ap: ap.unsqueeze(2).to_broadcast([128, NT, 16])
        dv(lambda e: e.tensor_reduce(out=r1["m1"][:], in_=t3(rt["sm"][:]), axis=AX.X, op=ALU.max), ["rt_sm"], ["m1"])
        dv(lambda e: e.tensor_tensor(out=t3(rt["sel"][:]), in0=t3(rt["sm"][:]), in1=bc16(r1["m1"][:]), op=ALU.is_ge), ["rt_sm", "m1"], ["rt_sel"])
        dv(lambda e: e.scalar_tensor_tensor(out=rt["sc"][:], in0=rt["sel"][:], scalar=-8.0, in1=rt["sm"][:], op0=ALU.mult, op1=ALU.add),
           ["rt_sel", "rt_sm"], ["rt_sc"])
        dv(lambda e: e.tensor_reduce(out=r1["m2"][:], in_=t3(rt["sc"][:]), axis=AX.X, op=ALU.max), ["rt_sc"], ["m2r"])
        dv(lambda e: e.tensor_tensor(out=t3(rt["sel"][:]), in0=t3(rt["sm"][:]), in1=bc16(r1["m2"][:]), op=ALU.is_ge), ["rt_sm", "m2r"], ["rt_sel"])
        dv(lambda e: e.tensor_tensor(out=rt["sc"][:], in0=rt["s"][:], in1=rt["sel"][:], op=ALU.mult), ["rt_s", "rt_sel"], ["rt_sc"])
        dv(lambda e: e.tensor_reduce(out=r1["den"][:], in_=t3(rt["sc"][:]), axis=AX.X, op=ALU.add), ["rt_sc"], ["den"])
        dv(lambda e: e.reciprocal(out=r1["den"][:], in_=r1["den"][:]), ["den"], ["den"])
        dv(lambda e: e.tensor_tensor(out=GATE[:], in0=t3(rt["sc"][:]), in1=bc16(r1["den"][:]), op=ALU.mult),
           ["rt_sc", "den"], [("GATE", t) for t in range(NT)])

        groups = [(g * 4, min(4, NT - g * 4)) for g in range((NT + 3) // 4)]
        NU = 2 * NE
        stg13_keys = ["STG0", "STG1", "STG2", "STG3"]
        stg2_keys = ["STG4", "STG5"]

        def load_unit(u):
            ex, hf = u // 2, u % 2
            S.dma("sp", S13[:, 0, :, :], w1_d[ex, :, hf * 256:(hf + 1) * 256].rearrange("(k p) n -> p k n", p=128),
                  "ld_s13", writes=stg13_keys)
            S.dma("sp", S13[:, 1, :, :], w3_d[ex, :, hf * 256:(hf + 1) * 256].rearrange("(k p) n -> p k n", p=128),
                  "ld_s13", writes=stg13_keys)
            S.dma("sp", S2, w2_d[ex, hf * 256:(hf + 1) * 256, :].rearrange("(k p) n -> p k n", p=128),
                  "ld_s2", writes=stg2_keys)

        def cast_unit(u):
            sl = u % 2
            for a in range(2):
                S.op("act", lambda e, a=a, sl=sl: e.activation(out=W13[sl][:, a, :, :], in_=S13[:, a, :, :], func=AF.Copy),
                     reads=stg13_keys, writes=[f"W13_{sl}"])
            gl = MOD[("gf", 0)].unsqueeze(1).to_broadcast([128, 2, D])
            S.op("dve", lambda e, sl=sl, gl=gl: e.tensor_tensor(out=W2[sl][:], in0=S2, in1=gl, op=ALU.mult),
                 reads=stg2_keys + [MKEY[("gf", 0)]], writes=[f"W2_{sl}"])
            if has_ctx:
                gc_ = MOD[("gf", 1)].unsqueeze(1).to_broadcast([128, 2, D])
                S.op("pool", lambda e, sl=sl, gc_=gc_: e.tensor_tensor(out=W2c[sl][:], in0=S2, in1=gc_, op=ALU.mult),
                     reads=stg2_keys + [MKEY[("gf", 1)]], writes=[f"W2c_{sl}"])

        yi = 0
        abi = 0
        load_unit(0)
        cast_unit(0)
        work = [(u, gi, t0, ntile) for u in range(NU) for gi, (t0, ntile) in enumerate(groups)]
        cast_gi = min(2, len(groups) - 1)
        yi_box = [0]

        def AB(idx):
            u, gi, t0, ntile = work[idx]
            sl = u % 2
            wk0 = f"W13_{sl}"
            ntok = ntile * 128
            c0 = t0 * 128
            htk = [("HT", t) for t in range(t0, t0 + ntile)]
            hb = H1[idx % 2]
            hk = f"H1_{idx % 2}"
            for dc in range(2):
                pa, pb = psA[dc], psB[dc]
                for k in range(8):
                    S.op("pe", lambda e, k=k, dc=dc, pa=pa: e.matmul(
                        pa[:, 0:ntok], lhsT=W13[sl][:, 0, k, dc * 128:(dc + 1) * 128], rhs=HT[:, k, c0:c0 + ntok],
                        start=(k == 0), stop=(k == 7)), reads=[wk0] + htk, writes=[f"psA{dc}"])
                for k in range(8):
                    S.op("pe", lambda e, k=k, dc=dc, pb=pb: e.matmul(
                        pb[:, 0:ntok], lhsT=W13[sl][:, 1, k, dc * 128:(dc + 1) * 128], rhs=HT[:, k, c0:c0 + ntok],
                        start=(k == 0), stop=(k == 7)), reads=[wk0] + htk, writes=[f"psB{dc}"])
                sa = SA[:, dc, 0:ntok]
                S.op("act", lambda e, pa=pa, sa=sa: e.activation(out=sa, in_=pa[:, 0:ntok], func=AF.Silu),
                     reads=[f"psA{dc}"], writes=[f"SA{dc}"])
                S.op("dve", lambda e, pb=pb, sa=sa, dc=dc: e.tensor_tensor(out=hb[:, dc, 0:ntok], in0=sa, in1=pb[:, 0:ntok], op=ALU.mult),
                     reads=[f"SA{dc}", f"psB{dc}"], writes=[hk])

        def Y(idx):
            u, gi, t0, ntile = work[idx]
            sl = u % 2
            ex = u // 2
            wk1 = f"W2_{sl}"
            hb = H1[idx % 2]
            hk = f"H1_{idx % 2}"
            for ti in range(ntile):
                t = t0 + ti
                s = tiles[t][1]
                yi = yi_box[0]
                yi_box[0] += 1
                py = psY[yi % 2]
                pyk = f"psY{yi % 2}"
                w2t, w2k = (W2[sl], f"W2_{sl}") if s == 0 else (W2c[sl], f"W2c_{sl}")
                for h2 in range(2):
                    for dc in range(2):
                        S.op("pe", lambda e, h2=h2, dc=dc, py=py, ti=ti, w2t=w2t: e.matmul(
                            py[:, h2 * 512:(h2 + 1) * 512], lhsT=hb[:, dc, ti * 128:(ti + 1) * 128],
                            rhs=w2t[:, dc, h2 * 512:(h2 + 1) * 512], start=(dc == 0), stop=(dc == 1)),
                            reads=[hk, w2k], writes=[pyk])
                S.op("dve", lambda e, py=py, t=t: e.scalar_tensor_tensor(
                    out=X[:, t, :], in0=py[:], scalar=GATE[:, t, ex:ex + 1], in1=X[:, t, :], op0=ALU.mult, op1=ALU.add),
                    reads=[pyk, ("GATE", t), ("X", t)], writes=[("X", t)])

        for idx in range(len(work)):
            u, gi, _, _ = work[idx]
            if gi == 0 and u + 1 < NU:
                load_unit(u + 1)
            if gi == cast_gi and u + 1 < NU:
                cast_unit(u + 1)
            AB(idx)
            if idx >= 1:
                Y(idx - 1)
        Y(len(work) - 1)

        LNG, LNB = STG[:, 0:D], STG[:, D:2 * D]
        S.dma("sp", LNG, lng_d.partition_broadcast(128), "ld_lng", writes=["STG0"])
        S.dma("sp", LNB, lnb_d.partition_broadcast(128), "ld_lnb", writes=["STG1"])
        for t in range(NT):
            o = STG[:, (2 + t % 2) * D:(3 + t % 2) * D]
            ok = f"STG{2 + t % 2}"
            layer_norm_tile(S, X[:, t, :], ("X", t), o, ok, T[0][:], "T0", LNG, "STG0", LNB, "STG1", small, eps_t)
            S.dma("sp", tiles[t][2], o, f"st{t % 2}", reads=[ok], store=True)
        S.emit()
        S.close()


class Ctx:
    pass


def std_inputs(nc):
    dt = lambda n, s, k="ExternalInput": nc.dram_tensor(n, s, F32, kind=k).ap()
    return dt, {"cvec": dt("cvec", [2, D]), "wmod": dt("wmod", [D, 3 * D]), "bmod": dt("bmod", [3 * D]), "lng": dt("lng", [D]),
                "lnb": dt("lnb", [D])}


def mixer_prologue(nc, es, pfx, A):
    C = Ctx()
    C.nc = nc
    C.es = es
    S = C.S = Sched(nc, es, pfx)
    sb = C.sb = lambda n, s, d=F32: es.enter_context(nc.sbuf_tensor(pfx + n, s, d))
    ps = C.ps = lambda n, s, d=F32: es.enter_context(nc.psum_tensor(pfx + n, s, d))
    C.cvec, C.wmod, C.bmod, C.lng_d, C.lnb_d = A["cvec"], A["wmod"], A["bmod"], A["lng"], A["lnb"]
    C.STG = [sb(f"STG{i}", [128, 4096]) for i in range(2)]
    C.stg_i = 0
    C.MOD = {}
    C.MKEY = {}
    for n in ("sh", "sc1", "ga"):
        for s in range(2):
            C.MOD[(n, s)] = sb(f"M{n}{s}", [128, D])[:]
            C.MKEY[(n, s)] = f"M{n}{s}"
    C.LNG = sb("LNG", [128, D])
    C.LNB = sb("LNB", [128, D])
    C.eps2 = sb("eps2", [128, 1])
    C.eps1 = sb("eps1", [128, 1])
    C.small = (sb("ln_st", [128, 2, 6]), sb("ln_mv", [128, 2]), sb("ln_rstd", [128, 1]), sb("ln_nmr", [128, 1]))
    C.psBig = [ps(f"psBig{i}", [128, D]) for i in range(2)]
    C.ident, C.ones = common_consts(S, nc, sb, ps)
    S.op("dve", lambda e: e.memset(C.eps2[:], EPS2), writes=["eps"])
    S.op("dve", lambda e: e.memset(C.eps1[:], LN_EPS), writes=["eps1"])
    return C


def mixer_mods(C):
    S = C.S
    stage = C.STG[0][:].rearrange("p (k n) -> p k n", k=8)
    SLC = C.STG[1][:, 0:2048].rearrange("p (s k n) -> p s k n", s=2, k=8)
    outs = []
    for s in range(2):
        outs.append((0, s, C.MOD[("sh", s)], C.MKEY[("sh", s)], "plain"))
        outs.append((1, s, C.MOD[("sc1", s)], C.MKEY[("sc1", s)], "plus1"))
        outs.append((2, s, C.MOD[("ga", s)], C.MKEY[("ga", s)], "invalpha"))
    mod_tiles(S, C.nc, C.sb, C.cvec, C.wmod, C.bmod, C.ident, C.ones, C.psBig[0], "psBig0", stage, ["STG0"], SLC, ["STG1"], outs)
    S.dma("sp", C.LNG[:], C.lng_d.partition_broadcast(128), "ld_lng", writes=["LNG"])
    S.dma("sp", C.LNB[:], C.lnb_d.partition_broadcast(128), "ld_lnb", writes=["LNB"])


def load_w_bf16(C, dst, dkey, src, kc, ncols):
    S = C.S
    cw = min(ncols, 512)
    kpp = max(1, min(kc, 4096 // cw))
    n = 0
    for c0 in range(0, ncols, cw):
        for k0 in range(0, kc, kpp):
            kk = min(kpp, kc - k0)
            i = C.stg_i % 2
            C.stg_i += 1
            st = C.STG[i][:, 0:kk * cw].rearrange("p (k n) -> p k n", k=kk)
            S.dma("sp", st, src[k0 * 128:(k0 + kk) * 128, c0:c0 + cw].rearrange("(k p) n -> p k n", p=128), f"ld_stg{i}",
                  writes=[f"STG{i}"])
            eng = "act" if n % 2 == 0 else "dve"
            n += 1
            d = dst[:, k0:k0 + kk, c0:c0 + cw]
            if eng == "act":
                S.op("act", lambda e, d=d, st=st: e.activation(out=d, in_=st, func=AF.Copy), reads=[f"STG{i}"], writes=[dkey])
            else:
                S.op("dve", lambda e, d=d, st=st: e.tensor_copy(out=d, in_=st), reads=[f"STG{i}"], writes=[dkey])


def modulate_T(C, xt, xkey, s, T0, t0key, pst, pskey, HTt, htkey):
    S = C.S
    S.op("dve", lambda e: e.tensor_tensor(out=T0, in0=xt, in1=C.MOD[("sc1", s)], op=ALU.mult), reads=[xkey, C.MKEY[("sc1", s)]], writes=[t0key])
    S.op("dve", lambda e: e.tensor_tensor(out=T0, in0=T0, in1=C.MOD[("sh", s)], op=ALU.add), reads=[t0key, C.MKEY[("sh", s)]], writes=[t0key])
    to_T(C, T0, t0key, pst, pskey, HTt, htkey)


def to_T(C, src, skey, pst, pskey, HTt, htkey):
    S = C.S
    for k in range(8):
        S.op("pe", lambda e, k=k: e.transpose(out=pst[:, k * 128:(k + 1) * 128], in_=src[:, k * 128:(k + 1) * 128], identity=C.ident[:]),
             reads=[skey, "ident"], writes=[pskey])
    S.op("act", lambda e: e.activation(out=HTt, in_=pst[:].rearrange("p (k n) -> p k n", k=8), func=AF.Copy), reads=[pskey], writes=[htkey])


def proj(C, pst_list, HTt, htkey, W, wkey, c0, ncols):
    S = C.S
    off = 0
    for (pa, pk) in pst_list:
        w = min(512, ncols - off)
        for k in range(8):
            S.op("pe", lambda e, k=k, pa=pa, w=w, off=off: e.matmul(pa[:, 0:w], lhsT=HTt[:, k, :], rhs=W[:, k, c0 + off:c0 + off + w],
                                                                    start=(k == 0), stop=(k == 7)), reads=[htkey, wkey], writes=[pk])
        off += w


def residual_ln_store(C, psy, pykey, xt, xkey, s, T1, t1key, T2, t2key, o, okey, out_ap, stsem):
    S = C.S
    S.op("dve", lambda e: e.tensor_tensor(out=T1, in0=psy[:], in1=C.MOD[("ga", s)], op=ALU.mult), reads=[pykey, C.MKEY[("ga", s)]], writes=[t1key])
    S.op("pool", lambda e: e.tensor_tensor(out=T1, in0=T1, in1=xt, op=ALU.add), reads=[t1key, xkey], writes=[t1key])
    layer_norm_tile(S, T1, t1key, o, okey, T2, t2key, C.LNG[:], "LNG", C.LNB[:], "LNB", C.small, C.eps2)
    S.dma("sp", out_ap, o, stsem, reads=[okey], store=True)


def build_gmlp():
    NT = 18
    nc = bass.Bass("TRN2", target_bir_lowering=False)
    dt, A = std_inputs(nc)
    xin = dt("xin", [NT * 128, D])
    A.update({"w_in": dt("w_in", [D, 2 * D]), "b_in": dt("b_in", [2 * D]), "g_ln_g": dt("g_ln_g", [D]), "g_ln_b": dt("g_ln_b", [D]),
              "w_s": dt("w_s", [8, 128, 128]), "b_s": dt("b_s", [128, 8]), "w_out": dt("w_out", [D, D])})
    xout = dt("xout", [NT * 128, D], "ExternalOutput")
    tiles = [(xin[t * 128:(t + 1) * 128, :], 0 if t < 16 else 1, xout[t * 128:(t + 1) * 128, :]) for t in range(NT)]
    stage_gmlp(nc, "", tiles, A)
    return nc


def stage_gmlp(nc, pfx, tiles, A):
    NT = len(tiles)
    es = ExitStack()
    with es:
        C = mixer_prologue(nc, es, pfx, A)
        S, sb, ps = C.S, C.sb, C.ps
        win_d, bin_d, glng_d, glnb_d, ws_d, bs_d, wout_d = (A[k] for k in ("w_in", "b_in", "g_ln_g", "g_ln_b", "w_s", "b_s", "w_out"))
        Win = sb("Win", [128, 8, 2 * D], BF16)
        Wout = sb("Wout", [128, 8, D], BF16)
        wsT = sb("wsT", [128, 8, 128], BF16)
        BIN = sb("BIN", [128, 2 * D])
        GLNG = sb("GLNG", [128, D])
        GLNB = sb("GLNB", [128, D])
        bs = sb("bs", [128, 8])
        Xt = [sb(f"Xt{i}", [128, D]) for i in range(2)]
        OUT = [sb(f"OUT{i}", [128, D]) for i in range(2)]
        T0 = sb("T0", [128, D]); T1 = sb("T1", [128, D]); T2 = sb("T2", [128, D])
        HTt = sb("HTt", [128, 8, 128], BF16)
        HT2 = sb("HT2", [128, 8, 128], BF16)
        Z = sb("Z", [128, 2 * D])
        TZ = sb("TZ", [128, 2 * D])
        TZ2 = sb("TZ2", [128, 2 * D])
        VN = sb("VN", [128, D], BF16)
        US = sb("US", [128, D])
        psZ = [ps(f"psZ{i}", [128, 512]) for i in range(4)]
        mixer_mods(C)
        load_w_bf16(C, Win[:], "Win", win_d, 8, 2 * D)
        load_w_bf16(C, Wout[:], "Wout", wout_d, 8, D)
        S.dma("sp", BIN[:], bin_d.partition_broadcast(128), "ld_bin", writes=["BIN"])
        S.dma("sp", GLNG[:], glng_d.partition_broadcast(128), "ld_glng", writes=["GLNG"])
        S.dma("sp", GLNB[:], glnb_d.partition_broadcast(128), "ld_glnb", writes=["GLNB"])
        S.dma("sp", bs[:], bs_d, "ld_bs", writes=["bs"])
        wst = C.STG[0][:, 0:1024].rearrange("p (g q) -> p g q", g=8)
        S.dma("sp", wst, ws_d.rearrange("g p q -> p g q"), "ld_stg0", writes=["STG0"])
        for g in range(8):
            S.op("pe", lambda e, g=g: e.transpose(out=C.psBig[0][:, g * 128:(g + 1) * 128], in_=wst[:, g, :], identity=C.ident[:]),
                 reads=["STG0", "ident"], writes=["psBig0"])
        S.op("act", lambda e: e.activation(out=wsT[:], in_=C.psBig[0][:].rearrange("p (g n) -> p g n", g=8), func=AF.Copy),
             reads=["psBig0"], writes=["wsT"])
        for t in range(NT):
            s = tiles[t][1]
            xt = Xt[t % 2][:]
            xk = f"Xt{t % 2}"
            S.dma("sp", xt, tiles[t][0], f"ld_x{t % 2}", writes=[xk])
            modulate_T(C, xt, xk, s, T0[:], "T0", C.psBig[0], "psBig0", HTt[:], "HTt")
            proj(C, [(psZ[i], f"psZ{i}") for i in range(4)], HTt, "HTt", Win, "Win", 0, 2 * D)
            for i in range(4):
                S.op("dve", lambda e, i=i: e.tensor_tensor(out=Z[:, i * 512:(i + 1) * 512], in0=psZ[i][:], in1=BIN[:, i * 512:(i + 1) * 512],
                                                           op=ALU.add), reads=[f"psZ{i}", "BIN"], writes=["Z"])
            S.op("act", lambda e: e.activation(out=TZ[:], in_=Z[:], func=AF.Square), reads=["Z"], writes=["TZ"])
            S.op("dve", lambda e: e.tensor_scalar(out=TZ[:], in0=TZ[:], scalar1=0.044715, scalar2=1.0, op0=ALU.mult, op1=ALU.add),
                 reads=["TZ"], writes=["TZ"])
            S.op("pool", lambda e: e.tensor_tensor(out=TZ[:], in0=TZ[:], in1=Z[:], op=ALU.mult), reads=["TZ", "Z"], writes=["TZ"])
            S.op("act", lambda e: e.activation(out=TZ[:], in_=TZ[:], func=AF.Sigmoid, scale=1.5957691216057308), reads=["TZ"], writes=["TZ"])
            S.op("pool", lambda e: e.tensor_tensor(out=TZ2[:], in0=TZ[:], in1=Z[:], op=ALU.mult), reads=["TZ", "Z"], writes=["TZ2"])
            layer_norm_tile(S, TZ2[:, D:2 * D], "TZ2", VN[:], "VN", T2[:], "T2", GLNG[:], "GLNG", GLNB[:], "GLNB", C.small, C.eps1)
            for g in range(8):
                S.op("pe", lambda e, g=g: e.matmul(C.psBig[0][:, g * 128:(g + 1) * 128], lhsT=wsT[:, g, :], rhs=VN[:, g * 128:(g + 1) * 128],
                                                   start=True, stop=True), reads=["wsT", "VN"], writes=["psBig0"])
            for g in range(8):
                S.op("dve", lambda e, g=g: e.scalar_tensor_tensor(out=US[:, g * 128:(g + 1) * 128], in0=C.psBig[0][:, g * 128:(g + 1) * 128],
                                                                  scalar=bs[:, g:g + 1], in1=TZ2[:, g * 128:(g + 1) * 128],
                                                                  op0=ALU.add, op1=ALU.mult), reads=["psBig0", "bs", "TZ2"], writes=["US"])
            to_T(C, US[:], "US", C.psBig[1], "psBig1", HT2[:], "HT2")
            proj(C, [(C.psBig[1][:, 0:512], "psBig1"), (C.psBig[1][:, 512:1024], "psBig1")], HT2, "HT2", Wout, "Wout", 0, D)
            residual_ln_store(C, C.psBig[1], "psBig1", xt, xk, s, T1[:], "T1", T2[:], "T2", OUT[t % 2][:], f"OUT{t % 2}",
                              tiles[t][2], f"st{t % 2}")
        S.emit()
        S.close()


def build_conv():
    nc = bass.Bass("TRN2", target_bir_lowering=False)
    dt, A = std_inputs(nc)
    xin = dt("xin", [20 * 128, D])
    A.update({"valid": dt("valid", [128, 20]), "w_in": dt("w_in", [D, 3 * D]), "w_conv": dt("w_conv", [3, D]), "w_out": dt("w_out", [D, D])})
    xout = dt("xout", [18 * 128, D], "ExternalOutput")
    lat = [(xin[e * 128:(e + 1) * 128, :], 0, e, (xout[(e - 1) * 128:e * 128, :] if 1 <= e <= 16 else None)) for e in range(18)]
    ctx = [(xin[e * 128:(e + 1) * 128, :], 1, e, xout[(e - 2) * 128:(e - 1) * 128, :]) for e in (18, 19)]
    stage_conv(nc, "", [lat, ctx], A, 20)
    return nc


def stage_conv(nc, pfx, seqs, A, nvalid):
    es = ExitStack()
    with es:
        C = mixer_prologue(nc, es, pfx, A)
        S, sb, ps = C.S, C.sb, C.ps
        valid_d, win_d, wconv_d, wout_d = (A[k] for k in ("valid", "w_in", "w_conv", "w_out"))
        Win = sb("Win", [128, 8, 3 * D], BF16)
        Wout = sb("Wout", [128, 8, D], BF16)
        WC = [sb(f"WC{i}", [128, D]) for i in range(3)]
        valid = sb("valid_sb", [128, nvalid])
        Xr = [sb(f"Xr{i}", [128, D]) for i in range(4)]
        Zr = [C.STG[1][:, i * D:(i + 1) * D] for i in range(4)]
        HTr = [sb(f"HTr{i}", [128, 8, 128], BF16) for i in range(4)]
        ZM = sb("ZM", [128, D]); ZP = sb("ZP", [128, D]); TC = sb("TC", [128, D]); TG = sb("TG", [128, D])
        OUT = [sb(f"OUT{i}", [128, D]) for i in range(2)]
        T0 = sb("T0", [128, D]); T1 = sb("T1", [128, D]); T2 = sb("T2", [128, D])
        HT2 = sb("HT2", [128, 8, 128], BF16)
        psP = [ps(f"psP{i}", [128, 512]) for i in range(4)]
        mixer_mods(C)
        load_w_bf16(C, Win[:], "Win", win_d, 8, 3 * D)
        load_w_bf16(C, Wout[:], "Wout", wout_d, 8, D)
        for i in range(3):
            S.dma("sp", WC[i][:], wconv_d[i, :].partition_broadcast(128), f"ld_wc{i}", writes=[f"WC{i}"])
        S.dma("sp", valid[:], valid_d, "ld_valid", writes=["valid"])
        S.op("dve", lambda e: e.memset(ZM[0:1, 0:1], 0.0), writes=["STG1", "Zr0", "Zr1", "Zr2", "Zr3"])

        cnt = {"a": 0, "o": 0}

        def Astep(tile):
            src, s, vcol, _ = tile
            r = cnt["a"] % 4
            cnt["a"] += 1
            xt, xk = Xr[r][:], f"Xr{r}"
            S.dma("sp", xt, src, f"ld_x{r}", writes=[xk])
            modulate_T(C, xt, xk, s, T0[:], "T0", C.psBig[0], "psBig0", HTr[r][:], f"HTr{r}")
            proj(C, [(psP[i], f"psP{i}") for i in range(4)], HTr[r], f"HTr{r}", Win, "Win", D, 2 * D)
            for h in range(2):
                S.op("act", lambda e_, h=h: e_.activation(out=TG[:, h * 512:(h + 1) * 512], in_=psP[h][:], func=AF.Copy, scale=valid[:, vcol:vcol + 1]),
                     reads=[f"psP{h}", "valid"], writes=["TG"])
            for h in range(2):
                S.op("dve", lambda e_, h=h: e_.tensor_tensor(out=Zr[r][:, h * 512:(h + 1) * 512], in0=TG[:, h * 512:(h + 1) * 512], in1=psP[2 + h][:],
                                                            op=ALU.mult), reads=["TG", f"psP{2 + h}"], writes=[f"Zr{r}"])
            return r

        def Bstep(tile, r, prev, nxt):
            _, s, _, out_ap = tile
            ot = cnt["o"]
            cnt["o"] += 1
            xt, xk = Xr[r][:], f"Xr{r}"
            zc, zk = Zr[r], f"Zr{r}"
            if prev is None:
                S.op("dve", lambda e_: e_.memset(ZM[:], 0.0), writes=["ZM"])
            if nxt is None:
                S.op("dve", lambda e_: e_.memset(ZP[:], 0.0), writes=["ZP"])
            S.dma("sp", ZM[1:128, :], zc[0:127, :], "sh_zm", reads=[zk], writes=["ZM"])
            if prev is not None:
                S.dma("sp", ZM[0:1, :], Zr[prev][127:128, :], "sh_zm", reads=[f"Zr{prev}"], writes=["ZM"])
            S.dma("sp", ZP[0:127, :], zc[1:128, :], "sh_zp", reads=[zk], writes=["ZP"])
            if nxt is not None:
                S.dma("sp", ZP[127:128, :], Zr[nxt][0:1, :], "sh_zp", reads=[f"Zr{nxt}"], writes=["ZP"])
            S.op("dve", lambda e_: e_.tensor_tensor(out=ZM[:], in0=ZM[:], in1=WC[0][:], op=ALU.mult), reads=["ZM", "WC0"], writes=["ZM"])
            S.op("pool", lambda e_: e_.tensor_tensor(out=ZP[:], in0=ZP[:], in1=WC[2][:], op=ALU.mult), reads=["ZP", "WC2"], writes=["ZP"])
            S.op("dve", lambda e_: e_.tensor_tensor(out=TC[:], in0=zc, in1=WC[1][:], op=ALU.mult), reads=[zk, "WC1"], writes=["TC"])
            S.op("pool", lambda e_: e_.tensor_tensor(out=TC[:], in0=TC[:], in1=ZM[:], op=ALU.add), reads=["TC", "ZM"], writes=["TC"])
            S.op("dve", lambda e_: e_.tensor_tensor(out=TC[:], in0=TC[:], in1=ZP[:], op=ALU.add), reads=["TC", "ZP"], writes=["TC"])
            proj(C, [(C.psBig[1][:, 0:512], "psBig1"), (C.psBig[1][:, 512:1024], "psBig1")], HTr[r], f"HTr{r}", Win, "Win", 0, D)
            S.op("dve", lambda e_: e_.tensor_tensor(out=TG[:], in0=C.psBig[1][:], in1=TC[:], op=ALU.mult), reads=["psBig1", "TC"], writes=["TG"])
            to_T(C, TG[:], "TG", C.psBig[1], "psBig1", HT2[:], "HT2")
            proj(C, [(C.psBig[1][:, 0:512], "psBig1"), (C.psBig[1][:, 512:1024], "psBig1")], HT2, "HT2", Wout, "Wout", 0, D)
            residual_ln_store(C, C.psBig[1], "psBig1", xt, xk, s, T1[:], "T1", T2[:], "T2", OUT[ot % 2][:], f"OUT{ot % 2}",
                              out_ap, f"st{ot % 2}")

        for seq in seqs:
            slots = {}
            n = len(seq)
            for i in range(n + 1):
                if i < n:
                    slots[i] = Astep(seq[i])
                j = i - 1
                if j >= 0 and seq[j][3] is not None:
                    Bstep(seq[j], slots[j], slots.get(j - 1), slots.get(j + 1) if j + 1 < n else None)
        S.emit()
        S.close()


def build_attn(want_ctx):
    nc = bass.Bass("TRN2", target_bir_lowering=False)
    dt, A = std_inputs(nc)
    NOUT = 18 if want_ctx else 16
    xin = dt("xin", [20 * 128, D])
    A.update({"kbias": dt("kbias", [128, 20]), "cos_t": dt("cos_t", [128, 20 * 64]), "sin_t": dt("sin_t", [128, 20 * 64]),
              "maskp": dt("maskp", [128, 512]), "maskn": dt("maskn", [128, 512]), "w_qkv": dt("w_qkv", [D, 1536]),
              "w_o": dt("w_o", [D, D]), "sink": dt("sink", [16])})
    xout = dt("xout", [NOUT * 128, D], "ExternalOutput")
    kv = [(xin[e * 128:(e + 1) * 128, :], 0 if e < 18 else 1, e, e) for e in range(20)]
    q = [(xin[(o + 1) * 128:(o + 2) * 128, :], 0, o + 1, [(o, "P"), (o + 1, None), (o + 2, "N"), (18, None), (19, None)],
          xout[o * 128:(o + 1) * 128, :]) for o in range(16)]
    if want_ctx:
        q += [(xin[e * 128:(e + 1) * 128, :], 1, e, [(18, None), (19, None)], xout[(e - 2) * 128:(e - 1) * 128, :]) for e in (18, 19)]
    stage_attn(nc, "", kv, q, A, 20)
    return nc


def stage_attn(nc, pfx, kv_tiles, q_tiles, A, ntbl):
    NKT = len(kv_tiles)
    es = ExitStack()
    with es:
        C = mixer_prologue(nc, es, pfx, A)
        S, sb, ps = C.S, C.sb, C.ps
        kbias_d, cos_d, sin_d, maskp_d, maskn_d, wqkv_d, wo_d, sink_d = (A[k] for k in ("kbias", "cos_t", "sin_t", "maskp", "maskn", "w_qkv", "w_o", "sink"))
        Wqkv = sb("Wqkv", [128, 8, 1536], BF16)
        Wo = sb("Wo", [128, 8, D], BF16)
        KT = sb("KT", [128, 2, NKT * 128], BF16)
        V = sb("V", [128, NKT, 256], BF16)
        kbias = sb("kbias_sb", [128, ntbl])
        COS = sb("COS", [128, ntbl, 64])
        SIN = sb("SIN", [128, ntbl, 64])
        MP = sb("MP", [128, 512])
        MN = sb("MN", [128, 512])
        SINKB = sb("SINKB", [128, 2, 512])
        ES = sb("ES", [128, 16])
        identb = sb("identb", [128, 128], BF16)
        onesb = sb("onesb", [128, 64], BF16)
        Xt = [sb(f"Xt{i}", [128, D]) for i in range(2)]
        OUT = [sb(f"OUT{i}", [128, D]) for i in range(2)]
        T0 = sb("T0", [128, D]); T1 = sb("T1", [128, D]); T2 = sb("T2", [128, D])
        HTt = sb("HTt", [128, 8, 128], BF16)
        R1 = sb("R1", [128, D]); R2 = sb("R2", [128, D])
        KR = sb("KR", [128, 256], BF16)
        QRp = sb("QRp", [128, 8, 128], BF16)
        QT = sb("QT", [128, 8, 128], BF16)
        PT = [sb(f"PT{i}", [128, 512], BF16) for i in range(3)]
        DEN = sb("DEN", [128, 512])
        OT = sb("OT", [128, 2, 512], BF16)
        psS = [ps(f"psS{i}", [128, 512]) for i in range(2)]
        psX = ps("psX", [128, 8, 128], BF16)
        psO = C.psBig[1][:, 0:512]
        psD = C.psBig[1][:, 512:1024]
        S.op("pool", lambda e: e.memset(onesb[:], 1.0), writes=["onesb"])
        S.op("pool", lambda e: e.tensor_copy(out=identb[:], in_=C.ident[:]), reads=["ident"], writes=["identb"])
        mixer_mods(C)
        load_w_bf16(C, Wqkv[:], "Wqkv", wqkv_d, 8, 1536)
        for half in range(2):
            i = C.stg_i % 2
            C.stg_i += 1
            st = C.STG[i][:].rearrange("p (c n) -> p c n", c=4)
            for cc in range(4):
                c = half * 4 + cc
                jp, g = c // 4, c % 4
                for r in range(2):
                    row = 512 * jp + 256 * r + 64 * g
                    S.dma("sp", st[r * 64:(r + 1) * 64, cc, :], wo_d[row:row + 64, :], f"ld_stg{i}", writes=[f"STG{i}"])
            S.op("act", lambda e, st=st, half=half: e.activation(out=Wo[:, half * 4:(half + 1) * 4, :], in_=st, func=AF.Copy),
                 reads=[f"STG{i}"], writes=["Wo"])
        S.dma("sp", kbias[:], kbias_d, "ld_kb", writes=["kbias"])
        S.dma("sp", COS[:].rearrange("p t n -> p (t n)"), cos_d, "ld_cos", writes=["COS"])
        S.dma("sp", SIN[:].rearrange("p t n -> p (t n)"), sin_d, "ld_sin", writes=["SIN"])
        S.dma("sp", MP[:], maskp_d, "ld_mp", writes=["MP"])
        S.dma("sp", MN[:], maskn_d, "ld_mn", writes=["MN"])
        S.dma("sp", ES[:], sink_d.partition_broadcast(128), "ld_sink", writes=["ES"])
        S.op("act", lambda e: e.activation(out=ES[:], in_=ES[:], func=AF.Exp), reads=["ES"], writes=["ES"])
        for jp in range(2):
            for g in range(4):
                for r in range(2):
                    h = 8 * jp + 4 * r + g
                    S.op("act", lambda e, jp=jp, g=g, r=r, h=h: e.activation(
                        out=SINKB[r * 64:(r + 1) * 64, jp, g * 128:(g + 1) * 128], in_=C.ones[r * 64:(r + 1) * 64, :], func=AF.Copy,
                        scale=ES[r * 64:(r + 1) * 64, h:h + 1]), reads=["ones", "ES"], writes=["SINKB"])

        T0s = [T0[:], C.STG[0][:, 0:D]]
        R1s = [R1[:], C.STG[0][:, D:2 * D]]
        R2s = [R2[:], C.STG[0][:, 2 * D:3 * D]]
        HTs = [HTt, sb("HTtb", [128, 8, 128], BF16)]
        QRs = [QRp, sb("QRpb", [128, 8, 128], BF16)]
        QTs = [QT, sb("QTb", [128, 8, 128], BF16)]
        S.op("dve", lambda e_: e_.memset(DEN[0:1, 0:1], 0.0), writes=["STG0", "T0_1", "R1_1", "R2_1"])

        def rope(src_ps, pskey, nh, tbl, b):
            n = nh * 64
            xv = src_ps.rearrange("p (h b f i) -> p h b f i", h=nh, b=2, f=2)
            r1v = R1s[b][:, 0:n].rearrange("p (h n) -> p h n", h=nh)
            r2v = R2s[b][:, 0:n].rearrange("p (h b f i) -> p h b f i", h=nh, b=2, f=2)
            cosb = COS[:, tbl, :].unsqueeze(1).to_broadcast([128, nh, 64])
            sv = SIN[:, tbl, :].rearrange("p (b f i) -> p b f i", b=2, f=2)
            S.op("dve", lambda e_: e_.tensor_tensor(out=r1v, in0=src_ps.rearrange("p (h n) -> p h n", h=nh), in1=cosb, op=ALU.mult),
                 reads=[pskey, "COS"], writes=[f"R1_{b}"])
            for f in range(2):
                sb_ = sv[:, :, f, :].unsqueeze(1).to_broadcast([128, nh, 2, 16])
                S.op("dve", lambda e_, f=f, sb_=sb_: e_.tensor_tensor(out=r2v[:, :, :, f, :], in0=xv[:, :, :, 1 - f, :], in1=sb_, op=ALU.mult),
                     reads=[pskey, "SIN"], writes=[f"R2_{b}"])

        def a_front(e):
            src_, s, tbl, kbc = kv_tiles[e]
            b = e % 2
            xt, xk = Xt[b][:], f"Xt{b}"
            S.dma("sp", xt, src_, f"ld_x{b}", writes=[xk])
            modulate_T(C, xt, xk, s, T0s[b], f"T0_{b}", C.psBig[0], "psBig0", HTs[b][:], f"HTt{b}")
            proj(C, [(psS[b], f"psS{b}")], HTs[b], f"HTt{b}", Wqkv, "Wqkv", 1024, 512)

        def a_back(e):
            src_, s, tbl, kbc = kv_tiles[e]
            b = e % 2
            S.op("act", lambda e_: e_.activation(out=V[:, e, :], in_=psS[b][:, 256:512], func=AF.Copy), reads=[f"psS{b}"], writes=["V"])
            rope(psS[b][:, 0:256], f"psS{b}", 4, tbl, b)
            S.op("pool", lambda e_: e_.tensor_tensor(out=KR[:], in0=R1s[b][:, 0:256], in1=R2s[b][:, 0:256], op=ALU.add),
                 reads=[f"R1_{b}", f"R2_{b}"], writes=["KR"])
            for jp in range(2):
                S.op("pe", lambda e_, jp=jp: e_.transpose(out=psX[:, jp, :], in_=KR[:, jp * 128:(jp + 1) * 128], identity=identb[:]),
                     reads=["KR", "identb"], writes=["psX"])
            S.op("act", lambda e_: e_.activation(out=KT[:, :, e * 128:(e + 1) * 128], in_=psX[:, 0:2, :], func=AF.Copy),
                 reads=["psX"], writes=["KT"])

        for e in range(NKT):
            a_front(e)
            if e >= 1:
                a_back(e - 1)
        a_back(NKT - 1)

        pti_box = [0]

        def b_front(i):
            src_, s, tbl, chunks_, out_ap = q_tiles[i]
            b = i % 2
            xt, xk = Xt[b][:], f"Xt{b}"
            S.dma("sp", xt, src_, f"ld_x{b}", writes=[xk])
            modulate_T(C, xt, xk, s, T0s[b], f"T0_{b}", C.psBig[0], "psBig0", HTs[b][:], f"HTt{b}")
            proj(C, [(C.psBig[0][:, 0:512], "psBig0"), (C.psBig[0][:, 512:1024], "psBig0")], HTs[b], f"HTt{b}", Wqkv, "Wqkv", 0, D)
            rope(C.psBig[0][:], "psBig0", 16, tbl, b)
            for jp in range(2):
                a_ = R1s[b][:, jp * 512:(jp + 1) * 512].rearrange("p (r g d) -> p r g d", r=2, g=4)
                b_ = R2s[b][:, jp * 512:(jp + 1) * 512].rearrange("p (r g d) -> p r g d", r=2, g=4)
                o_ = QRs[b][:, jp * 4:(jp + 1) * 4, :].rearrange("p g (r d) -> p r g d", r=2)
                S.op("pool", lambda e_, a_=a_, b_=b_, o_=o_: e_.tensor_tensor(out=o_, in0=a_, in1=b_, op=ALU.add),
                     reads=[f"R1_{b}", f"R2_{b}"], writes=[f"QRp{b}"])
            for c in range(8):
                S.op("pe", lambda e_, c=c: e_.transpose(out=psX[:, c, :], in_=QRs[b][:, c, :], identity=identb[:]),
                     reads=[f"QRp{b}", "identb"], writes=["psX"])
            S.op("act", lambda e_: e_.activation(out=QTs[b][:], in_=psX[:], func=AF.Copy), reads=["psX"], writes=[f"QT{b}"])

        def b_rest(i):
            src_, s, tbl, chunks_, out_ap = q_tiles[i]
            b = i % 2
            o = i
            xt, xk = Xt[b][:], f"Xt{b}"
            QTc, qtk = QTs[b], f"QT{b}"
            chunks = [(kt, {"P": MP, "N": MN, None: None}[m]) for (kt, m) in chunks_]
            steps = []
            for jp in range(2):
                for r in range(2):
                    for ci, (kt, mask) in enumerate(chunks):
                        steps.append((jp, r, ci, kt, mask))
            bufs = {}

            def score(n):
                jp, r, ci, kt, mask = steps[n]
                lo, hi = r * 64, (r + 1) * 64
                kbc = kv_tiles[kt][3]
                pti = pti_box[0]
                pti_box[0] += 1
                pss, psk = psS[pti % 2], f"psS{pti % 2}"
                pt, ptk = PT[pti % 3], f"PT{pti % 3}"
                bufs[n] = (pt, ptk)
                S.op("pe", lambda e_: e_.matmul(pss[:], lhsT=KT[lo:hi, jp, kt * 128:(kt + 1) * 128], rhs=QTc[lo:hi, jp * 4:(jp + 1) * 4, :],
                                                start=True, stop=True), reads=["KT", qtk], writes=[psk])
                S.op("act", lambda e_: e_.activation(out=pt[:], in_=pss[:], func=AF.Exp, bias=kbias[:, kbc:kbc + 1], scale=0.125),
                     reads=[psk, "kbias"], writes=[ptk])
                if mask is not None:
                    S.op("dve", lambda e_: e_.tensor_tensor(out=pt[:], in0=pt[:], in1=mask[:], op=ALU.mult), reads=[ptk, "MP", "MN"], writes=[ptk])

            def pv(n):
                jp, r, ci, kt, mask = steps[n]
                lo, hi = r * 64, (r + 1) * 64
                j = 2 * jp + r
                pt, ptk = bufs.pop(n)
                first, last = (ci == 0), (ci == len(chunks) - 1)
                S.op("pe", lambda e_: e_.matmul(psO[lo:hi, :], lhsT=V[:, kt, j * 64:(j + 1) * 64], rhs=pt[:], start=first, stop=last),
                     reads=["V", ptk], writes=["psBig1"])
                S.op("pe", lambda e_: e_.matmul(psD[lo:hi, :], lhsT=onesb[:, 0:64], rhs=pt[:], start=first, stop=last),
                     reads=["onesb", ptk], writes=["psBig1"])
                if r == 1 and last:
                    S.op("dve", lambda e_: e_.tensor_tensor(out=DEN[:], in0=psD, in1=SINKB[:, jp, :], op=ALU.add), reads=["psBig1", "SINKB"], writes=["DEN"])
                    S.op("dve", lambda e_: e_.reciprocal(out=DEN[:], in_=DEN[:]), reads=["DEN"], writes=["DEN"])
                    S.op("dve", lambda e_: e_.tensor_tensor(out=OT[:, jp, :], in0=psO, in1=DEN[:], op=ALU.mult), reads=["psBig1", "DEN"], writes=["OT"])

            score(0)
            for n in range(len(steps)):
                if n + 1 < len(steps):
                    score(n + 1)
                pv(n)
            for half in range(2):
                for c in range(8):
                    jp, g = c // 4, c % 4
                    S.op("pe", lambda e_, half=half, c=c, jp=jp, g=g: e_.matmul(
                        C.psBig[0][:, half * 512:(half + 1) * 512], lhsT=OT[:, jp, g * 128:(g + 1) * 128], rhs=Wo[:, c, half * 512:(half + 1) * 512],
                        start=(c == 0), stop=(c == 7)), reads=["OT", "Wo"], writes=["psBig0"])
            residual_ln_store(C, C.psBig[0], "psBig0", xt, xk, s, T1[:], "T1", T2[:], "T2", OUT[o % 2][:], f"OUT{o % 2}",
                              out_ap, f"st{o % 2}")

        nq = len(q_tiles)
        b_front(0)
        for i in range(nq):
            if i + 1 < nq:
                b_front(i + 1)
            b_rest(i)
        S.emit()
        S.close()


NWIN = 22
NEXT = 20


def build_fused(nstage=8, dbg=False):
    nc = bass.Bass("TRN2", target_bir_lowering=False)
    dt = lambda n, s, k="ExternalInput": nc.dram_tensor(n, s, F32, kind=k).ap()
    xw = dt("xw", [NWIN * 128, D])
    ctx = dt("ctx", [256, D])
    cvec = dt("cvec", [2, D])
    w_mod = dt("w_mod", [4, D, 6 * D]); b_mod = dt("b_mod", [4, 6 * D])
    ln1_g = dt("ln1_g", [4, D]); ln1_b = dt("ln1_b", [4, D]); ln2_g = dt("ln2_g", [4, D]); ln2_b = dt("ln2_b", [4, D])
    rw = dt("router_w", [D, NE]); rb = dt("router_bias", [NE])
    w1 = dt("moe_w1", [4, NE, D, DEXP]); w3 = dt("moe_w3", [4, NE, D, DEXP]); w2 = dt("moe_w2", [4, NE, DEXP, D])
    a_w_qkv = dt("a_w_qkv", [2, D, 1536]); a_w_o = dt("a_w_o", [2, D, D]); a_sink = dt("a_sink", [2, 16])
    b_w_in = dt("b_w_in", [1, D, 2 * D]); b_b_in = dt("b_b_in", [1, 2 * D]); b_ln_g = dt("b_ln_g", [1, D]); b_ln_b = dt("b_ln_b", [1, D])
    b_w_s = dt("b_w_s", [1, 8, 128, 128]); b_b_s = dt("b_b_s", [1, 128, 8]); b_w_out = dt("b_w_out", [1, D, D])
    c_w_in = dt("c_w_in", [1, D, 3 * D]); c_w_conv = dt("c_w_conv", [1, 3, D]); c_w_out = dt("c_w_out", [1, D, D])
    kbias = dt("kbias", [128, 24]); cos_t = dt("cos_t", [128, 24 * 64]); sin_t = dt("sin_t", [128, 24 * 64])
    maskp = dt("maskp", [128, 512]); maskn = dt("maskn", [128, 512]); valid = dt("valid", [128, 22])
    SA = dt("scrA", [22 * 128, D], "ExternalOutput" if dbg else "Internal")
    SB = dt("scrB", [22 * 128, D], "ExternalOutput" if dbg else "Internal")
    xout = dt("xout", [2048, D], "ExternalOutput")
    row = lambda T, i: T[i * 128:(i + 1) * 128, :]

    def AM(L):
        return {"cvec": cvec, "wmod": w_mod[L][:, 3 * D:6 * D], "bmod": b_mod[L][3 * D:6 * D], "lng": ln2_g[L], "lnb": ln2_b[L],
                "rw": rw, "rb": rb, "w1": w1[L], "w3": w3[L], "w2": w2[L]}

    def AX_(L):
        return {"cvec": cvec, "wmod": w_mod[L][:, 0:3 * D], "bmod": b_mod[L][0:3 * D], "lng": ln1_g[L], "lnb": ln1_b[L]}

    def moe(L, pfx, tiles):
        h = (len(tiles) + 1) // 2 if len(tiles) > 18 else len(tiles)
        stage_moe(nc, pfx + "a_", tiles[:h], AM(L))
        if h < len(tiles):
            stage_moe(nc, pfx + "b_", tiles[h:], AM(L))

    attn_tabs = {"kbias": kbias, "cos_t": cos_t, "sin_t": sin_t, "maskp": maskp, "maskn": maskn}
    kv = [(row(xw, v), 0, v, v) for v in range(NWIN)] + [(row(ctx, c), 1, 22 + c, 22 + c) for c in range(2)]
    q = [(row(xw, u + 1), 0, u + 1, [(u, "P"), (u + 1, None), (u + 2, "N"), (22, None), (23, None)], row(SA, u)) for u in range(NEXT)]
    q += [(row(ctx, c), 1, 22 + c, [(22, None), (23, None)], row(SA, 20 + c)) for c in range(2)]
    A = AX_(0); A.update(attn_tabs); A.update({"w_qkv": a_w_qkv[0], "w_o": a_w_o[0], "sink": a_sink[0]})
    stage_attn(nc, "s0_", kv, q, A, 24)
    if nstage <= 1:
        return nc
    allt = lambda Tsrc, Tdst: [(row(Tsrc, u), 0 if u < NEXT else 1, row(Tdst, u)) for u in range(22)]
    moe(0, "m0", allt(SA, SB))
    if nstage <= 2:
        return nc
    A = AX_(1); A.update({"w_in": b_w_in[0], "b_in": b_b_in[0], "g_ln_g": b_ln_g[0], "g_ln_b": b_ln_b[0], "w_s": b_w_s[0], "b_s": b_b_s[0],
                          "w_out": b_w_out[0]})
    stage_gmlp(nc, "s1_", allt(SB, SA), A)
    if nstage <= 3:
        return nc
    moe(1, "m1", allt(SA, SB))
    if nstage <= 4:
        return nc
    lat = [(row(SB, u), 0, u, (row(SA, u) if 1 <= u <= 18 else None)) for u in range(NEXT)]
    cx = [(row(SB, 20 + c), 1, 20 + c, row(SA, 20 + c)) for c in range(2)]
    A = AX_(2); A.update({"valid": valid, "w_in": c_w_in[0], "w_conv": c_w_conv[0], "w_out": c_w_out[0]})
    stage_conv(nc, "s2_", [lat, cx], A, 22)
    if nstage <= 5:
        return nc
    t2 = [(row(SA, u), 0, row(SB, u)) for u in range(1, 19)] + [(row(SA, 20 + c), 1, row(SB, 20 + c)) for c in range(2)]
    moe(2, "m2", t2)
    if nstage <= 6:
        return nc
    kv = [(row(SB, u), 0, u + 1, u + 1) for u in range(1, 19)] + [(row(SB, 20 + c), 1, 22 + c, 22 + c) for c in range(2)]
    q = [(row(SB, u), 0, u + 1, [(u - 2, "P"), (u - 1, None), (u, "N"), (18, None), (19, None)], row(SA, u)) for u in range(2, 18)]
    A = AX_(3); A.update(attn_tabs); A.update({"w_qkv": a_w_qkv[1], "w_o": a_w_o[1], "sink": a_sink[1]})
    stage_attn(nc, "s3_", kv, q, A, 24)
    moe(3, "m3", [(row(SA, u), 0, row(xout, u - 2)) for u in range(2, 18)])
    return nc


_NC = []


def _tables(core):
    L = 16384
    freqs = np.power(np.float32(10000.0), -np.arange(16, dtype=np.float32) / np.float32(16)).astype(np.float32)
    pos = (core * 2048 - 384 + np.arange(NWIN * 128)).astype(np.float32)
    row = np.floor(pos / 64.0).astype(np.float32)
    col = (pos - row * 64).astype(np.float32)
    ar = row[:, None] * freqs[None, :]
    ac = col[:, None] * freqs[None, :]
    ang = np.concatenate([ar, ar, ac, ac], -1)
    cos = np.cos(ang).astype(np.float32)
    sin = np.sin(ang).astype(np.float32)
    sgn = np.tile(np.concatenate([-np.ones(16, np.float32), np.ones(16, np.float32)]), 2)
    sinS = sin * sgn[None, :]
    cos = np.concatenate([cos, np.ones((256, 64), np.float32)], 0)
    sinS = np.concatenate([sinS, np.zeros((256, 64), np.float32)], 0)
    cos_t = np.ascontiguousarray(cos.reshape(24, 128, 64).transpose(1, 0, 2).reshape(128, 24 * 64))
    sin_t = np.ascontiguousarray(sinS.reshape(24, 128, 64).transpose(1, 0, 2).reshape(128, 24 * 64))
    kb = np.zeros((128, 24), np.float32)
    for v in range(NWIN):
        st = core * 2048 - 384 + 128 * v
        if st < 0 or st >= L:
            kb[:, v] = -30000.0
    valid = np.ones((128, 22), np.float32)
    for u in range(NEXT):
        st = core * 2048 - 256 + 128 * u
        if st < 0 or st >= L:
            valid[:, u] = 0.0
    return cos_t, sin_t, kb, valid


def kernel(x, c, ctx, c_ctx, w_mod, b_mod, ln1_g, ln1_b, ln2_g, ln2_b, router_w, router_bias, moe_w1, moe_w3, moe_w2,
           a_w_qkv, a_w_o, a_sink, b_w_in, b_b_in, b_ln_g, b_ln_b, b_w_s, b_b_s, b_w_out, c_w_in, c_w_conv, c_w_out):
    f32 = lambda a: np.ascontiguousarray(np.asarray(a, dtype=np.float32))
    if not _NC:
        _NC.append(build_fused())
    nc = _NC[0]
    xc = f32(x)[0]
    zpad = np.zeros((384, D), np.float32)
    xp = np.concatenate([zpad, xc, zpad], 0)
    kk = np.arange(128)[:, None]
    qq = np.arange(128)[None, :]
    com = {"ctx": f32(ctx)[0], "cvec": np.stack([f32(c)[0], f32(c_ctx)]).astype(np.float32),
           "w_mod": f32(w_mod), "b_mod": f32(b_mod), "ln1_g": f32(ln1_g), "ln1_b": f32(ln1_b), "ln2_g": f32(ln2_g), "ln2_b": f32(ln2_b),
           "router_w": f32(router_w), "router_bias": f32(router_bias), "moe_w1": f32(moe_w1), "moe_w3": f32(moe_w3), "moe_w2": f32(moe_w2),
           "a_w_qkv": f32(a_w_qkv), "a_w_o": f32(a_w_o), "a_sink": f32(a_sink), "b_w_in": f32(b_w_in), "b_b_in": f32(b_b_in),
           "b_ln_g": f32(b_ln_g), "b_ln_b": f32(b_ln_b), "b_w_s": f32(b_w_s), "b_b_s": f32(b_b_s), "b_w_out": f32(b_w_out),
           "c_w_in": f32(c_w_in), "c_w_conv": f32(c_w_conv), "c_w_out": f32(c_w_out),
           "maskp": np.tile((kk >= qq).astype(np.float32), (1, 4)), "maskn": np.tile((kk <= qq).astype(np.float32), (1, 4))}
    in_maps = []
    for core in range(8):
        cos_t, sin_t, kb, valid = _tables(core)
        in_maps.append(dict(com, xw=np.ascontiguousarray(xp[core * 2048:core * 2048 + NWIN * 128]), cos_t=cos_t, sin_t=sin_t, kbias=kb, valid=valid))
    res = run_bass_kernel_spmd(nc, in_maps, core_ids=list(range(8)))
    out = np.concatenate([r["xout"] for r in res.results], 0)
    return out[None].astype(np.float32)
```
